# Optimizing a Trainium2 kernel written in Bass

```python
import math
import jax, jax.numpy as jnp
from jax import lax
import numpy as np

D_MODEL = 1024
BATCH = 8
SEQ = 4096
DEPTH = 4

N_MIXERS = 2
N_ATTN_LAYERS = (DEPTH + 1) // 2
N_RWKV_LAYERS = DEPTH // 2
DA_HEADS = 8
DA_HEAD_DIM = D_MODEL // (2 * DA_HEADS)
DA_QBLOCK = 128
RMS_EPS = 1e-5
RW_HEAD = 64
RW_HEADS = D_MODEL // RW_HEAD
RW_DECAY_LORA = max(32, int(round(1.8 * D_MODEL ** 0.5 / 32)) * 32)
RW_AAA_LORA = max(32, int(round(1.8 * D_MODEL ** 0.5 / 32)) * 32)
RW_MV_LORA = max(32, int(round(1.3 * D_MODEL ** 0.5 / 32)) * 32)
RW_GATE_LORA = max(32, int(round(0.6 * D_MODEL ** 0.8 / 32)) * 32)
RW_GN_EPS = 64e-5
MOE_GROUPS = 4
MOE_EPG = 8
MOE_EXPERTS = MOE_GROUPS * MOE_EPG
MOE_TOPK = 2
MOE_HIDDEN = D_MODEL // 2
MOE_BLOCK = 128
DEEPNORM_ALPHA = (2 * DEPTH) ** 0.25
DEEPNORM_BETA = (8 * DEPTH) ** -0.25
LN_EPS = 1e-5

kernel_name = 'hybrid_diffattn_rwkv7_hmoe_deepnorm'


def layer_norm(x, g, b):
    xf = x.astype(jnp.float32)
    mu = jnp.mean(xf, -1, keepdims=True)
    var = jnp.mean(jnp.square(xf - mu), -1, keepdims=True)
    return ((xf - mu) * lax.rsqrt(var + LN_EPS) * g + b).astype(x.dtype)


def diff_attention(x, w_qkv, w_o, lam, subln_g, lambda_init):
    B, S, _ = x.shape
    q, k, v = jnp.split(x @ w_qkv, 3, axis=-1)
    q = q.reshape(B, S, DA_HEADS, 2, DA_HEAD_DIM)
    k = k.reshape(B, S, DA_HEADS, 2, DA_HEAD_DIM)
    v = v.reshape(B, S, DA_HEADS, 2 * DA_HEAD_DIM)
    lamf = lam.astype(jnp.float32)
    lam_full = jnp.exp(jnp.sum(lamf[0] * lamf[1])) - jnp.exp(jnp.sum(lamf[2] * lamf[3])) + lambda_init
    n_blk = S // DA_QBLOCK
    q_blocks = jnp.moveaxis(q.reshape(B, n_blk, DA_QBLOCK, DA_HEADS, 2, DA_HEAD_DIM), 1, 0)
    k_pos = jnp.arange(S)
    scale = DA_HEAD_DIM ** -0.5

    def one_block(args):
        q_blk, blk = args
        s = jnp.einsum('bqhcd,bkhcd->bhcqk', q_blk, k).astype(jnp.float32) * scale
        q_pos = blk * DA_QBLOCK + jnp.arange(DA_QBLOCK)
        causal = k_pos[None, :] <= q_pos[:, None]
        p = jax.nn.softmax(jnp.where(causal, s, -jnp.inf), axis=-1)
        attn = p[:, :, 0] - lam_full * p[:, :, 1]
        return jnp.einsum('bhqk,bkhe->bqhe', attn.astype(v.dtype), v)

    o = lax.map(one_block, (q_blocks, jnp.arange(n_blk)))
    o = jnp.moveaxis(o, 0, 1).reshape(B, S, DA_HEADS, 2 * DA_HEAD_DIM).astype(jnp.float32)
    o = o * lax.rsqrt(jnp.mean(o * o, -1, keepdims=True) + RMS_EPS) * subln_g * (1.0 - lambda_init)
    return o.reshape(B, S, D_MODEL).astype(x.dtype) @ w_o


def wkv7_scan(r, decay, k, v, a_vec, b_vec):
    B, S, H, N = r.shape

    def step(state, inp):
        r_t, w_t, k_t, v_t, a_t, b_t = inp
        sa = jnp.einsum('bhij,bhj->bhi', state, a_t)
        state = (state * w_t[:, :, None, :] + sa[..., None] * b_t[:, :, None, :]
                 + v_t[..., None] * k_t[:, :, None, :])
        return state, jnp.einsum('bhij,bhj->bhi', state, r_t)

    xs = tuple(jnp.moveaxis(t, 1, 0) for t in (r, decay, k, v, a_vec, b_vec))
    _, ys = lax.scan(step, jnp.zeros((B, H, N, N), jnp.float32), xs)
    return jnp.moveaxis(ys, 0, 1)


def rwkv7_time_mix(x, mu, w_rkv, w_o, w0, w1, w2, a0, a1, a2, g1, g2, k_k, k_a, r_k,
                   lnx_g, lnx_b, v_first, value_mix):
    B, S, D = x.shape
    H, N = RW_HEADS, RW_HEAD
    xx = jnp.pad(x, ((0, 0), (1, 0), (0, 0)))[:, :-1] - x
    xm = x[None] + xx[None] * mu[:, None, None, :]
    r, k, v = jnp.einsum('nbsd,nde->nbse', xm[:3], w_rkv)
    xv, xw, xa, xg = xm[2], xm[3], xm[4], xm[5]
    w = -jax.nn.softplus(-(w0 + jnp.tanh(xw @ w1) @ w2)) - 0.5
    if value_mix is None:
        v_first = v
    else:
        v0, v1, v2 = value_mix
        v = v + (v_first - v) * jax.nn.sigmoid(v0 + (xv @ v1) @ v2)
    a = jax.nn.sigmoid(a0 + (xa @ a1) @ a2)
    g = jax.nn.sigmoid(xg @ g1) @ g2
    kk = (k * k_k).reshape(B, S, H, N).astype(jnp.float32)
    kk = kk / jnp.maximum(jnp.sqrt(jnp.sum(kk * kk, -1, keepdims=True)), 1e-12)
    k = k * (1.0 + (a - 1.0) * k_a)
    rh, kh, vh, ah = [t.reshape(B, S, H, N).astype(jnp.float32) for t in (r, k, v, a)]
    decay = jnp.exp(-jnp.exp(w.reshape(B, S, H, N).astype(jnp.float32)))
    y = wkv7_scan(rh, decay, kh, vh, -kk, kk * ah)
    ym = jnp.mean(y, -1, keepdims=True)
    yv = jnp.mean(jnp.square(y - ym), -1, keepdims=True)
    y = (y - ym) * lax.rsqrt(yv + RW_GN_EPS) * lnx_g.reshape(H, N) + lnx_b.reshape(H, N)
    y = y + jnp.sum(rh * kh * r_k, -1, keepdims=True) * vh
    out = (y.reshape(B, S, D).astype(x.dtype) * g) @ w_o
    return out, v_first


def hier_moe(x, rg_w, rg_b, re_w, re_b, w_gu, w_down):
    B, S, D = x.shape
    n_tok = B * S
    t = x.reshape(n_tok, D)
    g_prob = jax.nn.softmax((t @ rg_w + rg_b).astype(jnp.float32), axis=-1)
    g_p, g_idx = lax.top_k(g_prob, 1)
    e_logits = (t @ re_w + re_b).astype(jnp.float32).reshape(n_tok, MOE_GROUPS, MOE_EPG)
    e_logits = jnp.take_along_axis(e_logits, g_idx[:, :, None], axis=1)[:, 0]
    e_p, e_idx = lax.top_k(jax.nn.softmax(e_logits, axis=-1), MOE_TOPK)
    e_p = e_p / jnp.sum(e_p, -1, keepdims=True)
    gate = (g_p * e_p).reshape(-1)
    expert = (g_idx * MOE_EPG + e_idx).reshape(-1).astype(jnp.int32)
    tok = jnp.repeat(jnp.arange(n_tok, dtype=jnp.int32), MOE_TOPK)
    n_assign = n_tok * MOE_TOPK
    order = jnp.argsort(expert)
    e_sorted, tok_sorted, gate_sorted = expert[order], tok[order], gate[order]
    counts = jnp.bincount(expert, length=MOE_EXPERTS).astype(jnp.int32)
    padded = (counts + MOE_BLOCK - 1) // MOE_BLOCK * MOE_BLOCK
    pad_end = jnp.cumsum(padded)
    pad_start = pad_end - padded
    start = jnp.cumsum(counts) - counts
    dest = pad_start[e_sorted] + jnp.arange(n_assign, dtype=jnp.int32) - start[e_sorted]
    n_blocks = -(-n_assign // MOE_BLOCK) + MOE_EXPERTS
    n_rows = n_blocks * MOE_BLOCK
    tok_buf = jnp.full((n_rows,), n_tok, jnp.int32).at[dest].set(tok_sorted)
    gate_buf = jnp.zeros((n_rows,), jnp.float32).at[dest].set(gate_sorted)
    t_pad = jnp.concatenate([t, jnp.zeros((1, D), t.dtype)], axis=0)
    x_buf = t_pad[tok_buf].reshape(n_blocks, MOE_BLOCK, D)
    block_e = jnp.minimum(jnp.searchsorted(pad_end, jnp.arange(n_blocks) * MOE_BLOCK, side='right'),
                          MOE_EXPERTS - 1)

    def expert_block(args):
        xb, e = args
        hg, hu = jnp.split(xb @ w_gu[e], 2, axis=-1)
        return (jax.nn.silu(hg) * hu) @ w_down[e]

    y = lax.map(expert_block, (x_buf, block_e)).reshape(n_rows, D)
    y = y * gate_buf[:, None].astype(y.dtype)
    out = jax.ops.segment_sum(y, tok_buf, num_segments=n_tok + 1)[:n_tok]
    return out.reshape(B, S, D)


def setup_inputs(seed: int = 0) -> dict:
    key = jax.random.key(seed)
    k = jax.random.split(key, 34)
    D, L, NA, NR = D_MODEL, DEPTH, N_ATTN_LAYERS, N_RWKV_LAYERS
    H, N = RW_HEADS, RW_HEAD
    f32 = jnp.float32

    def nrm(i, shape, scale):
        return scale * jax.random.normal(k[i], shape, f32)

    def unif(i, shape, lo, hi):
        return jax.random.uniform(k[i], shape, f32, lo, hi)

    return {
        'x': nrm(0, (BATCH, SEQ, D), 1.0),
        'ln1_g': 1.0 + nrm(1, (L, D), 0.02),
        'ln1_b': nrm(2, (L, D), 0.02),
        'ln2_g': 1.0 + nrm(3, (L, D), 0.02),
        'ln2_b': nrm(4, (L, D), 0.02),
        'attn_w_qkv': nrm(5, (NA, D, 3 * D), D ** -0.5),
        'attn_w_o': nrm(6, (NA, D, D), DEEPNORM_BETA * D ** -0.5),
        'attn_lambda': nrm(7, (NA, 4, DA_HEAD_DIM), 0.1),
        'attn_subln_g': 1.0 + nrm(8, (NA, 2 * DA_HEAD_DIM), 0.02),
        'rw_mu': unif(9, (NR, 6, D), 0.0, 1.0),
        'rw_w_rkv': nrm(10, (NR, 3, D, D), D ** -0.5),
        'rw_w_o': nrm(11, (NR, D, D), DEEPNORM_BETA * D ** -0.5),
        'rw_w0': unif(12, (NR, D), -6.0, -1.0),
        'rw_w1': nrm(13, (NR, D, RW_DECAY_LORA), D ** -0.5),
        'rw_w2': nrm(14, (NR, RW_DECAY_LORA, D), 0.1 * RW_DECAY_LORA ** -0.5),
        'rw_a0': nrm(15, (NR, D), 0.1),
        'rw_a1': nrm(16, (NR, D, RW_AAA_LORA), D ** -0.5),
        'rw_a2': nrm(17, (NR, RW_AAA_LORA, D), 0.5 * RW_AAA_LORA ** -0.5),
        'rw_g1': nrm(18, (NR, D, RW_GATE_LORA), D ** -0.5),
        'rw_g2': nrm(19, (NR, RW_GATE_LORA, D), RW_GATE_LORA ** -0.5),
        'rw_k_k': 0.85 + nrm(20, (NR, D), 0.02),
        'rw_k_a': 1.0 + nrm(21, (NR, D), 0.02),
        'rw_r_k': nrm(22, (NR, H, N), 0.1),
        'rw_lnx_g': 1.0 + nrm(23, (NR, D), 0.02),
        'rw_lnx_b': nrm(24, (NR, D), 0.02),
        'rw_v0': 1.0 + nrm(25, (NR - 1, D), 0.1),
        'rw_v1': nrm(26, (NR - 1, D, RW_MV_LORA), D ** -0.5),
        'rw_v2': nrm(27, (NR - 1, RW_MV_LORA, D), 0.5 * RW_MV_LORA ** -0.5),
        'moe_rg_w': nrm(28, (L, D, MOE_GROUPS), D ** -0.5),
        'moe_rg_b': nrm(29, (L, MOE_GROUPS), 0.01),
        'moe_re_w': nrm(30, (L, D, MOE_EXPERTS), D ** -0.5),
        'moe_re_b': nrm(31, (L, MOE_EXPERTS), 0.01),
        'moe_w_gu': nrm(32, (L, MOE_EXPERTS, D, 2 * MOE_HIDDEN), D ** -0.5),
        'moe_w_down': nrm(33, (L, MOE_EXPERTS, MOE_HIDDEN, D), DEEPNORM_BETA * MOE_HIDDEN ** -0.5),
    }


def reference(x, ln1_g, ln1_b, ln2_g, ln2_b, attn_w_qkv, attn_w_o, attn_lambda, attn_subln_g,
              rw_mu, rw_w_rkv, rw_w_o, rw_w0, rw_w1, rw_w2, rw_a0, rw_a1, rw_a2, rw_g1, rw_g2,
              rw_k_k, rw_k_a, rw_r_k, rw_lnx_g, rw_lnx_b, rw_v0, rw_v1, rw_v2,
              moe_rg_w, moe_rg_b, moe_re_w, moe_re_b, moe_w_gu, moe_w_down):
    v_first = None
    for i in range(DEPTH):
        j = i // N_MIXERS
        if i % N_MIXERS == 0:
            lambda_init = 0.8 - 0.6 * math.exp(-0.3 * i)
            h = diff_attention(x, attn_w_qkv[j], attn_w_o[j], attn_lambda[j], attn_subln_g[j], lambda_init)
        else:
            value_mix = None if j == 0 else (rw_v0[j - 1], rw_v1[j - 1], rw_v2[j - 1])
            h, v_first = rwkv7_time_mix(x, rw_mu[j], rw_w_rkv[j], rw_w_o[j], rw_w0[j], rw_w1[j], rw_w2[j],
                                        rw_a0[j], rw_a1[j], rw_a2[j], rw_g1[j], rw_g2[j], rw_k_k[j],
                                        rw_k_a[j], rw_r_k[j], rw_lnx_g[j], rw_lnx_b[j], v_first, value_mix)
        x = layer_norm(DEEPNORM_ALPHA * x + h, ln1_g[i], ln1_b[i])
        f = hier_moe(x, moe_rg_w[i], moe_rg_b[i], moe_re_w[i], moe_re_b[i], moe_w_gu[i], moe_w_down[i])
        x = layer_norm(DEEPNORM_ALPHA * x + f, ln2_g[i], ln2_b[i])
    return x
```

```python
import math
from contextlib import ExitStack
import numpy as np
import concourse.bass as bass
import concourse.mybir as mybir
from concourse.bass_utils import run_bass_kernel_spmd

F32 = mybir.dt.float32
BF16 = mybir.dt.bfloat16
I32 = mybir.dt.int32
AF = mybir.ActivationFunctionType
ALU = mybir.AluOpType
AX = mybir.AxisListType

D = 1024
DEPTH = 4
NH_A = 8
NE = 32
NG = 4
EPG = 8
HID = 512
ALPHA = (2 * DEPTH) ** 0.25
LN_EPS = 1e-5
RMS_EPS = 1e-5
GN_EPS = 64e-5


class Res:
    __slots__ = ("w", "r")

    def __init__(self):
        self.w = None
        self.r = {}


def RL(n):
    return [Res() for _ in range(n)]


class KB:
    EPOCH = 30000

    def __init__(self, nc, es):
        self.nc = nc
        self.es = es
        self.engs = {"pe": nc.tensor, "dve": nc.vector, "act": nc.scalar, "pool": nc.gpsimd, "sp": nc.sync}
        self.sems = {}
        self.cur = {}
        self.seen = {e: {} for e in self.engs}
        self.rings = {}
        self.ridx = {}
        self.nins = 0
        for q, n in (("sp", 16), ("pool", 12), ("act", 6)):
            self.rings[q] = [[self._new_sem("d%s%d" % (q, i)), 0] for i in range(n)]
            self.ridx[q] = 0

    def _new_sem(self, name):
        s = self.es.enter_context(self.nc.semaphore(name))
        self.sems[name] = s
        return name

    def sb(self, name, shape, dt):
        return self.es.enter_context(self.nc.sbuf_tensor(name, list(shape), dt))

    def ps(self, name, shape, dt=F32):
        return self.es.enter_context(self.nc.psum_tensor(name, list(shape), dt))

    def _deps(self, reads, writes):
        deps = {}
        for r in reads:
            if r.w:
                for k, v in r.w.items():
                    if deps.get(k, 0) < v:
                        deps[k] = v
        for w in writes:
            if w.w:
                for k, v in w.w.items():
                    if deps.get(k, 0) < v:
                        deps[k] = v
            for k, v in w.r.items():
                if deps.get(k, 0) < v:
                    deps[k] = v
        return deps

    def _waits(self, eng, deps):
        E = self.engs[eng]
        seen = self.seen[eng]
        for k, v in deps.items():
            if eng == "pe" and k.startswith("epe"):
                continue
            if seen.get(k, 0) >= v:
                continue
            E.wait_ge(self.sems[k], v)
            seen[k] = v

    def _mark(self, tok, reads, writes):
        (k, v), = tok.items()
        for w in writes:
            if w.w is None:
                w.w = dict(tok)
            else:
                w.w = dict(w.w)
                w.w[k] = v
            w.r = {}
        for r in reads:
            if r.r.get(k, 0) < v:
                r.r[k] = v

    def op(self, eng, fn, reads=(), writes=()):
        self._waits(eng, self._deps(reads, writes))
        c = self.cur.get(eng)
        if c is None or c[1] >= self.EPOCH:
            n = len([k for k in self.sems if k.startswith("e" + eng)])
            c = [self._new_sem("e%s%d" % (eng, n)), 0]
            self.cur[eng] = c
        ins = fn(self.engs[eng])
        c[1] += 1
        ins.then_inc(self.sems[c[0]], 1)
        self.nins += 1
        self._mark({c[0]: c[1]}, reads, writes)

    def dma(self, q, fn, reads=(), writes=()):
        ring = self.rings[q]
        slot = ring[self.ridx[q] % len(ring)]
        self.ridx[q] += 1
        deps = self._deps(reads, writes)
        if slot[1] > 0:
            deps[slot[0]] = max(deps.get(slot[0], 0), slot[1] * 16)
        self._waits(q, deps)
        ins = fn(self.engs[q])
        slot[1] += 1
        ins.then_inc(self.sems[slot[0]], 16)
        self.nins += 1
        self._mark({slot[0]: slot[1] * 16}, reads, writes)

    def finish(self):
        deps = {}
        for q, ring in self.rings.items():
            for name, cnt in ring:
                if cnt:
                    deps[name] = cnt * 16
        for name in self.sems:
            if name.startswith("e"):
                pass
        for e, c in self.cur.items():
            deps[c[0]] = c[1]
        self._waits("sp", deps)


class Consts:
    pass


def make_consts(kb):
    nc = kb.nc
    C = Consts()
    C.r = Res()
    it = kb.sb("c_iota", [128, 512], I32)
    itf = kb.sb("c_iotaf", [128, 512], F32)
    C.ident_f = kb.sb("c_identf", [128, 128], F32)
    C.ident_b = kb.sb("c_identb", [128, 128], BF16)
    C.ones_b = kb.sb("c_onesb", [128, 128], BF16)
    C.triu_b = kb.sb("c_triub", [128, 128], BF16)
    C.cmask = kb.sb("c_cmask", [128, 4, 512], BF16)
    C.m_st = kb.sb("c_mst", [128, 128], F32)
    C.m_in = kb.sb("c_min", [128, 128], F32)
    C.m_lo = kb.sb("c_mlo", [128, 128], F32)
    kb.op("pool", lambda e: e.iota(it[:], [[1, 512]], base=0, channel_multiplier=-1), writes=[C.r])
    kb.op("dve", lambda e: e.tensor_copy(itf[:], it[:]), reads=[C.r], writes=[C.r])
    kb.op("dve", lambda e: e.tensor_scalar(C.ident_f[:], itf[:, 0:128], 0.0, None, op0=ALU.is_equal), reads=[C.r], writes=[C.r])
    kb.op("dve", lambda e: e.tensor_copy(C.ident_b[:], C.ident_f[:]), reads=[C.r], writes=[C.r])
    kb.op("dve", lambda e: e.memset(C.ones_b[:], 1.0), writes=[C.r])
    kb.op("dve", lambda e: e.tensor_scalar(C.triu_b[:], itf[:, 0:128], 0.0, None, op0=ALU.is_ge), reads=[C.r], writes=[C.r])
    for o in range(4):
        kb.op("dve", lambda e, o=o: e.tensor_scalar(C.cmask[:, o, :], itf[:], float(128 * o), None, op0=ALU.is_ge), reads=[C.r], writes=[C.r])
    kb.op("dve", lambda e: e.tensor_scalar(C.m_st[:], itf[:, 0:128], 1.0, None, op0=ALU.is_ge), reads=[C.r], writes=[C.r])
    kb.op("dve", lambda e: e.tensor_scalar(C.m_in[:], itf[:, 0:128], 0.0, None, op0=ALU.is_ge), reads=[C.r], writes=[C.r])
    kb.op("dve", lambda e: e.tensor_scalar(C.m_lo[:], itf[:, 0:128], -1.0, None, op0=ALU.is_le), reads=[C.r], writes=[C.r])
    C.itf = itf
    return C


def kb_push(kb):
    kb._stack = getattr(kb, "_stack", [])
    kb._stack.append(kb.es_t)
    kb.es_t = kb.es_root.enter_context(ExitStack()) if False else ExitStack()
    kb.es_t.__enter__()


def kb_pop(kb):
    kb.barrier()
    kb.es_t.__exit__(None, None, None)
    kb.es_t = kb._stack.pop()


def _kb_sb(self, name, shape, dt):
    self.nalloc = getattr(self, "nalloc", 0) + 1
    return self.es_t.enter_context(self.nc.sbuf_tensor("%s_%d" % (name, self.nalloc), list(shape), dt))


def _kb_ps(self, name, shape, dt=F32):
    self.nalloc = getattr(self, "nalloc", 0) + 1
    return self.es_t.enter_context(self.nc.psum_tensor("%s_%d" % (name, self.nalloc), list(shape), dt))


def _kb_barrier(self):
    deps = {}
    for q, ring in self.rings.items():
        for name, cnt in ring:
            if cnt:
                deps[name] = cnt * 16
    for name in self.sems:
        if name.startswith("e"):
            eng = [e for e in self.engs if name.startswith("e" + e)][0]
            c = self.cur[eng]
            if c[0] == name:
                deps[name] = c[1]
    for eng in self.engs:
        self._waits(eng, dict(deps))


KB.sb = _kb_sb
KB.ps = _kb_ps
KB.barrier = _kb_barrier


def bcast_rows(ap_row, n=128):
    return ap_row.partition_broadcast(n)


def ln_tile(kb, S, z, z_r, out, out_r, g_bc, b_bc, par_r):
    st, mv, sd, rs, xn = S["st"], S["mv"], S["sd"], S["rs"], S["xn"]
    r = S["r"]
    kb.op("dve", lambda e: e.bn_stats(st[:, 0, :], z[:, 0:512]), reads=[z_r], writes=[r])
    kb.op("dve", lambda e: e.bn_stats(st[:, 1, :], z[:, 512:1024]), reads=[z_r], writes=[r])
    kb.op("dve", lambda e: e.bn_aggr(mv[:, 0:2], st[:].rearrange("p a b -> p (a b)")), reads=[r], writes=[r])
    kb.op("act", lambda e: e.activation(sd[:, 0:1], mv[:, 1:2], AF.Sqrt, bias=S["eps"][:, 0:1]), reads=[r], writes=[r])
    kb.op("dve", lambda e: e.reciprocal(rs[:, 0:1], sd[:, 0:1]), reads=[r], writes=[r])
    kb.op("dve", lambda e: e.tensor_scalar(xn[:], z[:], mv[:, 0:1], rs[:, 0:1], op0=ALU.subtract, op1=ALU.mult), reads=[r, z_r], writes=[S["xn_r"]])
    kb.op("pool", lambda e: e.tensor_tensor(xn[:], xn[:], g_bc[:], op=ALU.mult), reads=[S["xn_r"], par_r], writes=[S["xn_r"]])
    kb.op("pool", lambda e: e.tensor_tensor(out, xn[:], b_bc[:], op=ALU.add), reads=[S["xn_r"], par_r], writes=[out_r])


def ln_scratch(kb, pfx, eps):
    S = {}
    S["st"] = kb.sb(pfx + "st", [128, 2, 6], F32)
    S["mv"] = kb.sb(pfx + "mv", [128, 2], F32)
    S["sd"] = kb.sb(pfx + "sd", [128, 1], F32)
    S["rs"] = kb.sb(pfx + "rs", [128, 1], F32)
    S["xn"] = kb.sb(pfx + "xn", [128, 1024], F32)
    S["eps"] = kb.sb(pfx + "eps", [128, 1], F32)
    S["r"] = Res()
    S["xn_r"] = Res()
    kb.op("dve", lambda e: e.memset(S["eps"][:], eps), writes=[S["r"]])
    return S


def load_ln_params(kb, pfx, g_dram, b_dram, li):
    g = kb.sb(pfx + "g", [128, 1024], F32)
    b = kb.sb(pfx + "b", [128, 1024], F32)
    r = Res()
    kb.dma("sp", lambda e: e.dma_start(out=g[:], in_=bcast_rows(g_dram[li:li + 1, :])), writes=[r])
    kb.dma("sp", lambda e: e.dma_start(out=b[:], in_=bcast_rows(b_dram[li:li + 1, :])), writes=[r])
    return g, b, r


def attn_phase(kb, C, T, prm, j, li, xin, xin_r, xa, xa_r):
    nc = kb.nc
    NT = T // 128
    NS = T // 512
    lambda_init = 0.8 - 0.6 * math.exp(-0.3 * li)
    kb_push(kb)
    OT = kb.sb("a_OT", [128, 8, T], BF16)
    OT_r = [RL(NS) for _ in range(8)]
    ps = [kb.ps("a_ps%d" % b, [128, 512], F32) for b in range(8)]
    ps_r = RL(8)
    kb_push(kb)
    xld = [kb.sb("a_xld%d" % b, [128, 1024], F32) for b in range(2)]
    xld_r = RL(2)
    xT = kb.sb("a_xT", [128, 8, T], BF16)
    xT_r = RL(NT)
    QT = kb.sb("a_QT", [128, T], BF16)
    QT_r = RL(NS)
    KT = kb.sb("a_KT", [128, T], BF16)
    KT_r = RL(NS)
    V = kb.sb("a_V", [128, NT, 128], BF16)
    V_r = RL(NT // 4)
    wh = [kb.sb("a_wh%d" % b, [128, 8, 3, 128], BF16) for b in range(2)]
    wh_r = RL(2)
    pt = [kb.sb("a_pt%d" % b, [128, 512], BF16) for b in range(4)]
    pt_r = RL(4)
    lam = kb.sb("a_lam", [128, 256], F32)
    lsc = kb.sb("a_lsc", [128, 8], F32)
    lam_r = Res()
    s1 = kb.sb("a_s1", [128, 512], F32)
    s2 = kb.sb("a_s2", [128, 512], F32)
    t1 = kb.sb("a_t1", [128, 512], F32)
    t2 = kb.sb("a_t2", [128, 512], F32)
    sq = kb.sb("a_sq", [128, 512], BF16)
    op_ = t1
    s12 = s1
    e2 = s2
    tot = s2
    rstd = s1
    fin_r = Res()
    fin2_r = fin_r

    kb.dma("sp", lambda e: e.dma_start(out=lam[:], in_=bcast_rows(prm["attn_lambda"][j:j + 1].rearrange("o a b -> o (a b)"))), writes=[lam_r])
    kb.dma("sp", lambda e: e.dma_start(out=lsc[:, 4:5], in_=prm["attn_subln_g"][j].rearrange("(p o) -> p o", o=1)), writes=[lam_r])
    kb.op("dve", lambda e: e.tensor_tensor(lam[:, 0:64], lam[:, 0:64], lam[:, 64:128], op=ALU.mult), reads=[lam_r], writes=[lam_r])
    kb.op("dve", lambda e: e.tensor_tensor(lam[:, 128:192], lam[:, 128:192], lam[:, 192:256], op=ALU.mult), reads=[lam_r], writes=[lam_r])
    kb.op("dve", lambda e: e.reduce_sum(lsc[:, 0:1], lam[:, 0:64], axis=AX.X), reads=[lam_r], writes=[lam_r])
    kb.op("dve", lambda e: e.reduce_sum(lsc[:, 1:2], lam[:, 128:192], axis=AX.X), reads=[lam_r], writes=[lam_r])
    kb.op("act", lambda e: e.activation(lsc[:, 2:4], lsc[:, 0:2], AF.Exp), reads=[lam_r], writes=[lam_r])
    kb.op("dve", lambda e: e.tensor_tensor(lsc[:, 5:6], lsc[:, 3:4], lsc[:, 2:3], op=ALU.subtract), reads=[lam_r], writes=[lam_r])
    kb.op("dve", lambda e: e.tensor_scalar(lsc[:, 5:6], lsc[:, 5:6], -lambda_init, None, op0=ALU.add), reads=[lam_r], writes=[lam_r])
    kb.op("dve", lambda e: e.tensor_scalar(lsc[:, 6:7], lsc[:, 4:5], 1.0 - lambda_init, None, op0=ALU.mult), reads=[lam_r], writes=[lam_r])
    nlam = lsc[:, 5:6]
    gsc = lsc[:, 6:7]

    for i in range(NT):
        b = i % 2
        kb.dma("sp", lambda e, i=i, b=b: e.dma_start(out=xld[b][:], in_=xin[i * 128:(i + 1) * 128, :]), reads=[xin_r[i]], writes=[xld_r[b]])
        for hf in range(2):
            pb = (2 * i + hf) % 8
            for c4 in range(4):
                kc = hf * 4 + c4
                kb.op("pe", lambda e, pb=pb, c4=c4, kc=kc, b=b: e.transpose(ps[pb][:, c4 * 128:(c4 + 1) * 128], xld[b][:, kc * 128:(kc + 1) * 128], C.ident_f[:]),
                      reads=[xld_r[b], C.r], writes=[ps_r[pb]])
            eng = "act" if hf == 0 else "dve"
            if eng == "act":
                kb.op("act", lambda e, pb=pb, hf=hf, i=i: e.activation(xT[:, hf * 4:hf * 4 + 4, i * 128:(i + 1) * 128], ps[pb][:].rearrange("p (c t) -> p c t", c=4), AF.Copy),
                      reads=[ps_r[pb]], writes=[xT_r[i]])
            else:
                kb.op("dve", lambda e, pb=pb, hf=hf, i=i: e.tensor_copy(xT[:, hf * 4:hf * 4 + 4, i * 128:(i + 1) * 128], ps[pb][:].rearrange("p (c t) -> p c t", c=4)),
                      reads=[ps_r[pb]], writes=[xT_r[i]])


    wq = prm["attn_w_qkv"][j].rearrange("(kc p) n -> p kc n", p=128)
    pcnt = [0]

    def nps():
        pcnt[0] += 1
        return pcnt[0] % 2

    for h in range(NH_A):
        wb = h % 2
        for part in range(3):
            kb.dma("pool", lambda e, wb=wb, part=part, h=h: e.dma_start(out=wh[wb][:, :, part, :], in_=wq[:, :, part * 1024 + h * 128: part * 1024 + (h + 1) * 128]),
                   writes=[wh_r[wb]])
        for s in range(NS):
            for part, dst, dst_r, scl in ((0, QT, QT_r, 0.125), (1, KT, KT_r, 1.0)):
                pb = nps()
                for kc in range(8):
                    kb.op("pe", lambda e, pb=pb, wb=wb, kc=kc, part=part, s=s: e.matmul(ps[pb][:], wh[wb][:, kc, part, :], xT[:, kc, s * 512:(s + 1) * 512], start=(kc == 0), stop=(kc == 7)),
                          reads=[wh_r[wb]] + xT_r[s * 4:s * 4 + 4], writes=[ps_r[pb]])
                kb.op("act", lambda e, pb=pb, dst=dst, s=s, scl=scl: e.activation(dst[:, s * 512:(s + 1) * 512], ps[pb][:], AF.Copy, scale=scl),
                      reads=[ps_r[pb]], writes=[dst_r[s]])
        for g4 in range(NT // 4):
            pb = nps()
            for t4 in range(4):
                i = g4 * 4 + t4
                for kc in range(8):
                    kb.op("pe", lambda e, pb=pb, wb=wb, kc=kc, i=i, t4=t4: e.matmul(ps[pb][:, t4 * 128:(t4 + 1) * 128], xT[:, kc, i * 128:(i + 1) * 128], wh[wb][:, kc, 2, :], start=(kc == 0), stop=(kc == 7)),
                          reads=[wh_r[wb], xT_r[i]], writes=[ps_r[pb]])
            kb.op("dve", lambda e, pb=pb, g4=g4: e.tensor_copy(V[:, g4 * 4:g4 * 4 + 4, :], ps[pb][:].rearrange("p (c t) -> p c t", c=4)),
                  reads=[ps_r[pb]], writes=[V_r[g4]])
        pti = 0
        for qs in range(NS):
            nkt = qs * 4 + 4
            for kt in range(nkt):
                for c in range(2):
                    pb = nps()
                    kb.op("pe", lambda e, pb=pb, c=c, kt=kt, qs=qs: e.matmul(ps[pb][:], KT[c * 64:(c + 1) * 64, kt * 128:(kt + 1) * 128], QT[c * 64:(c + 1) * 64, qs * 512:(qs + 1) * 512], start=True, stop=True),
                          reads=[KT_r[kt // 4], QT_r[qs]], writes=[ps_r[pb]])
                    pi = pti % 4
                    pti += 1
                    kb.op("act", lambda e, pb=pb, pi=pi: e.activation(pt[pi][:], ps[pb][:], AF.Exp), reads=[ps_r[pb]], writes=[pt_r[pi]])
                    if kt >= qs * 4:
                        o = kt - qs * 4
                        kb.op("pool", lambda e, pi=pi, o=o: e.tensor_tensor(pt[pi][:], pt[pi][:], C.cmask[:, o, :], op=ALU.mult), reads=[pt_r[pi], C.r], writes=[pt_r[pi]])
                    kb.op("pe", lambda e, c=c, kt=kt, pi=pi, nkt=nkt: e.matmul(ps[2 + c][:], V[:, kt, :], pt[pi][:], start=(kt == 0), stop=(kt == nkt - 1)),
                          reads=[V_r[kt // 4], pt_r[pi]], writes=[ps_r[2 + c]])
                    kb.op("pe", lambda e, c=c, kt=kt, pi=pi, nkt=nkt: e.matmul(ps[4 + c][:], C.ones_b[:], pt[pi][:], start=(kt == 0), stop=(kt == nkt - 1)),
                          reads=[C.r, pt_r[pi]], writes=[ps_r[4 + c]])
            kb.op("act", lambda e: e.activation(s1[:], ps[4][:], AF.Copy), reads=[ps_r[4]], writes=[fin_r])
            kb.op("act", lambda e: e.activation(s2[:], ps[5][:], AF.Copy), reads=[ps_r[5]], writes=[fin_r])
            kb.op("dve", lambda e: e.tensor_tensor(t1[:], ps[2][:], s2[:], op=ALU.mult), reads=[ps_r[2], fin_r], writes=[fin2_r])
            kb.op("dve", lambda e: e.tensor_tensor(t2[:], ps[3][:], s1[:], op=ALU.mult), reads=[ps_r[3], fin_r], writes=[fin2_r])
            kb.op("dve", lambda e: e.scalar_tensor_tensor(op_[:], t2[:], nlam, t1[:], op0=ALU.mult, op1=ALU.add), reads=[fin2_r, lam_r], writes=[fin2_r])
            kb.op("pool", lambda e: e.tensor_tensor(sq[:], op_[:], op_[:], op=ALU.mult), reads=[fin2_r], writes=[fin2_r])
            kb.op("pool", lambda e: e.tensor_tensor(s12[:], s1[:], s2[:], op=ALU.mult), reads=[fin_r], writes=[fin2_r])
            kb.op("dve", lambda e: e.scalar_tensor_tensor(e2[:], s12[:], RMS_EPS, s12[:], op0=ALU.mult, op1=ALU.mult), reads=[fin2_r], writes=[fin2_r])
            kb.op("pe", lambda e: e.matmul(ps[6][:], C.ones_b[:], sq[:], start=True, stop=True), reads=[C.r, fin2_r], writes=[ps_r[6]])
            kb.op("dve", lambda e: e.scalar_tensor_tensor(tot[:], ps[6][:], 1.0 / 128.0, e2[:], op0=ALU.mult, op1=ALU.add), reads=[ps_r[6], fin2_r], writes=[fin2_r])
            kb.op("act", lambda e: e.activation(tot[:], tot[:], AF.Ln), reads=[fin2_r], writes=[fin2_r])
            kb.op("act", lambda e: e.activation(rstd[:], tot[:], AF.Exp, scale=-0.5), reads=[fin2_r], writes=[fin2_r])
            kb.op("dve", lambda e, h=h, qs=qs: e.scalar_tensor_tensor(OT[:, h, qs * 512:(qs + 1) * 512], op_[:], gsc, rstd[:], op0=ALU.mult, op1=ALU.mult),
                  reads=[fin2_r, lam_r], writes=[OT_r[h][qs]])

    kb_pop(kb)
    wo = kb.sb("a_wo", [128, 8, 1024], BF16)
    wo_r = Res()
    wo_src = prm["attn_w_o"][j].rearrange("(h p) n -> p h n", p=128)
    for hh in range(8):
        kb.dma("pool", lambda e, hh=hh: e.dma_start(out=wo[:, hh, :], in_=wo_src[:, hh, :]), writes=[wo_r])
    xld = [kb.sb("a_xldc%d" % b, [128, 1024], F32) for b in range(2)]
    xld_r = RL(2)
    zt = [kb.sb("a_z%d" % b, [128, 1024], F32) for b in range(2)]
    zt_r = RL(2)
    x1 = [kb.sb("a_x1%d" % b, [128, 1024], F32) for b in range(2)]
    x1_r = RL(2)
    LS = ln_scratch(kb, "a_ln", LN_EPS)
    g_bc, b_bc, lnp_r = load_ln_params(kb, "a_lnp", prm["ln1_g"], prm["ln1_b"], li)
    for i in range(NT):
        b = i % 2
        kb.dma("sp", lambda e, i=i, b=b: e.dma_start(out=xld[b][:], in_=xin[i * 128:(i + 1) * 128, :]), reads=[xin_r[i]], writes=[xld_r[b]])
        for hf in range(2):
            pb = 2 * b + hf
            for hh in range(8):
                kb.op("pe", lambda e, pb=pb, hh=hh, hf=hf, i=i: e.matmul(ps[pb][:], OT[:, hh, i * 128:(i + 1) * 128], wo[:, hh, hf * 512:(hf + 1) * 512], start=(hh == 0), stop=(hh == 7)),
                      reads=[OT_r[hh][i // 4], wo_r], writes=[ps_r[pb]])
            kb.op("dve", lambda e, pb=pb, hf=hf, b=b: e.scalar_tensor_tensor(zt[b][:, hf * 512:(hf + 1) * 512], xld[b][:, hf * 512:(hf + 1) * 512], ALPHA, ps[pb][:], op0=ALU.mult, op1=ALU.add),
                  reads=[xld_r[b], ps_r[pb]], writes=[zt_r[b]])
        ln_tile(kb, LS, zt[b], zt_r[b], x1[b][:], x1_r[b], g_bc, b_bc, lnp_r)
        kb.dma("sp", lambda e, i=i, b=b: e.dma_start(out=xa[i * 128:(i + 1) * 128, :], in_=x1[b][:]), reads=[x1_r[b]], writes=[xa_r[i]])
    kb_pop(kb)


PSHAPES = {
    "ln1_g": (4, 1024), "ln1_b": (4, 1024), "ln2_g": (4, 1024), "ln2_b": (4, 1024),
    "attn_w_qkv": (2, 1024, 3072), "attn_w_o": (2, 1024, 1024), "attn_lambda": (2, 4, 64), "attn_subln_g": (2, 128),
    "rw_mu": (2, 6, 1024), "rw_w_rkv": (2, 3, 1024, 1024), "rw_w_o": (2, 1024, 1024), "rw_w0": (2, 1024),
    "rw_w1": (2, 1024, 64), "rw_w2": (2, 64, 1024), "rw_a0": (2, 1024), "rw_a1": (2, 1024, 64), "rw_a2": (2, 64, 1024),
    "rw_g1": (2, 1024, 160), "rw_g2": (2, 160, 1024), "rw_k_k": (2, 1024), "rw_k_a": (2, 1024), "rw_r_k": (2, 16, 64),
    "rw_lnx_g": (2, 1024), "rw_lnx_b": (2, 1024), "rw_v0": (1, 1024), "rw_v1": (1, 1024, 32), "rw_v2": (1, 32, 1024),
    "moe_rg_w": (4, 1024, 4), "moe_rg_b": (4, 4), "moe_re_w": (4, 1024, 32), "moe_re_b": (4, 32),
    "moe_w_gu": (4, 32, 1024, 1024), "moe_w_down": (4, 32, 512, 1024),
}


class Params(dict):
    def __init__(self, nc):
        super().__init__()
        self.nc = nc

    def __missing__(self, k):
        ap = self.nc.dram_tensor(k, list(PSHAPES[k]), F32, kind="ExternalInput").ap()
        self[k] = ap
        return ap


def build(T, plan, cap=512):
    nc = bass.Bass("TRN2", target_bir_lowering=False)
    prm = Params(nc)
    x = nc.dram_tensor("x", [T, D], F32, kind="ExternalInput").ap()
    out = nc.dram_tensor("out", [T, D], F32, kind="ExternalOutput").ap()
    xa = nc.dram_tensor("xa_s", [T, D], F32, kind="Internal").ap()
    xb = nc.dram_tensor("xb_s", [T, D], F32, kind="Internal").ap()
    NT = T // 128
    es = ExitStack()
    with es:
        kb = KB(nc, es)
        kb.es_t = es
        C = make_consts(kb)
        cur, cur_r = x, RL(NT)
        xa_r, xb_r, out_r = RL(NT), RL(NT), RL(NT)
        ST = {}
        for n, step in enumerate(plan):
            last = n == len(plan) - 1
            kind = step[0]
            if kind == "attn":
                attn_phase(kb, C, T, prm, step[1], step[2], cur, cur_r, xa, xa_r)
                cur, cur_r = xa, xa_r
            elif kind == "rwkv":
                rwkv_phase(kb, C, T, prm, step[1], step[2], cur, cur_r, xa, xa_r, ST, nc)
                cur, cur_r = xa, xa_r
            elif kind == "moe":
                dst, dst_r = (out, out_r) if last else (xb, xb_r)
                moe_phase(kb, C, T, prm, step[1], cur, cur_r, dst, dst_r, cap, nc, ST)
                cur, cur_r = dst, dst_r
        if cur is not out:
            for i in range(NT):
                kb.dma("sp", lambda e, i=i: e.dma_start(out=out[i * 128:(i + 1) * 128, :], in_=cur[i * 128:(i + 1) * 128, :]), reads=[cur_r[i]], writes=[out_r[i]])
        kb.finish()
        ninst = kb.nins
    return nc, list(prm.keys()), ninst


FULL_PLAN = [("attn", 0, 0), ("moe", 0), ("rwkv", 0, 1), ("moe", 1), ("attn", 1, 2), ("moe", 2), ("rwkv", 1, 3), ("moe", 3)]


def moe_phase(kb, C, T, prm, li, xin, xin_r, dst, dst_r, cap, nc, ST):
    NT = T // 128
    NSLOT = NE * cap
    NB = cap // 128
    if "xbuf" not in ST:
        ST["xbuf"] = nc.dram_tensor("xbuf_s", [NSLOT, D], BF16, kind="Internal").ap()
        ST["ybuf"] = nc.dram_tensor("ybuf_s", [NSLOT, D], F32, kind="Internal").ap()
        ST["bc_reg"] = nc.gpsimd.to_reg(NSLOT - 1)
    xbuf, ybuf = ST["xbuf"], ST["ybuf"]
    bc_reg = ST["bc_reg"]
    kb_push(kb)
    slots = kb.sb("m_slots", [128, NT, 2], I32)
    gates = kb.sb("m_gates", [128, NT, 2], F32)
    sg_r = Res()

    kb_push(kb)
    ps = [kb.ps("m1_ps%d" % b, [128, 512], F32) for b in range(6)]
    ps_r = RL(6)
    xt = [kb.sb("m1_xt%d" % b, [128, 1024], F32) for b in range(2)]
    xt_r = RL(2)
    xb16 = [kb.sb("m1_xb%d" % b, [128, 1024], BF16) for b in range(2)]
    xb_r = RL(2)
    xT = [kb.sb("m1_xT%d" % b, [128, 8, 128], F32) for b in range(2)]
    xT_r = RL(2)
    wr = kb.sb("m1_wr", [128, 8, 36], F32)
    rb = kb.sb("m1_rb", [128, 36], F32)
    offs_i = kb.sb("m1_offi", [128, 32], I32)
    offs = kb.sb("m1_off", [128, 32], F32)
    base = kb.sb("m1_base", [128, 32], F32)
    cr = Res()
    base_r = Res()
    kb.dma("sp", lambda e: e.dma_start(out=wr[:, :, 0:4], in_=prm["moe_rg_w"][li].rearrange("(kc p) n -> p kc n", p=128)), writes=[cr])
    kb.dma("sp", lambda e: e.dma_start(out=wr[:, :, 4:36], in_=prm["moe_re_w"][li].rearrange("(kc p) n -> p kc n", p=128)), writes=[cr])
    kb.dma("sp", lambda e: e.dma_start(out=rb[:, 0:4], in_=bcast_rows(prm["moe_rg_b"][li:li + 1, :])), writes=[cr])
    kb.dma("sp", lambda e: e.dma_start(out=rb[:, 4:36], in_=bcast_rows(prm["moe_re_b"][li:li + 1, :])), writes=[cr])
    kb.op("pool", lambda e: e.iota(offs_i[:], [[cap, 32]], base=-1, channel_multiplier=0), writes=[cr])
    kb.op("dve", lambda e: e.tensor_copy(offs[:], offs_i[:]), reads=[cr], writes=[cr])
    kb.op("dve", lambda e: e.memset(base[:], 0.0), writes=[base_r])
    Wk = {}
    for nm, w in (("L", 36), ("ohg", 4), ("eg", 4), ("lsel", 8), ("oh1", 8), ("lsel2", 8), ("oh2", 8), ("E1", 32), ("E2", 32),
                  ("val", 32), ("valid", 32), ("val2", 32), ("tmp", 32), ("sc", 16)):
        Wk[nm] = kb.sb("m1_w" + nm, [128, w], F32)
    G01 = kb.sb("m1_G01", [128, 32], BF16)
    wr_ = Res()
    BIG = float(NSLOT)
    for i in range(NT):
        b = i % 2
        kb.dma("sp", lambda e, i=i, b=b: e.dma_start(out=xt[b][:], in_=xin[i * 128:(i + 1) * 128, :]), reads=[xin_r[i]], writes=[xt_r[b]])
        kb.op("act", lambda e, b=b: e.activation(xb16[b][:], xt[b][:], AF.Copy), reads=[xt_r[b]], writes=[xb_r[b]])
        for hf in range(2):
            pb = hf
            for c4 in range(4):
                kc = hf * 4 + c4
                kb.op("pe", lambda e, pb=pb, c4=c4, kc=kc, b=b: e.transpose(ps[pb][:, c4 * 128:(c4 + 1) * 128], xt[b][:, kc * 128:(kc + 1) * 128], C.ident_f[:]),
                      reads=[xt_r[b], C.r], writes=[ps_r[pb]])
            if hf == 0:
                kb.op("act", lambda e, pb=pb, b=b: e.activation(xT[b][:, 0:4, :], ps[pb][:].rearrange("p (c t) -> p c t", c=4), AF.Copy), reads=[ps_r[pb]], writes=[xT_r[b]])
            else:
                kb.op("dve", lambda e, pb=pb, b=b: e.tensor_copy(xT[b][:, 4:8, :], ps[pb][:].rearrange("p (c t) -> p c t", c=4)), reads=[ps_r[pb]], writes=[xT_r[b]])
        for kc in range(8):
            kb.op("pe", lambda e, kc=kc, b=b: e.matmul(ps[2][:, 0:36], xT[b][:, kc, :], wr[:, kc, :], start=(kc == 0), stop=(kc == 7)), reads=[xT_r[b], cr], writes=[ps_r[2]])
        L, ohg, eg, lsel, oh1, lsel2, oh2, E1, E2 = (Wk[k] for k in ("L", "ohg", "eg", "lsel", "oh1", "lsel2", "oh2", "E1", "E2"))
        val, valid, val2, tmp, sc = (Wk[k] for k in ("val", "valid", "val2", "tmp", "sc"))

        def dv(fn, extra_r=(), extra_w=()):
            kb.op("dve", fn, reads=[wr_] + list(extra_r), writes=[wr_] + list(extra_w))

        dv(lambda e: e.tensor_tensor(L[:], ps[2][:, 0:36], rb[:], op=ALU.add), extra_r=[ps_r[2], cr])
        dv(lambda e: e.reduce_max(sc[:, 0:1], L[:, 0:4], axis=AX.X))
        dv(lambda e: e.tensor_scalar(ohg[:], L[:, 0:4], sc[:, 0:1], None, op0=ALU.is_equal))
        dv(lambda e: e.tensor_scalar(sc[:, 1:2], sc[:, 0:1], -1.0, None, op0=ALU.mult))
        kb.op("act", lambda e: e.activation(eg[:], L[:, 0:4], AF.Exp, bias=sc[:, 1:2]), reads=[wr_], writes=[wr_])
        dv(lambda e: e.reduce_sum(sc[:, 2:3], eg[:], axis=AX.X))
        dv(lambda e: e.reciprocal(sc[:, 3:4], sc[:, 2:3]))
        dv(lambda e: e.tensor_scalar(lsel[:], L[:, 4:12], ohg[:, 0:1], None, op0=ALU.mult))
        for g in range(1, 4):
            dv(lambda e, g=g: e.scalar_tensor_tensor(lsel[:], L[:, 4 + 8 * g:12 + 8 * g], ohg[:, g:g + 1], lsel[:], op0=ALU.mult, op1=ALU.add))
        dv(lambda e: e.reduce_max(sc[:, 4:5], lsel[:], axis=AX.X))
        dv(lambda e: e.tensor_scalar(oh1[:], lsel[:], sc[:, 4:5], None, op0=ALU.is_equal))
        dv(lambda e: e.scalar_tensor_tensor(lsel2[:], oh1[:], -1e30, lsel[:], op0=ALU.mult, op1=ALU.add))
        dv(lambda e: e.reduce_max(sc[:, 5:6], lsel2[:], axis=AX.X))
        dv(lambda e: e.tensor_scalar(oh2[:], lsel2[:], sc[:, 5:6], None, op0=ALU.is_equal))
        dv(lambda e: e.tensor_tensor(sc[:, 6:7], sc[:, 5:6], sc[:, 4:5], op=ALU.subtract))
        kb.op("act", lambda e: e.activation(sc[:, 7:8], sc[:, 6:7], AF.Exp), reads=[wr_], writes=[wr_])
        dv(lambda e: e.tensor_scalar(sc[:, 8:9], sc[:, 7:8], 1.0, None, op0=ALU.add))
        dv(lambda e: e.reciprocal(sc[:, 9:10], sc[:, 8:9]))
        dv(lambda e: e.tensor_tensor(sc[:, 10:11], sc[:, 7:8], sc[:, 9:10], op=ALU.mult))
        for g in range(4):
            dv(lambda e, g=g: e.tensor_scalar(E1[:, 8 * g:8 * g + 8], oh1[:], ohg[:, g:g + 1], None, op0=ALU.mult))
            dv(lambda e, g=g: e.tensor_scalar(E2[:, 8 * g:8 * g + 8], oh2[:], ohg[:, g:g + 1], None, op0=ALU.mult))
        dv(lambda e: e.tensor_tensor(G01[:], E1[:], E2[:], op=ALU.add))
        kb.op("pe", lambda e: e.matmul(ps[3][:, 0:32], C.triu_b[:], G01[:], start=True, stop=True), reads=[wr_, C.r], writes=[ps_r[3]])
        kb.op("pe", lambda e: e.matmul(ps[3][:, 32:64], C.ones_b[:], G01[:], start=True, stop=True), reads=[wr_, C.r], writes=[ps_r[3]])
        dv(lambda e: e.tensor_tensor(val[:], ps[3][:, 0:32], base[:], op=ALU.add), extra_r=[ps_r[3], base_r])
        dv(lambda e: e.tensor_scalar(valid[:], val[:], float(cap), None, op0=ALU.is_le))
        dv(lambda e: e.tensor_tensor(val2[:], val[:], offs[:], op=ALU.add), extra_r=[cr])
        dv(lambda e: e.scalar_tensor_tensor(val2[:], val2[:], -BIG, valid[:], op0=ALU.add, op1=ALU.mult))
        dv(lambda e: e.tensor_scalar(val2[:], val2[:], BIG, None, op0=ALU.add))
        dv(lambda e: e.tensor_tensor(tmp[:], E1[:], val2[:], op=ALU.mult))
        dv(lambda e: e.reduce_sum(sc[:, 11:12], tmp[:], axis=AX.X))
        dv(lambda e: e.tensor_tensor(tmp[:], E2[:], val2[:], op=ALU.mult))
        dv(lambda e: e.reduce_sum(sc[:, 12:13], tmp[:], axis=AX.X))
        dv(lambda e: e.tensor_tensor(tmp[:], E1[:], valid[:], op=ALU.mult))
        dv(lambda e: e.reduce_sum(sc[:, 13:14], tmp[:], axis=AX.X))
        dv(lambda e: e.tensor_tensor(tmp[:], E2[:], valid[:], op=ALU.mult))
        dv(lambda e: e.reduce_sum(sc[:, 14:15], tmp[:], axis=AX.X))
        dv(lambda e: e.tensor_tensor(base[:], base[:], ps[3][:, 32:64], op=ALU.add), extra_r=[ps_r[3]], extra_w=[base_r])
        dv(lambda e, i=i: e.tensor_copy(slots[:, i, :], sc[:, 11:13]), extra_w=[sg_r])
        dv(lambda e: e.tensor_scalar(sc[:, 9:11], sc[:, 9:11], sc[:, 3:4], None, op0=ALU.mult))
        dv(lambda e, i=i: e.tensor_tensor(gates[:, i, :], sc[:, 9:11], sc[:, 13:15], op=ALU.mult), extra_w=[sg_r])
        for k in range(2):
            kb.dma("pool", lambda e, i=i, k=k, b=b: e.indirect_dma_start(out=xbuf[:, :], out_offset=bass.IndirectOffsetOnAxis(ap=slots[:, i, k:k + 1], axis=0),
                                                                       in_=xb16[b][:], in_offset=None, bounds_check=bc_reg, oob_is_err=False),
                   reads=[sg_r, xb_r[b]])
    kb_pop(kb)

    kb_push(kb)
    psT = [kb.ps("m2_pT%d" % b, [128, 1024], BF16) for b in range(2)]
    psT_r = RL(2)
    psH = [kb.ps("m2_pH%d" % b, [128, 512], F32) for b in range(4)]
    psH_r = RL(4)
    psY = [kb.ps("m2_pY%d" % b, [128, 512], F32) for b in range(2)]
    psY_r = RL(2)
    wgu = [kb.sb("m2_wgu%d" % b, [128, 8, 1024], BF16) for b in range(2)]
    wgu_r = RL(2)
    wd = [kb.sb("m2_wd%d" % b, [128, 4, 1024], BF16) for b in range(2)]
    wd_r = RL(2)
    xblk = [kb.sb("m2_xb%d" % b, [128, 1024], BF16) for b in range(2)]
    xblk_r = RL(2)
    XT = [kb.sb("m2_XT%d" % b, [128, 8, cap], BF16) for b in range(2)]
    XT_r = RL(2)
    sil = [kb.sb("m2_sil%d" % b, [128, cap], F32) for b in range(2)]
    sil_r = RL(2)
    AT = [kb.sb("m2_AT%d" % b, [128, 4, cap], BF16) for b in range(2)]
    AT_r = RL(2)
    ysb = [kb.sb("m2_y%d" % b, [128, 1024], F32) for b in range(2)]
    ysb_r = RL(2)
    nblk = 0
    for ex in range(NE):
        wb = ex % 2
        gsrc = prm["moe_w_gu"][li, ex].rearrange("(kc p) n -> p kc n", p=128)
        dsrc = prm["moe_w_down"][li, ex].rearrange("(m p) n -> p m n", p=128)
        for kc in range(8):
            kb.dma("pool", lambda e, wb=wb, kc=kc, gsrc=gsrc: e.dma_start(out=wgu[wb][:, kc, :], in_=gsrc[:, kc, :]), writes=[wgu_r[wb]])
        for m in range(4):
            kb.dma("pool", lambda e, wb=wb, m=m, dsrc=dsrc: e.dma_start(out=wd[wb][:, m, :], in_=dsrc[:, m, :]), writes=[wd_r[wb]])
        for blk in range(NB):
            bb = nblk % 2
            nblk += 1
            r0 = ex * cap + blk * 128
            kb.dma("sp", lambda e, bb=bb, r0=r0: e.dma_start(out=xblk[bb][:], in_=xbuf[r0:r0 + 128, :]), writes=[xblk_r[bb]])
            for kc in range(8):
                kb.op("pe", lambda e, bb=bb, kc=kc: e.transpose(psT[bb][:, kc * 128:(kc + 1) * 128], xblk[bb][:, kc * 128:(kc + 1) * 128], C.ident_b[:]),
                      reads=[xblk_r[bb], C.r], writes=[psT_r[bb]])
            eng = "act" if blk % 2 == 0 else "dve"
            if eng == "act":
                kb.op("act", lambda e, bb=bb, wb=wb, blk=blk: e.activation(XT[wb][:, :, blk * 128:(blk + 1) * 128], psT[bb][:].rearrange("p (c t) -> p c t", c=8), AF.Copy),
                      reads=[psT_r[bb]], writes=[XT_r[wb]])
            else:
                kb.op("dve", lambda e, bb=bb, wb=wb, blk=blk: e.tensor_copy(XT[wb][:, :, blk * 128:(blk + 1) * 128], psT[bb][:].rearrange("p (c t) -> p c t", c=8)),
                      reads=[psT_r[bb]], writes=[XT_r[wb]])
        for m in range(4):
            pg, pu = (m % 2) * 2, (m % 2) * 2 + 1
            for (pb, col) in ((pg, m * 128), (pu, 512 + m * 128)):
                for kc in range(8):
                    kb.op("pe", lambda e, pb=pb, col=col, kc=kc, wb=wb: e.matmul(psH[pb][:, 0:cap], wgu[wb][:, kc, col:col + 128], XT[wb][:, kc, :], start=(kc == 0), stop=(kc == 7)),
                          reads=[wgu_r[wb], XT_r[wb]], writes=[psH_r[pb]])
            sb_ = m % 2
            kb.op("act", lambda e, pg=pg, sb_=sb_: e.activation(sil[sb_][:], psH[pg][:, 0:cap], AF.Silu), reads=[psH_r[pg]], writes=[sil_r[sb_]])
            kb.op("dve", lambda e, pu=pu, sb_=sb_, m=m, wb=wb: e.tensor_tensor(AT[wb][:, m, :], sil[sb_][:], psH[pu][:, 0:cap], op=ALU.mult),
                  reads=[psH_r[pu], sil_r[sb_]], writes=[AT_r[wb]])
        for blk in range(NB):
            yb = blk % 2
            for hf in range(2):
                for m in range(4):
                    kb.op("pe", lambda e, hf=hf, m=m, wb=wb, blk=blk: e.matmul(psY[hf][:], AT[wb][:, m, blk * 128:(blk + 1) * 128], wd[wb][:, m, hf * 512:(hf + 1) * 512], start=(m == 0), stop=(m == 3)),
                          reads=[AT_r[wb], wd_r[wb]], writes=[psY_r[hf]])
                if hf == 0:
                    kb.op("act", lambda e, yb=yb: e.activation(ysb[yb][:, 0:512], psY[0][:], AF.Copy), reads=[psY_r[0]], writes=[ysb_r[yb]])
                else:
                    kb.op("dve", lambda e, yb=yb: e.tensor_copy(ysb[yb][:, 512:1024], psY[1][:]), reads=[psY_r[1]], writes=[ysb_r[yb]])
            r0 = ex * cap + blk * 128
            kb.dma("sp", lambda e, yb=yb, r0=r0: e.dma_start(out=ybuf[r0:r0 + 128, :], in_=ysb[yb][:]), reads=[ysb_r[yb]])
    kb_pop(kb)

    kb_push(kb)
    y1 = [kb.sb("m3_y1%d" % b, [128, 1024], F32) for b in range(2)]
    y2 = [kb.sb("m3_y2%d" % b, [128, 1024], F32) for b in range(2)]
    y_r = RL(2)
    xt3 = [kb.sb("m3_xt%d" % b, [128, 1024], F32) for b in range(2)]
    xt3_r = RL(2)
    z3 = [kb.sb("m3_z%d" % b, [128, 1024], F32) for b in range(2)]
    z3_r = RL(2)
    x2 = [kb.sb("m3_x2%d" % b, [128, 1024], F32) for b in range(2)]
    x2_r = RL(2)
    LS = ln_scratch(kb, "m3_ln", LN_EPS)
    g_bc, b_bc, lnp_r = load_ln_params(kb, "m3_lnp", prm["ln2_g"], prm["ln2_b"], li)
    for b in range(2):
        kb.op("pool", lambda e, b=b: e.memset(y1[b][:], 0.0), writes=[y_r[b]])
        kb.op("pool", lambda e, b=b: e.memset(y2[b][:], 0.0), writes=[y_r[b]])
    for i in range(NT):
        b = i % 2
        kb.dma("sp", lambda e, i=i, b=b: e.dma_start(out=xt3[b][:], in_=xin[i * 128:(i + 1) * 128, :]), reads=[xin_r[i]], writes=[xt3_r[b]])
        for k, yy in ((0, y1), (1, y2)):
            kb.dma("pool", lambda e, i=i, k=k, b=b, yy=yy: e.indirect_dma_start(out=yy[b][:], out_offset=None, in_=ybuf[:, :],
                                                                              in_offset=bass.IndirectOffsetOnAxis(ap=slots[:, i, k:k + 1], axis=0),
                                                                              bounds_check=bc_reg, oob_is_err=False),
                   reads=[sg_r], writes=[y_r[b]])
        kb.op("act", lambda e, b=b: e.activation(z3[b][:], xt3[b][:], AF.Copy, scale=ALPHA), reads=[xt3_r[b]], writes=[z3_r[b]])
        kb.op("dve", lambda e, b=b, i=i: e.scalar_tensor_tensor(z3[b][:], y1[b][:], gates[:, i, 0:1], z3[b][:], op0=ALU.mult, op1=ALU.add), reads=[y_r[b], sg_r], writes=[z3_r[b]])
        kb.op("dve", lambda e, b=b, i=i: e.scalar_tensor_tensor(z3[b][:], y2[b][:], gates[:, i, 1:2], z3[b][:], op0=ALU.mult, op1=ALU.add), reads=[y_r[b], sg_r], writes=[z3_r[b]])
        ln_tile(kb, LS, z3[b], z3_r[b], x2[b][:], x2_r[b], g_bc, b_bc, lnp_r)
        kb.dma("sp", lambda e, i=i, b=b: e.dma_start(out=dst[i * 128:(i + 1) * 128, :], in_=x2[b][:]), reads=[x2_r[b]], writes=[dst_r[i]])
    kb_pop(kb)
    kb_pop(kb)


C0 = math.exp(-0.5)


def rwkv_phase(kb, C, T, prm, j, li, xin, xin_r, xa, xa_r, ST, nc):
    NT = T // 128
    NSUP = T // 256
    if "ARd" not in ST:
        ST["ARd"] = nc.dram_tensor("ARd_s", [NT, 128, 8 * 2 * 128], BF16, kind="Internal").ap()
        ST["BKd"] = nc.dram_tensor("BKd_s", [NT, 128, 8 * 2 * 128], BF16, kind="Internal").ap()
        ST["rkd"] = nc.dram_tensor("rkd_s", [NT, 128, 8 * 128], BF16, kind="Internal").ap()
        ST["Pcd"] = nc.dram_tensor("Pcd_s", [NT, 128, 8], F32, kind="Internal").ap()
        ST["Vd"] = nc.dram_tensor("Vd_s", [T, D], BF16, kind="Internal").ap()
        ST["Gd"] = nc.dram_tensor("Gd_s", [T, D], BF16, kind="Internal").ap()
        ST["vfirst"] = nc.dram_tensor("vfirst_s", [T, D], F32, kind="Internal").ap()
    ARd, BKd, rkd, Pcd, Vd, Gd, vfd = (ST[k] for k in ("ARd", "BKd", "rkd", "Pcd", "Vd", "Gd", "vfirst"))

    kb_push(kb)
    ps = [kb.ps("r1_ps%d" % b, [128, 512], F32) for b in range(8)]
    ps_r = RL(8)
    wrkv = kb.sb("r1_wrkv", [128, 3, 8, 1024], BF16)
    w1 = kb.sb("r1_w1", [128, 8, 64], BF16)
    a1 = kb.sb("r1_a1", [128, 8, 64], BF16)
    g1 = kb.sb("r1_g1", [128, 8, 160], BF16)
    w2 = kb.sb("r1_w2", [64, 1024], BF16)
    a2 = kb.sb("r1_a2", [64, 1024], BF16)
    g2a = kb.sb("r1_g2a", [128, 1024], BF16)
    g2b = kb.sb("r1_g2b", [32, 1024], BF16)
    wr_ = Res()
    for n in range(3):
        src = prm["rw_w_rkv"][j, n].rearrange("(kc p) n -> p kc n", p=128)
        for kc in range(8):
            kb.dma("pool", lambda e, n=n, kc=kc, src=src: e.dma_start(out=wrkv[:, n, kc, :], in_=src[:, kc, :]), writes=[wr_])
    kb.dma("pool", lambda e: e.dma_start(out=w1[:], in_=prm["rw_w1"][j].rearrange("(kc p) n -> p kc n", p=128)), writes=[wr_])
    kb.dma("pool", lambda e: e.dma_start(out=a1[:], in_=prm["rw_a1"][j].rearrange("(kc p) n -> p kc n", p=128)), writes=[wr_])
    kb.dma("pool", lambda e: e.dma_start(out=g1[:], in_=prm["rw_g1"][j].rearrange("(kc p) n -> p kc n", p=128)), writes=[wr_])
    kb.dma("pool", lambda e: e.dma_start(out=w2[:], in_=prm["rw_w2"][j]), writes=[wr_])
    kb.dma("pool", lambda e: e.dma_start(out=a2[:], in_=prm["rw_a2"][j]), writes=[wr_])
    kb.dma("pool", lambda e: e.dma_start(out=g2a[:], in_=prm["rw_g2"][j, 0:128, :]), writes=[wr_])
    kb.dma("pool", lambda e: e.dma_start(out=g2b[:], in_=prm["rw_g2"][j, 128:160, :]), writes=[wr_])
    if j > 0:
        v1 = kb.sb("r1_v1", [128, 8, 32], BF16)
        v2 = kb.sb("r1_v2", [32, 1024], BF16)
        v0b = kb.sb("r1_v0b", [128, 1024], F32)
        kb.dma("pool", lambda e: e.dma_start(out=v1[:], in_=prm["rw_v1"][j - 1].rearrange("(kc p) n -> p kc n", p=128)), writes=[wr_])
        kb.dma("pool", lambda e: e.dma_start(out=v2[:], in_=prm["rw_v2"][j - 1]), writes=[wr_])
        kb.dma("sp", lambda e: e.dma_start(out=v0b[:], in_=bcast_rows(prm["rw_v0"][j - 1:j, :])), writes=[wr_])
    pvin = kb.sb("r1_pvin", [88, 128], F32)
    pvall = kb.sb("r1_pvall", [128, 88], F32)
    oma = kb.sb("r1_oma", [128, 8], F32)
    pv_r = Res()
    kb.dma("sp", lambda e: e.dma_start(out=pvin[0:48, :], in_=prm["rw_mu"][j].rearrange("n (kc p) -> (n kc) p", p=128)), writes=[pv_r])
    for idx, nm in enumerate(("rw_w0", "rw_a0", "rw_k_k", "rw_k_a")):
        kb.dma("sp", lambda e, idx=idx, nm=nm: e.dma_start(out=pvin[48 + 8 * idx:56 + 8 * idx, :], in_=prm[nm][j].rearrange("(oc p) -> oc p", p=128)), writes=[pv_r])
    kb.dma("sp", lambda e: e.dma_start(out=pvin[80:88, :], in_=prm["rw_r_k"][j].rearrange("(oc hh) n -> oc (hh n)", hh=2)), writes=[pv_r])
    kb.op("pe", lambda e: e.transpose(ps[0][:, 0:88], pvin[:], C.ident_f[0:88, 0:88]), reads=[pv_r, C.r], writes=[ps_r[0]])
    kb.op("dve", lambda e: e.tensor_copy(pvall[:], ps[0][:, 0:88]), reads=[ps_r[0]], writes=[pv_r])
    kb.op("dve", lambda e: e.tensor_scalar(oma[:], pvall[:, 72:80], -1.0, 1.0, op0=ALU.mult, op1=ALU.add), reads=[pv_r], writes=[pv_r])

    class _PV:
        def __getitem__(self, key):
            p, idx, oc = key
            if idx == 5:
                return oma[p, oc]
            return pvall[p, (oc.start + 48 + 8 * idx):(oc.stop + 48 + 8 * idx)]

    class _MU:
        def __getitem__(self, key):
            p, n, kc = key
            return pvall[p, (n * 8 + kc.start):(n * 8 + kc.stop)]

    pv = _PV()
    mu = _MU()
    rst = kb.sb("r1_rst", [128, 256], F32)
    kb.op("dve", lambda e: e.memset(rst[:], 1.0), writes=[pv_r])
    kb.op("dve", lambda e: e.memset(rst[:, 0:1], 0.0), writes=[pv_r])
    kb.op("dve", lambda e: e.memset(rst[:, 128:129], 0.0), writes=[pv_r])
    bd64 = kb.sb("r1_bd64", [128, 128], BF16)
    kb.op("dve", lambda e: e.memset(bd64[:], 0.0), writes=[pv_r])
    kb.op("dve", lambda e: e.memset(bd64[0:64, 0:64], 1.0), writes=[pv_r])
    kb.op("dve", lambda e: e.memset(bd64[64:128, 64:128], 1.0), writes=[pv_r])

    xld = [kb.sb("r1_xld%d" % b, [128, 1024], F32) for b in range(2)]
    xld_r = RL(2)
    xTs = [kb.sb("r1_xTs%d" % b, [128, 8, 257], BF16) for b in range(2)]
    xTs_r = RL(2)
    xx = kb.sb("r1_xx", [128, 8, 256], F32)
    xx_r = Res()
    xm = [kb.sb("r1_xm%d" % b, [128, 8, 256], BF16) for b in range(3)]
    xm_r = RL(3)
    AR = kb.sb("r1_AR", [128, 8, 2, 2, 128], BF16)
    BK = kb.sb("r1_BK", [128, 8, 2, 2, 128], BF16)
    rk = kb.sb("r1_rk", [128, 8, 256], BF16)
    Pc = kb.sb("r1_Pc", [128, 2, 8], F32)
    out_r = Res()
    hw = kb.sb("r1_hw", [64, 256], BF16)
    ha = kb.sb("r1_ha", [64, 256], BF16)
    hg1 = kb.sb("r1_hg1", [128, 256], BF16)
    hg2 = kb.sb("r1_hg2", [32, 256], BF16)
    hid_r = Res()
    if j > 0:
        hv = kb.sb("r1_hv", [32, 256], BF16)
    tnames = ("sgw", "cum", "cumx", "pin", "pinv", "pprev", "asig", "kk", "lns", "rn", "kkn", "t1", "k2", "tb")
    tm = {k: kb.sb("r1_t" + k, [128, 256], F32) for k in tnames}
    kk2 = kb.sb("r1_kk2", [128, 256], BF16)
    tm_r = Res()
    vsb = [kb.sb("r1_v%d" % b, [128, 1024], F32) for b in range(2)]
    vsb_r = RL(2)
    vb16 = [kb.sb("r1_vb%d" % b, [128, 1024], BF16) for b in range(2)]
    vb_r = RL(2)
    gsb = [kb.sb("r1_g%d" % b, [128, 1024], BF16) for b in range(2)]
    gsb_r = RL(2)
    if j > 0:
        vfs = [kb.sb("r1_vf%d" % b, [128, 1024], F32) for b in range(2)]
        vfs_r = RL(2)
        vmx = [kb.sb("r1_vm%d" % b, [128, 1024], F32) for b in range(2)]
        vmx_r = RL(2)
    kb.op("dve", lambda e: e.memset(xTs[0][:, :, 0:1], 0.0), writes=[xTs_r[0]])

    def mix(n, buf, xb):
        for kc in range(8):
            kb.op("dve", lambda e, kc=kc: e.scalar_tensor_tensor(xm[buf][:, kc, :], xx[:, kc, :], mu[:, n, kc:kc + 1], xTs[xb][:, kc, 1:257], op0=ALU.mult, op1=ALU.add),
                  reads=[xx_r, pv_r, xTs_r[xb]], writes=[xm_r[buf]])

    for s in range(NSUP):
        xb = s % 2
        if s > 0:
            kb.op("pool", lambda e, xb=xb: e.tensor_copy(xTs[xb][:, :, 0:1], xTs[1 - xb][:, :, 256:257]), reads=[xTs_r[1 - xb]], writes=[xTs_r[xb]])
        for tl in range(2):
            i = s * 2 + tl
            b = i % 2
            kb.dma("sp", lambda e, i=i, b=b: e.dma_start(out=xld[b][:], in_=xin[i * 128:(i + 1) * 128, :]), reads=[xin_r[i]], writes=[xld_r[b]])
            for hf in range(2):
                pb = 6 + hf
                for c4 in range(4):
                    kc = hf * 4 + c4
                    kb.op("pe", lambda e, pb=pb, c4=c4, kc=kc, b=b: e.transpose(ps[pb][:, c4 * 128:(c4 + 1) * 128], xld[b][:, kc * 128:(kc + 1) * 128], C.ident_f[:]),
                          reads=[xld_r[b], C.r], writes=[ps_r[pb]])
                if hf == 0:
                    kb.op("act", lambda e, pb=pb, xb=xb, tl=tl: e.activation(xTs[xb][:, 0:4, 1 + tl * 128:1 + (tl + 1) * 128], ps[pb][:].rearrange("p (c t) -> p c t", c=4), AF.Copy),
                          reads=[ps_r[pb]], writes=[xTs_r[xb]])
                else:
                    kb.op("dve", lambda e, pb=pb, xb=xb, tl=tl: e.tensor_copy(xTs[xb][:, 4:8, 1 + tl * 128:1 + (tl + 1) * 128], ps[pb][:].rearrange("p (c t) -> p c t", c=4)),
                          reads=[ps_r[pb]], writes=[xTs_r[xb]])
        kb.op("dve", lambda e, xb=xb: e.tensor_tensor(xx[:], xTs[xb][:, :, 0:256], xTs[xb][:, :, 1:257], op=ALU.subtract), reads=[xTs_r[xb]], writes=[xx_r])
        mix(3, 0, xb)
        for kc in range(8):
            kb.op("pe", lambda e, kc=kc: e.matmul(ps[5][0:64, 0:256], w1[:, kc, :], xm[0][:, kc, :], start=(kc == 0), stop=(kc == 7)), reads=[wr_, xm_r[0]], writes=[ps_r[5]])
        kb.op("act", lambda e: e.activation(hw[:], ps[5][0:64, 0:256], AF.Tanh), reads=[ps_r[5]], writes=[hid_r])
        mix(4, 1, xb)
        for kc in range(8):
            kb.op("pe", lambda e, kc=kc: e.matmul(ps[5][0:64, 256:512], a1[:, kc, :], xm[1][:, kc, :], start=(kc == 0), stop=(kc == 7)), reads=[wr_, xm_r[1]], writes=[ps_r[5]])
        kb.op("act", lambda e: e.activation(ha[:], ps[5][0:64, 256:512], AF.Copy), reads=[ps_r[5]], writes=[hid_r])
        mix(5, 2, xb)
        for kc in range(8):
            kb.op("pe", lambda e, kc=kc: e.matmul(ps[4][:, 0:256], g1[:, kc, 0:128], xm[2][:, kc, :], start=(kc == 0), stop=(kc == 7)), reads=[wr_, xm_r[2]], writes=[ps_r[4]])
        for kc in range(8):
            kb.op("pe", lambda e, kc=kc: e.matmul(ps[4][0:32, 256:512], g1[:, kc, 128:160], xm[2][:, kc, :], start=(kc == 0), stop=(kc == 7)), reads=[wr_, xm_r[2]], writes=[ps_r[4]])
        kb.op("act", lambda e: e.activation(hg1[:], ps[4][:, 0:256], AF.Sigmoid), reads=[ps_r[4]], writes=[hid_r])
        kb.op("act", lambda e: e.activation(hg2[:], ps[4][0:32, 256:512], AF.Sigmoid), reads=[ps_r[4]], writes=[hid_r])
        for tl in range(2):
            i = s * 2 + tl
            b = i % 2
            for hf in range(2):
                pb = 6 + hf
                kb.op("pe", lambda e, pb=pb, tl=tl, hf=hf: e.matmul(ps[pb][:], hg1[:, tl * 128:(tl + 1) * 128], g2a[:, hf * 512:(hf + 1) * 512], start=True, stop=False), reads=[hid_r, wr_], writes=[ps_r[pb]])
                kb.op("pe", lambda e, pb=pb, tl=tl, hf=hf: e.matmul(ps[pb][:], hg2[:, tl * 128:(tl + 1) * 128], g2b[:, hf * 512:(hf + 1) * 512], start=False, stop=True), reads=[hid_r, wr_], writes=[ps_r[pb]])
                if hf == 0:
                    kb.op("act", lambda e, pb=pb, b=b: e.activation(gsb[b][:, 0:512], ps[pb][:], AF.Copy), reads=[ps_r[pb]], writes=[gsb_r[b]])
                else:
                    kb.op("dve", lambda e, pb=pb, b=b: e.tensor_copy(gsb[b][:, 512:1024], ps[pb][:]), reads=[ps_r[pb]], writes=[gsb_r[b]])
            kb.dma("sp", lambda e, i=i, b=b: e.dma_start(out=Gd[i * 128:(i + 1) * 128, :], in_=gsb[b][:]), reads=[gsb_r[b]])
        mix(2, 0, xb)
        if j > 0:
            for kc in range(8):
                kb.op("pe", lambda e, kc=kc: e.matmul(ps[5][0:32, 0:256], v1[:, kc, :], xm[0][:, kc, :], start=(kc == 0), stop=(kc == 7)), reads=[wr_, xm_r[0]], writes=[ps_r[5]])
            kb.op("act", lambda e: e.activation(hv[:], ps[5][0:32, 0:256], AF.Copy), reads=[ps_r[5]], writes=[hid_r])
        for tl in range(2):
            i = s * 2 + tl
            b = i % 2
            for hf in range(2):
                pb = 6 + hf
                for kc in range(8):
                    kb.op("pe", lambda e, pb=pb, tl=tl, hf=hf, kc=kc: e.matmul(ps[pb][:], xm[0][:, kc, tl * 128:(tl + 1) * 128], wrkv[:, 2, kc, hf * 512:(hf + 1) * 512], start=(kc == 0), stop=(kc == 7)),
                          reads=[xm_r[0], wr_], writes=[ps_r[pb]])
                if hf == 0:
                    kb.op("act", lambda e, pb=pb, b=b: e.activation(vsb[b][:, 0:512], ps[pb][:], AF.Copy), reads=[ps_r[pb]], writes=[vsb_r[b]])
                else:
                    kb.op("dve", lambda e, pb=pb, b=b: e.tensor_copy(vsb[b][:, 512:1024], ps[pb][:]), reads=[ps_r[pb]], writes=[vsb_r[b]])
            if j == 0:
                kb.dma("sp", lambda e, i=i, b=b: e.dma_start(out=vfd[i * 128:(i + 1) * 128, :], in_=vsb[b][:]), reads=[vsb_r[b]])
                kb.op("pool", lambda e, b=b: e.tensor_copy(vb16[b][:], vsb[b][:]), reads=[vsb_r[b]], writes=[vb_r[b]])
            else:
                kb.dma("sp", lambda e, i=i, b=b: e.dma_start(out=vfs[b][:], in_=vfd[i * 128:(i + 1) * 128, :]), writes=[vfs_r[b]])
                for hf in range(2):
                    pb = 6 + hf
                    kb.op("pe", lambda e, pb=pb, tl=tl, hf=hf: e.matmul(ps[pb][:], hv[:, tl * 128:(tl + 1) * 128], v2[:, hf * 512:(hf + 1) * 512], start=True, stop=True), reads=[hid_r, wr_], writes=[ps_r[pb]])
                    kb.op("dve", lambda e, pb=pb, b=b, hf=hf: e.tensor_tensor(vmx[b][:, hf * 512:(hf + 1) * 512], ps[pb][:], v0b[:, hf * 512:(hf + 1) * 512], op=ALU.add), reads=[ps_r[pb], wr_], writes=[vmx_r[b]])
                kb.op("act", lambda e, b=b: e.activation(vmx[b][:], vmx[b][:], AF.Sigmoid), reads=[vmx_r[b]], writes=[vmx_r[b]])
                kb.op("pool", lambda e, b=b: e.tensor_tensor(vfs[b][:], vfs[b][:], vsb[b][:], op=ALU.subtract), reads=[vfs_r[b], vsb_r[b]], writes=[vfs_r[b]])
                kb.op("pool", lambda e, b=b: e.tensor_tensor(vfs[b][:], vfs[b][:], vmx[b][:], op=ALU.mult), reads=[vfs_r[b], vmx_r[b]], writes=[vfs_r[b]])
                kb.op("pool", lambda e, b=b: e.tensor_tensor(vb16[b][:], vfs[b][:], vsb[b][:], op=ALU.add), reads=[vfs_r[b], vsb_r[b]], writes=[vb_r[b]])
            kb.dma("sp", lambda e, i=i, b=b: e.dma_start(out=Vd[i * 128:(i + 1) * 128, :], in_=vb16[b][:]), reads=[vb_r[b]])
        mix(0, 1, xb)
        mix(1, 2, xb)
        for oc in range(8):
            osl = slice(oc * 128, (oc + 1) * 128)
            pr, pk = (oc % 2) * 2, (oc % 2) * 2 + 1
            for kc in range(8):
                kb.op("pe", lambda e, pr=pr, kc=kc, osl=osl: e.matmul(ps[pr][:, 0:256], wrkv[:, 0, kc, osl], xm[1][:, kc, :], start=(kc == 0), stop=(kc == 7)), reads=[wr_, xm_r[1]], writes=[ps_r[pr]])
            for kc in range(8):
                kb.op("pe", lambda e, pk=pk, kc=kc, osl=osl: e.matmul(ps[pk][:, 0:256], wrkv[:, 1, kc, osl], xm[2][:, kc, :], start=(kc == 0), stop=(kc == 7)), reads=[wr_, xm_r[2]], writes=[ps_r[pk]])
            kb.op("pe", lambda e, pr=pr, osl=osl: e.matmul(ps[pr][:, 256:512], w2[:, osl], hw[:], start=True, stop=True), reads=[wr_, hid_r], writes=[ps_r[pr]])
            kb.op("pe", lambda e, pk=pk, osl=osl: e.matmul(ps[pk][:, 256:512], a2[:, osl], ha[:], start=True, stop=True), reads=[wr_, hid_r], writes=[ps_r[pk]])
            r_ps, k_ps, w_ps, a_ps = ps[pr][:, 0:256], ps[pk][:, 0:256], ps[pr][:, 256:512], ps[pk][:, 256:512]
            R2 = [ps_r[pr], ps_r[pk], tm_r, pv_r]

            def o(eng, fn, extra_w=()):
                kb.op(eng, fn, reads=R2, writes=[tm_r] + list(extra_w))

            o("act", lambda e, oc=oc: e.activation(tm["sgw"][:], w_ps, AF.Sigmoid, bias=pv[:, 0, oc:oc + 1]))
            o("act", lambda e, oc=oc: e.activation(tm["asig"][:], a_ps, AF.Sigmoid, bias=pv[:, 1, oc:oc + 1]))
            o("act", lambda e, oc=oc: e.activation(tm["kk"][:], k_ps, AF.Identity, scale=pv[:, 2, oc:oc + 1]))
            o("dve", lambda e: e.tensor_tensor_scan(tm["cum"][:], rst[:], tm["sgw"][:], 0.0, op0=ALU.mult, op1=ALU.add))
            o("pool", lambda e: e.tensor_tensor(kk2[:], tm["kk"][:], tm["kk"][:], op=ALU.mult))
            kb.op("pe", lambda e: e.matmul(ps[5][:, 0:256], bd64[:], kk2[:], start=True, stop=True), reads=[tm_r, pv_r], writes=[ps_r[5]])
            o("pool", lambda e: e.tensor_tensor(tm["cumx"][:], tm["cum"][:], tm["sgw"][:], op=ALU.subtract))
            o("act", lambda e: e.activation(tm["pin"][:], tm["cum"][:], AF.Exp, scale=-C0))
            o("act", lambda e: e.activation(tm["pinv"][:], tm["cum"][:], AF.Exp, scale=C0))
            o("act", lambda e: e.activation(tm["pprev"][:], tm["cumx"][:], AF.Exp, scale=-C0))
            kb.op("dve", lambda e: e.tensor_scalar(tm["lns"][:], ps[5][:, 0:256], 1e-30, None, op0=ALU.add), reads=[ps_r[5]], writes=[tm_r])
            o("act", lambda e: e.activation(tm["lns"][:], tm["lns"][:], AF.Ln))
            o("act", lambda e: e.activation(tm["rn"][:], tm["lns"][:], AF.Exp, scale=-0.5))
            o("pool", lambda e: e.tensor_tensor(tm["kkn"][:], tm["kk"][:], tm["rn"][:], op=ALU.mult))
            o("dve", lambda e, oc=oc: e.tensor_scalar(tm["t1"][:], tm["asig"][:], pv[:, 3, oc:oc + 1], pv[:, 5, oc:oc + 1], op0=ALU.mult, op1=ALU.add))
            o("dve", lambda e: e.tensor_tensor(tm["k2"][:], k_ps, tm["t1"][:], op=ALU.mult))
            for tl_ in range(2):
                o("dve", lambda e, oc=oc, tl_=tl_: e.tensor_copy(Pc[:, tl_, oc:oc + 1], tm["pin"][:, 127 + 128 * tl_:128 + 128 * tl_]), extra_w=[out_r])
            o("dve", lambda e, oc=oc: e.scalar_tensor_tensor(AR[:, oc, :, 0, :], tm["kkn"][:].rearrange("p (a t) -> p a t", a=2), -1.0, tm["pprev"][:].rearrange("p (a t) -> p a t", a=2), op0=ALU.mult, op1=ALU.mult), extra_w=[out_r])
            o("dve", lambda e, oc=oc: e.tensor_tensor(AR[:, oc, :, 1, :], r_ps.rearrange("p (a t) -> p a t", a=2), tm["pin"][:].rearrange("p (a t) -> p a t", a=2), op=ALU.mult), extra_w=[out_r])
            o("pool", lambda e: e.tensor_tensor(tm["tb"][:], tm["kkn"][:], tm["asig"][:], op=ALU.mult))
            o("pool", lambda e, oc=oc: e.tensor_tensor(BK[:, oc, :, 0, :], tm["tb"][:].rearrange("p (a t) -> p a t", a=2), tm["pinv"][:].rearrange("p (a t) -> p a t", a=2), op=ALU.mult), extra_w=[out_r])
            o("pool", lambda e, oc=oc: e.tensor_tensor(BK[:, oc, :, 1, :], tm["k2"][:].rearrange("p (a t) -> p a t", a=2), tm["pinv"][:].rearrange("p (a t) -> p a t", a=2), op=ALU.mult), extra_w=[out_r])
            o("dve", lambda e, oc=oc: e.scalar_tensor_tensor(rk[:, oc, :], r_ps, pv[:, 4, oc:oc + 1], tm["k2"][:], op0=ALU.mult, op1=ALU.mult), extra_w=[out_r])
        for tl in range(2):
            i = s * 2 + tl
            kb.dma("sp", lambda e, i=i, tl=tl: e.dma_start(out=ARd[i].rearrange("p (o c t) -> p o c t", o=8, c=2), in_=AR[:, :, tl, :, :]), reads=[out_r])
            kb.dma("sp", lambda e, i=i, tl=tl: e.dma_start(out=BKd[i].rearrange("p (o c t) -> p o c t", o=8, c=2), in_=BK[:, :, tl, :, :]), reads=[out_r])
            kb.dma("sp", lambda e, i=i, tl=tl: e.dma_start(out=rkd[i].rearrange("p (o t) -> p o t", o=8), in_=rk[:, :, tl * 128:(tl + 1) * 128]), reads=[out_r])
            kb.dma("sp", lambda e, i=i, tl=tl: e.dma_start(out=Pcd[i], in_=Pc[:, tl, :]), reads=[out_r])
    kb_pop(kb)
    rwkv_pass2(kb, C, T, prm, j, li, xin, xin_r, xa, xa_r, ST, nc)


def rwkv_pass2(kb, C, T, prm, j, li, xin, xin_r, xa, xa_r, ST, nc):
    NT = T // 128
    ARd, BKd, rkd, Pcd, Vd, Gd = (ST[k] for k in ("ARd", "BKd", "rkd", "Pcd", "Vd", "Gd"))
    kb_push(kb)
    pF = [kb.ps("r2_pf%d" % b, [128, 512], F32) for b in range(4)]
    pF_r = RL(4)
    pT = kb.ps("r2_pT", [128, 1024], BF16)
    pT_r = Res()
    pY = [kb.ps("r2_pY%d" % b, [128, 512], F32) for b in range(2)]
    pY_r = RL(2)
    pH = kb.ps("r2_pH", [128, 512], F32)
    pH_r = Res()
    wo = kb.sb("r2_wo", [128, 8, 1024], BF16)
    wo_r = Res()
    wsrc = prm["rw_w_o"][j].rearrange("(kc p) n -> p kc n", p=128)
    for kc in range(8):
        kb.dma("pool", lambda e, kc=kc: e.dma_start(out=wo[:, kc, :], in_=wsrc[:, kc, :]), writes=[wo_r])
    cst_r = Res()
    m2 = kb.sb("r2_m2", [128, 2, 128], F32)
    kb.op("dve", lambda e: e.tensor_copy(m2[:, 0, :], C.m_st[:]), reads=[C.r], writes=[cst_r])
    kb.op("dve", lambda e: e.tensor_copy(m2[:, 1, :], C.m_in[:]), reads=[C.r], writes=[cst_r])
    bdm = kb.sb("r2_bdm", [128, 128], F32)
    kb.op("dve", lambda e: e.memset(bdm[:], 0.0), writes=[cst_r])
    kb.op("dve", lambda e: e.memset(bdm[0:64, 0:64], 1.0), writes=[cst_r])
    kb.op("dve", lambda e: e.memset(bdm[64:128, 64:128], 1.0), writes=[cst_r])
    HS = kb.sb("r2_HS", [128, 8, 16], BF16)
    kb.op("dve", lambda e: e.memset(HS[:], 0.0), writes=[cst_r])
    for oc in range(8):
        for hh in range(2):
            kb.op("dve", lambda e, oc=oc, hh=hh: e.memset(HS[hh * 64:(hh + 1) * 64, oc, 2 * oc + hh:2 * oc + hh + 1], 1.0), writes=[cst_r])
    lng = kb.sb("r2_lng", [128, 1024], F32)
    lnb = kb.sb("r2_lnb", [128, 1024], F32)
    kb.dma("sp", lambda e: e.dma_start(out=lng[:], in_=bcast_rows(prm["rw_lnx_g"][j:j + 1, :])), writes=[cst_r])
    kb.dma("sp", lambda e: e.dma_start(out=lnb[:], in_=bcast_rows(prm["rw_lnx_b"][j:j + 1, :])), writes=[cst_r])
    gneps = kb.sb("r2_gneps", [128, 1], F32)
    kb.op("dve", lambda e: e.memset(gneps[:], GN_EPS), writes=[cst_r])
    LS = ln_scratch(kb, "r2_ln", LN_EPS)
    g_bc, b_bc, lnp_r = load_ln_params(kb, "r2_lnp", prm["ln1_g"], prm["ln1_b"], li)

    ARt = [kb.sb("r2_AR%d" % b, [128, 8, 2, 128], BF16) for b in range(2)]
    BKt = [kb.sb("r2_BK%d" % b, [128, 8, 2, 128], BF16) for b in range(2)]
    rkt = [kb.sb("r2_rk%d" % b, [128, 8, 128], BF16) for b in range(2)]
    Pct = [kb.sb("r2_Pc%d" % b, [128, 8], F32) for b in range(2)]
    Vt = [kb.sb("r2_V%d" % b, [128, 1024], BF16) for b in range(2)]
    Gt = [kb.sb("r2_G%d" % b, [128, 1024], BF16) for b in range(2)]
    xt = [kb.sb("r2_xt%d" % b, [128, 1024], F32) for b in range(2)]
    in_r = RL(2)
    Hb = kb.sb("r2_H", [128, 8, 64], BF16)
    Hb_r = RL(8)
    kb.op("dve", lambda e: e.memset(Hb[:], 0.0), writes=Hb_r)
    TK = [kb.sb("r2_TK%d" % b, [128, 3, 128], BF16) for b in range(2)]
    TK_r = RL(2)
    XA = [kb.sb("r2_XA%d" % h, [128, 2, 128], BF16) for h in range(2)]
    KA = [kb.sb("r2_KA%d" % h, [128, 2, 128], BF16) for h in range(2)]
    XN = [[kb.sb("r2_XN%d_%d" % (h, q), [128, 2, 128], BF16) for q in range(2)] for h in range(2)]
    PQ = [[kb.sb("r2_PQ%d_%d" % (h, q), [128, 2, 128], BF16) for q in range(2)] for h in range(2)]
    AV = [kb.sb("r2_AV%d" % h, [128, 64], BF16) for h in range(2)]
    hd_r = [Res(), Res()]
    W12 = [kb.sb("r2_W12%d" % b, [128, 2, 2, 64], BF16) for b in range(2)]
    W12_r = RL(2)
    MT = [kb.sb("r2_MT%d" % b, [128, 128], BF16) for b in range(2)]
    MTf = kb.sb("r2_MTf", [128, 128], F32)
    GS = [kb.sb("r2_GS%d" % b, [128, 64], F32) for b in range(2)]
    QT = [kb.sb("r2_QT%d" % b, [128, 128], BF16) for b in range(2)]
    pr_r = RL(2)
    mtf_r = Res()
    ysb = kb.sb("r2_y", [128, 1024], F32)
    ysq = kb.sb("r2_ysq", [128, 1024], F32)
    yn = kb.sb("r2_yn", [128, 1024], F32)
    yg = kb.sb("r2_yg", [128, 1024], BF16)
    ygT = kb.sb("r2_ygT", [128, 8, 128], BF16)
    st = kb.sb("r2_st", [128, 8, 16], F32)
    y_r = Res()
    zt = [kb.sb("r2_z%d" % b, [128, 1024], F32) for b in range(2)]
    zt_r = RL(2)
    x1 = [kb.sb("r2_x1%d" % b, [128, 1024], F32) for b in range(2)]
    x1_r = RL(2)

    for i in range(NT):
        b = i % 2
        kb.dma("sp", lambda e, i=i, b=b: e.dma_start(out=ARt[b][:], in_=ARd[i].rearrange("p (o c t) -> p o c t", o=8, c=2)), writes=[in_r[b]])
        kb.dma("sp", lambda e, i=i, b=b: e.dma_start(out=BKt[b][:], in_=BKd[i].rearrange("p (o c t) -> p o c t", o=8, c=2)), writes=[in_r[b]])
        kb.dma("sp", lambda e, i=i, b=b: e.dma_start(out=rkt[b][:], in_=rkd[i].rearrange("p (o t) -> p o t", o=8)), writes=[in_r[b]])
        kb.dma("sp", lambda e, i=i, b=b: e.dma_start(out=Pct[b][:], in_=Pcd[i]), writes=[in_r[b]])
        kb.dma("sp", lambda e, i=i, b=b: e.dma_start(out=Vt[b][:], in_=Vd[i * 128:(i + 1) * 128, :]), writes=[in_r[b]])
        kb.dma("sp", lambda e, i=i, b=b: e.dma_start(out=Gt[b][:], in_=Gd[i * 128:(i + 1) * 128, :]), writes=[in_r[b]])
        kb.dma("sp", lambda e, i=i, b=b: e.dma_start(out=xt[b][:], in_=xin[i * 128:(i + 1) * 128, :]), reads=[xin_r[i]], writes=[in_r[b]])
        for hp in range(8):
            q = hp % 2
            for n, src in enumerate((ARt[b][:, hp, 0, :], BKt[b][:, hp, 0, :], BKt[b][:, hp, 1, :])):
                kb.op("pe", lambda e, n=n, src=src: e.transpose(pT[:, n * 128:(n + 1) * 128], src, C.ident_b[:]), reads=[in_r[b], C.r], writes=[pT_r])
            kb.op("act", lambda e, q=q: e.activation(TK[q][:], pT[:, 0:384].rearrange("p (c t) -> p c t", c=3), AF.Copy), reads=[pT_r], writes=[TK_r[q]])
            for hh in range(2):
                pb = 64 * hh
                psl = slice(pb, pb + 64)
                pf = pF[hh]
                rhs_ar = ARt[b][psl, hp, :, :].rearrange("p c t -> p (c t)")
                kb.op("pe", lambda e, pf=pf, psl=psl, rhs_ar=rhs_ar: e.matmul(pf[:, 0:256], BKt[b][psl, hp, 0, :], rhs_ar, start=True, stop=True), reads=[in_r[b]], writes=[pF_r[hh]])
                kb.op("pe", lambda e, pf=pf, psl=psl, rhs_ar=rhs_ar: e.matmul(pf[:, 256:512], BKt[b][psl, hp, 1, :], rhs_ar, start=True, stop=True), reads=[in_r[b]], writes=[pF_r[hh]])
                kb.op("dve", lambda e, pf=pf, hh=hh: e.tensor_tensor(XA[hh][:], pf[:, 0:256].rearrange("p (c t) -> p c t", c=2), m2[:], op=ALU.mult), reads=[pF_r[hh], cst_r], writes=[hd_r[hh]])
                kb.op("dve", lambda e, pf=pf, hh=hh: e.tensor_tensor(KA[hh][:], pf[:, 256:512].rearrange("p (c t) -> p c t", c=2), m2[:], op=ALU.mult), reads=[pF_r[hh], cst_r], writes=[hd_r[hh]])
                kb.op("pe", lambda e, pf=pf, psl=psl: e.matmul(pf[:, 0:128], ARt[b][psl, hp, 0, :], BKt[b][psl, hp, 0, :], start=True, stop=True), reads=[in_r[b]], writes=[pF_r[hh]])
                kb.op("pool", lambda e, hh=hh: e.tensor_copy(XN[hh][0][:, 0, :], XA[hh][:, 0, :]), reads=[hd_r[hh]], writes=[hd_r[hh]])
                kb.op("dve", lambda e, pf=pf, hh=hh: e.tensor_tensor(XN[hh][0][:, 1, :], pf[:, 0:128], C.m_lo[:], op=ALU.mult), reads=[pF_r[hh], C.r], writes=[hd_r[hh]])
                for c in range(2):
                    kb.op("pool", lambda e, hh=hh, c=c: e.tensor_tensor(PQ[hh][0][:, c, :], XN[hh][0][:, c, :], C.ident_b[:], op=ALU.add), reads=[hd_r[hh], C.r], writes=[hd_r[hh]])
            for k in range(1, 7):
                cur, prv = k % 2, (k - 1) % 2
                for hh in range(2):
                    pf = pF[2 + hh]
                    Xp, Np = XN[hh][prv][:, 0, :], XN[hh][prv][:, 1, :]
                    kb.op("pe", lambda e, pf=pf, Xp=Xp, Np=Np: e.matmul(pf[:, 0:128], Np, Xp, start=True, stop=True), reads=[hd_r[hh]], writes=[pF_r[2 + hh]])
                    if k < 6:
                        kb.op("pe", lambda e, pf=pf, Xp=Xp, Np=Np: e.matmul(pf[:, 128:256], Xp, Np, start=True, stop=True), reads=[hd_r[hh]], writes=[pF_r[2 + hh]])
                        kb.op("act", lambda e, pf=pf, hh=hh, cur=cur: e.activation(XN[hh][cur][:], pf[:, 0:256].rearrange("p (c t) -> p c t", c=2), AF.Copy), reads=[pF_r[2 + hh]], writes=[hd_r[hh]])
                    else:
                        kb.op("act", lambda e, pf=pf, hh=hh, cur=cur: e.activation(XN[hh][cur][:, 0, :], pf[:, 0:128], AF.Copy), reads=[pF_r[2 + hh]], writes=[hd_r[hh]])
                for hh in range(2):
                    pf = pF[2 + hh]
                    Xc = XN[hh][cur][:, 0, :]
                    Pp, Qp = PQ[hh][prv][:, 0, :], PQ[hh][prv][:, 1, :]
                    kb.op("pe", lambda e, pf=pf, Xc=Xc, Qp=Qp: e.matmul(pf[:, 256:384], Qp, Xc, start=True, stop=True), reads=[hd_r[hh]], writes=[pF_r[2 + hh]])
                    if k < 6:
                        kb.op("pe", lambda e, pf=pf, Xc=Xc, Qp=Qp: e.matmul(pf[:, 384:512], Xc, Qp, start=True, stop=True), reads=[hd_r[hh]], writes=[pF_r[2 + hh]])
                        kb.op("dve", lambda e, pf=pf, hh=hh, cur=cur, prv=prv: e.tensor_tensor(PQ[hh][cur][:], pf[:, 256:512].rearrange("p (c t) -> p c t", c=2), PQ[hh][prv][:], op=ALU.add), reads=[pF_r[2 + hh], hd_r[hh]], writes=[hd_r[hh]])
                    else:
                        kb.op("dve", lambda e, pf=pf, hh=hh, cur=cur, prv=prv: e.tensor_tensor(PQ[hh][cur][:, 0, :], pf[:, 256:384], PQ[hh][prv][:, 0, :], op=ALU.add), reads=[pF_r[2 + hh], hd_r[hh]], writes=[hd_r[hh]])
            Pfin = 6 % 2
            for hh in range(2):
                pf = pF[hh]
                hc = slice(hp * 128 + hh * 64, hp * 128 + hh * 64 + 64)
                kb.op("pe", lambda e, pf=pf, hh=hh, hc=hc: e.matmul(pf[:, 128:192], KA[hh][:, 0, :], Vt[b][:, hc], start=True, stop=True), reads=[hd_r[hh], in_r[b]], writes=[pF_r[hh]])
                kb.op("act", lambda e, pf=pf, hh=hh: e.activation(AV[hh][:], pf[:, 128:192], AF.Copy), reads=[pF_r[hh]], writes=[hd_r[hh]])
                kb.op("pe", lambda e, pf=pf, hh=hh: e.matmul(pf[:, 192:256], PQ[hh][Pfin][:, 0, :], AV[hh][:], start=True, stop=True), reads=[hd_r[hh]], writes=[pF_r[hh]])
                kb.op("pe", lambda e, pf=pf, hh=hh, q=q: e.matmul(pf[:, 256:320], PQ[hh][Pfin][:, 0, :], TK[q][:, 0, hh * 64:(hh + 1) * 64], start=True, stop=True), reads=[hd_r[hh], TK_r[q]], writes=[pF_r[hh]])
                kb.op("act", lambda e, pf=pf, hh=hh, q=q: e.activation(W12[q][:, :, hh, :], pf[:, 192:320].rearrange("p (c t) -> p c t", c=2), AF.Copy), reads=[pF_r[hh]], writes=[W12_r[q]])
            W1p = W12[q][:, 0, :, :].rearrange("p h t -> p (h t)")
            W2p = W12[q][:, 1, :, :].rearrange("p h t -> p (h t)")
            pg = pF[2]
            kb.op("pe", lambda e, W2p=W2p, q=q: e.matmul(pg[:, 0:128], W2p, TK[q][:, 1, :], start=True, stop=True), reads=[W12_r[q], TK_r[q]], writes=[pF_r[2]])
            kb.op("dve", lambda e: e.tensor_tensor(MTf[:], pg[:, 0:128], bdm[:], op=ALU.mult), reads=[pF_r[2], cst_r], writes=[mtf_r])
            kb.op("pool", lambda e, q=q: e.tensor_tensor(MT[q][:], MTf[:], C.ident_f[:], op=ALU.add), reads=[mtf_r, C.r], writes=[pr_r[q]])
            kb.op("pe", lambda e, W1p=W1p, q=q: e.matmul(pg[:, 128:256], TK[q][:, 1, :], W1p, start=True, stop=False), reads=[W12_r[q], TK_r[q]], writes=[pF_r[2]])
            kb.op("pe", lambda e, q=q: e.matmul(pg[:, 128:256], TK[q][:, 2, :], Vt[b][:, hp * 128:(hp + 1) * 128], start=False, stop=True), reads=[TK_r[q], in_r[b]], writes=[pF_r[2]])
            for hh in range(2):
                psl = slice(64 * hh, 64 * hh + 64)
                kb.op("dve", lambda e, psl=psl, hh=hh, q=q: e.tensor_scalar(GS[q][psl, :], pg[psl, 128 + 64 * hh:192 + 64 * hh], Pct[b][psl, hp:hp + 1], None, op0=ALU.mult), reads=[pF_r[2], in_r[b]], writes=[pr_r[q]])
                kb.op("pe", lambda e, hh=hh, W2p=W2p: e.matmul(pg[:, 256 + 128 * hh:384 + 128 * hh], W2p, XA[hh][:, 1, :], start=True, stop=True), reads=[W12_r[q], hd_r[hh]], writes=[pF_r[2]])
                kb.op("dve", lambda e, psl=psl, hh=hh, q=q: e.tensor_tensor(QT[q][psl, :], pg[psl, 256 + 128 * hh:384 + 128 * hh], ARt[b][psl, hp, 1, :], op=ALU.add), reads=[pF_r[2], in_r[b]], writes=[pr_r[q]])
            yb_, yc = pY[hp // 4], (hp % 4) * 128
            for hh in range(2):
                psl = slice(64 * hh, 64 * hh + 64)
                hc = slice(hp * 128 + hh * 64, hp * 128 + hh * 64 + 64)
                yo = yb_[:, yc + 64 * hh:yc + 64 * hh + 64]
                kb.op("pe", lambda e, yo=yo, hh=hh, q=q: e.matmul(yo, XA[hh][:, 1, :], W12[q][:, 0, hh, :], start=True, stop=False), reads=[hd_r[hh], W12_r[q]], writes=[pY_r[hp // 4]])
                kb.op("pe", lambda e, yo=yo, hh=hh, hc=hc: e.matmul(yo, KA[hh][:, 1, :], Vt[b][:, hc], start=False, stop=False), reads=[hd_r[hh], in_r[b]], writes=[pY_r[hp // 4]])
                kb.op("pe", lambda e, yo=yo, psl=psl, q=q: e.matmul(yo, QT[q][psl, :], Hb[psl, hp, :], start=False, stop=True), reads=[pr_r[q], Hb_r[hp]], writes=[pY_r[hp // 4]])
            kb.op("pe", lambda e, q=q: e.matmul(pF[3][:, 0:64], MT[q][:], Hb[:, hp, :], start=True, stop=True), reads=[pr_r[q], Hb_r[hp]], writes=[pF_r[3]])
            kb.op("dve", lambda e, q=q: e.scalar_tensor_tensor(Hb[:, hp, :], pF[3][:, 0:64], Pct[b][:, hp:hp + 1], GS[q][:], op0=ALU.mult, op1=ALU.add), reads=[pF_r[3], in_r[b], pr_r[q]], writes=[Hb_r[hp]])
        kb.op("act", lambda e: e.activation(ysb[:, 0:512], pY[0][:], AF.Copy), reads=[pY_r[0]], writes=[y_r])
        kb.op("dve", lambda e: e.tensor_copy(ysb[:, 512:1024], pY[1][:]), reads=[pY_r[1]], writes=[y_r])
        kb.op("pool", lambda e: e.tensor_tensor(ysq[:], ysb[:], ysb[:], op=ALU.mult), reads=[y_r], writes=[y_r])
        kb.op("dve", lambda e: e.reduce_sum(st[:, 0, :], ysb[:].rearrange("p (h n) -> p h n", h=16), axis=AX.X), reads=[y_r], writes=[y_r])
        kb.op("dve", lambda e: e.reduce_sum(st[:, 1, :], ysq[:].rearrange("p (h n) -> p h n", h=16), axis=AX.X), reads=[y_r], writes=[y_r])
        kb.op("dve", lambda e: e.tensor_scalar(st[:, 2, :], st[:, 0, :], 1.0 / 64, None, op0=ALU.mult), reads=[y_r], writes=[y_r])
        kb.op("dve", lambda e: e.tensor_tensor(st[:, 3, :], st[:, 2, :], st[:, 2, :], op=ALU.mult), reads=[y_r], writes=[y_r])
        kb.op("dve", lambda e: e.scalar_tensor_tensor(st[:, 4, :], st[:, 1, :], 1.0 / 64, st[:, 3, :], op0=ALU.mult, op1=ALU.subtract), reads=[y_r], writes=[y_r])
        kb.op("act", lambda e: e.activation(st[:, 5, :], st[:, 4, :], AF.Sqrt, bias=gneps[:, 0:1]), reads=[y_r, cst_r], writes=[y_r])
        kb.op("dve", lambda e: e.reciprocal(st[:, 6, :], st[:, 5, :]), reads=[y_r], writes=[y_r])
        for hd in range(16):
            eng = "dve" if hd % 2 == 0 else "pool"
            kb.op(eng, lambda e, hd=hd: e.tensor_scalar(yn[:, hd * 64:(hd + 1) * 64], ysb[:, hd * 64:(hd + 1) * 64], st[:, 2, hd:hd + 1], st[:, 6, hd:hd + 1], op0=ALU.subtract, op1=ALU.mult), reads=[y_r], writes=[y_r])
        kb.op("pool", lambda e: e.tensor_tensor(yn[:], yn[:], lng[:], op=ALU.mult), reads=[y_r, cst_r], writes=[y_r])
        kb.op("pool", lambda e: e.tensor_tensor(yn[:], yn[:], lnb[:], op=ALU.add), reads=[y_r, cst_r], writes=[y_r])
        for oc in range(8):
            kb.op("pe", lambda e, oc=oc: e.matmul(pH[:, 0:16], rkt[b][:, oc, :], HS[:, oc, :], start=(oc == 0), stop=(oc == 7)), reads=[in_r[b], cst_r], writes=[pH_r])
        kb.op("act", lambda e: e.activation(st[:, 7, :], pH[:, 0:16], AF.Copy), reads=[pH_r], writes=[y_r])
        for hd in range(16):
            kb.op("dve", lambda e, hd=hd: e.scalar_tensor_tensor(yn[:, hd * 64:(hd + 1) * 64], Vt[b][:, hd * 64:(hd + 1) * 64], st[:, 7, hd:hd + 1], yn[:, hd * 64:(hd + 1) * 64], op0=ALU.mult, op1=ALU.add), reads=[y_r, in_r[b]], writes=[y_r])
        kb.op("pool", lambda e: e.tensor_tensor(yg[:], yn[:], Gt[b][:], op=ALU.mult), reads=[y_r, in_r[b]], writes=[y_r])
        for hf in range(2):
            for c4 in range(4):
                kc = hf * 4 + c4
                kb.op("pe", lambda e, c4=c4, kc=kc: e.transpose(pT[:, 512 + c4 * 128:512 + (c4 + 1) * 128], yg[:, kc * 128:(kc + 1) * 128], C.ident_b[:]), reads=[y_r, C.r], writes=[pT_r])
            kb.op("act", lambda e, hf=hf: e.activation(ygT[:, hf * 4:hf * 4 + 4, :], pT[:, 512:1024].rearrange("p (c t) -> p c t", c=4), AF.Copy), reads=[pT_r], writes=[y_r])
        for hf in range(2):
            for kc in range(8):
                kb.op("pe", lambda e, kc=kc, hf=hf: e.matmul(pH[:], ygT[:, kc, :], wo[:, kc, hf * 512:(hf + 1) * 512], start=(kc == 0), stop=(kc == 7)), reads=[y_r, wo_r], writes=[pH_r])
            kb.op("dve", lambda e, hf=hf, b=b: e.scalar_tensor_tensor(zt[b][:, hf * 512:(hf + 1) * 512], xt[b][:, hf * 512:(hf + 1) * 512], ALPHA, pH[:], op0=ALU.mult, op1=ALU.add),
                  reads=[in_r[b], pH_r], writes=[zt_r[b]])
        ln_tile(kb, LS, zt[b], zt_r[b], x1[b][:], x1_r[b], g_bc, b_bc, lnp_r)
        kb.dma("sp", lambda e, i=i, b=b: e.dma_start(out=xa[i * 128:(i + 1) * 128, :], in_=x1[b][:]), reads=[x1_r[b]], writes=[xa_r[i]])
    kb_pop(kb)


T_FULL = 4096
N_CORES = 8
_CACHE = {}


def kernel(**inputs):
    if "prog" not in _CACHE:
        _CACHE["prog"] = build(T_FULL, FULL_PLAN, cap=512)
    nc, names, _ = _CACHE["prog"]
    x = np.asarray(inputs["x"], dtype=np.float32)
    in_maps = []
    for c in range(N_CORES):
        m = {"x": np.ascontiguousarray(x[c])}
        for n in names:
            m[n] = np.ascontiguousarray(np.asarray(inputs[n], dtype=np.float32))
        in_maps.append(m)
    res = run_bass_kernel_spmd(nc, in_maps, core_ids=list(range(N_CORES)))
    return np.stack([np.asarray(res.results[c]["out"]) for c in range(N_CORES)], axis=0).astype(np.float32)
```

```python
import math
from contextlib import ExitStack
import numpy as np
import concourse.bass as bass
import concourse.mybir as mybir
from concourse.bass_utils import run_bass_kernel_spmd

F32 = mybir.dt.float32
BF16 = mybir.dt.bfloat16
I32 = mybir.dt.int32
AF = mybir.ActivationFunctionType
ALU = mybir.AluOpType
AX = mybir.AxisListType

D = 1024
DEPTH = 4
NH_A = 8
NE = 32
NG = 4
EPG = 8
HID = 512
ALPHA = (2 * DEPTH) ** 0.25
LN_EPS = 1e-5
RMS_EPS = 1e-5
GN_EPS = 64e-5


class Res:
    __slots__ = ("w", "r")

    def __init__(self):
        self.w = None
        self.r = {}


def RL(n):
    return [Res() for _ in range(n)]


class KB:
    EPOCH = 30000

    def __init__(self, nc, es):
        self.nc = nc
        self.es = es
        self.engs = {"pe": nc.tensor, "dve": nc.vector, "act": nc.scalar, "pool": nc.gpsimd, "sp": nc.sync}
        self.sems = {}
        self.cur = {}
        self.seen = {e: {} for e in self.engs}
        self.rings = {}
        self.ridx = {}
        self.nins = 0
        for q, n in (("sp", 16), ("pool", 12), ("act", 6)):
            self.rings[q] = [[self._new_sem("d%s%d" % (q, i)), 0] for i in range(n)]
            self.ridx[q] = 0

    def _new_sem(self, name):
        s = self.es.enter_context(self.nc.semaphore(name))
        self.sems[name] = s
        return name

    def sb(self, name, shape, dt):
        return self.es.enter_context(self.nc.sbuf_tensor(name, list(shape), dt))

    def ps(self, name, shape, dt=F32):
        return self.es.enter_context(self.nc.psum_tensor(name, list(shape), dt))

    def _deps(self, reads, writes):
        deps = {}
        for r in reads:
            if r.w:
                for k, v in r.w.items():
                    if deps.get(k, 0) < v:
                        deps[k] = v
        for w in writes:
            if w.w:
                for k, v in w.w.items():
                    if deps.get(k, 0) < v:
                        deps[k] = v
            for k, v in w.r.items():
                if deps.get(k, 0) < v:
                    deps[k] = v
        return deps

    def _waits(self, eng, deps):
        E = self.engs[eng]
        seen = self.seen[eng]
        for k, v in deps.items():
            if eng == "pe" and k.startswith("epe"):
                continue
            if seen.get(k, 0) >= v:
                continue
            E.wait_ge(self.sems[k], v)
            seen[k] = v

    def _mark(self, tok, reads, writes):
        (k, v), = tok.items()
        for w in writes:
            if w.w is None:
                w.w = dict(tok)
            else:
                w.w = dict(w.w)
                w.w[k] = v
            w.r = {}
        for r in reads:
            if r.r.get(k, 0) < v:
                r.r[k] = v

    def op(self, eng, fn, reads=(), writes=()):
        self._waits(eng, self._deps(reads, writes))
        c = self.cur.get(eng)
        if c is None or c[1] >= self.EPOCH:
            n = len([k for k in self.sems if k.startswith("e" + eng)])
            c = [self._new_sem("e%s%d" % (eng, n)), 0]
            self.cur[eng] = c
        ins = fn(self.engs[eng])
        c[1] += 1
        ins.then_inc(self.sems[c[0]], 1)
        self.nins += 1
        self._mark({c[0]: c[1]}, reads, writes)

    def dma(self, q, fn, reads=(), writes=()):
        ring = self.rings[q]
        slot = ring[self.ridx[q] % len(ring)]
        self.ridx[q] += 1
        deps = self._deps(reads, writes)
        if slot[1] > 0:
            deps[slot[0]] = max(deps.get(slot[0], 0), slot[1] * 16)
        self._waits(q, deps)
        ins = fn(self.engs[q])
        slot[1] += 1
        ins.then_inc(self.sems[slot[0]], 16)
        self.nins += 1
        self._mark({slot[0]: slot[1] * 16}, reads, writes)

    def finish(self):
        deps = {}
        for q, ring in self.rings.items():
            for name, cnt in ring:
                if cnt:
                    deps[name] = cnt * 16
        for name in self.sems:
            if name.startswith("e"):
                pass
        for e, c in self.cur.items():
            deps[c[0]] = c[1]
        self._waits("sp", deps)


class Consts:
    pass


def make_consts(kb):
    nc = kb.nc
    C = Consts()
    C.r = Res()
    it = kb.sb("c_iota", [128, 512], I32)
    itf = kb.sb("c_iotaf", [128, 512], F32)
    C.ident_f = kb.sb("c_identf", [128, 128], F32)
    C.ident_b = kb.sb("c_identb", [128, 128], BF16)
    C.ones_b = kb.sb("c_onesb", [128, 128], BF16)
    C.triu_b = kb.sb("c_triub", [128, 128], BF16)
    C.cmask = kb.sb("c_cmask", [128, 4, 512], BF16)
    C.m_st = kb.sb("c_mst", [128, 128], F32)
    C.m_in = kb.sb("c_min", [128, 128], F32)
    C.m_lo = kb.sb("c_mlo", [128, 128], F32)
    kb.op("pool", lambda e: e.iota(it[:], [[1, 512]], base=0, channel_multiplier=-1), writes=[C.r])
    kb.op("dve", lambda e: e.tensor_copy(itf[:], it[:]), reads=[C.r], writes=[C.r])
    kb.op("dve", lambda e: e.tensor_scalar(C.ident_f[:], itf[:, 0:128], 0.0, None, op0=ALU.is_equal), reads=[C.r], writes=[C.r])
    kb.op("dve", lambda e: e.tensor_copy(C.ident_b[:], C.ident_f[:]), reads=[C.r], writes=[C.r])
    kb.op("dve", lambda e: e.memset(C.ones_b[:], 1.0), writes=[C.r])
    kb.op("dve", lambda e: e.tensor_scalar(C.triu_b[:], itf[:, 0:128], 0.0, None, op0=ALU.is_ge), reads=[C.r], writes=[C.r])
    for o in range(4):
        kb.op("dve", lambda e, o=o: e.tensor_scalar(C.cmask[:, o, :], itf[:], float(128 * o), None, op0=ALU.is_ge), reads=[C.r], writes=[C.r])
    kb.op("dve", lambda e: e.tensor_scalar(C.m_st[:], itf[:, 0:128], 1.0, None, op0=ALU.is_ge), reads=[C.r], writes=[C.r])
    kb.op("dve", lambda e: e.tensor_scalar(C.m_in[:], itf[:, 0:128], 0.0, None, op0=ALU.is_ge), reads=[C.r], writes=[C.r])
    kb.op("dve", lambda e: e.tensor_scalar(C.m_lo[:], itf[:, 0:128], -1.0, None, op0=ALU.is_le), reads=[C.r], writes=[C.r])
    C.itf = itf
    return C


def kb_push(kb):
    kb._stack = getattr(kb, "_stack", [])
    kb._stack.append(kb.es_t)
    kb.es_t = kb.es_root.enter_context(ExitStack()) if False else ExitStack()
    kb.es_t.__enter__()


def kb_pop(kb):
    kb.barrier()
    kb.es_t.__exit__(None, None, None)
    kb.es_t = kb._stack.pop()


def _kb_sb(self, name, shape, dt):
    self.nalloc = getattr(self, "nalloc", 0) + 1
    return self.es_t.enter_context(self.nc.sbuf_tensor("%s_%d" % (name, self.nalloc), list(shape), dt))


def _kb_ps(self, name, shape, dt=F32):
    self.nalloc = getattr(self, "nalloc", 0) + 1
    return self.es_t.enter_context(self.nc.psum_tensor("%s_%d" % (name, self.nalloc), list(shape), dt))


def _kb_barrier(self):
    deps = {}
    for q, ring in self.rings.items():
        for name, cnt in ring:
            if cnt:
                deps[name] = cnt * 16
    for name in self.sems:
        if name.startswith("e"):
            eng = [e for e in self.engs if name.startswith("e" + e)][0]
            c = self.cur[eng]
            if c[0] == name:
                deps[name] = c[1]
    for eng in self.engs:
        self._waits(eng, dict(deps))


KB.sb = _kb_sb
KB.ps = _kb_ps
KB.barrier = _kb_barrier


def bcast_rows(ap_row, n=128):
    return ap_row.partition_broadcast(n)


def ln_tile(kb, S, z, z_r, out, out_r, g_bc, b_bc, par_r):
    st, mv, sd, rs, xn = S["st"], S["mv"], S["sd"], S["rs"], S["xn"]
    r = S["r"]
    kb.op("dve", lambda e: e.bn_stats(st[:, 0, :], z[:, 0:512]), reads=[z_r], writes=[r])
    kb.op("dve", lambda e: e.bn_stats(st[:, 1, :], z[:, 512:1024]), reads=[z_r], writes=[r])
    kb.op("dve", lambda e: e.bn_aggr(mv[:, 0:2], st[:].rearrange("p a b -> p (a b)")), reads=[r], writes=[r])
    kb.op("act", lambda e: e.activation(sd[:, 0:1], mv[:, 1:2], AF.Sqrt, bias=S["eps"][:, 0:1]), reads=[r], writes=[r])
    kb.op("dve", lambda e: e.reciprocal(rs[:, 0:1], sd[:, 0:1]), reads=[r], writes=[r])
    kb.op("dve", lambda e: e.tensor_scalar(xn[:], z[:], mv[:, 0:1], rs[:, 0:1], op0=ALU.subtract, op1=ALU.mult), reads=[r, z_r], writes=[S["xn_r"]])
    kb.op("pool", lambda e: e.tensor_tensor(xn[:], xn[:], g_bc[:], op=ALU.mult), reads=[S["xn_r"], par_r], writes=[S["xn_r"]])
    kb.op("pool", lambda e: e.tensor_tensor(out, xn[:], b_bc[:], op=ALU.add), reads=[S["xn_r"], par_r], writes=[out_r])


def ln_scratch(kb, pfx, eps):
    S = {}
    S["st"] = kb.sb(pfx + "st", [128, 2, 6], F32)
    S["mv"] = kb.sb(pfx + "mv", [128, 2], F32)
    S["sd"] = kb.sb(pfx + "sd", [128, 1], F32)
    S["rs"] = kb.sb(pfx + "rs", [128, 1], F32)
    S["xn"] = kb.sb(pfx + "xn", [128, 1024], F32)
    S["eps"] = kb.sb(pfx + "eps", [128, 1], F32)
    S["r"] = Res()
    S["xn_r"] = Res()
    kb.op("dve", lambda e: e.memset(S["eps"][:], eps), writes=[S["r"]])
    return S


def load_ln_params(kb, pfx, g_dram, b_dram, li):
    g = kb.sb(pfx + "g", [128, 1024], F32)
    b = kb.sb(pfx + "b", [128, 1024], F32)
    r = Res()
    kb.dma("sp", lambda e: e.dma_start(out=g[:], in_=bcast_rows(g_dram[li:li + 1, :])), writes=[r])
    kb.dma("sp", lambda e: e.dma_start(out=b[:], in_=bcast_rows(b_dram[li:li + 1, :])), writes=[r])
    return g, b, r


def attn_phase(kb, C, T, prm, j, li, xin, xin_r, xa, xa_r):
    nc = kb.nc
    NT = T // 128
    NS = T // 512
    lambda_init = 0.8 - 0.6 * math.exp(-0.3 * li)
    kb_push(kb)
    OT = kb.sb("a_OT", [128, 8, T], BF16)
    OT_r = [RL(NS) for _ in range(8)]
    ps = [kb.ps("a_ps%d" % b, [128, 512], F32) for b in range(8)]
    ps_r = RL(8)
    kb_push(kb)
    xld = [kb.sb("a_xld%d" % b, [128, 1024], F32) for b in range(2)]
    xld_r = RL(2)
    xT = kb.sb("a_xT", [128, 8, T], BF16)
    xT_r = RL(NT)
    QT = kb.sb("a_QT", [128, T], BF16)
    QT_r = RL(NS)
    KT = kb.sb("a_KT", [128, T], BF16)
    KT_r = RL(NS)
    V = kb.sb("a_V", [128, NT, 128], BF16)
    V_r = RL(NT // 4)
    wh = [kb.sb("a_wh%d" % b, [128, 8, 3, 128], BF16) for b in range(2)]
    wh_r = RL(2)
    pt = [kb.sb("a_pt%d" % b, [128, 512], BF16) for b in range(4)]
    pt_r = RL(4)
    lam = kb.sb("a_lam", [128, 256], F32)
    lsc = kb.sb("a_lsc", [128, 8], F32)
    lam_r = Res()
    s1 = kb.sb("a_s1", [128, 512], F32)
    s2 = kb.sb("a_s2", [128, 512], F32)
    t1 = kb.sb("a_t1", [128, 512], F32)
    t2 = kb.sb("a_t2", [128, 512], F32)
    sq = kb.sb("a_sq", [128, 512], BF16)
    op_ = t1
    s12 = s1
    e2 = s2
    tot = s2
    rstd = s1
    fin_r = Res()
    fin2_r = fin_r

    kb.dma("sp", lambda e: e.dma_start(out=lam[:], in_=bcast_rows(prm["attn_lambda"][j:j + 1].rearrange("o a b -> o (a b)"))), writes=[lam_r])
    kb.dma("sp", lambda e: e.dma_start(out=lsc[:, 4:5], in_=prm["attn_subln_g"][j].rearrange("(p o) -> p o", o=1)), writes=[lam_r])
    kb.op("dve", lambda e: e.tensor_tensor(lam[:, 0:64], lam[:, 0:64], lam[:, 64:128], op=ALU.mult), reads=[lam_r], writes=[lam_r])
    kb.op("dve", lambda e: e.tensor_tensor(lam[:, 128:192], lam[:, 128:192], lam[:, 192:256], op=ALU.mult), reads=[lam_r], writes=[lam_r])
    kb.op("dve", lambda e: e.reduce_sum(lsc[:, 0:1], lam[:, 0:64], axis=AX.X), reads=[lam_r], writes=[lam_r])
    kb.op("dve", lambda e: e.reduce_sum(lsc[:, 1:2], lam[:, 128:192], axis=AX.X), reads=[lam_r], writes=[lam_r])
    kb.op("act", lambda e: e.activation(lsc[:, 2:4], lsc[:, 0:2], AF.Exp), reads=[lam_r], writes=[lam_r])
    kb.op("dve", lambda e: e.tensor_tensor(lsc[:, 5:6], lsc[:, 3:4], lsc[:, 2:3], op=ALU.subtract), reads=[lam_r], writes=[lam_r])
    kb.op("dve", lambda e: e.tensor_scalar(lsc[:, 5:6], lsc[:, 5:6], -lambda_init, None, op0=ALU.add), reads=[lam_r], writes=[lam_r])
    kb.op("dve", lambda e: e.tensor_scalar(lsc[:, 6:7], lsc[:, 4:5], 1.0 - lambda_init, None, op0=ALU.mult), reads=[lam_r], writes=[lam_r])
    nlam = lsc[:, 5:6]
    gsc = lsc[:, 6:7]

    for i in range(NT):
        b = i % 2
        kb.dma("sp", lambda e, i=i, b=b: e.dma_start(out=xld[b][:], in_=xin[i * 128:(i + 1) * 128, :]), reads=[xin_r[i]], writes=[xld_r[b]])
        for hf in range(2):
            pb = (2 * i + hf) % 8
            for c4 in range(4):
                kc = hf * 4 + c4
                kb.op("pe", lambda e, pb=pb, c4=c4, kc=kc, b=b: e.transpose(ps[pb][:, c4 * 128:(c4 + 1) * 128], xld[b][:, kc * 128:(kc + 1) * 128], C.ident_f[:]),
                      reads=[xld_r[b], C.r], writes=[ps_r[pb]])
            eng = "act" if hf == 0 else "dve"
            if eng == "act":
                kb.op("act", lambda e, pb=pb, hf=hf, i=i: e.activation(xT[:, hf * 4:hf * 4 + 4, i * 128:(i + 1) * 128], ps[pb][:].rearrange("p (c t) -> p c t", c=4), AF.Copy),
                      reads=[ps_r[pb]], writes=[xT_r[i]])
            else:
                kb.op("dve", lambda e, pb=pb, hf=hf, i=i: e.tensor_copy(xT[:, hf * 4:hf * 4 + 4, i * 128:(i + 1) * 128], ps[pb][:].rearrange("p (c t) -> p c t", c=4)),
                      reads=[ps_r[pb]], writes=[xT_r[i]])


    wq = prm["attn_w_qkv"][j].rearrange("(kc p) n -> p kc n", p=128)
    pcnt = [0]

    def nps():
        pcnt[0] += 1
        return pcnt[0] % 2

    for h in range(NH_A):
        wb = h % 2
        for part in range(3):
            kb.dma("pool", lambda e, wb=wb, part=part, h=h: e.dma_start(out=wh[wb][:, :, part, :], in_=wq[:, :, part * 1024 + h * 128: part * 1024 + (h + 1) * 128]),
                   writes=[wh_r[wb]])
        for s in range(NS):
            for part, dst, dst_r, scl in ((0, QT, QT_r, 0.125), (1, KT, KT_r, 1.0)):
                pb = nps()
                for kc in range(8):
                    kb.op("pe", lambda e, pb=pb, wb=wb, kc=kc, part=part, s=s: e.matmul(ps[pb][:], wh[wb][:, kc, part, :], xT[:, kc, s * 512:(s + 1) * 512], start=(kc == 0), stop=(kc == 7)),
                          reads=[wh_r[wb]] + xT_r[s * 4:s * 4 + 4], writes=[ps_r[pb]])
                kb.op("act", lambda e, pb=pb, dst=dst, s=s, scl=scl: e.activation(dst[:, s * 512:(s + 1) * 512], ps[pb][:], AF.Copy, scale=scl),
                      reads=[ps_r[pb]], writes=[dst_r[s]])
        for g4 in range(NT // 4):
            pb = nps()
            for t4 in range(4):
                i = g4 * 4 + t4
                for kc in range(8):
                    kb.op("pe", lambda e, pb=pb, wb=wb, kc=kc, i=i, t4=t4: e.matmul(ps[pb][:, t4 * 128:(t4 + 1) * 128], xT[:, kc, i * 128:(i + 1) * 128], wh[wb][:, kc, 2, :], start=(kc == 0), stop=(kc == 7)),
                          reads=[wh_r[wb], xT_r[i]], writes=[ps_r[pb]])
            kb.op("dve", lambda e, pb=pb, g4=g4: e.tensor_copy(V[:, g4 * 4:g4 * 4 + 4, :], ps[pb][:].rearrange("p (c t) -> p c t", c=4)),
                  reads=[ps_r[pb]], writes=[V_r[g4]])
        pti = 0
        scnt = [0]
        for qs in range(NS):
            nkt = qs * 4 + 4
            items = [(kt, c) for kt in range(nkt) for c in range(2)]
            sbank = {}
            LOOK = 2

            def emit_s(n):
                kt, c = items[n]
                pb = (0, 1, 7)[scnt[0] % 3]
                scnt[0] += 1
                sbank[n] = pb
                kb.op("pe", lambda e, pb=pb, c=c, kt=kt, qs=qs: e.matmul(ps[pb][:], KT[c * 64:(c + 1) * 64, kt * 128:(kt + 1) * 128], QT[c * 64:(c + 1) * 64, qs * 512:(qs + 1) * 512], start=True, stop=True),
                      reads=[KT_r[kt // 4], QT_r[qs]], writes=[ps_r[pb]])

            for n in range(min(LOOK, len(items))):
                emit_s(n)
            for n, (kt, c) in enumerate(items):
                if n + LOOK < len(items):
                    emit_s(n + LOOK)
                pb = sbank[n]
                pi = pti % 4
                pti += 1
                kb.op("act", lambda e, pb=pb, pi=pi: e.activation(pt[pi][:], ps[pb][:], AF.Exp), reads=[ps_r[pb]], writes=[pt_r[pi]])
                if kt >= qs * 4:
                    o = kt - qs * 4
                    kb.op("pool", lambda e, pi=pi, o=o: e.tensor_tensor(pt[pi][:], pt[pi][:], C.cmask[:, o, :], op=ALU.mult), reads=[pt_r[pi], C.r], writes=[pt_r[pi]])
                kb.op("pe", lambda e, c=c, kt=kt, pi=pi, nkt=nkt: e.matmul(ps[2 + c][:], V[:, kt, :], pt[pi][:], start=(kt == 0), stop=(kt == nkt - 1)),
                      reads=[V_r[kt // 4], pt_r[pi]], writes=[ps_r[2 + c]])
                kb.op("pe", lambda e, c=c, kt=kt, pi=pi, nkt=nkt: e.matmul(ps[4 + c][:], C.ones_b[:], pt[pi][:], start=(kt == 0), stop=(kt == nkt - 1)),
                      reads=[C.r, pt_r[pi]], writes=[ps_r[4 + c]])
            kb.op("act", lambda e: e.activation(s1[:], ps[4][:], AF.Copy), reads=[ps_r[4]], writes=[fin_r])
            kb.op("act", lambda e: e.activation(s2[:], ps[5][:], AF.Copy), reads=[ps_r[5]], writes=[fin_r])
            kb.op("dve", lambda e: e.tensor_tensor(t1[:], ps[2][:], s2[:], op=ALU.mult), reads=[ps_r[2], fin_r], writes=[fin2_r])
            kb.op("dve", lambda e: e.tensor_tensor(t2[:], ps[3][:], s1[:], op=ALU.mult), reads=[ps_r[3], fin_r], writes=[fin2_r])
            kb.op("dve", lambda e: e.scalar_tensor_tensor(op_[:], t2[:], nlam, t1[:], op0=ALU.mult, op1=ALU.add), reads=[fin2_r, lam_r], writes=[fin2_r])
            kb.op("pool", lambda e: e.tensor_tensor(sq[:], op_[:], op_[:], op=ALU.mult), reads=[fin2_r], writes=[fin2_r])
            kb.op("pool", lambda e: e.tensor_tensor(s12[:], s1[:], s2[:], op=ALU.mult), reads=[fin_r], writes=[fin2_r])
            kb.op("dve", lambda e: e.scalar_tensor_tensor(e2[:], s12[:], RMS_EPS, s12[:], op0=ALU.mult, op1=ALU.mult), reads=[fin2_r], writes=[fin2_r])
            kb.op("pe", lambda e: e.matmul(ps[6][:], C.ones_b[:], sq[:], start=True, stop=True), reads=[C.r, fin2_r], writes=[ps_r[6]])
            kb.op("dve", lambda e: e.scalar_tensor_tensor(tot[:], ps[6][:], 1.0 / 128.0, e2[:], op0=ALU.mult, op1=ALU.add), reads=[ps_r[6], fin2_r], writes=[fin2_r])
            kb.op("act", lambda e: e.activation(tot[:], tot[:], AF.Ln), reads=[fin2_r], writes=[fin2_r])
            kb.op("act", lambda e: e.activation(rstd[:], tot[:], AF.Exp, scale=-0.5), reads=[fin2_r], writes=[fin2_r])
            kb.op("dve", lambda e, h=h, qs=qs: e.scalar_tensor_tensor(OT[:, h, qs * 512:(qs + 1) * 512], op_[:], gsc, rstd[:], op0=ALU.mult, op1=ALU.mult),
                  reads=[fin2_r, lam_r], writes=[OT_r[h][qs]])

    kb_pop(kb)
    wo = kb.sb("a_wo", [128, 8, 1024], BF16)
    wo_r = Res()
    wo_src = prm["attn_w_o"][j].rearrange("(h p) n -> p h n", p=128)
    for hh in range(8):
        kb.dma("pool", lambda e, hh=hh: e.dma_start(out=wo[:, hh, :], in_=wo_src[:, hh, :]), writes=[wo_r])
    xld = [kb.sb("a_xldc%d" % b, [128, 1024], F32) for b in range(2)]
    xld_r = RL(2)
    zt = [kb.sb("a_z%d" % b, [128, 1024], F32) for b in range(2)]
    zt_r = RL(2)
    x1 = [kb.sb("a_x1%d" % b, [128, 1024], F32) for b in range(2)]
    x1_r = RL(2)
    LS = ln_scratch(kb, "a_ln", LN_EPS)
    g_bc, b_bc, lnp_r = load_ln_params(kb, "a_lnp", prm["ln1_g"], prm["ln1_b"], li)
    for i in range(NT):
        b = i % 2
        kb.dma("sp", lambda e, i=i, b=b: e.dma_start(out=xld[b][:], in_=xin[i * 128:(i + 1) * 128, :]), reads=[xin_r[i]], writes=[xld_r[b]])
        for hf in range(2):
            pb = 2 * b + hf
            for hh in range(8):
                kb.op("pe", lambda e, pb=pb, hh=hh, hf=hf, i=i: e.matmul(ps[pb][:], OT[:, hh, i * 128:(i + 1) * 128], wo[:, hh, hf * 512:(hf + 1) * 512], start=(hh == 0), stop=(hh == 7)),
                      reads=[OT_r[hh][i // 4], wo_r], writes=[ps_r[pb]])
            kb.op("dve", lambda e, pb=pb, hf=hf, b=b: e.scalar_tensor_tensor(zt[b][:, hf * 512:(hf + 1) * 512], xld[b][:, hf * 512:(hf + 1) * 512], ALPHA, ps[pb][:], op0=ALU.mult, op1=ALU.add),
                  reads=[xld_r[b], ps_r[pb]], writes=[zt_r[b]])
        ln_tile(kb, LS, zt[b], zt_r[b], x1[b][:], x1_r[b], g_bc, b_bc, lnp_r)
        kb.dma("sp", lambda e, i=i, b=b: e.dma_start(out=xa[i * 128:(i + 1) * 128, :], in_=x1[b][:]), reads=[x1_r[b]], writes=[xa_r[i]])
    kb_pop(kb)


PSHAPES = {
    "ln1_g": (4, 1024), "ln1_b": (4, 1024), "ln2_g": (4, 1024), "ln2_b": (4, 1024),
    "attn_w_qkv": (2, 1024, 3072), "attn_w_o": (2, 1024, 1024), "attn_lambda": (2, 4, 64), "attn_subln_g": (2, 128),
    "rw_mu": (2, 6, 1024), "rw_w_rkv": (2, 3, 1024, 1024), "rw_w_o": (2, 1024, 1024), "rw_w0": (2, 1024),
    "rw_w1": (2, 1024, 64), "rw_w2": (2, 64, 1024), "rw_a0": (2, 1024), "rw_a1": (2, 1024, 64), "rw_a2": (2, 64, 1024),
    "rw_g1": (2, 1024, 160), "rw_g2": (2, 160, 1024), "rw_k_k": (2, 1024), "rw_k_a": (2, 1024), "rw_r_k": (2, 16, 64),
    "rw_lnx_g": (2, 1024), "rw_lnx_b": (2, 1024), "rw_v0": (1, 1024), "rw_v1": (1, 1024, 32), "rw_v2": (1, 32, 1024),
    "moe_rg_w": (4, 1024, 4), "moe_rg_b": (4, 4), "moe_re_w": (4, 1024, 32), "moe_re_b": (4, 32),
    "moe_w_gu": (4, 32, 1024, 1024), "moe_w_down": (4, 32, 512, 1024),
}


class Params(dict):
    def __init__(self, nc):
        super().__init__()
        self.nc = nc

    def __missing__(self, k):
        ap = self.nc.dram_tensor(k, list(PSHAPES[k]), F32, kind="ExternalInput").ap()
        self[k] = ap
        return ap


def build(T, plan, cap=512):
    nc = bass.Bass("TRN2", target_bir_lowering=False)
    prm = Params(nc)
    x = nc.dram_tensor("x", [T, D], F32, kind="ExternalInput").ap()
    out = nc.dram_tensor("out", [T, D], F32, kind="ExternalOutput").ap()
    xa = nc.dram_tensor("xa_s", [T, D], F32, kind="Internal").ap()
    xb = nc.dram_tensor("xb_s", [T, D], F32, kind="Internal").ap()
    NT = T // 128
    es = ExitStack()
    with es:
        kb = KB(nc, es)
        kb.es_t = es
        C = make_consts(kb)
        cur, cur_r = x, RL(NT)
        xa_r, xb_r, out_r = RL(NT), RL(NT), RL(NT)
        ST = {}
        for n, step in enumerate(plan):
            last = n == len(plan) - 1
            kind = step[0]
            if kind == "attn":
                attn_phase(kb, C, T, prm, step[1], step[2], cur, cur_r, xa, xa_r)
                cur, cur_r = xa, xa_r
            elif kind == "rwkv":
                rwkv_phase(kb, C, T, prm, step[1], step[2], cur, cur_r, xa, xa_r, ST, nc)
                cur, cur_r = xa, xa_r
            elif kind == "moe":
                dst, dst_r = (out, out_r) if last else (xb, xb_r)
                moe_phase(kb, C, T, prm, step[1], cur, cur_r, dst, dst_r, cap, nc, ST)
                cur, cur_r = dst, dst_r
        if cur is not out:
            for i in range(NT):
                kb.dma("sp", lambda e, i=i: e.dma_start(out=out[i * 128:(i + 1) * 128, :], in_=cur[i * 128:(i + 1) * 128, :]), reads=[cur_r[i]], writes=[out_r[i]])
        kb.finish()
        ninst = kb.nins
    return nc, list(prm.keys()), ninst


FULL_PLAN = [("attn", 0, 0), ("moe", 0), ("rwkv", 0, 1), ("moe", 1), ("attn", 1, 2), ("moe", 2), ("rwkv", 1, 3), ("moe", 3)]


def moe_phase(kb, C, T, prm, li, xin, xin_r, dst, dst_r, cap, nc, ST):
    NT = T // 128
    NSLOT = NE * cap
    NB = cap // 128
    if "xbuf" not in ST:
        ST["xbuf"] = nc.dram_tensor("xbuf_s", [NSLOT, D], BF16, kind="Internal").ap()
        ST["ybuf"] = nc.dram_tensor("ybuf_s", [NSLOT, D], F32, kind="Internal").ap()
        ST["bc_reg"] = nc.gpsimd.to_reg(NSLOT - 1)
    xbuf, ybuf = ST["xbuf"], ST["ybuf"]
    bc_reg = ST["bc_reg"]
    kb_push(kb)
    slots = kb.sb("m_slots", [128, NT, 2], I32)
    gates = kb.sb("m_gates", [128, NT, 2], F32)
    sg_r = Res()

    kb_push(kb)
    ps = [kb.ps("m1_ps%d" % b, [128, 512], F32) for b in range(6)]
    ps_r = RL(6)
    xt = [kb.sb("m1_xt%d" % b, [128, 1024], F32) for b in range(2)]
    xt_r = RL(2)
    xb16 = [kb.sb("m1_xb%d" % b, [128, 1024], BF16) for b in range(2)]
    xb_r = RL(2)
    xT = [kb.sb("m1_xT%d" % b, [128, 8, 128], F32) for b in range(2)]
    xT_r = RL(2)
    wr = kb.sb("m1_wr", [128, 8, 36], F32)
    rb = kb.sb("m1_rb", [128, 36], F32)
    offs_i = kb.sb("m1_offi", [128, 32], I32)
    offs = kb.sb("m1_off", [128, 32], F32)
    base = kb.sb("m1_base", [128, 32], F32)
    cr = Res()
    base_r = Res()
    kb.dma("sp", lambda e: e.dma_start(out=wr[:, :, 0:4], in_=prm["moe_rg_w"][li].rearrange("(kc p) n -> p kc n", p=128)), writes=[cr])
    kb.dma("sp", lambda e: e.dma_start(out=wr[:, :, 4:36], in_=prm["moe_re_w"][li].rearrange("(kc p) n -> p kc n", p=128)), writes=[cr])
    kb.dma("sp", lambda e: e.dma_start(out=rb[:, 0:4], in_=bcast_rows(prm["moe_rg_b"][li:li + 1, :])), writes=[cr])
    kb.dma("sp", lambda e: e.dma_start(out=rb[:, 4:36], in_=bcast_rows(prm["moe_re_b"][li:li + 1, :])), writes=[cr])
    kb.op("pool", lambda e: e.iota(offs_i[:], [[cap, 32]], base=-1, channel_multiplier=0), writes=[cr])
    kb.op("dve", lambda e: e.tensor_copy(offs[:], offs_i[:]), reads=[cr], writes=[cr])
    kb.op("dve", lambda e: e.memset(base[:], 0.0), writes=[base_r])
    Wk = {}
    for nm, w in (("L", 36), ("ohg", 4), ("eg", 4), ("lsel", 8), ("oh1", 8), ("lsel2", 8), ("oh2", 8), ("E1", 32), ("E2", 32),
                  ("val", 32), ("valid", 32), ("val2", 32), ("tmp", 32), ("sc", 16)):
        Wk[nm] = kb.sb("m1_w" + nm, [128, w], F32)
    G01 = kb.sb("m1_G01", [128, 32], BF16)
    wr_ = Res()
    BIG = float(NSLOT)
    for i in range(NT):
        b = i % 2
        kb.dma("sp", lambda e, i=i, b=b: e.dma_start(out=xt[b][:], in_=xin[i * 128:(i + 1) * 128, :]), reads=[xin_r[i]], writes=[xt_r[b]])
        kb.op("act", lambda e, b=b: e.activation(xb16[b][:], xt[b][:], AF.Copy), reads=[xt_r[b]], writes=[xb_r[b]])
        for hf in range(2):
            pb = hf
            for c4 in range(4):
                kc = hf * 4 + c4
                kb.op("pe", lambda e, pb=pb, c4=c4, kc=kc, b=b: e.transpose(ps[pb][:, c4 * 128:(c4 + 1) * 128], xt[b][:, kc * 128:(kc + 1) * 128], C.ident_f[:]),
                      reads=[xt_r[b], C.r], writes=[ps_r[pb]])
            if hf == 0:
                kb.op("act", lambda e, pb=pb, b=b: e.activation(xT[b][:, 0:4, :], ps[pb][:].rearrange("p (c t) -> p c t", c=4), AF.Copy), reads=[ps_r[pb]], writes=[xT_r[b]])
            else:
                kb.op("dve", lambda e, pb=pb, b=b: e.tensor_copy(xT[b][:, 4:8, :], ps[pb][:].rearrange("p (c t) -> p c t", c=4)), reads=[ps_r[pb]], writes=[xT_r[b]])
        for kc in range(8):
            kb.op("pe", lambda e, kc=kc, b=b: e.matmul(ps[2][:, 0:36], xT[b][:, kc, :], wr[:, kc, :], start=(kc == 0), stop=(kc == 7)), reads=[xT_r[b], cr], writes=[ps_r[2]])
        L, ohg, eg, lsel, oh1, lsel2, oh2, E1, E2 = (Wk[k] for k in ("L", "ohg", "eg", "lsel", "oh1", "lsel2", "oh2", "E1", "E2"))
        val, valid, val2, tmp, sc = (Wk[k] for k in ("val", "valid", "val2", "tmp", "sc"))

        def dv(fn, extra_r=(), extra_w=()):
            kb.op("dve", fn, reads=[wr_] + list(extra_r), writes=[wr_] + list(extra_w))

        dv(lambda e: e.tensor_tensor(L[:], ps[2][:, 0:36], rb[:], op=ALU.add), extra_r=[ps_r[2], cr])
        dv(lambda e: e.reduce_max(sc[:, 0:1], L[:, 0:4], axis=AX.X))
        dv(lambda e: e.tensor_scalar(ohg[:], L[:, 0:4], sc[:, 0:1], None, op0=ALU.is_equal))
        dv(lambda e: e.tensor_scalar(sc[:, 1:2], sc[:, 0:1], -1.0, None, op0=ALU.mult))
        kb.op("act", lambda e: e.activation(eg[:], L[:, 0:4], AF.Exp, bias=sc[:, 1:2]), reads=[wr_], writes=[wr_])
        dv(lambda e: e.reduce_sum(sc[:, 2:3], eg[:], axis=AX.X))
        dv(lambda e: e.reciprocal(sc[:, 3:4], sc[:, 2:3]))
        dv(lambda e: e.tensor_scalar(lsel[:], L[:, 4:12], ohg[:, 0:1], None, op0=ALU.mult))
        for g in range(1, 4):
            dv(lambda e, g=g: e.scalar_tensor_tensor(lsel[:], L[:, 4 + 8 * g:12 + 8 * g], ohg[:, g:g + 1], lsel[:], op0=ALU.mult, op1=ALU.add))
        dv(lambda e: e.reduce_max(sc[:, 4:5], lsel[:], axis=AX.X))
        dv(lambda e: e.tensor_scalar(oh1[:], lsel[:], sc[:, 4:5], None, op0=ALU.is_equal))
        dv(lambda e: e.scalar_tensor_tensor(lsel2[:], oh1[:], -1e30, lsel[:], op0=ALU.mult, op1=ALU.add))
        dv(lambda e: e.reduce_max(sc[:, 5:6], lsel2[:], axis=AX.X))
        dv(lambda e: e.tensor_scalar(oh2[:], lsel2[:], sc[:, 5:6], None, op0=ALU.is_equal))
        dv(lambda e: e.tensor_tensor(sc[:, 6:7], sc[:, 5:6], sc[:, 4:5], op=ALU.subtract))
        kb.op("act", lambda e: e.activation(sc[:, 7:8], sc[:, 6:7], AF.Exp), reads=[wr_], writes=[wr_])
        dv(lambda e: e.tensor_scalar(sc[:, 8:9], sc[:, 7:8], 1.0, None, op0=ALU.add))
        dv(lambda e: e.reciprocal(sc[:, 9:10], sc[:, 8:9]))
        dv(lambda e: e.tensor_tensor(sc[:, 10:11], sc[:, 7:8], sc[:, 9:10], op=ALU.mult))
        for g in range(4):
            dv(lambda e, g=g: e.tensor_scalar(E1[:, 8 * g:8 * g + 8], oh1[:], ohg[:, g:g + 1], None, op0=ALU.mult))
            dv(lambda e, g=g: e.tensor_scalar(E2[:, 8 * g:8 * g + 8], oh2[:], ohg[:, g:g + 1], None, op0=ALU.mult))
        dv(lambda e: e.tensor_tensor(G01[:], E1[:], E2[:], op=ALU.add))
        kb.op("pe", lambda e: e.matmul(ps[3][:, 0:32], C.triu_b[:], G01[:], start=True, stop=True), reads=[wr_, C.r], writes=[ps_r[3]])
        kb.op("pe", lambda e: e.matmul(ps[3][:, 32:64], C.ones_b[:], G01[:], start=True, stop=True), reads=[wr_, C.r], writes=[ps_r[3]])
        dv(lambda e: e.tensor_tensor(val[:], ps[3][:, 0:32], base[:], op=ALU.add), extra_r=[ps_r[3], base_r])
        dv(lambda e: e.tensor_scalar(valid[:], val[:], float(cap), None, op0=ALU.is_le))
        dv(lambda e: e.tensor_tensor(val2[:], val[:], offs[:], op=ALU.add), extra_r=[cr])
        dv(lambda e: e.scalar_tensor_tensor(val2[:], val2[:], -BIG, valid[:], op0=ALU.add, op1=ALU.mult))
        dv(lambda e: e.tensor_scalar(val2[:], val2[:], BIG, None, op0=ALU.add))
        dv(lambda e: e.tensor_tensor(tmp[:], E1[:], val2[:], op=ALU.mult))
        dv(lambda e: e.reduce_sum(sc[:, 11:12], tmp[:], axis=AX.X))
        dv(lambda e: e.tensor_tensor(tmp[:], E2[:], val2[:], op=ALU.mult))
        dv(lambda e: e.reduce_sum(sc[:, 12:13], tmp[:], axis=AX.X))
        dv(lambda e: e.tensor_tensor(tmp[:], E1[:], valid[:], op=ALU.mult))
        dv(lambda e: e.reduce_sum(sc[:, 13:14], tmp[:], axis=AX.X))
        dv(lambda e: e.tensor_tensor(tmp[:], E2[:], valid[:], op=ALU.mult))
        dv(lambda e: e.reduce_sum(sc[:, 14:15], tmp[:], axis=AX.X))
        dv(lambda e: e.tensor_tensor(base[:], base[:], ps[3][:, 32:64], op=ALU.add), extra_r=[ps_r[3]], extra_w=[base_r])
        dv(lambda e, i=i: e.tensor_copy(slots[:, i, :], sc[:, 11:13]), extra_w=[sg_r])
        dv(lambda e: e.tensor_scalar(sc[:, 9:11], sc[:, 9:11], sc[:, 3:4], None, op0=ALU.mult))
        dv(lambda e, i=i: e.tensor_tensor(gates[:, i, :], sc[:, 9:11], sc[:, 13:15], op=ALU.mult), extra_w=[sg_r])
        for k in range(2):
            kb.dma("pool", lambda e, i=i, k=k, b=b: e.indirect_dma_start(out=xbuf[:, :], out_offset=bass.IndirectOffsetOnAxis(ap=slots[:, i, k:k + 1], axis=0),
                                                                       in_=xb16[b][:], in_offset=None, bounds_check=bc_reg, oob_is_err=False),
                   reads=[sg_r, xb_r[b]])
    kb_pop(kb)

    kb_push(kb)
    psT = [kb.ps("m2_pT%d" % b, [128, 1024], BF16) for b in range(2)]
    psT_r = RL(2)
    psH = [kb.ps("m2_pH%d" % b, [128, 512], F32) for b in range(4)]
    psH_r = RL(4)
    psY = [kb.ps("m2_pY%d" % b, [128, 512], F32) for b in range(2)]
    psY_r = RL(2)
    wgu = [kb.sb("m2_wgu%d" % b, [128, 8, 1024], BF16) for b in range(2)]
    wgu_r = RL(2)
    wd = [kb.sb("m2_wd%d" % b, [128, 4, 1024], BF16) for b in range(2)]
    wd_r = RL(2)
    xblk = [kb.sb("m2_xb%d" % b, [128, 1024], BF16) for b in range(2)]
    xblk_r = RL(2)
    XT = [kb.sb("m2_XT%d" % b, [128, 8, cap], BF16) for b in range(2)]
    XT_r = RL(2)
    sil = [kb.sb("m2_sil%d" % b, [128, cap], F32) for b in range(2)]
    sil_r = RL(2)
    AT = [kb.sb("m2_AT%d" % b, [128, 4, cap], BF16) for b in range(2)]
    AT_r = RL(2)
    ysb = [kb.sb("m2_y%d" % b, [128, 1024], F32) for b in range(2)]
    ysb_r = RL(2)
    nblk = 0
    for ex in range(NE):
        wb = ex % 2
        gsrc = prm["moe_w_gu"][li, ex].rearrange("(kc p) n -> p kc n", p=128)
        dsrc = prm["moe_w_down"][li, ex].rearrange("(m p) n -> p m n", p=128)
        for kc in range(8):
            kb.dma("pool", lambda e, wb=wb, kc=kc, gsrc=gsrc: e.dma_start(out=wgu[wb][:, kc, :], in_=gsrc[:, kc, :]), writes=[wgu_r[wb]])
        for m in range(4):
            kb.dma("pool", lambda e, wb=wb, m=m, dsrc=dsrc: e.dma_start(out=wd[wb][:, m, :], in_=dsrc[:, m, :]), writes=[wd_r[wb]])
        for blk in range(NB):
            bb = nblk % 2
            nblk += 1
            r0 = ex * cap + blk * 128
            kb.dma("sp", lambda e, bb=bb, r0=r0: e.dma_start(out=xblk[bb][:], in_=xbuf[r0:r0 + 128, :]), writes=[xblk_r[bb]])
            for kc in range(8):
                kb.op("pe", lambda e, bb=bb, kc=kc: e.transpose(psT[bb][:, kc * 128:(kc + 1) * 128], xblk[bb][:, kc * 128:(kc + 1) * 128], C.ident_b[:]),
                      reads=[xblk_r[bb], C.r], writes=[psT_r[bb]])
            eng = "act" if blk % 2 == 0 else "dve"
            if eng == "act":
                kb.op("act", lambda e, bb=bb, wb=wb, blk=blk: e.activation(XT[wb][:, :, blk * 128:(blk + 1) * 128], psT[bb][:].rearrange("p (c t) -> p c t", c=8), AF.Copy),
                      reads=[psT_r[bb]], writes=[XT_r[wb]])
            else:
                kb.op("dve", lambda e, bb=bb, wb=wb, blk=blk: e.tensor_copy(XT[wb][:, :, blk * 128:(blk + 1) * 128], psT[bb][:].rearrange("p (c t) -> p c t", c=8)),
                      reads=[psT_r[bb]], writes=[XT_r[wb]])
        for m in range(4):
            pg, pu = (m % 2) * 2, (m % 2) * 2 + 1
            for (pb, col) in ((pg, m * 128), (pu, 512 + m * 128)):
                for kc in range(8):
                    kb.op("pe", lambda e, pb=pb, col=col, kc=kc, wb=wb: e.matmul(psH[pb][:, 0:cap], wgu[wb][:, kc, col:col + 128], XT[wb][:, kc, :], start=(kc == 0), stop=(kc == 7)),
                          reads=[wgu_r[wb], XT_r[wb]], writes=[psH_r[pb]])
            sb_ = m % 2
            kb.op("act", lambda e, pg=pg, sb_=sb_: e.activation(sil[sb_][:], psH[pg][:, 0:cap], AF.Silu), reads=[psH_r[pg]], writes=[sil_r[sb_]])
            kb.op("dve", lambda e, pu=pu, sb_=sb_, m=m, wb=wb: e.tensor_tensor(AT[wb][:, m, :], sil[sb_][:], psH[pu][:, 0:cap], op=ALU.mult),
                  reads=[psH_r[pu], sil_r[sb_]], writes=[AT_r[wb]])
        for blk in range(NB):
            yb = blk % 2
            for hf in range(2):
                for m in range(4):
                    kb.op("pe", lambda e, hf=hf, m=m, wb=wb, blk=blk: e.matmul(psY[hf][:], AT[wb][:, m, blk * 128:(blk + 1) * 128], wd[wb][:, m, hf * 512:(hf + 1) * 512], start=(m == 0), stop=(m == 3)),
                          reads=[AT_r[wb], wd_r[wb]], writes=[psY_r[hf]])
                if hf == 0:
                    kb.op("act", lambda e, yb=yb: e.activation(ysb[yb][:, 0:512], psY[0][:], AF.Copy), reads=[psY_r[0]], writes=[ysb_r[yb]])
                else:
                    kb.op("dve", lambda e, yb=yb: e.tensor_copy(ysb[yb][:, 512:1024], psY[1][:]), reads=[psY_r[1]], writes=[ysb_r[yb]])
            r0 = ex * cap + blk * 128
            kb.dma("sp", lambda e, yb=yb, r0=r0: e.dma_start(out=ybuf[r0:r0 + 128, :], in_=ysb[yb][:]), reads=[ysb_r[yb]])
    kb_pop(kb)

    kb_push(kb)
    y1 = [kb.sb("m3_y1%d" % b, [128, 1024], F32) for b in range(2)]
    y2 = [kb.sb("m3_y2%d" % b, [128, 1024], F32) for b in range(2)]
    y_r = RL(2)
    xt3 = [kb.sb("m3_xt%d" % b, [128, 1024], F32) for b in range(2)]
    xt3_r = RL(2)
    z3 = [kb.sb("m3_z%d" % b, [128, 1024], F32) for b in range(2)]
    z3_r = RL(2)
    x2 = [kb.sb("m3_x2%d" % b, [128, 1024], F32) for b in range(2)]
    x2_r = RL(2)
    LS = ln_scratch(kb, "m3_ln", LN_EPS)
    g_bc, b_bc, lnp_r = load_ln_params(kb, "m3_lnp", prm["ln2_g"], prm["ln2_b"], li)
    for b in range(2):
        kb.op("pool", lambda e, b=b: e.memset(y1[b][:], 0.0), writes=[y_r[b]])
        kb.op("pool", lambda e, b=b: e.memset(y2[b][:], 0.0), writes=[y_r[b]])
    for i in range(NT):
        b = i % 2
        kb.dma("sp", lambda e, i=i, b=b: e.dma_start(out=xt3[b][:], in_=xin[i * 128:(i + 1) * 128, :]), reads=[xin_r[i]], writes=[xt3_r[b]])
        for k, yy in ((0, y1), (1, y2)):
            kb.dma("pool", lambda e, i=i, k=k, b=b, yy=yy: e.indirect_dma_start(out=yy[b][:], out_offset=None, in_=ybuf[:, :],
                                                                              in_offset=bass.IndirectOffsetOnAxis(ap=slots[:, i, k:k + 1], axis=0),
                                                                              bounds_check=bc_reg, oob_is_err=False),
                   reads=[sg_r], writes=[y_r[b]])
        kb.op("act", lambda e, b=b: e.activation(z3[b][:], xt3[b][:], AF.Copy, scale=ALPHA), reads=[xt3_r[b]], writes=[z3_r[b]])
        kb.op("dve", lambda e, b=b, i=i: e.scalar_tensor_tensor(z3[b][:], y1[b][:], gates[:, i, 0:1], z3[b][:], op0=ALU.mult, op1=ALU.add), reads=[y_r[b], sg_r], writes=[z3_r[b]])
        kb.op("dve", lambda e, b=b, i=i: e.scalar_tensor_tensor(z3[b][:], y2[b][:], gates[:, i, 1:2], z3[b][:], op0=ALU.mult, op1=ALU.add), reads=[y_r[b], sg_r], writes=[z3_r[b]])
        ln_tile(kb, LS, z3[b], z3_r[b], x2[b][:], x2_r[b], g_bc, b_bc, lnp_r)
        kb.dma("sp", lambda e, i=i, b=b: e.dma_start(out=dst[i * 128:(i + 1) * 128, :], in_=x2[b][:]), reads=[x2_r[b]], writes=[dst_r[i]])
    kb_pop(kb)
    kb_pop(kb)


C0 = math.exp(-0.5)


def rwkv_phase(kb, C, T, prm, j, li, xin, xin_r, xa, xa_r, ST, nc):
    NT = T // 128
    NSUP = T // 256
    if "ARd" not in ST:
        ST["ARd"] = nc.dram_tensor("ARd_s", [NT, 128, 8 * 2 * 128], BF16, kind="Internal").ap()
        ST["BKd"] = nc.dram_tensor("BKd_s", [NT, 128, 8 * 2 * 128], BF16, kind="Internal").ap()
        ST["rkd"] = nc.dram_tensor("rkd_s", [NT, 128, 8 * 128], BF16, kind="Internal").ap()
        ST["Pcd"] = nc.dram_tensor("Pcd_s", [NT, 128, 8], F32, kind="Internal").ap()
        ST["Vd"] = nc.dram_tensor("Vd_s", [T, D], BF16, kind="Internal").ap()
        ST["Gd"] = nc.dram_tensor("Gd_s", [T, D], BF16, kind="Internal").ap()
        ST["vfirst"] = nc.dram_tensor("vfirst_s", [T, D], F32, kind="Internal").ap()
    ARd, BKd, rkd, Pcd, Vd, Gd, vfd = (ST[k] for k in ("ARd", "BKd", "rkd", "Pcd", "Vd", "Gd", "vfirst"))

    kb_push(kb)
    ps = [kb.ps("r1_ps%d" % b, [128, 512], F32) for b in range(8)]
    ps_r = RL(8)
    wrkv = kb.sb("r1_wrkv", [128, 3, 8, 1024], BF16)
    w1 = kb.sb("r1_w1", [128, 8, 64], BF16)
    a1 = kb.sb("r1_a1", [128, 8, 64], BF16)
    g1 = kb.sb("r1_g1", [128, 8, 160], BF16)
    w2 = kb.sb("r1_w2", [64, 1024], BF16)
    a2 = kb.sb("r1_a2", [64, 1024], BF16)
    g2a = kb.sb("r1_g2a", [128, 1024], BF16)
    g2b = kb.sb("r1_g2b", [32, 1024], BF16)
    wr_ = Res()
    for n in range(3):
        src = prm["rw_w_rkv"][j, n].rearrange("(kc p) n -> p kc n", p=128)
        for kc in range(8):
            kb.dma("pool", lambda e, n=n, kc=kc, src=src: e.dma_start(out=wrkv[:, n, kc, :], in_=src[:, kc, :]), writes=[wr_])
    kb.dma("pool", lambda e: e.dma_start(out=w1[:], in_=prm["rw_w1"][j].rearrange("(kc p) n -> p kc n", p=128)), writes=[wr_])
    kb.dma("pool", lambda e: e.dma_start(out=a1[:], in_=prm["rw_a1"][j].rearrange("(kc p) n -> p kc n", p=128)), writes=[wr_])
    kb.dma("pool", lambda e: e.dma_start(out=g1[:], in_=prm["rw_g1"][j].rearrange("(kc p) n -> p kc n", p=128)), writes=[wr_])
    kb.dma("pool", lambda e: e.dma_start(out=w2[:], in_=prm["rw_w2"][j]), writes=[wr_])
    kb.dma("pool", lambda e: e.dma_start(out=a2[:], in_=prm["rw_a2"][j]), writes=[wr_])
    kb.dma("pool", lambda e: e.dma_start(out=g2a[:], in_=prm["rw_g2"][j, 0:128, :]), writes=[wr_])
    kb.dma("pool", lambda e: e.dma_start(out=g2b[:], in_=prm["rw_g2"][j, 128:160, :]), writes=[wr_])
    if j > 0:
        v1 = kb.sb("r1_v1", [128, 8, 32], BF16)
        v2 = kb.sb("r1_v2", [32, 1024], BF16)
        v0b = kb.sb("r1_v0b", [128, 1024], F32)
        kb.dma("pool", lambda e: e.dma_start(out=v1[:], in_=prm["rw_v1"][j - 1].rearrange("(kc p) n -> p kc n", p=128)), writes=[wr_])
        kb.dma("pool", lambda e: e.dma_start(out=v2[:], in_=prm["rw_v2"][j - 1]), writes=[wr_])
        kb.dma("sp", lambda e: e.dma_start(out=v0b[:], in_=bcast_rows(prm["rw_v0"][j - 1:j, :])), writes=[wr_])
    pvin = kb.sb("r1_pvin", [88, 128], F32)
    pvall = kb.sb("r1_pvall", [128, 88], F32)
    oma = kb.sb("r1_oma", [128, 8], F32)
    pv_r = Res()
    kb.dma("sp", lambda e: e.dma_start(out=pvin[0:48, :], in_=prm["rw_mu"][j].rearrange("n (kc p) -> (n kc) p", p=128)), writes=[pv_r])
    for idx, nm in enumerate(("rw_w0", "rw_a0", "rw_k_k", "rw_k_a")):
        kb.dma("sp", lambda e, idx=idx, nm=nm: e.dma_start(out=pvin[48 + 8 * idx:56 + 8 * idx, :], in_=prm[nm][j].rearrange("(oc p) -> oc p", p=128)), writes=[pv_r])
    kb.dma("sp", lambda e: e.dma_start(out=pvin[80:88, :], in_=prm["rw_r_k"][j].rearrange("(oc hh) n -> oc (hh n)", hh=2)), writes=[pv_r])
    kb.op("pe", lambda e: e.transpose(ps[0][:, 0:88], pvin[:], C.ident_f[0:88, 0:88]), reads=[pv_r, C.r], writes=[ps_r[0]])
    kb.op("dve", lambda e: e.tensor_copy(pvall[:], ps[0][:, 0:88]), reads=[ps_r[0]], writes=[pv_r])
    kb.op("dve", lambda e: e.tensor_scalar(oma[:], pvall[:, 72:80], -1.0, 1.0, op0=ALU.mult, op1=ALU.add), reads=[pv_r], writes=[pv_r])

    class _PV:
        def __getitem__(self, key):
            p, idx, oc = key
            if idx == 5:
                return oma[p, oc]
            return pvall[p, (oc.start + 48 + 8 * idx):(oc.stop + 48 + 8 * idx)]

    class _MU:
        def __getitem__(self, key):
            p, n, kc = key
            return pvall[p, (n * 8 + kc.start):(n * 8 + kc.stop)]

    pv = _PV()
    mu = _MU()
    rst = kb.sb("r1_rst", [128, 256], F32)
    kb.op("dve", lambda e: e.memset(rst[:], 1.0), writes=[pv_r])
    kb.op("dve", lambda e: e.memset(rst[:, 0:1], 0.0), writes=[pv_r])
    kb.op("dve", lambda e: e.memset(rst[:, 128:129], 0.0), writes=[pv_r])
    bd64 = kb.sb("r1_bd64", [128, 128], BF16)
    kb.op("dve", lambda e: e.memset(bd64[:], 0.0), writes=[pv_r])
    kb.op("dve", lambda e: e.memset(bd64[0:64, 0:64], 1.0), writes=[pv_r])
    kb.op("dve", lambda e: e.memset(bd64[64:128, 64:128], 1.0), writes=[pv_r])

    xld = [kb.sb("r1_xld%d" % b, [128, 1024], F32) for b in range(2)]
    xld_r = RL(2)
    xTs = [kb.sb("r1_xTs%d" % b, [128, 8, 257], BF16) for b in range(2)]
    xTs_r = RL(2)
    xx = kb.sb("r1_xx", [128, 8, 256], F32)
    xx_r = Res()
    xm = [kb.sb("r1_xm%d" % b, [128, 8, 256], BF16) for b in range(3)]
    xm_r = RL(3)
    AR = kb.sb("r1_AR", [128, 8, 2, 2, 128], BF16)
    BK = kb.sb("r1_BK", [128, 8, 2, 2, 128], BF16)
    rk = kb.sb("r1_rk", [128, 8, 256], BF16)
    Pc = kb.sb("r1_Pc", [128, 2, 8], F32)
    out_r = Res()
    hw = kb.sb("r1_hw", [64, 256], BF16)
    ha = kb.sb("r1_ha", [64, 256], BF16)
    hg1 = kb.sb("r1_hg1", [128, 256], BF16)
    hg2 = kb.sb("r1_hg2", [32, 256], BF16)
    hid_r = Res()
    if j > 0:
        hv = kb.sb("r1_hv", [32, 256], BF16)
    tnames = ("sgw", "cum", "cumx", "pin", "pinv", "pprev", "asig", "kk", "lns", "rn", "kkn", "t1", "k2", "tb")
    tm = {k: kb.sb("r1_t" + k, [128, 256], F32) for k in tnames}
    kk2 = kb.sb("r1_kk2", [128, 256], BF16)
    tm_r = Res()
    vsb = [kb.sb("r1_v%d" % b, [128, 1024], F32) for b in range(2)]
    vsb_r = RL(2)
    vb16 = [kb.sb("r1_vb%d" % b, [128, 1024], BF16) for b in range(2)]
    vb_r = RL(2)
    gsb = [kb.sb("r1_g%d" % b, [128, 1024], BF16) for b in range(2)]
    gsb_r = RL(2)
    if j > 0:
        vfs = [kb.sb("r1_vf%d" % b, [128, 1024], F32) for b in range(2)]
        vfs_r = RL(2)
        vmx = [kb.sb("r1_vm%d" % b, [128, 1024], F32) for b in range(2)]
        vmx_r = RL(2)
    kb.op("dve", lambda e: e.memset(xTs[0][:, :, 0:1], 0.0), writes=[xTs_r[0]])

    def mix(n, buf, xb):
        for kc in range(8):
            kb.op("dve", lambda e, kc=kc: e.scalar_tensor_tensor(xm[buf][:, kc, :], xx[:, kc, :], mu[:, n, kc:kc + 1], xTs[xb][:, kc, 1:257], op0=ALU.mult, op1=ALU.add),
                  reads=[xx_r, pv_r, xTs_r[xb]], writes=[xm_r[buf]])

    for s in range(NSUP):
        xb = s % 2
        if s > 0:
            kb.op("pool", lambda e, xb=xb: e.tensor_copy(xTs[xb][:, :, 0:1], xTs[1 - xb][:, :, 256:257]), reads=[xTs_r[1 - xb]], writes=[xTs_r[xb]])
        for tl in range(2):
            i = s * 2 + tl
            b = i % 2
            kb.dma("sp", lambda e, i=i, b=b: e.dma_start(out=xld[b][:], in_=xin[i * 128:(i + 1) * 128, :]), reads=[xin_r[i]], writes=[xld_r[b]])
            for hf in range(2):
                pb = 6 + hf
                for c4 in range(4):
                    kc = hf * 4 + c4
                    kb.op("pe", lambda e, pb=pb, c4=c4, kc=kc, b=b: e.transpose(ps[pb][:, c4 * 128:(c4 + 1) * 128], xld[b][:, kc * 128:(kc + 1) * 128], C.ident_f[:]),
                          reads=[xld_r[b], C.r], writes=[ps_r[pb]])
                if hf == 0:
                    kb.op("act", lambda e, pb=pb, xb=xb, tl=tl: e.activation(xTs[xb][:, 0:4, 1 + tl * 128:1 + (tl + 1) * 128], ps[pb][:].rearrange("p (c t) -> p c t", c=4), AF.Copy),
                          reads=[ps_r[pb]], writes=[xTs_r[xb]])
                else:
                    kb.op("dve", lambda e, pb=pb, xb=xb, tl=tl: e.tensor_copy(xTs[xb][:, 4:8, 1 + tl * 128:1 + (tl + 1) * 128], ps[pb][:].rearrange("p (c t) -> p c t", c=4)),
                          reads=[ps_r[pb]], writes=[xTs_r[xb]])
        kb.op("dve", lambda e, xb=xb: e.tensor_tensor(xx[:], xTs[xb][:, :, 0:256], xTs[xb][:, :, 1:257], op=ALU.subtract), reads=[xTs_r[xb]], writes=[xx_r])
        mix(3, 0, xb)
        for kc in range(8):
            kb.op("pe", lambda e, kc=kc: e.matmul(ps[5][0:64, 0:256], w1[:, kc, :], xm[0][:, kc, :], start=(kc == 0), stop=(kc == 7)), reads=[wr_, xm_r[0]], writes=[ps_r[5]])
        kb.op("act", lambda e: e.activation(hw[:], ps[5][0:64, 0:256], AF.Tanh), reads=[ps_r[5]], writes=[hid_r])
        mix(4, 1, xb)
        for kc in range(8):
            kb.op("pe", lambda e, kc=kc: e.matmul(ps[5][0:64, 256:512], a1[:, kc, :], xm[1][:, kc, :], start=(kc == 0), stop=(kc == 7)), reads=[wr_, xm_r[1]], writes=[ps_r[5]])
        kb.op("act", lambda e: e.activation(ha[:], ps[5][0:64, 256:512], AF.Copy), reads=[ps_r[5]], writes=[hid_r])
        mix(5, 2, xb)
        for kc in range(8):
            kb.op("pe", lambda e, kc=kc: e.matmul(ps[4][:, 0:256], g1[:, kc, 0:128], xm[2][:, kc, :], start=(kc == 0), stop=(kc == 7)), reads=[wr_, xm_r[2]], writes=[ps_r[4]])
        for kc in range(8):
            kb.op("pe", lambda e, kc=kc: e.matmul(ps[4][0:32, 256:512], g1[:, kc, 128:160], xm[2][:, kc, :], start=(kc == 0), stop=(kc == 7)), reads=[wr_, xm_r[2]], writes=[ps_r[4]])
        kb.op("act", lambda e: e.activation(hg1[:], ps[4][:, 0:256], AF.Sigmoid), reads=[ps_r[4]], writes=[hid_r])
        kb.op("act", lambda e: e.activation(hg2[:], ps[4][0:32, 256:512], AF.Sigmoid), reads=[ps_r[4]], writes=[hid_r])
        for tl in range(2):
            i = s * 2 + tl
            b = i % 2
            for hf in range(2):
                pb = 6 + hf
                kb.op("pe", lambda e, pb=pb, tl=tl, hf=hf: e.matmul(ps[pb][:], hg1[:, tl * 128:(tl + 1) * 128], g2a[:, hf * 512:(hf + 1) * 512], start=True, stop=False), reads=[hid_r, wr_], writes=[ps_r[pb]])
                kb.op("pe", lambda e, pb=pb, tl=tl, hf=hf: e.matmul(ps[pb][:], hg2[:, tl * 128:(tl + 1) * 128], g2b[:, hf * 512:(hf + 1) * 512], start=False, stop=True), reads=[hid_r, wr_], writes=[ps_r[pb]])
                if hf == 0:
                    kb.op("act", lambda e, pb=pb, b=b: e.activation(gsb[b][:, 0:512], ps[pb][:], AF.Copy), reads=[ps_r[pb]], writes=[gsb_r[b]])
                else:
                    kb.op("dve", lambda e, pb=pb, b=b: e.tensor_copy(gsb[b][:, 512:1024], ps[pb][:]), reads=[ps_r[pb]], writes=[gsb_r[b]])
            kb.dma("sp", lambda e, i=i, b=b: e.dma_start(out=Gd[i * 128:(i + 1) * 128, :], in_=gsb[b][:]), reads=[gsb_r[b]])
        mix(2, 0, xb)
        if j > 0:
            for kc in range(8):
                kb.op("pe", lambda e, kc=kc: e.matmul(ps[5][0:32, 0:256], v1[:, kc, :], xm[0][:, kc, :], start=(kc == 0), stop=(kc == 7)), reads=[wr_, xm_r[0]], writes=[ps_r[5]])
            kb.op("act", lambda e: e.activation(hv[:], ps[5][0:32, 0:256], AF.Copy), reads=[ps_r[5]], writes=[hid_r])
        for tl in range(2):
            i = s * 2 + tl
            b = i % 2
            for hf in range(2):
                pb = 6 + hf
                for kc in range(8):
                    kb.op("pe", lambda e, pb=pb, tl=tl, hf=hf, kc=kc: e.matmul(ps[pb][:], xm[0][:, kc, tl * 128:(tl + 1) * 128], wrkv[:, 2, kc, hf * 512:(hf + 1) * 512], start=(kc == 0), stop=(kc == 7)),
                          reads=[xm_r[0], wr_], writes=[ps_r[pb]])
                if hf == 0:
                    kb.op("act", lambda e, pb=pb, b=b: e.activation(vsb[b][:, 0:512], ps[pb][:], AF.Copy), reads=[ps_r[pb]], writes=[vsb_r[b]])
                else:
                    kb.op("dve", lambda e, pb=pb, b=b: e.tensor_copy(vsb[b][:, 512:1024], ps[pb][:]), reads=[ps_r[pb]], writes=[vsb_r[b]])
            if j == 0:
                kb.dma("sp", lambda e, i=i, b=b: e.dma_start(out=vfd[i * 128:(i + 1) * 128, :], in_=vsb[b][:]), reads=[vsb_r[b]])
                kb.op("pool", lambda e, b=b: e.tensor_copy(vb16[b][:], vsb[b][:]), reads=[vsb_r[b]], writes=[vb_r[b]])
            else:
                kb.dma("sp", lambda e, i=i, b=b: e.dma_start(out=vfs[b][:], in_=vfd[i * 128:(i + 1) * 128, :]), writes=[vfs_r[b]])
                for hf in range(2):
                    pb = 6 + hf
                    kb.op("pe", lambda e, pb=pb, tl=tl, hf=hf: e.matmul(ps[pb][:], hv[:, tl * 128:(tl + 1) * 128], v2[:, hf * 512:(hf + 1) * 512], start=True, stop=True), reads=[hid_r, wr_], writes=[ps_r[pb]])
                    kb.op("dve", lambda e, pb=pb, b=b, hf=hf: e.tensor_tensor(vmx[b][:, hf * 512:(hf + 1) * 512], ps[pb][:], v0b[:, hf * 512:(hf + 1) * 512], op=ALU.add), reads=[ps_r[pb], wr_], writes=[vmx_r[b]])
                kb.op("act", lambda e, b=b: e.activation(vmx[b][:], vmx[b][:], AF.Sigmoid), reads=[vmx_r[b]], writes=[vmx_r[b]])
                kb.op("pool", lambda e, b=b: e.tensor_tensor(vfs[b][:], vfs[b][:], vsb[b][:], op=ALU.subtract), reads=[vfs_r[b], vsb_r[b]], writes=[vfs_r[b]])
                kb.op("pool", lambda e, b=b: e.tensor_tensor(vfs[b][:], vfs[b][:], vmx[b][:], op=ALU.mult), reads=[vfs_r[b], vmx_r[b]], writes=[vfs_r[b]])
                kb.op("pool", lambda e, b=b: e.tensor_tensor(vb16[b][:], vfs[b][:], vsb[b][:], op=ALU.add), reads=[vfs_r[b], vsb_r[b]], writes=[vb_r[b]])
            kb.dma("sp", lambda e, i=i, b=b: e.dma_start(out=Vd[i * 128:(i + 1) * 128, :], in_=vb16[b][:]), reads=[vb_r[b]])
        mix(0, 1, xb)
        mix(1, 2, xb)
        for oc in range(8):
            osl = slice(oc * 128, (oc + 1) * 128)
            pr, pk = (oc % 2) * 2, (oc % 2) * 2 + 1
            for kc in range(8):
                kb.op("pe", lambda e, pr=pr, kc=kc, osl=osl: e.matmul(ps[pr][:, 0:256], wrkv[:, 0, kc, osl], xm[1][:, kc, :], start=(kc == 0), stop=(kc == 7)), reads=[wr_, xm_r[1]], writes=[ps_r[pr]])
            for kc in range(8):
                kb.op("pe", lambda e, pk=pk, kc=kc, osl=osl: e.matmul(ps[pk][:, 0:256], wrkv[:, 1, kc, osl], xm[2][:, kc, :], start=(kc == 0), stop=(kc == 7)), reads=[wr_, xm_r[2]], writes=[ps_r[pk]])
            kb.op("pe", lambda e, pr=pr, osl=osl: e.matmul(ps[pr][:, 256:512], w2[:, osl], hw[:], start=True, stop=True), reads=[wr_, hid_r], writes=[ps_r[pr]])
            kb.op("pe", lambda e, pk=pk, osl=osl: e.matmul(ps[pk][:, 256:512], a2[:, osl], ha[:], start=True, stop=True), reads=[wr_, hid_r], writes=[ps_r[pk]])
            r_ps, k_ps, w_ps, a_ps = ps[pr][:, 0:256], ps[pk][:, 0:256], ps[pr][:, 256:512], ps[pk][:, 256:512]
            R2 = [ps_r[pr], ps_r[pk], tm_r, pv_r]

            def o(eng, fn, extra_w=()):
                kb.op(eng, fn, reads=R2, writes=[tm_r] + list(extra_w))

            o("act", lambda e, oc=oc: e.activation(tm["sgw"][:], w_ps, AF.Sigmoid, bias=pv[:, 0, oc:oc + 1]))
            o("act", lambda e, oc=oc: e.activation(tm["asig"][:], a_ps, AF.Sigmoid, bias=pv[:, 1, oc:oc + 1]))
            o("act", lambda e, oc=oc: e.activation(tm["kk"][:], k_ps, AF.Identity, scale=pv[:, 2, oc:oc + 1]))
            o("dve", lambda e: e.tensor_tensor_scan(tm["cum"][:], rst[:], tm["sgw"][:], 0.0, op0=ALU.mult, op1=ALU.add))
            o("pool", lambda e: e.tensor_tensor(kk2[:], tm["kk"][:], tm["kk"][:], op=ALU.mult))
            kb.op("pe", lambda e: e.matmul(ps[5][:, 0:256], bd64[:], kk2[:], start=True, stop=True), reads=[tm_r, pv_r], writes=[ps_r[5]])
            o("pool", lambda e: e.tensor_tensor(tm["cumx"][:], tm["cum"][:], tm["sgw"][:], op=ALU.subtract))
            o("act", lambda e: e.activation(tm["pin"][:], tm["cum"][:], AF.Exp, scale=-C0))
            o("act", lambda e: e.activation(tm["pinv"][:], tm["cum"][:], AF.Exp, scale=C0))
            o("act", lambda e: e.activation(tm["pprev"][:], tm["cumx"][:], AF.Exp, scale=-C0))
            kb.op("dve", lambda e: e.tensor_scalar(tm["lns"][:], ps[5][:, 0:256], 1e-30, None, op0=ALU.add), reads=[ps_r[5]], writes=[tm_r])
            o("act", lambda e: e.activation(tm["lns"][:], tm["lns"][:], AF.Ln))
            o("act", lambda e: e.activation(tm["rn"][:], tm["lns"][:], AF.Exp, scale=-0.5))
            o("pool", lambda e: e.tensor_tensor(tm["kkn"][:], tm["kk"][:], tm["rn"][:], op=ALU.mult))
            o("dve", lambda e, oc=oc: e.tensor_scalar(tm["t1"][:], tm["asig"][:], pv[:, 3, oc:oc + 1], pv[:, 5, oc:oc + 1], op0=ALU.mult, op1=ALU.add))
            o("dve", lambda e: e.tensor_tensor(tm["k2"][:], k_ps, tm["t1"][:], op=ALU.mult))
            for tl_ in range(2):
                o("dve", lambda e, oc=oc, tl_=tl_: e.tensor_copy(Pc[:, tl_, oc:oc + 1], tm["pin"][:, 127 + 128 * tl_:128 + 128 * tl_]), extra_w=[out_r])
            o("dve", lambda e, oc=oc: e.scalar_tensor_tensor(AR[:, oc, :, 0, :], tm["kkn"][:].rearrange("p (a t) -> p a t", a=2), -1.0, tm["pprev"][:].rearrange("p (a t) -> p a t", a=2), op0=ALU.mult, op1=ALU.mult), extra_w=[out_r])
            o("dve", lambda e, oc=oc: e.tensor_tensor(AR[:, oc, :, 1, :], r_ps.rearrange("p (a t) -> p a t", a=2), tm["pin"][:].rearrange("p (a t) -> p a t", a=2), op=ALU.mult), extra_w=[out_r])
            o("pool", lambda e: e.tensor_tensor(tm["tb"][:], tm["kkn"][:], tm["asig"][:], op=ALU.mult))
            o("pool", lambda e, oc=oc: e.tensor_tensor(BK[:, oc, :, 0, :], tm["tb"][:].rearrange("p (a t) -> p a t", a=2), tm["pinv"][:].rearrange("p (a t) -> p a t", a=2), op=ALU.mult), extra_w=[out_r])
            o("pool", lambda e, oc=oc: e.tensor_tensor(BK[:, oc, :, 1, :], tm["k2"][:].rearrange("p (a t) -> p a t", a=2), tm["pinv"][:].rearrange("p (a t) -> p a t", a=2), op=ALU.mult), extra_w=[out_r])
            o("dve", lambda e, oc=oc: e.scalar_tensor_tensor(rk[:, oc, :], r_ps, pv[:, 4, oc:oc + 1], tm["k2"][:], op0=ALU.mult, op1=ALU.mult), extra_w=[out_r])
        for tl in range(2):
            i = s * 2 + tl
            kb.dma("sp", lambda e, i=i, tl=tl: e.dma_start(out=ARd[i].rearrange("p (o c t) -> p o c t", o=8, c=2), in_=AR[:, :, tl, :, :]), reads=[out_r])
            kb.dma("sp", lambda e, i=i, tl=tl: e.dma_start(out=BKd[i].rearrange("p (o c t) -> p o c t", o=8, c=2), in_=BK[:, :, tl, :, :]), reads=[out_r])
            kb.dma("sp", lambda e, i=i, tl=tl: e.dma_start(out=rkd[i].rearrange("p (o t) -> p o t", o=8), in_=rk[:, :, tl * 128:(tl + 1) * 128]), reads=[out_r])
            kb.dma("sp", lambda e, i=i, tl=tl: e.dma_start(out=Pcd[i], in_=Pc[:, tl, :]), reads=[out_r])
    kb_pop(kb)
    rwkv_pass2(kb, C, T, prm, j, li, xin, xin_r, xa, xa_r, ST, nc)


def rwkv_pass2(kb, C, T, prm, j, li, xin, xin_r, xa, xa_r, ST, nc):
    NT = T // 128
    ARd, BKd, rkd, Pcd, Vd, Gd = (ST[k] for k in ("ARd", "BKd", "rkd", "Pcd", "Vd", "Gd"))
    kb_push(kb)
    pF = [kb.ps("r2_pf%d" % b, [128, 512], F32) for b in range(4)]
    pF_r = RL(4)
    pT = kb.ps("r2_pT", [128, 1024], BF16)
    pT_r = Res()
    pY = [kb.ps("r2_pY%d" % b, [128, 512], F32) for b in range(2)]
    pY_r = RL(2)
    pH = kb.ps("r2_pH", [128, 512], F32)
    pH_r = Res()
    wo = kb.sb("r2_wo", [128, 8, 1024], BF16)
    wo_r = Res()
    wsrc = prm["rw_w_o"][j].rearrange("(kc p) n -> p kc n", p=128)
    for kc in range(8):
        kb.dma("pool", lambda e, kc=kc: e.dma_start(out=wo[:, kc, :], in_=wsrc[:, kc, :]), writes=[wo_r])
    cst_r = Res()
    m2 = kb.sb("r2_m2", [128, 2, 128], F32)
    kb.op("dve", lambda e: e.tensor_copy(m2[:, 0, :], C.m_st[:]), reads=[C.r], writes=[cst_r])
    kb.op("dve", lambda e: e.tensor_copy(m2[:, 1, :], C.m_in[:]), reads=[C.r], writes=[cst_r])
    bdm = kb.sb("r2_bdm", [128, 128], F32)
    kb.op("dve", lambda e: e.memset(bdm[:], 0.0), writes=[cst_r])
    kb.op("dve", lambda e: e.memset(bdm[0:64, 0:64], 1.0), writes=[cst_r])
    kb.op("dve", lambda e: e.memset(bdm[64:128, 64:128], 1.0), writes=[cst_r])
    HS = kb.sb("r2_HS", [128, 8, 16], BF16)
    kb.op("dve", lambda e: e.memset(HS[:], 0.0), writes=[cst_r])
    for oc in range(8):
        for hh in range(2):
            kb.op("dve", lambda e, oc=oc, hh=hh: e.memset(HS[hh * 64:(hh + 1) * 64, oc, 2 * oc + hh:2 * oc + hh + 1], 1.0), writes=[cst_r])
    lng = kb.sb("r2_lng", [128, 1024], F32)
    lnb = kb.sb("r2_lnb", [128, 1024], F32)
    kb.dma("sp", lambda e: e.dma_start(out=lng[:], in_=bcast_rows(prm["rw_lnx_g"][j:j + 1, :])), writes=[cst_r])
    kb.dma("sp", lambda e: e.dma_start(out=lnb[:], in_=bcast_rows(prm["rw_lnx_b"][j:j + 1, :])), writes=[cst_r])
    gneps = kb.sb("r2_gneps", [128, 1], F32)
    kb.op("dve", lambda e: e.memset(gneps[:], GN_EPS), writes=[cst_r])
    LS = ln_scratch(kb, "r2_ln", LN_EPS)
    g_bc, b_bc, lnp_r = load_ln_params(kb, "r2_lnp", prm["ln1_g"], prm["ln1_b"], li)

    ARt = [kb.sb("r2_AR%d" % b, [128, 8, 2, 128], BF16) for b in range(2)]
    BKt = [kb.sb("r2_BK%d" % b, [128, 8, 2, 128], BF16) for b in range(2)]
    rkt = [kb.sb("r2_rk%d" % b, [128, 8, 128], BF16) for b in range(2)]
    Pct = [kb.sb("r2_Pc%d" % b, [128, 8], F32) for b in range(2)]
    Vt = [kb.sb("r2_V%d" % b, [128, 1024], BF16) for b in range(2)]
    Gt = [kb.sb("r2_G%d" % b, [128, 1024], BF16) for b in range(2)]
    xt = [kb.sb("r2_xt%d" % b, [128, 1024], F32) for b in range(2)]
    in_r = RL(2)
    Hb = kb.sb("r2_H", [128, 8, 64], BF16)
    Hb_r = RL(8)
    kb.op("dve", lambda e: e.memset(Hb[:], 0.0), writes=Hb_r)
    TK = [kb.sb("r2_TK%d" % b, [128, 3, 128], BF16) for b in range(2)]
    TK_r = RL(2)
    XA = [kb.sb("r2_XA%d" % h, [128, 2, 128], BF16) for h in range(2)]
    KA = [kb.sb("r2_KA%d" % h, [128, 2, 128], BF16) for h in range(2)]
    XN = [[kb.sb("r2_XN%d_%d" % (h, q), [128, 2, 128], BF16) for q in range(2)] for h in range(2)]
    PQ = [[kb.sb("r2_PQ%d_%d" % (h, q), [128, 2, 128], BF16) for q in range(2)] for h in range(2)]
    AV = [kb.sb("r2_AV%d" % h, [128, 64], BF16) for h in range(2)]
    hd_r = [Res(), Res()]
    W12 = [kb.sb("r2_W12%d" % b, [128, 2, 2, 64], BF16) for b in range(2)]
    W12_r = RL(2)
    MT = [kb.sb("r2_MT%d" % b, [128, 128], BF16) for b in range(2)]
    MTf = kb.sb("r2_MTf", [128, 128], F32)
    GS = [kb.sb("r2_GS%d" % b, [128, 64], F32) for b in range(2)]
    QT = [kb.sb("r2_QT%d" % b, [128, 128], BF16) for b in range(2)]
    pr_r = RL(2)
    mtf_r = Res()
    ysb = kb.sb("r2_y", [128, 1024], F32)
    ysq = kb.sb("r2_ysq", [128, 1024], F32)
    yn = kb.sb("r2_yn", [128, 1024], F32)
    yg = kb.sb("r2_yg", [128, 1024], BF16)
    ygT = kb.sb("r2_ygT", [128, 8, 128], BF16)
    st = kb.sb("r2_st", [128, 8, 16], F32)
    y_r = Res()
    zt = [kb.sb("r2_z%d" % b, [128, 1024], F32) for b in range(2)]
    zt_r = RL(2)
    x1 = [kb.sb("r2_x1%d" % b, [128, 1024], F32) for b in range(2)]
    x1_r = RL(2)

    for i in range(NT):
        b = i % 2
        kb.dma("sp", lambda e, i=i, b=b: e.dma_start(out=ARt[b][:], in_=ARd[i].rearrange("p (o c t) -> p o c t", o=8, c=2)), writes=[in_r[b]])
        kb.dma("sp", lambda e, i=i, b=b: e.dma_start(out=BKt[b][:], in_=BKd[i].rearrange("p (o c t) -> p o c t", o=8, c=2)), writes=[in_r[b]])
        kb.dma("sp", lambda e, i=i, b=b: e.dma_start(out=rkt[b][:], in_=rkd[i].rearrange("p (o t) -> p o t", o=8)), writes=[in_r[b]])
        kb.dma("sp", lambda e, i=i, b=b: e.dma_start(out=Pct[b][:], in_=Pcd[i]), writes=[in_r[b]])
        kb.dma("sp", lambda e, i=i, b=b: e.dma_start(out=Vt[b][:], in_=Vd[i * 128:(i + 1) * 128, :]), writes=[in_r[b]])
        kb.dma("sp", lambda e, i=i, b=b: e.dma_start(out=Gt[b][:], in_=Gd[i * 128:(i + 1) * 128, :]), writes=[in_r[b]])
        kb.dma("sp", lambda e, i=i, b=b: e.dma_start(out=xt[b][:], in_=xin[i * 128:(i + 1) * 128, :]), reads=[xin_r[i]], writes=[in_r[b]])
        for hp in range(8):
            q = hp % 2
            for n, src in enumerate((ARt[b][:, hp, 0, :], BKt[b][:, hp, 0, :], BKt[b][:, hp, 1, :])):
                kb.op("pe", lambda e, n=n, src=src: e.transpose(pT[:, n * 128:(n + 1) * 128], src, C.ident_b[:]), reads=[in_r[b], C.r], writes=[pT_r])
            kb.op("act", lambda e, q=q: e.activation(TK[q][:], pT[:, 0:384].rearrange("p (c t) -> p c t", c=3), AF.Copy), reads=[pT_r], writes=[TK_r[q]])
            for hh in range(2):
                pb = 64 * hh
                psl = slice(pb, pb + 64)
                pf = pF[hh]
                rhs_ar = ARt[b][psl, hp, :, :].rearrange("p c t -> p (c t)")
                kb.op("pe", lambda e, pf=pf, psl=psl, rhs_ar=rhs_ar: e.matmul(pf[:, 0:256], BKt[b][psl, hp, 0, :], rhs_ar, start=True, stop=True), reads=[in_r[b]], writes=[pF_r[hh]])
                kb.op("pe", lambda e, pf=pf, psl=psl, rhs_ar=rhs_ar: e.matmul(pf[:, 256:512], BKt[b][psl, hp, 1, :], rhs_ar, start=True, stop=True), reads=[in_r[b]], writes=[pF_r[hh]])
                kb.op("dve", lambda e, pf=pf, hh=hh: e.tensor_tensor(XA[hh][:], pf[:, 0:256].rearrange("p (c t) -> p c t", c=2), m2[:], op=ALU.mult), reads=[pF_r[hh], cst_r], writes=[hd_r[hh]])
                kb.op("dve", lambda e, pf=pf, hh=hh: e.tensor_tensor(KA[hh][:], pf[:, 256:512].rearrange("p (c t) -> p c t", c=2), m2[:], op=ALU.mult), reads=[pF_r[hh], cst_r], writes=[hd_r[hh]])
                kb.op("pe", lambda e, pf=pf, psl=psl: e.matmul(pf[:, 0:128], ARt[b][psl, hp, 0, :], BKt[b][psl, hp, 0, :], start=True, stop=True), reads=[in_r[b]], writes=[pF_r[hh]])
                kb.op("pool", lambda e, hh=hh: e.tensor_copy(XN[hh][0][:, 0, :], XA[hh][:, 0, :]), reads=[hd_r[hh]], writes=[hd_r[hh]])
                kb.op("dve", lambda e, pf=pf, hh=hh: e.tensor_tensor(XN[hh][0][:, 1, :], pf[:, 0:128], C.m_lo[:], op=ALU.mult), reads=[pF_r[hh], C.r], writes=[hd_r[hh]])
                for c in range(2):
                    kb.op("pool", lambda e, hh=hh, c=c: e.tensor_tensor(PQ[hh][0][:, c, :], XN[hh][0][:, c, :], C.ident_b[:], op=ALU.add), reads=[hd_r[hh], C.r], writes=[hd_r[hh]])
            for k in range(1, 7):
                cur, prv = k % 2, (k - 1) % 2
                for hh in range(2):
                    pf = pF[2 + hh]
                    Xp, Np = XN[hh][prv][:, 0, :], XN[hh][prv][:, 1, :]
                    kb.op("pe", lambda e, pf=pf, Xp=Xp, Np=Np: e.matmul(pf[:, 0:128], Np, Xp, start=True, stop=True), reads=[hd_r[hh]], writes=[pF_r[2 + hh]])
                    if k < 6:
                        kb.op("pe", lambda e, pf=pf, Xp=Xp, Np=Np: e.matmul(pf[:, 128:256], Xp, Np, start=True, stop=True), reads=[hd_r[hh]], writes=[pF_r[2 + hh]])
                        kb.op("act", lambda e, pf=pf, hh=hh, cur=cur: e.activation(XN[hh][cur][:], pf[:, 0:256].rearrange("p (c t) -> p c t", c=2), AF.Copy), reads=[pF_r[2 + hh]], writes=[hd_r[hh]])
                    else:
                        kb.op("act", lambda e, pf=pf, hh=hh, cur=cur: e.activation(XN[hh][cur][:, 0, :], pf[:, 0:128], AF.Copy), reads=[pF_r[2 + hh]], writes=[hd_r[hh]])
                for hh in range(2):
                    pf = pF[2 + hh]
                    Xc = XN[hh][cur][:, 0, :]
                    Pp, Qp = PQ[hh][prv][:, 0, :], PQ[hh][prv][:, 1, :]
                    kb.op("pe", lambda e, pf=pf, Xc=Xc, Qp=Qp: e.matmul(pf[:, 256:384], Qp, Xc, start=True, stop=True), reads=[hd_r[hh]], writes=[pF_r[2 + hh]])
                    if k < 6:
                        kb.op("pe", lambda e, pf=pf, Xc=Xc, Qp=Qp: e.matmul(pf[:, 384:512], Xc, Qp, start=True, stop=True), reads=[hd_r[hh]], writes=[pF_r[2 + hh]])
                        kb.op("dve", lambda e, pf=pf, hh=hh, cur=cur, prv=prv: e.tensor_tensor(PQ[hh][cur][:], pf[:, 256:512].rearrange("p (c t) -> p c t", c=2), PQ[hh][prv][:], op=ALU.add), reads=[pF_r[2 + hh], hd_r[hh]], writes=[hd_r[hh]])
                    else:
                        kb.op("dve", lambda e, pf=pf, hh=hh, cur=cur, prv=prv: e.tensor_tensor(PQ[hh][cur][:, 0, :], pf[:, 256:384], PQ[hh][prv][:, 0, :], op=ALU.add), reads=[pF_r[2 + hh], hd_r[hh]], writes=[hd_r[hh]])
            Pfin = 6 % 2
            for hh in range(2):
                pf = pF[hh]
                hc = slice(hp * 128 + hh * 64, hp * 128 + hh * 64 + 64)
                kb.op("pe", lambda e, pf=pf, hh=hh, hc=hc: e.matmul(pf[:, 128:192], KA[hh][:, 0, :], Vt[b][:, hc], start=True, stop=True), reads=[hd_r[hh], in_r[b]], writes=[pF_r[hh]])
                kb.op("act", lambda e, pf=pf, hh=hh: e.activation(AV[hh][:], pf[:, 128:192], AF.Copy), reads=[pF_r[hh]], writes=[hd_r[hh]])
                kb.op("pe", lambda e, pf=pf, hh=hh: e.matmul(pf[:, 192:256], PQ[hh][Pfin][:, 0, :], AV[hh][:], start=True, stop=True), reads=[hd_r[hh]], writes=[pF_r[hh]])
                kb.op("pe", lambda e, pf=pf, hh=hh, q=q: e.matmul(pf[:, 256:320], PQ[hh][Pfin][:, 0, :], TK[q][:, 0, hh * 64:(hh + 1) * 64], start=True, stop=True), reads=[hd_r[hh], TK_r[q]], writes=[pF_r[hh]])
                kb.op("act", lambda e, pf=pf, hh=hh, q=q: e.activation(W12[q][:, :, hh, :], pf[:, 192:320].rearrange("p (c t) -> p c t", c=2), AF.Copy), reads=[pF_r[hh]], writes=[W12_r[q]])
            W1p = W12[q][:, 0, :, :].rearrange("p h t -> p (h t)")
            W2p = W12[q][:, 1, :, :].rearrange("p h t -> p (h t)")
            pg = pF[2]
            kb.op("pe", lambda e, W2p=W2p, q=q: e.matmul(pg[:, 0:128], W2p, TK[q][:, 1, :], start=True, stop=True), reads=[W12_r[q], TK_r[q]], writes=[pF_r[2]])
            kb.op("dve", lambda e: e.tensor_tensor(MTf[:], pg[:, 0:128], bdm[:], op=ALU.mult), reads=[pF_r[2], cst_r], writes=[mtf_r])
            kb.op("pool", lambda e, q=q: e.tensor_tensor(MT[q][:], MTf[:], C.ident_f[:], op=ALU.add), reads=[mtf_r, C.r], writes=[pr_r[q]])
            kb.op("pe", lambda e, W1p=W1p, q=q: e.matmul(pg[:, 128:256], TK[q][:, 1, :], W1p, start=True, stop=False), reads=[W12_r[q], TK_r[q]], writes=[pF_r[2]])
            kb.op("pe", lambda e, q=q: e.matmul(pg[:, 128:256], TK[q][:, 2, :], Vt[b][:, hp * 128:(hp + 1) * 128], start=False, stop=True), reads=[TK_r[q], in_r[b]], writes=[pF_r[2]])
            for hh in range(2):
                psl = slice(64 * hh, 64 * hh + 64)
                kb.op("dve", lambda e, psl=psl, hh=hh, q=q: e.tensor_scalar(GS[q][psl, :], pg[psl, 128 + 64 * hh:192 + 64 * hh], Pct[b][psl, hp:hp + 1], None, op0=ALU.mult), reads=[pF_r[2], in_r[b]], writes=[pr_r[q]])
                kb.op("pe", lambda e, hh=hh, W2p=W2p: e.matmul(pg[:, 256 + 128 * hh:384 + 128 * hh], W2p, XA[hh][:, 1, :], start=True, stop=True), reads=[W12_r[q], hd_r[hh]], writes=[pF_r[2]])
                kb.op("dve", lambda e, psl=psl, hh=hh, q=q: e.tensor_tensor(QT[q][psl, :], pg[psl, 256 + 128 * hh:384 + 128 * hh], ARt[b][psl, hp, 1, :], op=ALU.add), reads=[pF_r[2], in_r[b]], writes=[pr_r[q]])
            yb_, yc = pY[hp // 4], (hp % 4) * 128
            for hh in range(2):
                psl = slice(64 * hh, 64 * hh + 64)
                hc = slice(hp * 128 + hh * 64, hp * 128 + hh * 64 + 64)
                yo = yb_[:, yc + 64 * hh:yc + 64 * hh + 64]
                kb.op("pe", lambda e, yo=yo, hh=hh, q=q: e.matmul(yo, XA[hh][:, 1, :], W12[q][:, 0, hh, :], start=True, stop=False), reads=[hd_r[hh], W12_r[q]], writes=[pY_r[hp // 4]])
                kb.op("pe", lambda e, yo=yo, hh=hh, hc=hc: e.matmul(yo, KA[hh][:, 1, :], Vt[b][:, hc], start=False, stop=False), reads=[hd_r[hh], in_r[b]], writes=[pY_r[hp // 4]])
                kb.op("pe", lambda e, yo=yo, psl=psl, q=q: e.matmul(yo, QT[q][psl, :], Hb[psl, hp, :], start=False, stop=True), reads=[pr_r[q], Hb_r[hp]], writes=[pY_r[hp // 4]])
            kb.op("pe", lambda e, q=q: e.matmul(pF[3][:, 0:64], MT[q][:], Hb[:, hp, :], start=True, stop=True), reads=[pr_r[q], Hb_r[hp]], writes=[pF_r[3]])
            kb.op("dve", lambda e, q=q: e.scalar_tensor_tensor(Hb[:, hp, :], pF[3][:, 0:64], Pct[b][:, hp:hp + 1], GS[q][:], op0=ALU.mult, op1=ALU.add), reads=[pF_r[3], in_r[b], pr_r[q]], writes=[Hb_r[hp]])
        kb.op("act", lambda e: e.activation(ysb[:, 0:512], pY[0][:], AF.Copy), reads=[pY_r[0]], writes=[y_r])
        kb.op("dve", lambda e: e.tensor_copy(ysb[:, 512:1024], pY[1][:]), reads=[pY_r[1]], writes=[y_r])
        kb.op("pool", lambda e: e.tensor_tensor(ysq[:], ysb[:], ysb[:], op=ALU.mult), reads=[y_r], writes=[y_r])
        kb.op("dve", lambda e: e.reduce_sum(st[:, 0, :], ysb[:].rearrange("p (h n) -> p h n", h=16), axis=AX.X), reads=[y_r], writes=[y_r])
        kb.op("dve", lambda e: e.reduce_sum(st[:, 1, :], ysq[:].rearrange("p (h n) -> p h n", h=16), axis=AX.X), reads=[y_r], writes=[y_r])
        kb.op("dve", lambda e: e.tensor_scalar(st[:, 2, :], st[:, 0, :], 1.0 / 64, None, op0=ALU.mult), reads=[y_r], writes=[y_r])
        kb.op("dve", lambda e: e.tensor_tensor(st[:, 3, :], st[:, 2, :], st[:, 2, :], op=ALU.mult), reads=[y_r], writes=[y_r])
        kb.op("dve", lambda e: e.scalar_tensor_tensor(st[:, 4, :], st[:, 1, :], 1.0 / 64, st[:, 3, :], op0=ALU.mult, op1=ALU.subtract), reads=[y_r], writes=[y_r])
        kb.op("act", lambda e: e.activation(st[:, 5, :], st[:, 4, :], AF.Sqrt, bias=gneps[:, 0:1]), reads=[y_r, cst_r], writes=[y_r])
        kb.op("dve", lambda e: e.reciprocal(st[:, 6, :], st[:, 5, :]), reads=[y_r], writes=[y_r])
        for hd in range(16):
            eng = "dve" if hd % 2 == 0 else "pool"
            kb.op(eng, lambda e, hd=hd: e.tensor_scalar(yn[:, hd * 64:(hd + 1) * 64], ysb[:, hd * 64:(hd + 1) * 64], st[:, 2, hd:hd + 1], st[:, 6, hd:hd + 1], op0=ALU.subtract, op1=ALU.mult), reads=[y_r], writes=[y_r])
        kb.op("pool", lambda e: e.tensor_tensor(yn[:], yn[:], lng[:], op=ALU.mult), reads=[y_r, cst_r], writes=[y_r])
        kb.op("pool", lambda e: e.tensor_tensor(yn[:], yn[:], lnb[:], op=ALU.add), reads=[y_r, cst_r], writes=[y_r])
        for oc in range(8):
            kb.op("pe", lambda e, oc=oc: e.matmul(pH[:, 0:16], rkt[b][:, oc, :], HS[:, oc, :], start=(oc == 0), stop=(oc == 7)), reads=[in_r[b], cst_r], writes=[pH_r])
        kb.op("act", lambda e: e.activation(st[:, 7, :], pH[:, 0:16], AF.Copy), reads=[pH_r], writes=[y_r])
        for hd in range(16):
            kb.op("dve", lambda e, hd=hd: e.scalar_tensor_tensor(yn[:, hd * 64:(hd + 1) * 64], Vt[b][:, hd * 64:(hd + 1) * 64], st[:, 7, hd:hd + 1], yn[:, hd * 64:(hd + 1) * 64], op0=ALU.mult, op1=ALU.add), reads=[y_r, in_r[b]], writes=[y_r])
        kb.op("pool", lambda e: e.tensor_tensor(yg[:], yn[:], Gt[b][:], op=ALU.mult), reads=[y_r, in_r[b]], writes=[y_r])
        for hf in range(2):
            for c4 in range(4):
                kc = hf * 4 + c4
                kb.op("pe", lambda e, c4=c4, kc=kc: e.transpose(pT[:, 512 + c4 * 128:512 + (c4 + 1) * 128], yg[:, kc * 128:(kc + 1) * 128], C.ident_b[:]), reads=[y_r, C.r], writes=[pT_r])
            kb.op("act", lambda e, hf=hf: e.activation(ygT[:, hf * 4:hf * 4 + 4, :], pT[:, 512:1024].rearrange("p (c t) -> p c t", c=4), AF.Copy), reads=[pT_r], writes=[y_r])
        for hf in range(2):
            for kc in range(8):
                kb.op("pe", lambda e, kc=kc, hf=hf: e.matmul(pH[:], ygT[:, kc, :], wo[:, kc, hf * 512:(hf + 1) * 512], start=(kc == 0), stop=(kc == 7)), reads=[y_r, wo_r], writes=[pH_r])
            kb.op("dve", lambda e, hf=hf, b=b: e.scalar_tensor_tensor(zt[b][:, hf * 512:(hf + 1) * 512], xt[b][:, hf * 512:(hf + 1) * 512], ALPHA, pH[:], op0=ALU.mult, op1=ALU.add),
                  reads=[in_r[b], pH_r], writes=[zt_r[b]])
        ln_tile(kb, LS, zt[b], zt_r[b], x1[b][:], x1_r[b], g_bc, b_bc, lnp_r)
        kb.dma("sp", lambda e, i=i, b=b: e.dma_start(out=xa[i * 128:(i + 1) * 128, :], in_=x1[b][:]), reads=[x1_r[b]], writes=[xa_r[i]])
    kb_pop(kb)


T_FULL = 4096
N_CORES = 8
_CACHE = {}


def kernel(**inputs):
    if "prog" not in _CACHE:
        _CACHE["prog"] = build(T_FULL, FULL_PLAN, cap=512)
    nc, names, _ = _CACHE["prog"]
    x = np.asarray(inputs["x"], dtype=np.float32)
    in_maps = []
    for c in range(N_CORES):
        m = {"x": np.ascontiguousarray(x[c])}
        for n in names:
            m[n] = np.ascontiguousarray(np.asarray(inputs[n], dtype=np.float32))
        in_maps.append(m)
    res = run_bass_kernel_spmd(nc, in_maps, core_ids=list(range(N_CORES)))
    return np.stack([np.asarray(res.results[c]["out"]) for c in range(N_CORES)], axis=0).astype(np.float32)
```

```python
import math
from contextlib import ExitStack
import numpy as np
import concourse.bass as bass
import concourse.mybir as mybir
from concourse.bass_utils import run_bass_kernel_spmd

F32 = mybir.dt.float32
BF16 = mybir.dt.bfloat16
I32 = mybir.dt.int32
AF = mybir.ActivationFunctionType
ALU = mybir.AluOpType
AX = mybir.AxisListType

D = 1024
DEPTH = 4
NH_A = 8
NE = 32
NG = 4
EPG = 8
HID = 512
ALPHA = (2 * DEPTH) ** 0.25
LN_EPS = 1e-5
RMS_EPS = 1e-5
GN_EPS = 64e-5


class Res:
    __slots__ = ("w", "r")

    def __init__(self):
        self.w = None
        self.r = {}


def RL(n):
    return [Res() for _ in range(n)]


class KB:
    EPOCH = 30000

    def __init__(self, nc, es):
        self.nc = nc
        self.es = es
        self.engs = {"pe": nc.tensor, "dve": nc.vector, "act": nc.scalar, "pool": nc.gpsimd, "sp": nc.sync}
        self.sems = {}
        self.cur = {}
        self.seen = {e: {} for e in self.engs}
        self.rings = {}
        self.ridx = {}
        self.nins = 0
        self.rec = None
        for q, n in (("sp", 16), ("pool", 12), ("act", 6)):
            self.rings[q] = [[self._new_sem("d%s%d" % (q, i)), 0] for i in range(n)]
            self.ridx[q] = 0

    def _new_sem(self, name):
        s = self.es.enter_context(self.nc.semaphore(name))
        self.sems[name] = s
        return name

    def sb(self, name, shape, dt):
        return self.es.enter_context(self.nc.sbuf_tensor(name, list(shape), dt))

    def ps(self, name, shape, dt=F32):
        return self.es.enter_context(self.nc.psum_tensor(name, list(shape), dt))

    def _deps(self, reads, writes):
        deps = {}
        for r in reads:
            if r.w:
                for k, v in r.w.items():
                    if deps.get(k, 0) < v:
                        deps[k] = v
        for w in writes:
            if w.w:
                for k, v in w.w.items():
                    if deps.get(k, 0) < v:
                        deps[k] = v
            for k, v in w.r.items():
                if deps.get(k, 0) < v:
                    deps[k] = v
        return deps

    def _waits(self, eng, deps):
        E = self.engs[eng]
        seen = self.seen[eng]
        for k, v in deps.items():
            if eng == "pe" and k.startswith("epe"):
                continue
            if seen.get(k, 0) >= v:
                continue
            E.wait_ge(self.sems[k], v)
            seen[k] = v
            if self.rec is not None:
                self.rec.append((eng, "w", k, v))

    def _mark(self, tok, reads, writes):
        (k, v), = tok.items()
        for w in writes:
            if w.w is None:
                w.w = dict(tok)
            else:
                w.w = dict(w.w)
                w.w[k] = v
            w.r = {}
        for r in reads:
            if r.r.get(k, 0) < v:
                r.r[k] = v

    def op(self, eng, fn, reads=(), writes=()):
        self._waits(eng, self._deps(reads, writes))
        c = self.cur.get(eng)
        if c is None or c[1] >= self.EPOCH:
            n = len([k for k in self.sems if k.startswith("e" + eng)])
            c = [self._new_sem("e%s%d" % (eng, n)), 0]
            self.cur[eng] = c
        ins = fn(self.engs[eng])
        c[1] += 1
        ins.then_inc(self.sems[c[0]], 1)
        self.nins += 1
        if self.rec is not None:
            self.rec.append((eng, "i", c[0], 1))
        self._mark({c[0]: c[1]}, reads, writes)

    def dma(self, q, fn, reads=(), writes=()):
        ring = self.rings[q]
        slot = ring[self.ridx[q] % len(ring)]
        self.ridx[q] += 1
        deps = self._deps(reads, writes)
        if slot[1] > 0:
            deps[slot[0]] = max(deps.get(slot[0], 0), slot[1] * 16)
        self._waits(q, deps)
        ins = fn(self.engs[q])
        slot[1] += 1
        ins.then_inc(self.sems[slot[0]], 16)
        self.nins += 1
        if self.rec is not None:
            self.rec.append((q, "i", slot[0], 16))
        self._mark({slot[0]: slot[1] * 16}, reads, writes)

    def finish(self):
        deps = {}
        for q, ring in self.rings.items():
            for name, cnt in ring:
                if cnt:
                    deps[name] = cnt * 16
        for name in self.sems:
            if name.startswith("e"):
                pass
        for e, c in self.cur.items():
            deps[c[0]] = c[1]
        self._waits("sp", deps)


class Consts:
    pass


def make_consts(kb):
    nc = kb.nc
    C = Consts()
    C.r = Res()
    it = kb.sb("c_iota", [128, 512], I32)
    itf = kb.sb("c_iotaf", [128, 512], F32)
    C.ident_f = kb.sb("c_identf", [128, 128], F32)
    C.ident_b = kb.sb("c_identb", [128, 128], BF16)
    C.ones_b = kb.sb("c_onesb", [128, 128], BF16)
    C.triu_b = kb.sb("c_triub", [128, 128], BF16)
    C.cmask = kb.sb("c_cmask", [128, 4, 512], BF16)
    C.m_st = kb.sb("c_mst", [128, 128], F32)
    C.m_in = kb.sb("c_min", [128, 128], F32)
    C.m_lo = kb.sb("c_mlo", [128, 128], F32)
    kb.op("pool", lambda e: e.iota(it[:], [[1, 512]], base=0, channel_multiplier=-1), writes=[C.r])
    kb.op("dve", lambda e: e.tensor_copy(itf[:], it[:]), reads=[C.r], writes=[C.r])
    kb.op("dve", lambda e: e.tensor_scalar(C.ident_f[:], itf[:, 0:128], 0.0, None, op0=ALU.is_equal), reads=[C.r], writes=[C.r])
    kb.op("dve", lambda e: e.tensor_copy(C.ident_b[:], C.ident_f[:]), reads=[C.r], writes=[C.r])
    kb.op("dve", lambda e: e.memset(C.ones_b[:], 1.0), writes=[C.r])
    kb.op("dve", lambda e: e.tensor_scalar(C.triu_b[:], itf[:, 0:128], 0.0, None, op0=ALU.is_ge), reads=[C.r], writes=[C.r])
    for o in range(4):
        kb.op("dve", lambda e, o=o: e.tensor_scalar(C.cmask[:, o, :], itf[:], float(128 * o), None, op0=ALU.is_ge), reads=[C.r], writes=[C.r])
    kb.op("dve", lambda e: e.tensor_scalar(C.m_st[:], itf[:, 0:128], 1.0, None, op0=ALU.is_ge), reads=[C.r], writes=[C.r])
    kb.op("dve", lambda e: e.tensor_scalar(C.m_in[:], itf[:, 0:128], 0.0, None, op0=ALU.is_ge), reads=[C.r], writes=[C.r])
    kb.op("dve", lambda e: e.tensor_scalar(C.m_lo[:], itf[:, 0:128], -1.0, None, op0=ALU.is_le), reads=[C.r], writes=[C.r])
    C.itf = itf
    return C


def kb_push(kb):
    kb._stack = getattr(kb, "_stack", [])
    kb._stack.append(kb.es_t)
    kb.es_t = kb.es_root.enter_context(ExitStack()) if False else ExitStack()
    kb.es_t.__enter__()


def kb_pop(kb):
    kb.barrier()
    kb.es_t.__exit__(None, None, None)
    kb.es_t = kb._stack.pop()


def _kb_sb(self, name, shape, dt):
    self.nalloc = getattr(self, "nalloc", 0) + 1
    return self.es_t.enter_context(self.nc.sbuf_tensor("%s_%d" % (name, self.nalloc), list(shape), dt))


def _kb_ps(self, name, shape, dt=F32):
    self.nalloc = getattr(self, "nalloc", 0) + 1
    return self.es_t.enter_context(self.nc.psum_tensor("%s_%d" % (name, self.nalloc), list(shape), dt))


def _kb_barrier(self):
    deps = {}
    for q, ring in self.rings.items():
        for name, cnt in ring:
            if cnt:
                deps[name] = cnt * 16
    for name in self.sems:
        if name.startswith("e"):
            eng = [e for e in self.engs if name.startswith("e" + e)][0]
            c = self.cur[eng]
            if c[0] == name:
                deps[name] = c[1]
    for eng in self.engs:
        self._waits(eng, dict(deps))


KB.sb = _kb_sb
KB.ps = _kb_ps
KB.barrier = _kb_barrier


def bcast_rows(ap_row, n=128):
    return ap_row.partition_broadcast(n)


def ln_tile(kb, S, z, z_r, out, out_r, g_bc, b_bc, par_r):
    st, mv, sd, rs, xn = S["st"], S["mv"], S["sd"], S["rs"], S["xn"]
    r = S["r"]
    kb.op("dve", lambda e: e.bn_stats(st[:, 0, :], z[:, 0:512]), reads=[z_r], writes=[r])
    kb.op("dve", lambda e: e.bn_stats(st[:, 1, :], z[:, 512:1024]), reads=[z_r], writes=[r])
    kb.op("dve", lambda e: e.bn_aggr(mv[:, 0:2], st[:].rearrange("p a b -> p (a b)")), reads=[r], writes=[r])
    kb.op("act", lambda e: e.activation(sd[:, 0:1], mv[:, 1:2], AF.Sqrt, bias=S["eps"][:, 0:1]), reads=[r], writes=[r])
    kb.op("dve", lambda e: e.reciprocal(rs[:, 0:1], sd[:, 0:1]), reads=[r], writes=[r])
    kb.op("dve", lambda e: e.tensor_scalar(xn[:], z[:], mv[:, 0:1], rs[:, 0:1], op0=ALU.subtract, op1=ALU.mult), reads=[r, z_r], writes=[S["xn_r"]])
    kb.op("pool", lambda e: e.tensor_tensor(xn[:], xn[:], g_bc[:], op=ALU.mult), reads=[S["xn_r"], par_r], writes=[S["xn_r"]])
    kb.op("pool", lambda e: e.tensor_tensor(out, xn[:], b_bc[:], op=ALU.add), reads=[S["xn_r"], par_r], writes=[out_r])


def ln_scratch(kb, pfx, eps):
    S = {}
    S["st"] = kb.sb(pfx + "st", [128, 2, 6], F32)
    S["mv"] = kb.sb(pfx + "mv", [128, 2], F32)
    S["sd"] = kb.sb(pfx + "sd", [128, 1], F32)
    S["rs"] = kb.sb(pfx + "rs", [128, 1], F32)
    S["xn"] = kb.sb(pfx + "xn", [128, 1024], F32)
    S["eps"] = kb.sb(pfx + "eps", [128, 1], F32)
    S["r"] = Res()
    S["xn_r"] = Res()
    kb.op("dve", lambda e: e.memset(S["eps"][:], eps), writes=[S["r"]])
    return S


def load_ln_params(kb, pfx, g_dram, b_dram, li):
    g = kb.sb(pfx + "g", [128, 1024], F32)
    b = kb.sb(pfx + "b", [128, 1024], F32)
    r = Res()
    kb.dma("sp", lambda e: e.dma_start(out=g[:], in_=bcast_rows(g_dram[li:li + 1, :])), writes=[r])
    kb.dma("sp", lambda e: e.dma_start(out=b[:], in_=bcast_rows(b_dram[li:li + 1, :])), writes=[r])
    return g, b, r


def attn_phase(kb, C, T, prm, j, li, xin, xin_r, xa, xa_r):
    nc = kb.nc
    NT = T // 128
    NS = T // 512
    lambda_init = 0.8 - 0.6 * math.exp(-0.3 * li)
    kb_push(kb)
    OT = kb.sb("a_OT", [128, 8, T], BF16)
    OT_r = [RL(NS) for _ in range(8)]
    ps = [kb.ps("a_ps%d" % b, [128, 512], F32) for b in range(8)]
    ps_r = RL(8)
    kb_push(kb)
    xld = [kb.sb("a_xld%d" % b, [128, 1024], F32) for b in range(2)]
    xld_r = RL(2)
    xT = kb.sb("a_xT", [128, 8, T], BF16)
    xT_r = RL(NT)
    QT = kb.sb("a_QT", [128, T], BF16)
    QT_r = RL(NS)
    KT = kb.sb("a_KT", [128, T], BF16)
    KT_r = RL(NS)
    V = kb.sb("a_V", [128, NT, 128], BF16)
    V_r = RL(NT // 4)
    wh = [kb.sb("a_wh%d" % b, [128, 8, 3, 128], BF16) for b in range(2)]
    wh_r = RL(2)
    pt = [kb.sb("a_pt%d" % b, [128, 512], BF16) for b in range(4)]
    pt_r = RL(4)
    lam = kb.sb("a_lam", [128, 256], F32)
    lsc = kb.sb("a_lsc", [128, 8], F32)
    lam_r = Res()
    s1 = kb.sb("a_s1", [128, 512], F32)
    s2 = kb.sb("a_s2", [128, 512], F32)
    t1 = kb.sb("a_t1", [128, 512], F32)
    t2 = kb.sb("a_t2", [128, 512], F32)
    sq = kb.sb("a_sq", [128, 512], BF16)
    op_ = t1
    s12 = s1
    e2 = s2
    tot = s2
    rstd = s1
    fin_r = Res()
    fin2_r = fin_r

    kb.dma("sp", lambda e: e.dma_start(out=lam[:], in_=bcast_rows(prm["attn_lambda"][j:j + 1].rearrange("o a b -> o (a b)"))), writes=[lam_r])
    kb.dma("sp", lambda e: e.dma_start(out=lsc[:, 4:5], in_=prm["attn_subln_g"][j].rearrange("(p o) -> p o", o=1)), writes=[lam_r])
    kb.op("dve", lambda e: e.tensor_tensor(lam[:, 0:64], lam[:, 0:64], lam[:, 64:128], op=ALU.mult), reads=[lam_r], writes=[lam_r])
    kb.op("dve", lambda e: e.tensor_tensor(lam[:, 128:192], lam[:, 128:192], lam[:, 192:256], op=ALU.mult), reads=[lam_r], writes=[lam_r])
    kb.op("dve", lambda e: e.reduce_sum(lsc[:, 0:1], lam[:, 0:64], axis=AX.X), reads=[lam_r], writes=[lam_r])
    kb.op("dve", lambda e: e.reduce_sum(lsc[:, 1:2], lam[:, 128:192], axis=AX.X), reads=[lam_r], writes=[lam_r])
    kb.op("act", lambda e: e.activation(lsc[:, 2:4], lsc[:, 0:2], AF.Exp), reads=[lam_r], writes=[lam_r])
    kb.op("dve", lambda e: e.tensor_tensor(lsc[:, 5:6], lsc[:, 3:4], lsc[:, 2:3], op=ALU.subtract), reads=[lam_r], writes=[lam_r])
    kb.op("dve", lambda e: e.tensor_scalar(lsc[:, 5:6], lsc[:, 5:6], -lambda_init, None, op0=ALU.add), reads=[lam_r], writes=[lam_r])
    kb.op("dve", lambda e: e.tensor_scalar(lsc[:, 6:7], lsc[:, 4:5], 1.0 - lambda_init, None, op0=ALU.mult), reads=[lam_r], writes=[lam_r])
    nlam = lsc[:, 5:6]
    gsc = lsc[:, 6:7]

    for i in range(NT):
        b = i % 2
        kb.dma("sp", lambda e, i=i, b=b: e.dma_start(out=xld[b][:], in_=xin[i * 128:(i + 1) * 128, :]), reads=[xin_r[i]], writes=[xld_r[b]])
        for hf in range(2):
            pb = (2 * i + hf) % 8
            for c4 in range(4):
                kc = hf * 4 + c4
                kb.op("pe", lambda e, pb=pb, c4=c4, kc=kc, b=b: e.transpose(ps[pb][:, c4 * 128:(c4 + 1) * 128], xld[b][:, kc * 128:(kc + 1) * 128], C.ident_f[:]),
                      reads=[xld_r[b], C.r], writes=[ps_r[pb]])
            eng = "act" if hf == 0 else "dve"
            if eng == "act":
                kb.op("act", lambda e, pb=pb, hf=hf, i=i: e.activation(xT[:, hf * 4:hf * 4 + 4, i * 128:(i + 1) * 128], ps[pb][:].rearrange("p (c t) -> p c t", c=4), AF.Copy),
                      reads=[ps_r[pb]], writes=[xT_r[i]])
            else:
                kb.op("dve", lambda e, pb=pb, hf=hf, i=i: e.tensor_copy(xT[:, hf * 4:hf * 4 + 4, i * 128:(i + 1) * 128], ps[pb][:].rearrange("p (c t) -> p c t", c=4)),
                      reads=[ps_r[pb]], writes=[xT_r[i]])


    wq = prm["attn_w_qkv"][j].rearrange("(kc p) n -> p kc n", p=128)
    pcnt = [0]

    def nps():
        pcnt[0] += 1
        return pcnt[0] % 2

    for h in range(NH_A):
        wb = h % 2
        for part in range(3):
            kb.dma("pool", lambda e, wb=wb, part=part, h=h: e.dma_start(out=wh[wb][:, :, part, :], in_=wq[:, :, part * 1024 + h * 128: part * 1024 + (h + 1) * 128]),
                   writes=[wh_r[wb]])
        for s in range(NS):
            for part, dst, dst_r, scl in ((0, QT, QT_r, 0.125), (1, KT, KT_r, 1.0)):
                pb = nps()
                for kc in range(8):
                    kb.op("pe", lambda e, pb=pb, wb=wb, kc=kc, part=part, s=s: e.matmul(ps[pb][:], wh[wb][:, kc, part, :], xT[:, kc, s * 512:(s + 1) * 512], start=(kc == 0), stop=(kc == 7)),
                          reads=[wh_r[wb]] + xT_r[s * 4:s * 4 + 4], writes=[ps_r[pb]])
                kb.op("act", lambda e, pb=pb, dst=dst, s=s, scl=scl: e.activation(dst[:, s * 512:(s + 1) * 512], ps[pb][:], AF.Copy, scale=scl),
                      reads=[ps_r[pb]], writes=[dst_r[s]])
        for g4 in range(NT // 4):
            pb = nps()
            for t4 in range(4):
                i = g4 * 4 + t4
                for kc in range(8):
                    kb.op("pe", lambda e, pb=pb, wb=wb, kc=kc, i=i, t4=t4: e.matmul(ps[pb][:, t4 * 128:(t4 + 1) * 128], xT[:, kc, i * 128:(i + 1) * 128], wh[wb][:, kc, 2, :], start=(kc == 0), stop=(kc == 7)),
                          reads=[wh_r[wb], xT_r[i]], writes=[ps_r[pb]])
            kb.op("dve", lambda e, pb=pb, g4=g4: e.tensor_copy(V[:, g4 * 4:g4 * 4 + 4, :], ps[pb][:].rearrange("p (c t) -> p c t", c=4)),
                  reads=[ps_r[pb]], writes=[V_r[g4]])
        pti = 0
        scnt = [0]
        for qs in range(NS):
            nkt = qs * 4 + 4
            items = [(kt, c) for kt in range(nkt) for c in range(2)]
            sbank = {}
            LOOK = 2

            def emit_s(n):
                kt, c = items[n]
                pb = (0, 1, 7)[scnt[0] % 3]
                scnt[0] += 1
                sbank[n] = pb
                kb.op("pe", lambda e, pb=pb, c=c, kt=kt, qs=qs: e.matmul(ps[pb][:], KT[c * 64:(c + 1) * 64, kt * 128:(kt + 1) * 128], QT[c * 64:(c + 1) * 64, qs * 512:(qs + 1) * 512], start=True, stop=True),
                      reads=[KT_r[kt // 4], QT_r[qs]], writes=[ps_r[pb]])

            for n in range(min(LOOK, len(items))):
                emit_s(n)
            for n, (kt, c) in enumerate(items):
                if n + LOOK < len(items):
                    emit_s(n + LOOK)
                pb = sbank[n]
                pi = pti % 4
                pti += 1
                kb.op("act", lambda e, pb=pb, pi=pi: e.activation(pt[pi][:], ps[pb][:], AF.Exp), reads=[ps_r[pb]], writes=[pt_r[pi]])
                if kt >= qs * 4:
                    o = kt - qs * 4
                    kb.op("pool", lambda e, pi=pi, o=o: e.tensor_tensor(pt[pi][:], pt[pi][:], C.cmask[:, o, :], op=ALU.mult), reads=[pt_r[pi], C.r], writes=[pt_r[pi]])
                kb.op("pe", lambda e, c=c, kt=kt, pi=pi, nkt=nkt: e.matmul(ps[2 + c][:], V[:, kt, :], pt[pi][:], start=(kt == 0), stop=(kt == nkt - 1)),
                      reads=[V_r[kt // 4], pt_r[pi]], writes=[ps_r[2 + c]])
                kb.op("pe", lambda e, c=c, kt=kt, pi=pi, nkt=nkt: e.matmul(ps[4 + c][:], C.ones_b[:], pt[pi][:], start=(kt == 0), stop=(kt == nkt - 1)),
                      reads=[C.r, pt_r[pi]], writes=[ps_r[4 + c]])
            kb.op("act", lambda e: e.activation(s1[:], ps[4][:], AF.Copy), reads=[ps_r[4]], writes=[fin_r])
            kb.op("act", lambda e: e.activation(s2[:], ps[5][:], AF.Copy), reads=[ps_r[5]], writes=[fin_r])
            kb.op("dve", lambda e: e.tensor_tensor(t1[:], ps[2][:], s2[:], op=ALU.mult), reads=[ps_r[2], fin_r], writes=[fin2_r])
            kb.op("dve", lambda e: e.tensor_tensor(t2[:], ps[3][:], s1[:], op=ALU.mult), reads=[ps_r[3], fin_r], writes=[fin2_r])
            kb.op("dve", lambda e: e.scalar_tensor_tensor(op_[:], t2[:], nlam, t1[:], op0=ALU.mult, op1=ALU.add), reads=[fin2_r, lam_r], writes=[fin2_r])
            kb.op("pool", lambda e: e.tensor_tensor(sq[:], op_[:], op_[:], op=ALU.mult), reads=[fin2_r], writes=[fin2_r])
            kb.op("pool", lambda e: e.tensor_tensor(s12[:], s1[:], s2[:], op=ALU.mult), reads=[fin_r], writes=[fin2_r])
            kb.op("dve", lambda e: e.scalar_tensor_tensor(e2[:], s12[:], RMS_EPS, s12[:], op0=ALU.mult, op1=ALU.mult), reads=[fin2_r], writes=[fin2_r])
            kb.op("pe", lambda e: e.matmul(ps[6][:], C.ones_b[:], sq[:], start=True, stop=True), reads=[C.r, fin2_r], writes=[ps_r[6]])
            kb.op("dve", lambda e: e.scalar_tensor_tensor(tot[:], ps[6][:], 1.0 / 128.0, e2[:], op0=ALU.mult, op1=ALU.add), reads=[ps_r[6], fin2_r], writes=[fin2_r])
            kb.op("act", lambda e: e.activation(tot[:], tot[:], AF.Ln), reads=[fin2_r], writes=[fin2_r])
            kb.op("act", lambda e: e.activation(rstd[:], tot[:], AF.Exp, scale=-0.5), reads=[fin2_r], writes=[fin2_r])
            kb.op("dve", lambda e, h=h, qs=qs: e.scalar_tensor_tensor(OT[:, h, qs * 512:(qs + 1) * 512], op_[:], gsc, rstd[:], op0=ALU.mult, op1=ALU.mult),
                  reads=[fin2_r, lam_r], writes=[OT_r[h][qs]])

    kb_pop(kb)
    wo = kb.sb("a_wo", [128, 8, 1024], BF16)
    wo_r = Res()
    wo_src = prm["attn_w_o"][j].rearrange("(h p) n -> p h n", p=128)
    for hh in range(8):
        kb.dma("pool", lambda e, hh=hh: e.dma_start(out=wo[:, hh, :], in_=wo_src[:, hh, :]), writes=[wo_r])
    xld = [kb.sb("a_xldc%d" % b, [128, 1024], F32) for b in range(2)]
    xld_r = RL(2)
    zt = [kb.sb("a_z%d" % b, [128, 1024], F32) for b in range(2)]
    zt_r = RL(2)
    x1 = [kb.sb("a_x1%d" % b, [128, 1024], F32) for b in range(2)]
    x1_r = RL(2)
    LS = ln_scratch(kb, "a_ln", LN_EPS)
    g_bc, b_bc, lnp_r = load_ln_params(kb, "a_lnp", prm["ln1_g"], prm["ln1_b"], li)
    for i in range(NT):
        b = i % 2
        kb.dma("sp", lambda e, i=i, b=b: e.dma_start(out=xld[b][:], in_=xin[i * 128:(i + 1) * 128, :]), reads=[xin_r[i]], writes=[xld_r[b]])
        for hf in range(2):
            pb = 2 * b + hf
            for hh in range(8):
                kb.op("pe", lambda e, pb=pb, hh=hh, hf=hf, i=i: e.matmul(ps[pb][:], OT[:, hh, i * 128:(i + 1) * 128], wo[:, hh, hf * 512:(hf + 1) * 512], start=(hh == 0), stop=(hh == 7)),
                      reads=[OT_r[hh][i // 4], wo_r], writes=[ps_r[pb]])
            kb.op("dve", lambda e, pb=pb, hf=hf, b=b: e.scalar_tensor_tensor(zt[b][:, hf * 512:(hf + 1) * 512], xld[b][:, hf * 512:(hf + 1) * 512], ALPHA, ps[pb][:], op0=ALU.mult, op1=ALU.add),
                  reads=[xld_r[b], ps_r[pb]], writes=[zt_r[b]])
        ln_tile(kb, LS, zt[b], zt_r[b], x1[b][:], x1_r[b], g_bc, b_bc, lnp_r)
        kb.dma("sp", lambda e, i=i, b=b: e.dma_start(out=xa[i * 128:(i + 1) * 128, :], in_=x1[b][:]), reads=[x1_r[b]], writes=[xa_r[i]])
    kb_pop(kb)


PSHAPES = {
    "ln1_g": (4, 1024), "ln1_b": (4, 1024), "ln2_g": (4, 1024), "ln2_b": (4, 1024),
    "attn_w_qkv": (2, 1024, 3072), "attn_w_o": (2, 1024, 1024), "attn_lambda": (2, 4, 64), "attn_subln_g": (2, 128),
    "rw_mu": (2, 6, 1024), "rw_w_rkv": (2, 3, 1024, 1024), "rw_w_o": (2, 1024, 1024), "rw_w0": (2, 1024),
    "rw_w1": (2, 1024, 64), "rw_w2": (2, 64, 1024), "rw_a0": (2, 1024), "rw_a1": (2, 1024, 64), "rw_a2": (2, 64, 1024),
    "rw_g1": (2, 1024, 160), "rw_g2": (2, 160, 1024), "rw_k_k": (2, 1024), "rw_k_a": (2, 1024), "rw_r_k": (2, 16, 64),
    "rw_lnx_g": (2, 1024), "rw_lnx_b": (2, 1024), "rw_v0": (1, 1024), "rw_v1": (1, 1024, 32), "rw_v2": (1, 32, 1024),
    "moe_rg_w": (4, 1024, 4), "moe_rg_b": (4, 4), "moe_re_w": (4, 1024, 32), "moe_re_b": (4, 32),
    "moe_w_gu": (4, 32, 1024, 1024), "moe_w_down": (4, 32, 512, 1024),
}


class Params(dict):
    def __init__(self, nc):
        super().__init__()
        self.nc = nc

    def __missing__(self, k):
        ap = self.nc.dram_tensor(k, list(PSHAPES[k]), F32, kind="ExternalInput").ap()
        self[k] = ap
        return ap


def build(T, plan, cap=512):
    nc = bass.Bass("TRN2", target_bir_lowering=False)
    prm = Params(nc)
    x = nc.dram_tensor("x", [T, D], F32, kind="ExternalInput").ap()
    out = nc.dram_tensor("out", [T, D], F32, kind="ExternalOutput").ap()
    xa = nc.dram_tensor("xa_s", [T, D], F32, kind="Internal").ap()
    xb = nc.dram_tensor("xb_s", [T, D], F32, kind="Internal").ap()
    NT = T // 128
    es = ExitStack()
    with es:
        kb = KB(nc, es)
        kb.es_t = es
        C = make_consts(kb)
        cur, cur_r = x, RL(NT)
        xa_r, xb_r, out_r = RL(NT), RL(NT), RL(NT)
        ST = dict(DEBUG_ST)
        for n, step in enumerate(plan):
            last = n == len(plan) - 1
            kind = step[0]
            if kind == "attn":
                attn_phase(kb, C, T, prm, step[1], step[2], cur, cur_r, xa, xa_r)
                cur, cur_r = xa, xa_r
            elif kind == "rwkv":
                rwkv_phase(kb, C, T, prm, step[1], step[2], cur, cur_r, xa, xa_r, ST, nc)
                cur, cur_r = xa, xa_r
            elif kind == "moe":
                dst, dst_r = (out, out_r) if last else (xb, xb_r)
                moe_phase(kb, C, T, prm, step[1], cur, cur_r, dst, dst_r, cap, nc, ST)
                cur, cur_r = dst, dst_r
        if cur is not out:
            for i in range(NT):
                kb.dma("sp", lambda e, i=i: e.dma_start(out=out[i * 128:(i + 1) * 128, :], in_=cur[i * 128:(i + 1) * 128, :]), reads=[cur_r[i]], writes=[out_r[i]])
        kb.finish()
        ninst = kb.nins
    return nc, list(prm.keys()), ninst


DEBUG_ST = {}
FULL_PLAN = [("attn", 0, 0), ("moe", 0), ("rwkv", 0, 1), ("moe", 1), ("attn", 1, 2), ("moe", 2), ("rwkv", 1, 3), ("moe", 3)]


def moe_phase(kb, C, T, prm, li, xin, xin_r, dst, dst_r, cap, nc, ST):
    NT = T // 128
    NSLOT = NE * cap
    NB = cap // 128
    if "xbuf" not in ST:
        ST["xbuf"] = nc.dram_tensor("xbuf_s", [NSLOT, D], BF16, kind="Internal").ap()
        ST["ybuf"] = nc.dram_tensor("ybuf_s", [NSLOT, D], F32, kind="Internal").ap()
        ST["bc_reg"] = nc.gpsimd.to_reg(NSLOT - 1)
    xbuf, ybuf = ST["xbuf"], ST["ybuf"]
    bc_reg = ST["bc_reg"]
    kb_push(kb)
    slots = kb.sb("m_slots", [128, NT, 2], I32)
    gates = kb.sb("m_gates", [128, NT, 2], F32)
    sg_r = Res()

    kb_push(kb)
    ps = [kb.ps("m1_ps%d" % b, [128, 512], F32) for b in range(6)]
    ps_r = RL(6)
    xt = [kb.sb("m1_xt%d" % b, [128, 1024], F32) for b in range(2)]
    xt_r = RL(2)
    xb16 = [kb.sb("m1_xb%d" % b, [128, 1024], BF16) for b in range(2)]
    xb_r = RL(2)
    xT = [kb.sb("m1_xT%d" % b, [128, 8, 128], F32) for b in range(2)]
    xT_r = RL(2)
    wr = kb.sb("m1_wr", [128, 8, 36], F32)
    rb = kb.sb("m1_rb", [128, 36], F32)
    offs_i = kb.sb("m1_offi", [128, 32], I32)
    offs = kb.sb("m1_off", [128, 32], F32)
    base = kb.sb("m1_base", [128, 32], F32)
    cr = Res()
    base_r = Res()
    kb.dma("sp", lambda e: e.dma_start(out=wr[:, :, 0:4], in_=prm["moe_rg_w"][li].rearrange("(kc p) n -> p kc n", p=128)), writes=[cr])
    kb.dma("sp", lambda e: e.dma_start(out=wr[:, :, 4:36], in_=prm["moe_re_w"][li].rearrange("(kc p) n -> p kc n", p=128)), writes=[cr])
    kb.dma("sp", lambda e: e.dma_start(out=rb[:, 0:4], in_=bcast_rows(prm["moe_rg_b"][li:li + 1, :])), writes=[cr])
    kb.dma("sp", lambda e: e.dma_start(out=rb[:, 4:36], in_=bcast_rows(prm["moe_re_b"][li:li + 1, :])), writes=[cr])
    kb.op("pool", lambda e: e.iota(offs_i[:], [[cap, 32]], base=-1, channel_multiplier=0), writes=[cr])
    kb.op("dve", lambda e: e.tensor_copy(offs[:], offs_i[:]), reads=[cr], writes=[cr])
    kb.op("dve", lambda e: e.memset(base[:], 0.0), writes=[base_r])
    Wk = {}
    for nm, w in (("L", 36), ("ohg", 4), ("eg", 4), ("lsel", 8), ("oh1", 8), ("lsel2", 8), ("oh2", 8), ("E1", 32), ("E2", 32),
                  ("val", 32), ("valid", 32), ("val2", 32), ("tmp", 32), ("sc", 16)):
        Wk[nm] = kb.sb("m1_w" + nm, [128, w], F32)
    G01 = kb.sb("m1_G01", [128, 32], BF16)
    wr_ = Res()
    BIG = float(NSLOT)
    for i in range(NT):
        b = i % 2
        kb.dma("sp", lambda e, i=i, b=b: e.dma_start(out=xt[b][:], in_=xin[i * 128:(i + 1) * 128, :]), reads=[xin_r[i]], writes=[xt_r[b]])
        kb.op("act", lambda e, b=b: e.activation(xb16[b][:], xt[b][:], AF.Copy), reads=[xt_r[b]], writes=[xb_r[b]])
        for hf in range(2):
            pb = hf
            for c4 in range(4):
                kc = hf * 4 + c4
                kb.op("pe", lambda e, pb=pb, c4=c4, kc=kc, b=b: e.transpose(ps[pb][:, c4 * 128:(c4 + 1) * 128], xt[b][:, kc * 128:(kc + 1) * 128], C.ident_f[:]),
                      reads=[xt_r[b], C.r], writes=[ps_r[pb]])
            if hf == 0:
                kb.op("act", lambda e, pb=pb, b=b: e.activation(xT[b][:, 0:4, :], ps[pb][:].rearrange("p (c t) -> p c t", c=4), AF.Copy), reads=[ps_r[pb]], writes=[xT_r[b]])
            else:
                kb.op("dve", lambda e, pb=pb, b=b: e.tensor_copy(xT[b][:, 4:8, :], ps[pb][:].rearrange("p (c t) -> p c t", c=4)), reads=[ps_r[pb]], writes=[xT_r[b]])
        for kc in range(8):
            kb.op("pe", lambda e, kc=kc, b=b: e.matmul(ps[2][:, 0:36], xT[b][:, kc, :], wr[:, kc, :], start=(kc == 0), stop=(kc == 7)), reads=[xT_r[b], cr], writes=[ps_r[2]])
        L, ohg, eg, lsel, oh1, lsel2, oh2, E1, E2 = (Wk[k] for k in ("L", "ohg", "eg", "lsel", "oh1", "lsel2", "oh2", "E1", "E2"))
        val, valid, val2, tmp, sc = (Wk[k] for k in ("val", "valid", "val2", "tmp", "sc"))

        def dv(fn, extra_r=(), extra_w=()):
            kb.op("dve", fn, reads=[wr_] + list(extra_r), writes=[wr_] + list(extra_w))

        dv(lambda e: e.tensor_tensor(L[:], ps[2][:, 0:36], rb[:], op=ALU.add), extra_r=[ps_r[2], cr])
        dv(lambda e: e.reduce_max(sc[:, 0:1], L[:, 0:4], axis=AX.X))
        dv(lambda e: e.tensor_scalar(ohg[:], L[:, 0:4], sc[:, 0:1], None, op0=ALU.is_equal))
        dv(lambda e: e.tensor_scalar(sc[:, 1:2], sc[:, 0:1], -1.0, None, op0=ALU.mult))
        kb.op("act", lambda e: e.activation(eg[:], L[:, 0:4], AF.Exp, bias=sc[:, 1:2]), reads=[wr_], writes=[wr_])
        dv(lambda e: e.reduce_sum(sc[:, 2:3], eg[:], axis=AX.X))
        dv(lambda e: e.reciprocal(sc[:, 3:4], sc[:, 2:3]))
        dv(lambda e: e.tensor_scalar(lsel[:], L[:, 4:12], ohg[:, 0:1], None, op0=ALU.mult))
        for g in range(1, 4):
            dv(lambda e, g=g: e.scalar_tensor_tensor(lsel[:], L[:, 4 + 8 * g:12 + 8 * g], ohg[:, g:g + 1], lsel[:], op0=ALU.mult, op1=ALU.add))
        dv(lambda e: e.reduce_max(sc[:, 4:5], lsel[:], axis=AX.X))
        dv(lambda e: e.tensor_scalar(oh1[:], lsel[:], sc[:, 4:5], None, op0=ALU.is_equal))
        dv(lambda e: e.scalar_tensor_tensor(lsel2[:], oh1[:], -1e30, lsel[:], op0=ALU.mult, op1=ALU.add))
        dv(lambda e: e.reduce_max(sc[:, 5:6], lsel2[:], axis=AX.X))
        dv(lambda e: e.tensor_scalar(oh2[:], lsel2[:], sc[:, 5:6], None, op0=ALU.is_equal))
        dv(lambda e: e.tensor_tensor(sc[:, 6:7], sc[:, 5:6], sc[:, 4:5], op=ALU.subtract))
        kb.op("act", lambda e: e.activation(sc[:, 7:8], sc[:, 6:7], AF.Exp), reads=[wr_], writes=[wr_])
        dv(lambda e: e.tensor_scalar(sc[:, 8:9], sc[:, 7:8], 1.0, None, op0=ALU.add))
        dv(lambda e: e.reciprocal(sc[:, 9:10], sc[:, 8:9]))
        dv(lambda e: e.tensor_tensor(sc[:, 10:11], sc[:, 7:8], sc[:, 9:10], op=ALU.mult))
        for g in range(4):
            dv(lambda e, g=g: e.tensor_scalar(E1[:, 8 * g:8 * g + 8], oh1[:], ohg[:, g:g + 1], None, op0=ALU.mult))
            dv(lambda e, g=g: e.tensor_scalar(E2[:, 8 * g:8 * g + 8], oh2[:], ohg[:, g:g + 1], None, op0=ALU.mult))
        dv(lambda e: e.tensor_tensor(G01[:], E1[:], E2[:], op=ALU.add))
        kb.op("pe", lambda e: e.matmul(ps[3][:, 0:32], C.triu_b[:], G01[:], start=True, stop=True), reads=[wr_, C.r], writes=[ps_r[3]])
        kb.op("pe", lambda e: e.matmul(ps[3][:, 32:64], C.ones_b[:], G01[:], start=True, stop=True), reads=[wr_, C.r], writes=[ps_r[3]])
        dv(lambda e: e.tensor_tensor(val[:], ps[3][:, 0:32], base[:], op=ALU.add), extra_r=[ps_r[3], base_r])
        dv(lambda e: e.tensor_scalar(valid[:], val[:], float(cap), None, op0=ALU.is_le))
        dv(lambda e: e.tensor_tensor(val2[:], val[:], offs[:], op=ALU.add), extra_r=[cr])
        dv(lambda e: e.scalar_tensor_tensor(val2[:], val2[:], -BIG, valid[:], op0=ALU.add, op1=ALU.mult))
        dv(lambda e: e.tensor_scalar(val2[:], val2[:], BIG, None, op0=ALU.add))
        dv(lambda e: e.tensor_tensor(tmp[:], E1[:], val2[:], op=ALU.mult))
        dv(lambda e: e.reduce_sum(sc[:, 11:12], tmp[:], axis=AX.X))
        dv(lambda e: e.tensor_tensor(tmp[:], E2[:], val2[:], op=ALU.mult))
        dv(lambda e: e.reduce_sum(sc[:, 12:13], tmp[:], axis=AX.X))
        dv(lambda e: e.tensor_tensor(tmp[:], E1[:], valid[:], op=ALU.mult))
        dv(lambda e: e.reduce_sum(sc[:, 13:14], tmp[:], axis=AX.X))
        dv(lambda e: e.tensor_tensor(tmp[:], E2[:], valid[:], op=ALU.mult))
        dv(lambda e: e.reduce_sum(sc[:, 14:15], tmp[:], axis=AX.X))
        dv(lambda e: e.tensor_tensor(base[:], base[:], ps[3][:, 32:64], op=ALU.add), extra_r=[ps_r[3]], extra_w=[base_r])
        dv(lambda e, i=i: e.tensor_copy(slots[:, i, :], sc[:, 11:13]), extra_w=[sg_r])
        dv(lambda e: e.tensor_scalar(sc[:, 9:11], sc[:, 9:11], sc[:, 3:4], None, op0=ALU.mult))
        dv(lambda e, i=i: e.tensor_tensor(gates[:, i, :], sc[:, 9:11], sc[:, 13:15], op=ALU.mult), extra_w=[sg_r])
        for k in range(2):
            kb.dma("pool", lambda e, i=i, k=k, b=b: e.indirect_dma_start(out=xbuf[:, :], out_offset=bass.IndirectOffsetOnAxis(ap=slots[:, i, k:k + 1], axis=0),
                                                                       in_=xb16[b][:], in_offset=None, bounds_check=bc_reg, oob_is_err=False),
                   reads=[sg_r, xb_r[b]])
    kb_pop(kb)

    kb_push(kb)
    psT = [kb.ps("m2_pT%d" % b, [128, 1024], BF16) for b in range(2)]
    psT_r = RL(2)
    psH = [kb.ps("m2_pH%d" % b, [128, 512], F32) for b in range(4)]
    psH_r = RL(4)
    psY = [kb.ps("m2_pY%d" % b, [128, 512], F32) for b in range(2)]
    psY_r = RL(2)
    wgu = [kb.sb("m2_wgu%d" % b, [128, 8, 1024], BF16) for b in range(2)]
    wgu_r = RL(2)
    wd = [kb.sb("m2_wd%d" % b, [128, 4, 1024], BF16) for b in range(2)]
    wd_r = RL(2)
    xblk = [kb.sb("m2_xb%d" % b, [128, 1024], BF16) for b in range(2)]
    xblk_r = RL(2)
    XT = [kb.sb("m2_XT%d" % b, [128, 8, cap], BF16) for b in range(2)]
    XT_r = RL(2)
    sil = [kb.sb("m2_sil%d" % b, [128, cap], F32) for b in range(2)]
    sil_r = RL(2)
    AT = [kb.sb("m2_AT%d" % b, [128, 4, cap], BF16) for b in range(2)]
    AT_r = RL(2)
    ysb = [kb.sb("m2_y%d" % b, [128, 1024], F32) for b in range(2)]
    ysb_r = RL(2)
    nblk = 0
    for ex in range(NE):
        wb = ex % 2
        gsrc = prm["moe_w_gu"][li, ex].rearrange("(kc p) n -> p kc n", p=128)
        dsrc = prm["moe_w_down"][li, ex].rearrange("(m p) n -> p m n", p=128)
        for kc in range(8):
            kb.dma("pool", lambda e, wb=wb, kc=kc, gsrc=gsrc: e.dma_start(out=wgu[wb][:, kc, :], in_=gsrc[:, kc, :]), writes=[wgu_r[wb]])
        for m in range(4):
            kb.dma("pool", lambda e, wb=wb, m=m, dsrc=dsrc: e.dma_start(out=wd[wb][:, m, :], in_=dsrc[:, m, :]), writes=[wd_r[wb]])
        for blk in range(NB):
            bb = nblk % 2
            nblk += 1
            r0 = ex * cap + blk * 128
            kb.dma("sp", lambda e, bb=bb, r0=r0: e.dma_start(out=xblk[bb][:], in_=xbuf[r0:r0 + 128, :]), writes=[xblk_r[bb]])
            for kc in range(8):
                kb.op("pe", lambda e, bb=bb, kc=kc: e.transpose(psT[bb][:, kc * 128:(kc + 1) * 128], xblk[bb][:, kc * 128:(kc + 1) * 128], C.ident_b[:]),
                      reads=[xblk_r[bb], C.r], writes=[psT_r[bb]])
            eng = "act" if blk % 2 == 0 else "dve"
            if eng == "act":
                kb.op("act", lambda e, bb=bb, wb=wb, blk=blk: e.activation(XT[wb][:, :, blk * 128:(blk + 1) * 128], psT[bb][:].rearrange("p (c t) -> p c t", c=8), AF.Copy),
                      reads=[psT_r[bb]], writes=[XT_r[wb]])
            else:
                kb.op("dve", lambda e, bb=bb, wb=wb, blk=blk: e.tensor_copy(XT[wb][:, :, blk * 128:(blk + 1) * 128], psT[bb][:].rearrange("p (c t) -> p c t", c=8)),
                      reads=[psT_r[bb]], writes=[XT_r[wb]])
        for m in range(4):
            pg, pu = (m % 2) * 2, (m % 2) * 2 + 1
            for (pb, col) in ((pg, m * 128), (pu, 512 + m * 128)):
                for kc in range(8):
                    kb.op("pe", lambda e, pb=pb, col=col, kc=kc, wb=wb: e.matmul(psH[pb][:, 0:cap], wgu[wb][:, kc, col:col + 128], XT[wb][:, kc, :], start=(kc == 0), stop=(kc == 7)),
                          reads=[wgu_r[wb], XT_r[wb]], writes=[psH_r[pb]])
            sb_ = m % 2
            kb.op("act", lambda e, pg=pg, sb_=sb_: e.activation(sil[sb_][:], psH[pg][:, 0:cap], AF.Silu), reads=[psH_r[pg]], writes=[sil_r[sb_]])
            kb.op("dve", lambda e, pu=pu, sb_=sb_, m=m, wb=wb: e.tensor_tensor(AT[wb][:, m, :], sil[sb_][:], psH[pu][:, 0:cap], op=ALU.mult),
                  reads=[psH_r[pu], sil_r[sb_]], writes=[AT_r[wb]])
        for blk in range(NB):
            yb = blk % 2
            for hf in range(2):
                for m in range(4):
                    kb.op("pe", lambda e, hf=hf, m=m, wb=wb, blk=blk: e.matmul(psY[hf][:], AT[wb][:, m, blk * 128:(blk + 1) * 128], wd[wb][:, m, hf * 512:(hf + 1) * 512], start=(m == 0), stop=(m == 3)),
                          reads=[AT_r[wb], wd_r[wb]], writes=[psY_r[hf]])
                if hf == 0:
                    kb.op("act", lambda e, yb=yb: e.activation(ysb[yb][:, 0:512], psY[0][:], AF.Copy), reads=[psY_r[0]], writes=[ysb_r[yb]])
                else:
                    kb.op("dve", lambda e, yb=yb: e.tensor_copy(ysb[yb][:, 512:1024], psY[1][:]), reads=[psY_r[1]], writes=[ysb_r[yb]])
            r0 = ex * cap + blk * 128
            kb.dma("sp", lambda e, yb=yb, r0=r0: e.dma_start(out=ybuf[r0:r0 + 128, :], in_=ysb[yb][:]), reads=[ysb_r[yb]])
    kb_pop(kb)

    kb_push(kb)
    y1 = [kb.sb("m3_y1%d" % b, [128, 1024], F32) for b in range(2)]
    y2 = [kb.sb("m3_y2%d" % b, [128, 1024], F32) for b in range(2)]
    y_r = RL(2)
    xt3 = [kb.sb("m3_xt%d" % b, [128, 1024], F32) for b in range(2)]
    xt3_r = RL(2)
    z3 = [kb.sb("m3_z%d" % b, [128, 1024], F32) for b in range(2)]
    z3_r = RL(2)
    x2 = [kb.sb("m3_x2%d" % b, [128, 1024], F32) for b in range(2)]
    x2_r = RL(2)
    LS = ln_scratch(kb, "m3_ln", LN_EPS)
    g_bc, b_bc, lnp_r = load_ln_params(kb, "m3_lnp", prm["ln2_g"], prm["ln2_b"], li)
    for b in range(2):
        kb.op("pool", lambda e, b=b: e.memset(y1[b][:], 0.0), writes=[y_r[b]])
        kb.op("pool", lambda e, b=b: e.memset(y2[b][:], 0.0), writes=[y_r[b]])
    for i in range(NT):
        b = i % 2
        kb.dma("sp", lambda e, i=i, b=b: e.dma_start(out=xt3[b][:], in_=xin[i * 128:(i + 1) * 128, :]), reads=[xin_r[i]], writes=[xt3_r[b]])
        for k, yy in ((0, y1), (1, y2)):
            kb.dma("pool", lambda e, i=i, k=k, b=b, yy=yy: e.indirect_dma_start(out=yy[b][:], out_offset=None, in_=ybuf[:, :],
                                                                              in_offset=bass.IndirectOffsetOnAxis(ap=slots[:, i, k:k + 1], axis=0),
                                                                              bounds_check=bc_reg, oob_is_err=False),
                   reads=[sg_r], writes=[y_r[b]])
        kb.op("act", lambda e, b=b: e.activation(z3[b][:], xt3[b][:], AF.Copy, scale=ALPHA), reads=[xt3_r[b]], writes=[z3_r[b]])
        kb.op("dve", lambda e, b=b, i=i: e.scalar_tensor_tensor(z3[b][:], y1[b][:], gates[:, i, 0:1], z3[b][:], op0=ALU.mult, op1=ALU.add), reads=[y_r[b], sg_r], writes=[z3_r[b]])
        kb.op("dve", lambda e, b=b, i=i: e.scalar_tensor_tensor(z3[b][:], y2[b][:], gates[:, i, 1:2], z3[b][:], op0=ALU.mult, op1=ALU.add), reads=[y_r[b], sg_r], writes=[z3_r[b]])
        ln_tile(kb, LS, z3[b], z3_r[b], x2[b][:], x2_r[b], g_bc, b_bc, lnp_r)
        kb.dma("sp", lambda e, i=i, b=b: e.dma_start(out=dst[i * 128:(i + 1) * 128, :], in_=x2[b][:]), reads=[x2_r[b]], writes=[dst_r[i]])
    kb_pop(kb)
    kb_pop(kb)


C0 = math.exp(-0.5)


def rwkv_phase(kb, C, T, prm, j, li, xin, xin_r, xa, xa_r, ST, nc):
    NT = T // 128
    NSUP = T // 256
    if "ARd" not in ST:
        ST["ARd"] = nc.dram_tensor("ARd_s", [NT, 128, 8 * 2 * 128], BF16, kind="Internal").ap()
        ST["BKd"] = nc.dram_tensor("BKd_s", [NT, 128, 8 * 2 * 128], BF16, kind="Internal").ap()
        ST["rkd"] = nc.dram_tensor("rkd_s", [NT, 128, 8 * 128], BF16, kind="Internal").ap()
        ST["Pcd"] = nc.dram_tensor("Pcd_s", [NT, 128, 8], F32, kind="Internal").ap()
        ST["Vd"] = nc.dram_tensor("Vd_s", [T, D], BF16, kind="Internal").ap()
        ST["Gd"] = nc.dram_tensor("Gd_s", [T, D], BF16, kind="Internal").ap()
        ST["vfirst"] = nc.dram_tensor("vfirst_s", [T, D], F32, kind="Internal").ap()
    ARd, BKd, rkd, Pcd, Vd, Gd, vfd = (ST[k] for k in ("ARd", "BKd", "rkd", "Pcd", "Vd", "Gd", "vfirst"))

    kb_push(kb)
    ps = [kb.ps("r1_ps%d" % b, [128, 512], F32) for b in range(8)]
    ps_r = RL(8)
    wrkv = kb.sb("r1_wrkv", [128, 3, 8, 1024], BF16)
    w1 = kb.sb("r1_w1", [128, 8, 64], BF16)
    a1 = kb.sb("r1_a1", [128, 8, 64], BF16)
    g1 = kb.sb("r1_g1", [128, 8, 160], BF16)
    w2 = kb.sb("r1_w2", [64, 1024], BF16)
    a2 = kb.sb("r1_a2", [64, 1024], BF16)
    g2a = kb.sb("r1_g2a", [128, 1024], BF16)
    g2b = kb.sb("r1_g2b", [32, 1024], BF16)
    wr_ = Res()
    for n in range(3):
        src = prm["rw_w_rkv"][j, n].rearrange("(kc p) n -> p kc n", p=128)
        for kc in range(8):
            kb.dma("pool", lambda e, n=n, kc=kc, src=src: e.dma_start(out=wrkv[:, n, kc, :], in_=src[:, kc, :]), writes=[wr_])
    kb.dma("pool", lambda e: e.dma_start(out=w1[:], in_=prm["rw_w1"][j].rearrange("(kc p) n -> p kc n", p=128)), writes=[wr_])
    kb.dma("pool", lambda e: e.dma_start(out=a1[:], in_=prm["rw_a1"][j].rearrange("(kc p) n -> p kc n", p=128)), writes=[wr_])
    kb.dma("pool", lambda e: e.dma_start(out=g1[:], in_=prm["rw_g1"][j].rearrange("(kc p) n -> p kc n", p=128)), writes=[wr_])
    kb.dma("pool", lambda e: e.dma_start(out=w2[:], in_=prm["rw_w2"][j]), writes=[wr_])
    kb.dma("pool", lambda e: e.dma_start(out=a2[:], in_=prm["rw_a2"][j]), writes=[wr_])
    kb.dma("pool", lambda e: e.dma_start(out=g2a[:], in_=prm["rw_g2"][j, 0:128, :]), writes=[wr_])
    kb.dma("pool", lambda e: e.dma_start(out=g2b[:], in_=prm["rw_g2"][j, 128:160, :]), writes=[wr_])
    if j > 0:
        v1 = kb.sb("r1_v1", [128, 8, 32], BF16)
        v2 = kb.sb("r1_v2", [32, 1024], BF16)
        v0b = kb.sb("r1_v0b", [128, 1024], F32)
        kb.dma("pool", lambda e: e.dma_start(out=v1[:], in_=prm["rw_v1"][j - 1].rearrange("(kc p) n -> p kc n", p=128)), writes=[wr_])
        kb.dma("pool", lambda e: e.dma_start(out=v2[:], in_=prm["rw_v2"][j - 1]), writes=[wr_])
        kb.dma("sp", lambda e: e.dma_start(out=v0b[:], in_=bcast_rows(prm["rw_v0"][j - 1:j, :])), writes=[wr_])
    pvin = kb.sb("r1_pvin", [88, 128], F32)
    pvall = kb.sb("r1_pvall", [128, 88], F32)
    oma = kb.sb("r1_oma", [128, 8], F32)
    pv_r = Res()
    kb.dma("sp", lambda e: e.dma_start(out=pvin[0:48, :], in_=prm["rw_mu"][j].rearrange("n (kc p) -> (n kc) p", p=128)), writes=[pv_r])
    for idx, nm in enumerate(("rw_w0", "rw_a0", "rw_k_k", "rw_k_a")):
        kb.dma("sp", lambda e, idx=idx, nm=nm: e.dma_start(out=pvin[48 + 8 * idx:56 + 8 * idx, :], in_=prm[nm][j].rearrange("(oc p) -> oc p", p=128)), writes=[pv_r])
    kb.dma("sp", lambda e: e.dma_start(out=pvin[80:88, :], in_=prm["rw_r_k"][j].rearrange("(oc hh) n -> oc (hh n)", hh=2)), writes=[pv_r])
    kb.op("pe", lambda e: e.transpose(ps[0][:, 0:88], pvin[:], C.ident_f[0:88, 0:88]), reads=[pv_r, C.r], writes=[ps_r[0]])
    kb.op("dve", lambda e: e.tensor_copy(pvall[:], ps[0][:, 0:88]), reads=[ps_r[0]], writes=[pv_r])
    kb.op("dve", lambda e: e.tensor_scalar(oma[:], pvall[:, 72:80], -1.0, 1.0, op0=ALU.mult, op1=ALU.add), reads=[pv_r], writes=[pv_r])

    class _PV:
        def __getitem__(self, key):
            p, idx, oc = key
            if idx == 5:
                return oma[p, oc]
            return pvall[p, (oc.start + 48 + 8 * idx):(oc.stop + 48 + 8 * idx)]

    class _MU:
        def __getitem__(self, key):
            p, n, kc = key
            return pvall[p, (n * 8 + kc.start):(n * 8 + kc.stop)]

    pv = _PV()
    mu = _MU()
    rst = kb.sb("r1_rst", [128, 256], F32)
    kb.op("dve", lambda e: e.memset(rst[:], 1.0), writes=[pv_r])
    kb.op("dve", lambda e: e.memset(rst[:, 0:1], 0.0), writes=[pv_r])
    kb.op("dve", lambda e: e.memset(rst[:, 128:129], 0.0), writes=[pv_r])
    bd64 = kb.sb("r1_bd64", [128, 128], BF16)
    kb.op("dve", lambda e: e.memset(bd64[:], 0.0), writes=[pv_r])
    kb.op("dve", lambda e: e.memset(bd64[0:64, 0:64], 1.0), writes=[pv_r])
    kb.op("dve", lambda e: e.memset(bd64[64:128, 64:128], 1.0), writes=[pv_r])

    xld = [kb.sb("r1_xld%d" % b, [128, 1024], F32) for b in range(2)]
    xld_r = RL(2)
    xTs = [kb.sb("r1_xTs%d" % b, [128, 8, 257], BF16) for b in range(2)]
    xTs_r = RL(2)
    xx = kb.sb("r1_xx", [128, 8, 256], F32)
    xx_r = Res()
    xm = [kb.sb("r1_xm%d" % b, [128, 8, 256], BF16) for b in range(3)]
    xm_r = RL(3)
    AR = kb.sb("r1_AR", [128, 8, 2, 2, 128], BF16)
    BK = kb.sb("r1_BK", [128, 8, 2, 2, 128], BF16)
    rk = kb.sb("r1_rk", [128, 8, 256], BF16)
    Pc = kb.sb("r1_Pc", [128, 2, 8], F32)
    out_r = Res()
    hw = kb.sb("r1_hw", [64, 256], BF16)
    ha = kb.sb("r1_ha", [64, 256], BF16)
    hg1 = kb.sb("r1_hg1", [128, 256], BF16)
    hg2 = kb.sb("r1_hg2", [32, 256], BF16)
    hid_r = Res()
    if j > 0:
        hv = kb.sb("r1_hv", [32, 256], BF16)
    tnames = ("sgw", "cum", "cumx", "pin", "pinv", "pprev", "asig", "kk", "lns", "rn", "kkn", "t1", "k2", "tb")
    tm = {k: kb.sb("r1_t" + k, [128, 256], F32) for k in tnames}
    kk2 = kb.sb("r1_kk2", [128, 256], BF16)
    tm_r = Res()
    vsb = [kb.sb("r1_v%d" % b, [128, 1024], F32) for b in range(2)]
    vsb_r = RL(2)
    vb16 = [kb.sb("r1_vb%d" % b, [128, 1024], BF16) for b in range(2)]
    vb_r = RL(2)
    gsb = [kb.sb("r1_g%d" % b, [128, 1024], BF16) for b in range(2)]
    gsb_r = RL(2)
    if j > 0:
        vfs = [kb.sb("r1_vf%d" % b, [128, 1024], F32) for b in range(2)]
        vfs_r = RL(2)
        vmx = [kb.sb("r1_vm%d" % b, [128, 1024], F32) for b in range(2)]
        vmx_r = RL(2)
    kb.op("dve", lambda e: e.memset(xTs[0][:, :, 0:1], 0.0), writes=[xTs_r[0]])

    def mix(n, buf, xb):
        for kc in range(8):
            kb.op("dve", lambda e, kc=kc: e.scalar_tensor_tensor(xm[buf][:, kc, :], xx[:, kc, :], mu[:, n, kc:kc + 1], xTs[xb][:, kc, 1:257], op0=ALU.mult, op1=ALU.add),
                  reads=[xx_r, pv_r, xTs_r[xb]], writes=[xm_r[buf]])

    for s in range(NSUP):
        xb = s % 2
        if s > 0:
            kb.op("pool", lambda e, xb=xb: e.tensor_copy(xTs[xb][:, :, 0:1], xTs[1 - xb][:, :, 256:257]), reads=[xTs_r[1 - xb]], writes=[xTs_r[xb]])
        for tl in range(2):
            i = s * 2 + tl
            b = i % 2
            kb.dma("sp", lambda e, i=i, b=b: e.dma_start(out=xld[b][:], in_=xin[i * 128:(i + 1) * 128, :]), reads=[xin_r[i]], writes=[xld_r[b]])
            for hf in range(2):
                pb = 6 + hf
                for c4 in range(4):
                    kc = hf * 4 + c4
                    kb.op("pe", lambda e, pb=pb, c4=c4, kc=kc, b=b: e.transpose(ps[pb][:, c4 * 128:(c4 + 1) * 128], xld[b][:, kc * 128:(kc + 1) * 128], C.ident_f[:]),
                          reads=[xld_r[b], C.r], writes=[ps_r[pb]])
                if hf == 0:
                    kb.op("act", lambda e, pb=pb, xb=xb, tl=tl: e.activation(xTs[xb][:, 0:4, 1 + tl * 128:1 + (tl + 1) * 128], ps[pb][:].rearrange("p (c t) -> p c t", c=4), AF.Copy),
                          reads=[ps_r[pb]], writes=[xTs_r[xb]])
                else:
                    kb.op("dve", lambda e, pb=pb, xb=xb, tl=tl: e.tensor_copy(xTs[xb][:, 4:8, 1 + tl * 128:1 + (tl + 1) * 128], ps[pb][:].rearrange("p (c t) -> p c t", c=4)),
                          reads=[ps_r[pb]], writes=[xTs_r[xb]])
        kb.op("dve", lambda e, xb=xb: e.tensor_tensor(xx[:], xTs[xb][:, :, 0:256], xTs[xb][:, :, 1:257], op=ALU.subtract), reads=[xTs_r[xb]], writes=[xx_r])
        mix(3, 0, xb)
        for kc in range(8):
            kb.op("pe", lambda e, kc=kc: e.matmul(ps[5][0:64, 0:256], w1[:, kc, :], xm[0][:, kc, :], start=(kc == 0), stop=(kc == 7)), reads=[wr_, xm_r[0]], writes=[ps_r[5]])
        kb.op("act", lambda e: e.activation(hw[:], ps[5][0:64, 0:256], AF.Tanh), reads=[ps_r[5]], writes=[hid_r])
        mix(4, 1, xb)
        for kc in range(8):
            kb.op("pe", lambda e, kc=kc: e.matmul(ps[5][0:64, 256:512], a1[:, kc, :], xm[1][:, kc, :], start=(kc == 0), stop=(kc == 7)), reads=[wr_, xm_r[1]], writes=[ps_r[5]])
        kb.op("act", lambda e: e.activation(ha[:], ps[5][0:64, 256:512], AF.Copy), reads=[ps_r[5]], writes=[hid_r])
        mix(5, 2, xb)
        for kc in range(8):
            kb.op("pe", lambda e, kc=kc: e.matmul(ps[4][:, 0:256], g1[:, kc, 0:128], xm[2][:, kc, :], start=(kc == 0), stop=(kc == 7)), reads=[wr_, xm_r[2]], writes=[ps_r[4]])
        for kc in range(8):
            kb.op("pe", lambda e, kc=kc: e.matmul(ps[4][0:32, 256:512], g1[:, kc, 128:160], xm[2][:, kc, :], start=(kc == 0), stop=(kc == 7)), reads=[wr_, xm_r[2]], writes=[ps_r[4]])
        kb.op("act", lambda e: e.activation(hg1[:], ps[4][:, 0:256], AF.Sigmoid), reads=[ps_r[4]], writes=[hid_r])
        kb.op("act", lambda e: e.activation(hg2[:], ps[4][0:32, 256:512], AF.Sigmoid), reads=[ps_r[4]], writes=[hid_r])
        for tl in range(2):
            i = s * 2 + tl
            b = i % 2
            for hf in range(2):
                pb = 6 + hf
                kb.op("pe", lambda e, pb=pb, tl=tl, hf=hf: e.matmul(ps[pb][:], hg1[:, tl * 128:(tl + 1) * 128], g2a[:, hf * 512:(hf + 1) * 512], start=True, stop=False), reads=[hid_r, wr_], writes=[ps_r[pb]])
                kb.op("pe", lambda e, pb=pb, tl=tl, hf=hf: e.matmul(ps[pb][:], hg2[:, tl * 128:(tl + 1) * 128], g2b[:, hf * 512:(hf + 1) * 512], start=False, stop=True), reads=[hid_r, wr_], writes=[ps_r[pb]])
                if hf == 0:
                    kb.op("act", lambda e, pb=pb, b=b: e.activation(gsb[b][:, 0:512], ps[pb][:], AF.Copy), reads=[ps_r[pb]], writes=[gsb_r[b]])
                else:
                    kb.op("dve", lambda e, pb=pb, b=b: e.tensor_copy(gsb[b][:, 512:1024], ps[pb][:]), reads=[ps_r[pb]], writes=[gsb_r[b]])
            kb.dma("sp", lambda e, i=i, b=b: e.dma_start(out=Gd[i * 128:(i + 1) * 128, :], in_=gsb[b][:]), reads=[gsb_r[b]])
        mix(2, 0, xb)
        if j > 0:
            for kc in range(8):
                kb.op("pe", lambda e, kc=kc: e.matmul(ps[5][0:32, 0:256], v1[:, kc, :], xm[0][:, kc, :], start=(kc == 0), stop=(kc == 7)), reads=[wr_, xm_r[0]], writes=[ps_r[5]])
            kb.op("act", lambda e: e.activation(hv[:], ps[5][0:32, 0:256], AF.Copy), reads=[ps_r[5]], writes=[hid_r])
        for tl in range(2):
            i = s * 2 + tl
            b = i % 2
            for hf in range(2):
                pb = 6 + hf
                for kc in range(8):
                    kb.op("pe", lambda e, pb=pb, tl=tl, hf=hf, kc=kc: e.matmul(ps[pb][:], xm[0][:, kc, tl * 128:(tl + 1) * 128], wrkv[:, 2, kc, hf * 512:(hf + 1) * 512], start=(kc == 0), stop=(kc == 7)),
                          reads=[xm_r[0], wr_], writes=[ps_r[pb]])
                if hf == 0:
                    kb.op("act", lambda e, pb=pb, b=b: e.activation(vsb[b][:, 0:512], ps[pb][:], AF.Copy), reads=[ps_r[pb]], writes=[vsb_r[b]])
                else:
                    kb.op("dve", lambda e, pb=pb, b=b: e.tensor_copy(vsb[b][:, 512:1024], ps[pb][:]), reads=[ps_r[pb]], writes=[vsb_r[b]])
            if j == 0:
                kb.dma("sp", lambda e, i=i, b=b: e.dma_start(out=vfd[i * 128:(i + 1) * 128, :], in_=vsb[b][:]), reads=[vsb_r[b]])
                kb.op("pool", lambda e, b=b: e.tensor_copy(vb16[b][:], vsb[b][:]), reads=[vsb_r[b]], writes=[vb_r[b]])
            else:
                kb.dma("sp", lambda e, i=i, b=b: e.dma_start(out=vfs[b][:], in_=vfd[i * 128:(i + 1) * 128, :]), writes=[vfs_r[b]])
                for hf in range(2):
                    pb = 6 + hf
                    kb.op("pe", lambda e, pb=pb, tl=tl, hf=hf: e.matmul(ps[pb][:], hv[:, tl * 128:(tl + 1) * 128], v2[:, hf * 512:(hf + 1) * 512], start=True, stop=True), reads=[hid_r, wr_], writes=[ps_r[pb]])
                    kb.op("dve", lambda e, pb=pb, b=b, hf=hf: e.tensor_tensor(vmx[b][:, hf * 512:(hf + 1) * 512], ps[pb][:], v0b[:, hf * 512:(hf + 1) * 512], op=ALU.add), reads=[ps_r[pb], wr_], writes=[vmx_r[b]])
                kb.op("act", lambda e, b=b: e.activation(vmx[b][:], vmx[b][:], AF.Sigmoid), reads=[vmx_r[b]], writes=[vmx_r[b]])
                kb.op("pool", lambda e, b=b: e.tensor_tensor(vfs[b][:], vfs[b][:], vsb[b][:], op=ALU.subtract), reads=[vfs_r[b], vsb_r[b]], writes=[vfs_r[b]])
                kb.op("pool", lambda e, b=b: e.tensor_tensor(vfs[b][:], vfs[b][:], vmx[b][:], op=ALU.mult), reads=[vfs_r[b], vmx_r[b]], writes=[vfs_r[b]])
                kb.op("pool", lambda e, b=b: e.tensor_tensor(vb16[b][:], vfs[b][:], vsb[b][:], op=ALU.add), reads=[vfs_r[b], vsb_r[b]], writes=[vb_r[b]])
            kb.dma("sp", lambda e, i=i, b=b: e.dma_start(out=Vd[i * 128:(i + 1) * 128, :], in_=vb16[b][:]), reads=[vb_r[b]])
        mix(0, 1, xb)
        mix(1, 2, xb)
        for oc in range(8):
            osl = slice(oc * 128, (oc + 1) * 128)
            pr, pk = (oc % 2) * 2, (oc % 2) * 2 + 1
            for kc in range(8):
                kb.op("pe", lambda e, pr=pr, kc=kc, osl=osl: e.matmul(ps[pr][:, 0:256], wrkv[:, 0, kc, osl], xm[1][:, kc, :], start=(kc == 0), stop=(kc == 7)), reads=[wr_, xm_r[1]], writes=[ps_r[pr]])
            for kc in range(8):
                kb.op("pe", lambda e, pk=pk, kc=kc, osl=osl: e.matmul(ps[pk][:, 0:256], wrkv[:, 1, kc, osl], xm[2][:, kc, :], start=(kc == 0), stop=(kc == 7)), reads=[wr_, xm_r[2]], writes=[ps_r[pk]])
            kb.op("pe", lambda e, pr=pr, osl=osl: e.matmul(ps[pr][:, 256:512], w2[:, osl], hw[:], start=True, stop=True), reads=[wr_, hid_r], writes=[ps_r[pr]])
            kb.op("pe", lambda e, pk=pk, osl=osl: e.matmul(ps[pk][:, 256:512], a2[:, osl], ha[:], start=True, stop=True), reads=[wr_, hid_r], writes=[ps_r[pk]])
            r_ps, k_ps, w_ps, a_ps = ps[pr][:, 0:256], ps[pk][:, 0:256], ps[pr][:, 256:512], ps[pk][:, 256:512]
            R2 = [ps_r[pr], ps_r[pk], tm_r, pv_r]

            def o(eng, fn, extra_w=()):
                kb.op(eng, fn, reads=R2, writes=[tm_r] + list(extra_w))

            o("act", lambda e, oc=oc: e.activation(tm["sgw"][:], w_ps, AF.Sigmoid, bias=pv[:, 0, oc:oc + 1]))
            o("act", lambda e, oc=oc: e.activation(tm["asig"][:], a_ps, AF.Sigmoid, bias=pv[:, 1, oc:oc + 1]))
            o("act", lambda e, oc=oc: e.activation(tm["kk"][:], k_ps, AF.Identity, scale=pv[:, 2, oc:oc + 1]))
            o("dve", lambda e: e.tensor_tensor_scan(tm["cum"][:], rst[:], tm["sgw"][:], 0.0, op0=ALU.mult, op1=ALU.add))
            o("pool", lambda e: e.tensor_tensor(kk2[:], tm["kk"][:], tm["kk"][:], op=ALU.mult))
            kb.op("pe", lambda e: e.matmul(ps[5][:, 0:256], bd64[:], kk2[:], start=True, stop=True), reads=[tm_r, pv_r], writes=[ps_r[5]])
            o("pool", lambda e: e.tensor_tensor(tm["cumx"][:], tm["cum"][:], tm["sgw"][:], op=ALU.subtract))
            o("act", lambda e: e.activation(tm["pin"][:], tm["cum"][:], AF.Exp, scale=-C0))
            o("act", lambda e: e.activation(tm["pinv"][:], tm["cum"][:], AF.Exp, scale=C0))
            o("act", lambda e: e.activation(tm["pprev"][:], tm["cumx"][:], AF.Exp, scale=-C0))
            kb.op("dve", lambda e: e.tensor_scalar(tm["lns"][:], ps[5][:, 0:256], 1e-30, None, op0=ALU.add), reads=[ps_r[5]], writes=[tm_r])
            o("act", lambda e: e.activation(tm["lns"][:], tm["lns"][:], AF.Ln))
            o("act", lambda e: e.activation(tm["rn"][:], tm["lns"][:], AF.Exp, scale=-0.5))
            o("pool", lambda e: e.tensor_tensor(tm["kkn"][:], tm["kk"][:], tm["rn"][:], op=ALU.mult))
            o("dve", lambda e, oc=oc: e.tensor_scalar(tm["t1"][:], tm["asig"][:], pv[:, 3, oc:oc + 1], pv[:, 5, oc:oc + 1], op0=ALU.mult, op1=ALU.add))
            o("dve", lambda e: e.tensor_tensor(tm["k2"][:], k_ps, tm["t1"][:], op=ALU.mult))
            for tl_ in range(2):
                o("dve", lambda e, oc=oc, tl_=tl_: e.tensor_copy(Pc[:, tl_, oc:oc + 1], tm["pin"][:, 127 + 128 * tl_:128 + 128 * tl_]), extra_w=[out_r])
            o("dve", lambda e, oc=oc: e.scalar_tensor_tensor(AR[:, oc, :, 0, :], tm["kkn"][:].rearrange("p (a t) -> p a t", a=2), -1.0, tm["pprev"][:].rearrange("p (a t) -> p a t", a=2), op0=ALU.mult, op1=ALU.mult), extra_w=[out_r])
            o("dve", lambda e, oc=oc: e.tensor_tensor(AR[:, oc, :, 1, :], r_ps.rearrange("p (a t) -> p a t", a=2), tm["pin"][:].rearrange("p (a t) -> p a t", a=2), op=ALU.mult), extra_w=[out_r])
            o("pool", lambda e: e.tensor_tensor(tm["tb"][:], tm["kkn"][:], tm["asig"][:], op=ALU.mult))
            o("pool", lambda e, oc=oc: e.tensor_tensor(BK[:, oc, :, 0, :], tm["tb"][:].rearrange("p (a t) -> p a t", a=2), tm["pinv"][:].rearrange("p (a t) -> p a t", a=2), op=ALU.mult), extra_w=[out_r])
            o("pool", lambda e, oc=oc: e.tensor_tensor(BK[:, oc, :, 1, :], tm["k2"][:].rearrange("p (a t) -> p a t", a=2), tm["pinv"][:].rearrange("p (a t) -> p a t", a=2), op=ALU.mult), extra_w=[out_r])
            o("dve", lambda e, oc=oc: e.scalar_tensor_tensor(rk[:, oc, :], r_ps, pv[:, 4, oc:oc + 1], tm["k2"][:], op0=ALU.mult, op1=ALU.mult), extra_w=[out_r])
        for tl in range(2):
            i = s * 2 + tl
            kb.dma("sp", lambda e, i=i, tl=tl: e.dma_start(out=ARd[i].rearrange("p (o c t) -> p o c t", o=8, c=2), in_=AR[:, :, tl, :, :]), reads=[out_r])
            kb.dma("sp", lambda e, i=i, tl=tl: e.dma_start(out=BKd[i].rearrange("p (o c t) -> p o c t", o=8, c=2), in_=BK[:, :, tl, :, :]), reads=[out_r])
            kb.dma("sp", lambda e, i=i, tl=tl: e.dma_start(out=rkd[i].rearrange("p (o t) -> p o t", o=8), in_=rk[:, :, tl * 128:(tl + 1) * 128]), reads=[out_r])
            kb.dma("sp", lambda e, i=i, tl=tl: e.dma_start(out=Pcd[i], in_=Pc[:, tl, :]), reads=[out_r])
    kb_pop(kb)
    if not ST.get("skip_p2"):
        rwkv_pass2(kb, C, T, prm, j, li, xin, xin_r, xa, xa_r, ST, nc)


def rwkv_pass2(kb, C, T, prm, j, li, xin, xin_r, xa, xa_r, ST, nc):
    NT = T // 128
    ARd, BKd, rkd, Pcd, Vd, Gd = (ST[k] for k in ("ARd", "BKd", "rkd", "Pcd", "Vd", "Gd"))
    kb_push(kb)
    pF = [kb.ps("r2_pf%d" % b, [128, 512], F32) for b in range(4)]
    RG = [pF[b][:, 0:256] for b in range(4)]
    RG_r = RL(4)
    rgc = [0]

    def nrg():
        rgc[0] += 1
        return rgc[0] % 4
    pT = kb.ps("r2_pT", [128, 1024], BF16)
    pT_r = Res()
    pY = [kb.ps("r2_pY%d" % b, [128, 512], F32) for b in range(2)]
    pY_r = RL(2)
    pH = kb.ps("r2_pH", [128, 512], F32)
    pH_r = Res()
    wo = kb.sb("r2_wo", [128, 8, 1024], BF16)
    wo_r = Res()
    wsrc = prm["rw_w_o"][j].rearrange("(kc p) n -> p kc n", p=128)
    for kc in range(8):
        kb.dma("pool", lambda e, kc=kc: e.dma_start(out=wo[:, kc, :], in_=wsrc[:, kc, :]), writes=[wo_r])
    cst_r = Res()
    m2 = kb.sb("r2_m2", [128, 2, 128], F32)
    kb.op("dve", lambda e: e.tensor_copy(m2[:, 0, :], C.m_st[:]), reads=[C.r], writes=[cst_r])
    kb.op("dve", lambda e: e.tensor_copy(m2[:, 1, :], C.m_in[:]), reads=[C.r], writes=[cst_r])
    bdm = kb.sb("r2_bdm", [128, 128], F32)
    kb.op("dve", lambda e: e.memset(bdm[:], 0.0), writes=[cst_r])
    kb.op("dve", lambda e: e.memset(bdm[0:64, 0:64], 1.0), writes=[cst_r])
    kb.op("dve", lambda e: e.memset(bdm[64:128, 64:128], 1.0), writes=[cst_r])
    HS = kb.sb("r2_HS", [128, 8, 16], BF16)
    kb.op("dve", lambda e: e.memset(HS[:], 0.0), writes=[cst_r])
    for oc in range(8):
        for hh in range(2):
            kb.op("dve", lambda e, oc=oc, hh=hh: e.memset(HS[hh * 64:(hh + 1) * 64, oc, 2 * oc + hh:2 * oc + hh + 1], 1.0), writes=[cst_r])
    lng = kb.sb("r2_lng", [128, 1024], F32)
    lnb = kb.sb("r2_lnb", [128, 1024], F32)
    kb.dma("sp", lambda e: e.dma_start(out=lng[:], in_=bcast_rows(prm["rw_lnx_g"][j:j + 1, :])), writes=[cst_r])
    kb.dma("sp", lambda e: e.dma_start(out=lnb[:], in_=bcast_rows(prm["rw_lnx_b"][j:j + 1, :])), writes=[cst_r])
    gneps = kb.sb("r2_gneps", [128, 1], F32)
    kb.op("dve", lambda e: e.memset(gneps[:], GN_EPS), writes=[cst_r])
    LS = ln_scratch(kb, "r2_ln", LN_EPS)
    g_bc, b_bc, lnp_r = load_ln_params(kb, "r2_lnp", prm["ln1_g"], prm["ln1_b"], li)

    ARt = [kb.sb("r2_AR%d" % b, [128, 8, 2, 128], BF16) for b in range(2)]
    BKt = [kb.sb("r2_BK%d" % b, [128, 8, 2, 128], BF16) for b in range(2)]
    rkt = [kb.sb("r2_rk%d" % b, [128, 8, 128], BF16) for b in range(2)]
    Pct = [kb.sb("r2_Pc%d" % b, [128, 8], F32) for b in range(2)]
    Vt = [kb.sb("r2_V%d" % b, [128, 1024], BF16) for b in range(2)]
    Gt = [kb.sb("r2_G%d" % b, [128, 1024], BF16) for b in range(2)]
    xt = [kb.sb("r2_xt%d" % b, [128, 1024], F32) for b in range(2)]
    in_r = RL(2)
    Hb = kb.sb("r2_H", [128, 8, 64], BF16)
    Hb_r = RL(8)
    kb.op("dve", lambda e: e.memset(Hb[:], 0.0), writes=Hb_r)
    TK = [kb.sb("r2_TK%d" % u, [128, 3, 128], BF16) for u in range(8)]
    TK_r = RL(8)
    XA = [kb.sb("r2_XA%d" % u, [128, 2, 128], BF16) for u in range(16)]
    KA = [kb.sb("r2_KA%d" % u, [128, 2, 128], BF16) for u in range(16)]
    XAr, KAr = RL(16), RL(16)
    XN = [[kb.sb("r2_XN%d_%d" % (u, q), [128, 2, 128], BF16) for q in range(2)] for u in range(16)]
    XNr = [RL(2) for _ in range(16)]
    PQ = [[kb.sb("r2_PQ%d_%d" % (u, q), [128, 2, 128], BF16) for q in range(2)] for u in range(16)]
    PQr = [RL(2) for _ in range(16)]
    AV = [kb.sb("r2_AV%d" % u, [128, 64], BF16) for u in range(16)]
    AVr = RL(16)
    W12 = [kb.sb("r2_W12%d" % u, [128, 2, 2, 64], BF16) for u in range(8)]
    W12_r = RL(8)
    MT = [kb.sb("r2_MT%d" % u, [128, 128], BF16) for u in range(8)]
    MTf = [kb.sb("r2_MTf%d" % u, [128, 128], F32) for u in range(2)]
    mtf_r = RL(2)
    GS = [kb.sb("r2_GS%d" % u, [128, 64], F32) for u in range(8)]
    QT = [kb.sb("r2_QT%d" % u, [128, 128], BF16) for u in range(8)]
    MT_r, GS_r, QT_r = RL(8), RL(8), RL(8)
    pT_rr = RL(2)
    ysb = kb.sb("r2_y", [128, 1024], F32)
    ysq = kb.sb("r2_ysq", [128, 1024], F32)
    yn = kb.sb("r2_yn", [128, 1024], F32)
    yg = kb.sb("r2_yg", [128, 1024], BF16)
    ygT = kb.sb("r2_ygT", [128, 8, 128], BF16)
    st = kb.sb("r2_st", [128, 8, 16], F32)
    y_r = Res()
    zt = [kb.sb("r2_z%d" % b, [128, 1024], F32) for b in range(2)]
    zt_r = RL(2)
    x1 = [kb.sb("r2_x1%d" % b, [128, 1024], F32) for b in range(2)]
    x1_r = RL(2)

    for i in range(NT):
        b = i % 2
        kb.dma("sp", lambda e, i=i, b=b: e.dma_start(out=ARt[b][:], in_=ARd[i].rearrange("p (o c t) -> p o c t", o=8, c=2)), writes=[in_r[b]])
        kb.dma("sp", lambda e, i=i, b=b: e.dma_start(out=BKt[b][:], in_=BKd[i].rearrange("p (o c t) -> p o c t", o=8, c=2)), writes=[in_r[b]])
        kb.dma("sp", lambda e, i=i, b=b: e.dma_start(out=rkt[b][:], in_=rkd[i].rearrange("p (o t) -> p o t", o=8)), writes=[in_r[b]])
        kb.dma("sp", lambda e, i=i, b=b: e.dma_start(out=Pct[b][:], in_=Pcd[i]), writes=[in_r[b]])
        kb.dma("sp", lambda e, i=i, b=b: e.dma_start(out=Vt[b][:], in_=Vd[i * 128:(i + 1) * 128, :]), writes=[in_r[b]])
        kb.dma("sp", lambda e, i=i, b=b: e.dma_start(out=Gt[b][:], in_=Gd[i * 128:(i + 1) * 128, :]), writes=[in_r[b]])
        kb.dma("sp", lambda e, i=i, b=b: e.dma_start(out=xt[b][:], in_=xin[i * 128:(i + 1) * 128, :]), reads=[xin_r[i]], writes=[in_r[b]])
        ARb, BKb, Vb, Pcb = ARt[b], BKt[b], Vt[b], Pct[b]
        inr = in_r[b]
        for hp in range(8):
            tr = 0
            for n, src in enumerate((ARb[:, hp, 0, :], BKb[:, hp, 0, :], BKb[:, hp, 1, :])):
                kb.op("pe", lambda e, n=n, src=src, tr=tr: e.transpose(pT[:, tr * 384 + n * 128:tr * 384 + (n + 1) * 128], src, C.ident_b[:]), reads=[inr, C.r], writes=[pT_rr[tr]])
            kb.op("act", lambda e, hp=hp, tr=tr: e.activation(TK[hp][:], pT[:, tr * 384:tr * 384 + 384].rearrange("p (c t) -> p c t", c=3), AF.Copy), reads=[pT_rr[tr]], writes=[TK_r[hp]])
        for u in range(16):
            hp, hh = u // 2, u % 2
            psl = slice(64 * hh, 64 * hh + 64)
            rhs_ar = ARb[psl, hp, :, :].rearrange("p c t -> p (c t)")
            for which, dstT, dst_r in ((0, XA, XAr), (1, KA, KAr)):
                g = nrg()
                kb.op("pe", lambda e, g=g, psl=psl, hp=hp, which=which, rhs_ar=rhs_ar: e.matmul(RG[g], BKb[psl, hp, which, :], rhs_ar, start=True, stop=True), reads=[inr], writes=[RG_r[g]])
                kb.op("dve", lambda e, g=g, u=u, dstT=dstT: e.tensor_tensor(dstT[u][:], RG[g].rearrange("p (c t) -> p c t", c=2), m2[:], op=ALU.mult), reads=[RG_r[g], cst_r], writes=[dst_r[u]])
            g = nrg()
            kb.op("pe", lambda e, g=g, psl=psl, hp=hp: e.matmul(RG[g][:, 0:128], ARb[psl, hp, 0, :], BKb[psl, hp, 0, :], start=True, stop=True), reads=[inr], writes=[RG_r[g]])
            kb.op("dve", lambda e, g=g, u=u: e.tensor_tensor(XN[u][0][:, 1, :], RG[g][:, 0:128], C.m_lo[:], op=ALU.mult), reads=[RG_r[g], C.r], writes=[XNr[u][0]])
            kb.op("pool", lambda e, u=u: e.tensor_copy(XN[u][0][:, 0, :], XA[u][:, 0, :]), reads=[XAr[u]], writes=[XNr[u][0]])
        for u in range(16):
            for c in range(2):
                kb.op("pool", lambda e, u=u, c=c: e.tensor_tensor(PQ[u][0][:, c, :], XN[u][0][:, c, :], C.ident_b[:], op=ALU.add), reads=[XNr[u][0], C.r], writes=[PQr[u][0]])
        for k in range(1, 7):
            cur, prv = k % 2, (k - 1) % 2
            for u in range(16):
                g = nrg()
                Xp, Np = XN[u][prv][:, 0, :], XN[u][prv][:, 1, :]
                kb.op("pe", lambda e, g=g, Xp=Xp, Np=Np: e.matmul(RG[g][:, 0:128], Np, Xp, start=True, stop=True), reads=[XNr[u][prv]], writes=[RG_r[g]])
                if k < 6:
                    kb.op("pe", lambda e, g=g, Xp=Xp, Np=Np: e.matmul(RG[g][:, 128:256], Xp, Np, start=True, stop=True), reads=[XNr[u][prv]], writes=[RG_r[g]])
                    kb.op("act", lambda e, g=g, u=u, cur=cur: e.activation(XN[u][cur][:], RG[g].rearrange("p (c t) -> p c t", c=2), AF.Copy), reads=[RG_r[g]], writes=[XNr[u][cur]])
                else:
                    kb.op("act", lambda e, g=g, u=u, cur=cur: e.activation(XN[u][cur][:, 0, :], RG[g][:, 0:128], AF.Copy), reads=[RG_r[g]], writes=[XNr[u][cur]])
            for u in range(16):
                g = nrg()
                Xc = XN[u][cur][:, 0, :]
                Qp = PQ[u][prv][:, 1, :]
                kb.op("pe", lambda e, g=g, Xc=Xc, Qp=Qp: e.matmul(RG[g][:, 0:128], Qp, Xc, start=True, stop=True), reads=[XNr[u][cur], PQr[u][prv]], writes=[RG_r[g]])
                if k < 6:
                    kb.op("pe", lambda e, g=g, Xc=Xc, Qp=Qp: e.matmul(RG[g][:, 128:256], Xc, Qp, start=True, stop=True), reads=[XNr[u][cur], PQr[u][prv]], writes=[RG_r[g]])
                    kb.op("dve", lambda e, g=g, u=u, cur=cur, prv=prv: e.tensor_tensor(PQ[u][cur][:], RG[g].rearrange("p (c t) -> p c t", c=2), PQ[u][prv][:], op=ALU.add), reads=[RG_r[g], PQr[u][prv]], writes=[PQr[u][cur]])
                else:
                    kb.op("dve", lambda e, g=g, u=u, cur=cur, prv=prv: e.tensor_tensor(PQ[u][cur][:, 0, :], RG[g][:, 0:128], PQ[u][prv][:, 0, :], op=ALU.add), reads=[RG_r[g], PQr[u][prv]], writes=[PQr[u][cur]])
        Pfin = 0
        for u in range(16):
            hp, hh = u // 2, u % 2
            hc = slice(hp * 128 + hh * 64, hp * 128 + hh * 64 + 64)
            g = nrg()
            kb.op("pe", lambda e, g=g, u=u, hc=hc: e.matmul(RG[g][:, 0:64], KA[u][:, 0, :], Vb[:, hc], start=True, stop=True), reads=[KAr[u], inr], writes=[RG_r[g]])
            kb.op("act", lambda e, g=g, u=u: e.activation(AV[u][:], RG[g][:, 0:64], AF.Copy), reads=[RG_r[g]], writes=[AVr[u]])
        for u in range(16):
            hp, hh = u // 2, u % 2
            g = nrg()
            kb.op("pe", lambda e, g=g, u=u: e.matmul(RG[g][:, 0:64], PQ[u][Pfin][:, 0, :], AV[u][:], start=True, stop=True), reads=[PQr[u][Pfin], AVr[u]], writes=[RG_r[g]])
            kb.op("pe", lambda e, g=g, u=u, hp=hp, hh=hh: e.matmul(RG[g][:, 64:128], PQ[u][Pfin][:, 0, :], TK[hp][:, 0, hh * 64:(hh + 1) * 64], start=True, stop=True), reads=[PQr[u][Pfin], TK_r[hp]], writes=[RG_r[g]])
            kb.op("act", lambda e, g=g, hp=hp, hh=hh: e.activation(W12[hp][:, :, hh, :], RG[g][:, 0:128].rearrange("p (c t) -> p c t", c=2), AF.Copy), reads=[RG_r[g]], writes=[W12_r[hp]])
        for hp in range(8):
            W1p = W12[hp][:, 0, :, :].rearrange("p h t -> p (h t)")
            W2p = W12[hp][:, 1, :, :].rearrange("p h t -> p (h t)")
            g = nrg()
            mq = hp % 2
            kb.op("pe", lambda e, g=g, W2p=W2p, hp=hp: e.matmul(RG[g][:, 0:128], W2p, TK[hp][:, 1, :], start=True, stop=True), reads=[W12_r[hp], TK_r[hp]], writes=[RG_r[g]])
            kb.op("pe", lambda e, g=g, W1p=W1p, hp=hp: e.matmul(RG[g][:, 128:256], TK[hp][:, 1, :], W1p, start=True, stop=False), reads=[W12_r[hp], TK_r[hp]], writes=[RG_r[g]])
            kb.op("pe", lambda e, g=g, hp=hp: e.matmul(RG[g][:, 128:256], TK[hp][:, 2, :], Vb[:, hp * 128:(hp + 1) * 128], start=False, stop=True), reads=[TK_r[hp], inr], writes=[RG_r[g]])
            kb.op("dve", lambda e, g=g, mq=mq: e.tensor_tensor(MTf[mq][:], RG[g][:, 0:128], bdm[:], op=ALU.mult), reads=[RG_r[g], cst_r], writes=[mtf_r[mq]])
            kb.op("pool", lambda e, hp=hp, mq=mq: e.tensor_tensor(MT[hp][:], MTf[mq][:], C.ident_f[:], op=ALU.add), reads=[mtf_r[mq], C.r], writes=[MT_r[hp]])
            for hh in range(2):
                psl = slice(64 * hh, 64 * hh + 64)
                kb.op("dve", lambda e, g=g, psl=psl, hh=hh, hp=hp: e.tensor_scalar(GS[hp][psl, :], RG[g][psl, 128 + 64 * hh:192 + 64 * hh], Pcb[psl, hp:hp + 1], None, op0=ALU.mult), reads=[RG_r[g], inr], writes=[GS_r[hp]])
            g2 = nrg()
            for hh in range(2):
                u = 2 * hp + hh
                psl = slice(64 * hh, 64 * hh + 64)
                kb.op("pe", lambda e, g2=g2, hh=hh, W2p=W2p, u=u: e.matmul(RG[g2][:, 128 * hh:128 * hh + 128], W2p, XA[u][:, 1, :], start=True, stop=True), reads=[W12_r[hp], XAr[u]], writes=[RG_r[g2]])
            for hh in range(2):
                psl = slice(64 * hh, 64 * hh + 64)
                kb.op("dve", lambda e, g2=g2, psl=psl, hh=hh, hp=hp: e.tensor_tensor(QT[hp][psl, :], RG[g2][psl, 128 * hh:128 * hh + 128], ARb[psl, hp, 1, :], op=ALU.add), reads=[RG_r[g2], inr], writes=[QT_r[hp]])
        for hp in range(8):
            yb_, yc = pY[hp // 4], (hp % 4) * 128
            for hh in range(2):
                u = 2 * hp + hh
                psl = slice(64 * hh, 64 * hh + 64)
                hc = slice(hp * 128 + hh * 64, hp * 128 + hh * 64 + 64)
                yo = yb_[:, yc + 64 * hh:yc + 64 * hh + 64]
                kb.op("pe", lambda e, yo=yo, hh=hh, u=u, hp=hp: e.matmul(yo, XA[u][:, 1, :], W12[hp][:, 0, hh, :], start=True, stop=False), reads=[XAr[u], W12_r[hp]], writes=[pY_r[hp // 4]])
                kb.op("pe", lambda e, yo=yo, u=u, hc=hc: e.matmul(yo, KA[u][:, 1, :], Vb[:, hc], start=False, stop=False), reads=[KAr[u], inr], writes=[pY_r[hp // 4]])
                kb.op("pe", lambda e, yo=yo, psl=psl, hp=hp: e.matmul(yo, QT[hp][psl, :], Hb[psl, hp, :], start=False, stop=True), reads=[QT_r[hp], Hb_r[hp]], writes=[pY_r[hp // 4]])
            g = nrg()
            kb.op("pe", lambda e, g=g, hp=hp: e.matmul(RG[g][:, 0:64], MT[hp][:], Hb[:, hp, :], start=True, stop=True), reads=[MT_r[hp], Hb_r[hp]], writes=[RG_r[g]])
            kb.op("dve", lambda e, g=g, hp=hp: e.scalar_tensor_tensor(Hb[:, hp, :], RG[g][:, 0:64], Pcb[:, hp:hp + 1], GS[hp][:], op0=ALU.mult, op1=ALU.add), reads=[RG_r[g], inr, GS_r[hp]], writes=[Hb_r[hp]])
        kb.op("act", lambda e: e.activation(ysb[:, 0:512], pY[0][:], AF.Copy), reads=[pY_r[0]], writes=[y_r])
        kb.op("dve", lambda e: e.tensor_copy(ysb[:, 512:1024], pY[1][:]), reads=[pY_r[1]], writes=[y_r])
        kb.op("pool", lambda e: e.tensor_tensor(ysq[:], ysb[:], ysb[:], op=ALU.mult), reads=[y_r], writes=[y_r])
        kb.op("dve", lambda e: e.reduce_sum(st[:, 0, :], ysb[:].rearrange("p (h n) -> p h n", h=16), axis=AX.X), reads=[y_r], writes=[y_r])
        kb.op("dve", lambda e: e.reduce_sum(st[:, 1, :], ysq[:].rearrange("p (h n) -> p h n", h=16), axis=AX.X), reads=[y_r], writes=[y_r])
        kb.op("dve", lambda e: e.tensor_scalar(st[:, 2, :], st[:, 0, :], 1.0 / 64, None, op0=ALU.mult), reads=[y_r], writes=[y_r])
        kb.op("dve", lambda e: e.tensor_tensor(st[:, 3, :], st[:, 2, :], st[:, 2, :], op=ALU.mult), reads=[y_r], writes=[y_r])
        kb.op("dve", lambda e: e.scalar_tensor_tensor(st[:, 4, :], st[:, 1, :], 1.0 / 64, st[:, 3, :], op0=ALU.mult, op1=ALU.subtract), reads=[y_r], writes=[y_r])
        kb.op("act", lambda e: e.activation(st[:, 5, :], st[:, 4, :], AF.Sqrt, bias=gneps[:, 0:1]), reads=[y_r, cst_r], writes=[y_r])
        kb.op("dve", lambda e: e.reciprocal(st[:, 6, :], st[:, 5, :]), reads=[y_r], writes=[y_r])
        for hd in range(16):
            eng = "dve" if hd % 2 == 0 else "pool"
            kb.op(eng, lambda e, hd=hd: e.tensor_scalar(yn[:, hd * 64:(hd + 1) * 64], ysb[:, hd * 64:(hd + 1) * 64], st[:, 2, hd:hd + 1], st[:, 6, hd:hd + 1], op0=ALU.subtract, op1=ALU.mult), reads=[y_r], writes=[y_r])
        kb.op("pool", lambda e: e.tensor_tensor(yn[:], yn[:], lng[:], op=ALU.mult), reads=[y_r, cst_r], writes=[y_r])
        kb.op("pool", lambda e: e.tensor_tensor(yn[:], yn[:], lnb[:], op=ALU.add), reads=[y_r, cst_r], writes=[y_r])
        for oc in range(8):
            kb.op("pe", lambda e, oc=oc: e.matmul(pH[:, 0:16], rkt[b][:, oc, :], HS[:, oc, :], start=(oc == 0), stop=(oc == 7)), reads=[in_r[b], cst_r], writes=[pH_r])
        kb.op("act", lambda e: e.activation(st[:, 7, :], pH[:, 0:16], AF.Copy), reads=[pH_r], writes=[y_r])
        for hd in range(16):
            kb.op("dve", lambda e, hd=hd: e.scalar_tensor_tensor(yn[:, hd * 64:(hd + 1) * 64], Vt[b][:, hd * 64:(hd + 1) * 64], st[:, 7, hd:hd + 1], yn[:, hd * 64:(hd + 1) * 64], op0=ALU.mult, op1=ALU.add), reads=[y_r, in_r[b]], writes=[y_r])
        kb.op("pool", lambda e: e.tensor_tensor(yg[:], yn[:], Gt[b][:], op=ALU.mult), reads=[y_r, in_r[b]], writes=[y_r])
        for hf in range(2):
            for c4 in range(4):
                kc = hf * 4 + c4
                kb.op("pe", lambda e, c4=c4, kc=kc: e.transpose(pT[:, 512 + c4 * 128:512 + (c4 + 1) * 128], yg[:, kc * 128:(kc + 1) * 128], C.ident_b[:]), reads=[y_r, C.r], writes=[pT_rr[0], pT_rr[1]])
            kb.op("act", lambda e, hf=hf: e.activation(ygT[:, hf * 4:hf * 4 + 4, :], pT[:, 512:1024].rearrange("p (c t) -> p c t", c=4), AF.Copy), reads=[pT_rr[0], pT_rr[1]], writes=[y_r])
        for hf in range(2):
            for kc in range(8):
                kb.op("pe", lambda e, kc=kc, hf=hf: e.matmul(pH[:], ygT[:, kc, :], wo[:, kc, hf * 512:(hf + 1) * 512], start=(kc == 0), stop=(kc == 7)), reads=[y_r, wo_r], writes=[pH_r])
            kb.op("dve", lambda e, hf=hf, b=b: e.scalar_tensor_tensor(zt[b][:, hf * 512:(hf + 1) * 512], xt[b][:, hf * 512:(hf + 1) * 512], ALPHA, pH[:], op0=ALU.mult, op1=ALU.add),
                  reads=[in_r[b], pH_r], writes=[zt_r[b]])
        ln_tile(kb, LS, zt[b], zt_r[b], x1[b][:], x1_r[b], g_bc, b_bc, lnp_r)
        kb.dma("sp", lambda e, i=i, b=b: e.dma_start(out=xa[i * 128:(i + 1) * 128, :], in_=x1[b][:]), reads=[x1_r[b]], writes=[xa_r[i]])
    kb_pop(kb)


T_FULL = 4096
N_CORES = 8
_CACHE = {}


def kernel(**inputs):
    if "prog" not in _CACHE:
        _CACHE["prog"] = build(T_FULL, FULL_PLAN, cap=512)
    nc, names, _ = _CACHE["prog"]
    x = np.asarray(inputs["x"], dtype=np.float32)
    in_maps = []
    for c in range(N_CORES):
        m = {"x": np.ascontiguousarray(x[c])}
        for n in names:
            m[n] = np.ascontiguousarray(np.asarray(inputs[n], dtype=np.float32))
        in_maps.append(m)
    res = run_bass_kernel_spmd(nc, in_maps, core_ids=list(range(N_CORES)))
    return np.stack([np.asarray(res.results[c]["out"]) for c in range(N_CORES)], axis=0).astype(np.float32)
```

```python
import math
from contextlib import ExitStack
import numpy as np
import concourse.bass as bass
import concourse.mybir as mybir
from concourse.bass_utils import run_bass_kernel_spmd

F32 = mybir.dt.float32
BF16 = mybir.dt.bfloat16
I32 = mybir.dt.int32
AF = mybir.ActivationFunctionType
ALU = mybir.AluOpType
AX = mybir.AxisListType

D = 1024
DEPTH = 4
NH_A = 8
NE = 32
NG = 4
EPG = 8
HID = 512
ALPHA = (2 * DEPTH) ** 0.25
LN_EPS = 1e-5
RMS_EPS = 1e-5
GN_EPS = 64e-5


class Res:
    __slots__ = ("w", "r")

    def __init__(self):
        self.w = None
        self.r = {}


def RL(n):
    return [Res() for _ in range(n)]


class KB:
    EPOCH = 30000

    def __init__(self, nc, es):
        self.nc = nc
        self.es = es
        self.engs = {"pe": nc.tensor, "dve": nc.vector, "act": nc.scalar, "pool": nc.gpsimd, "sp": nc.sync}
        self.sems = {}
        self.cur = {}
        self.seen = {e: {} for e in self.engs}
        self.rings = {}
        self.ridx = {}
        self.nins = 0
        self.rec = None
        for q, n in (("sp", 16), ("pool", 12), ("act", 6)):
            self.rings[q] = [[self._new_sem("d%s%d" % (q, i)), 0] for i in range(n)]
            self.ridx[q] = 0

    def _new_sem(self, name):
        s = self.es.enter_context(self.nc.semaphore(name))
        self.sems[name] = s
        return name

    def sb(self, name, shape, dt):
        return self.es.enter_context(self.nc.sbuf_tensor(name, list(shape), dt))

    def ps(self, name, shape, dt=F32):
        return self.es.enter_context(self.nc.psum_tensor(name, list(shape), dt))

    def _deps(self, reads, writes):
        deps = {}
        for r in reads:
            if r.w:
                for k, v in r.w.items():
                    if deps.get(k, 0) < v:
                        deps[k] = v
        for w in writes:
            if w.w:
                for k, v in w.w.items():
                    if deps.get(k, 0) < v:
                        deps[k] = v
            for k, v in w.r.items():
                if deps.get(k, 0) < v:
                    deps[k] = v
        return deps

    def _waits(self, eng, deps):
        E = self.engs[eng]
        seen = self.seen[eng]
        for k, v in deps.items():
            if eng == "pe" and k.startswith("epe"):
                continue
            if seen.get(k, 0) >= v:
                continue
            E.wait_ge(self.sems[k], v)
            seen[k] = v
            if self.rec is not None:
                self.rec.append((eng, "w", k, v))

    def _mark(self, tok, reads, writes):
        (k, v), = tok.items()
        for w in writes:
            if w.w is None:
                w.w = dict(tok)
            else:
                w.w = dict(w.w)
                w.w[k] = v
            w.r = {}
        for r in reads:
            if r.r.get(k, 0) < v:
                r.r[k] = v

    def op(self, eng, fn, reads=(), writes=()):
        self._waits(eng, self._deps(reads, writes))
        c = self.cur.get(eng)
        if c is None or c[1] >= self.EPOCH:
            n = len([k for k in self.sems if k.startswith("e" + eng)])
            c = [self._new_sem("e%s%d" % (eng, n)), 0]
            self.cur[eng] = c
        ins = fn(self.engs[eng])
        c[1] += 1
        ins.then_inc(self.sems[c[0]], 1)
        self.nins += 1
        if self.rec is not None:
            self.rec.append((eng, "i", c[0], 1))
        self._mark({c[0]: c[1]}, reads, writes)

    def dma(self, q, fn, reads=(), writes=()):
        ring = self.rings[q]
        slot = ring[self.ridx[q] % len(ring)]
        self.ridx[q] += 1
        deps = self._deps(reads, writes)
        if slot[1] > 0:
            deps[slot[0]] = max(deps.get(slot[0], 0), slot[1] * 16)
        self._waits(q, deps)
        ins = fn(self.engs[q])
        slot[1] += 1
        ins.then_inc(self.sems[slot[0]], 16)
        self.nins += 1
        if self.rec is not None:
            self.rec.append((q, "i", slot[0], 16))
        self._mark({slot[0]: slot[1] * 16}, reads, writes)

    def finish(self):
        deps = {}
        for q, ring in self.rings.items():
            for name, cnt in ring:
                if cnt:
                    deps[name] = cnt * 16
        for name in self.sems:
            if name.startswith("e"):
                pass
        for e, c in self.cur.items():
            deps[c[0]] = c[1]
        self._waits("sp", deps)


class Consts:
    pass


def make_consts(kb):
    nc = kb.nc
    C = Consts()
    C.r = Res()
    it = kb.sb("c_iota", [128, 512], I32)
    itf = kb.sb("c_iotaf", [128, 512], F32)
    C.ident_f = kb.sb("c_identf", [128, 128], F32)
    C.ident_b = kb.sb("c_identb", [128, 128], BF16)
    C.ones_b = kb.sb("c_onesb", [128, 128], BF16)
    C.triu_b = kb.sb("c_triub", [128, 128], BF16)
    C.cmask = kb.sb("c_cmask", [128, 4, 512], BF16)
    C.m_st = kb.sb("c_mst", [128, 128], F32)
    C.m_in = kb.sb("c_min", [128, 128], F32)
    C.m_lo = kb.sb("c_mlo", [128, 128], F32)
    kb.op("pool", lambda e: e.iota(it[:], [[1, 512]], base=0, channel_multiplier=-1), writes=[C.r])
    kb.op("dve", lambda e: e.tensor_copy(itf[:], it[:]), reads=[C.r], writes=[C.r])
    kb.op("dve", lambda e: e.tensor_scalar(C.ident_f[:], itf[:, 0:128], 0.0, None, op0=ALU.is_equal), reads=[C.r], writes=[C.r])
    kb.op("dve", lambda e: e.tensor_copy(C.ident_b[:], C.ident_f[:]), reads=[C.r], writes=[C.r])
    kb.op("dve", lambda e: e.memset(C.ones_b[:], 1.0), writes=[C.r])
    kb.op("dve", lambda e: e.tensor_scalar(C.triu_b[:], itf[:, 0:128], 0.0, None, op0=ALU.is_ge), reads=[C.r], writes=[C.r])
    for o in range(4):
        kb.op("dve", lambda e, o=o: e.tensor_scalar(C.cmask[:, o, :], itf[:], float(128 * o), None, op0=ALU.is_ge), reads=[C.r], writes=[C.r])
    kb.op("dve", lambda e: e.tensor_scalar(C.m_st[:], itf[:, 0:128], 1.0, None, op0=ALU.is_ge), reads=[C.r], writes=[C.r])
    kb.op("dve", lambda e: e.tensor_scalar(C.m_in[:], itf[:, 0:128], 0.0, None, op0=ALU.is_ge), reads=[C.r], writes=[C.r])
    kb.op("dve", lambda e: e.tensor_scalar(C.m_lo[:], itf[:, 0:128], -1.0, None, op0=ALU.is_le), reads=[C.r], writes=[C.r])
    C.itf = itf
    return C


def kb_push(kb):
    kb._stack = getattr(kb, "_stack", [])
    kb._stack.append(kb.es_t)
    kb.es_t = kb.es_root.enter_context(ExitStack()) if False else ExitStack()
    kb.es_t.__enter__()


def kb_pop(kb):
    kb.barrier()
    kb.es_t.__exit__(None, None, None)
    kb.es_t = kb._stack.pop()


def _kb_sb(self, name, shape, dt):
    self.nalloc = getattr(self, "nalloc", 0) + 1
    return self.es_t.enter_context(self.nc.sbuf_tensor("%s_%d" % (name, self.nalloc), list(shape), dt))


def _kb_ps(self, name, shape, dt=F32):
    self.nalloc = getattr(self, "nalloc", 0) + 1
    return self.es_t.enter_context(self.nc.psum_tensor("%s_%d" % (name, self.nalloc), list(shape), dt))


def _kb_barrier(self):
    deps = {}
    for q, ring in self.rings.items():
        for name, cnt in ring:
            if cnt:
                deps[name] = cnt * 16
    for name in self.sems:
        if name.startswith("e"):
            eng = [e for e in self.engs if name.startswith("e" + e)][0]
            c = self.cur[eng]
            if c[0] == name:
                deps[name] = c[1]
    for eng in self.engs:
        self._waits(eng, dict(deps))


KB.sb = _kb_sb
KB.ps = _kb_ps
KB.barrier = _kb_barrier


def bcast_rows(ap_row, n=128):
    return ap_row.partition_broadcast(n)


def ln_tile(kb, S, z, z_r, out, out_r, g_bc, b_bc, par_r):
    st, mv, sd, rs, xn = S["st"], S["mv"], S["sd"], S["rs"], S["xn"]
    r = S["r"]
    kb.op("dve", lambda e: e.bn_stats(st[:, 0, :], z[:, 0:512]), reads=[z_r], writes=[r])
    kb.op("dve", lambda e: e.bn_stats(st[:, 1, :], z[:, 512:1024]), reads=[z_r], writes=[r])
    kb.op("dve", lambda e: e.bn_aggr(mv[:, 0:2], st[:].rearrange("p a b -> p (a b)")), reads=[r], writes=[r])
    kb.op("act", lambda e: e.activation(sd[:, 0:1], mv[:, 1:2], AF.Sqrt, bias=S["eps"][:, 0:1]), reads=[r], writes=[r])
    kb.op("dve", lambda e: e.reciprocal(rs[:, 0:1], sd[:, 0:1]), reads=[r], writes=[r])
    kb.op("dve", lambda e: e.tensor_scalar(xn[:], z[:], mv[:, 0:1], rs[:, 0:1], op0=ALU.subtract, op1=ALU.mult), reads=[r, z_r], writes=[S["xn_r"]])
    kb.op("pool", lambda e: e.tensor_tensor(xn[:], xn[:], g_bc[:], op=ALU.mult), reads=[S["xn_r"], par_r], writes=[S["xn_r"]])
    kb.op("pool", lambda e: e.tensor_tensor(out, xn[:], b_bc[:], op=ALU.add), reads=[S["xn_r"], par_r], writes=[out_r])


def ln_scratch(kb, pfx, eps):
    S = {}
    S["st"] = kb.sb(pfx + "st", [128, 2, 6], F32)
    S["mv"] = kb.sb(pfx + "mv", [128, 2], F32)
    S["sd"] = kb.sb(pfx + "sd", [128, 1], F32)
    S["rs"] = kb.sb(pfx + "rs", [128, 1], F32)
    S["xn"] = kb.sb(pfx + "xn", [128, 1024], F32)
    S["eps"] = kb.sb(pfx + "eps", [128, 1], F32)
    S["r"] = Res()
    S["xn_r"] = Res()
    kb.op("dve", lambda e: e.memset(S["eps"][:], eps), writes=[S["r"]])
    return S


def load_ln_params(kb, pfx, g_dram, b_dram, li):
    g = kb.sb(pfx + "g", [128, 1024], F32)
    b = kb.sb(pfx + "b", [128, 1024], F32)
    r = Res()
    kb.dma("sp", lambda e: e.dma_start(out=g[:], in_=bcast_rows(g_dram[li:li + 1, :])), writes=[r])
    kb.dma("sp", lambda e: e.dma_start(out=b[:], in_=bcast_rows(b_dram[li:li + 1, :])), writes=[r])
    return g, b, r


def attn_phase(kb, C, T, prm, j, li, xin, xin_r, xa, xa_r):
    nc = kb.nc
    NT = T // 128
    NS = T // 512
    lambda_init = 0.8 - 0.6 * math.exp(-0.3 * li)
    kb_push(kb)
    OT = kb.sb("a_OT", [128, 8, T], BF16)
    OT_r = [RL(NS) for _ in range(8)]
    ps = [kb.ps("a_ps%d" % b, [128, 512], F32) for b in range(8)]
    ps_r = RL(8)
    kb_push(kb)
    xld = [kb.sb("a_xld%d" % b, [128, 1024], F32) for b in range(2)]
    xld_r = RL(2)
    xT = kb.sb("a_xT", [128, 8, T], BF16)
    xT_r = RL(NT)
    QT = kb.sb("a_QT", [128, T], BF16)
    QT_r = RL(NS)
    KT = kb.sb("a_KT", [128, T], BF16)
    KT_r = RL(NS)
    V = kb.sb("a_V", [128, NT, 128], BF16)
    V_r = RL(NT // 4)
    wh = [kb.sb("a_wh%d" % b, [128, 8, 3, 128], BF16) for b in range(2)]
    wh_r = RL(2)
    pt = [kb.sb("a_pt%d" % b, [128, 512], BF16) for b in range(4)]
    pt_r = RL(4)
    lam = kb.sb("a_lam", [128, 256], F32)
    lsc = kb.sb("a_lsc", [128, 8], F32)
    lam_r = Res()
    s1 = kb.sb("a_s1", [128, 512], F32)
    s2 = kb.sb("a_s2", [128, 512], F32)
    t1 = kb.sb("a_t1", [128, 512], F32)
    t2 = kb.sb("a_t2", [128, 512], F32)
    sq = kb.sb("a_sq", [128, 512], BF16)
    op_ = t1
    s12 = s1
    e2 = s2
    tot = s2
    rstd = s1
    fin_r = Res()
    fin2_r = fin_r

    kb.dma("sp", lambda e: e.dma_start(out=lam[:], in_=bcast_rows(prm["attn_lambda"][j:j + 1].rearrange("o a b -> o (a b)"))), writes=[lam_r])
    kb.dma("sp", lambda e: e.dma_start(out=lsc[:, 4:5], in_=prm["attn_subln_g"][j].rearrange("(p o) -> p o", o=1)), writes=[lam_r])
    kb.op("dve", lambda e: e.tensor_tensor(lam[:, 0:64], lam[:, 0:64], lam[:, 64:128], op=ALU.mult), reads=[lam_r], writes=[lam_r])
    kb.op("dve", lambda e: e.tensor_tensor(lam[:, 128:192], lam[:, 128:192], lam[:, 192:256], op=ALU.mult), reads=[lam_r], writes=[lam_r])
    kb.op("dve", lambda e: e.reduce_sum(lsc[:, 0:1], lam[:, 0:64], axis=AX.X), reads=[lam_r], writes=[lam_r])
    kb.op("dve", lambda e: e.reduce_sum(lsc[:, 1:2], lam[:, 128:192], axis=AX.X), reads=[lam_r], writes=[lam_r])
    kb.op("act", lambda e: e.activation(lsc[:, 2:4], lsc[:, 0:2], AF.Exp), reads=[lam_r], writes=[lam_r])
    kb.op("dve", lambda e: e.tensor_tensor(lsc[:, 5:6], lsc[:, 3:4], lsc[:, 2:3], op=ALU.subtract), reads=[lam_r], writes=[lam_r])
    kb.op("dve", lambda e: e.tensor_scalar(lsc[:, 5:6], lsc[:, 5:6], -lambda_init, None, op0=ALU.add), reads=[lam_r], writes=[lam_r])
    kb.op("dve", lambda e: e.tensor_scalar(lsc[:, 6:7], lsc[:, 4:5], 1.0 - lambda_init, None, op0=ALU.mult), reads=[lam_r], writes=[lam_r])
    nlam = lsc[:, 5:6]
    gsc = lsc[:, 6:7]

    for i in range(NT):
        b = i % 2
        kb.dma("sp", lambda e, i=i, b=b: e.dma_start(out=xld[b][:], in_=xin[i * 128:(i + 1) * 128, :]), reads=[xin_r[i]], writes=[xld_r[b]])
        for hf in range(2):
            pb = (2 * i + hf) % 8
            for c4 in range(4):
                kc = hf * 4 + c4
                kb.op("pe", lambda e, pb=pb, c4=c4, kc=kc, b=b: e.transpose(ps[pb][:, c4 * 128:(c4 + 1) * 128], xld[b][:, kc * 128:(kc + 1) * 128], C.ident_f[:]),
                      reads=[xld_r[b], C.r], writes=[ps_r[pb]])
            eng = "act" if hf == 0 else "dve"
            if eng == "act":
                kb.op("act", lambda e, pb=pb, hf=hf, i=i: e.activation(xT[:, hf * 4:hf * 4 + 4, i * 128:(i + 1) * 128], ps[pb][:].rearrange("p (c t) -> p c t", c=4), AF.Copy),
                      reads=[ps_r[pb]], writes=[xT_r[i]])
            else:
                kb.op("dve", lambda e, pb=pb, hf=hf, i=i: e.tensor_copy(xT[:, hf * 4:hf * 4 + 4, i * 128:(i + 1) * 128], ps[pb][:].rearrange("p (c t) -> p c t", c=4)),
                      reads=[ps_r[pb]], writes=[xT_r[i]])


    wq = prm["attn_w_qkv"][j].rearrange("(kc p) n -> p kc n", p=128)
    pcnt = [0]

    def nps():
        pcnt[0] += 1
        return pcnt[0] % 2

    for h in range(NH_A):
        wb = h % 2
        for part in range(3):
            kb.dma("pool", lambda e, wb=wb, part=part, h=h: e.dma_start(out=wh[wb][:, :, part, :], in_=wq[:, :, part * 1024 + h * 128: part * 1024 + (h + 1) * 128]),
                   writes=[wh_r[wb]])
        for s in range(NS):
            for part, dst, dst_r, scl in ((0, QT, QT_r, 0.125), (1, KT, KT_r, 1.0)):
                pb = nps()
                for kc in range(8):
                    kb.op("pe", lambda e, pb=pb, wb=wb, kc=kc, part=part, s=s: e.matmul(ps[pb][:], wh[wb][:, kc, part, :], xT[:, kc, s * 512:(s + 1) * 512], start=(kc == 0), stop=(kc == 7)),
                          reads=[wh_r[wb]] + xT_r[s * 4:s * 4 + 4], writes=[ps_r[pb]])
                kb.op("act", lambda e, pb=pb, dst=dst, s=s, scl=scl: e.activation(dst[:, s * 512:(s + 1) * 512], ps[pb][:], AF.Copy, scale=scl),
                      reads=[ps_r[pb]], writes=[dst_r[s]])
        for g4 in range(NT // 4):
            pb = nps()
            for t4 in range(4):
                i = g4 * 4 + t4
                for kc in range(8):
                    kb.op("pe", lambda e, pb=pb, wb=wb, kc=kc, i=i, t4=t4: e.matmul(ps[pb][:, t4 * 128:(t4 + 1) * 128], xT[:, kc, i * 128:(i + 1) * 128], wh[wb][:, kc, 2, :], start=(kc == 0), stop=(kc == 7)),
                          reads=[wh_r[wb], xT_r[i]], writes=[ps_r[pb]])
            kb.op("dve", lambda e, pb=pb, g4=g4: e.tensor_copy(V[:, g4 * 4:g4 * 4 + 4, :], ps[pb][:].rearrange("p (c t) -> p c t", c=4)),
                  reads=[ps_r[pb]], writes=[V_r[g4]])
        pti = 0
        scnt = [0]
        for qs in range(NS):
            nkt = qs * 4 + 4
            items = [(kt, c) for kt in range(nkt) for c in range(2)]
            sbank = {}
            LOOK = 2

            def emit_s(n):
                kt, c = items[n]
                pb = (0, 1, 7)[scnt[0] % 3]
                scnt[0] += 1
                sbank[n] = pb
                kb.op("pe", lambda e, pb=pb, c=c, kt=kt, qs=qs: e.matmul(ps[pb][:], KT[c * 64:(c + 1) * 64, kt * 128:(kt + 1) * 128], QT[c * 64:(c + 1) * 64, qs * 512:(qs + 1) * 512], start=True, stop=True),
                      reads=[KT_r[kt // 4], QT_r[qs]], writes=[ps_r[pb]])

            for n in range(min(LOOK, len(items))):
                emit_s(n)
            for n, (kt, c) in enumerate(items):
                if n + LOOK < len(items):
                    emit_s(n + LOOK)
                pb = sbank[n]
                pi = pti % 4
                pti += 1
                kb.op("act", lambda e, pb=pb, pi=pi: e.activation(pt[pi][:], ps[pb][:], AF.Exp), reads=[ps_r[pb]], writes=[pt_r[pi]])
                if kt >= qs * 4:
                    o = kt - qs * 4
                    kb.op("pool", lambda e, pi=pi, o=o: e.tensor_tensor(pt[pi][:], pt[pi][:], C.cmask[:, o, :], op=ALU.mult), reads=[pt_r[pi], C.r], writes=[pt_r[pi]])
                kb.op("pe", lambda e, c=c, kt=kt, pi=pi, nkt=nkt: e.matmul(ps[2 + c][:], V[:, kt, :], pt[pi][:], start=(kt == 0), stop=(kt == nkt - 1)),
                      reads=[V_r[kt // 4], pt_r[pi]], writes=[ps_r[2 + c]])
                kb.op("pe", lambda e, c=c, kt=kt, pi=pi, nkt=nkt: e.matmul(ps[4 + c][:], C.ones_b[:], pt[pi][:], start=(kt == 0), stop=(kt == nkt - 1)),
                      reads=[C.r, pt_r[pi]], writes=[ps_r[4 + c]])
            kb.op("act", lambda e: e.activation(s1[:], ps[4][:], AF.Copy), reads=[ps_r[4]], writes=[fin_r])
            kb.op("act", lambda e: e.activation(s2[:], ps[5][:], AF.Copy), reads=[ps_r[5]], writes=[fin_r])
            kb.op("dve", lambda e: e.tensor_tensor(t1[:], ps[2][:], s2[:], op=ALU.mult), reads=[ps_r[2], fin_r], writes=[fin2_r])
            kb.op("dve", lambda e: e.tensor_tensor(t2[:], ps[3][:], s1[:], op=ALU.mult), reads=[ps_r[3], fin_r], writes=[fin2_r])
            kb.op("dve", lambda e: e.scalar_tensor_tensor(op_[:], t2[:], nlam, t1[:], op0=ALU.mult, op1=ALU.add), reads=[fin2_r, lam_r], writes=[fin2_r])
            kb.op("pool", lambda e: e.tensor_tensor(sq[:], op_[:], op_[:], op=ALU.mult), reads=[fin2_r], writes=[fin2_r])
            kb.op("pool", lambda e: e.tensor_tensor(s12[:], s1[:], s2[:], op=ALU.mult), reads=[fin_r], writes=[fin2_r])
            kb.op("dve", lambda e: e.scalar_tensor_tensor(e2[:], s12[:], RMS_EPS, s12[:], op0=ALU.mult, op1=ALU.mult), reads=[fin2_r], writes=[fin2_r])
            kb.op("pe", lambda e: e.matmul(ps[6][:], C.ones_b[:], sq[:], start=True, stop=True), reads=[C.r, fin2_r], writes=[ps_r[6]])
            kb.op("dve", lambda e: e.scalar_tensor_tensor(tot[:], ps[6][:], 1.0 / 128.0, e2[:], op0=ALU.mult, op1=ALU.add), reads=[ps_r[6], fin2_r], writes=[fin2_r])
            kb.op("act", lambda e: e.activation(tot[:], tot[:], AF.Ln), reads=[fin2_r], writes=[fin2_r])
            kb.op("act", lambda e: e.activation(rstd[:], tot[:], AF.Exp, scale=-0.5), reads=[fin2_r], writes=[fin2_r])
            kb.op("dve", lambda e, h=h, qs=qs: e.scalar_tensor_tensor(OT[:, h, qs * 512:(qs + 1) * 512], op_[:], gsc, rstd[:], op0=ALU.mult, op1=ALU.mult),
                  reads=[fin2_r, lam_r], writes=[OT_r[h][qs]])

    kb_pop(kb)
    wo = kb.sb("a_wo", [128, 8, 1024], BF16)
    wo_r = Res()
    wo_src = prm["attn_w_o"][j].rearrange("(h p) n -> p h n", p=128)
    for hh in range(8):
        kb.dma("pool", lambda e, hh=hh: e.dma_start(out=wo[:, hh, :], in_=wo_src[:, hh, :]), writes=[wo_r])
    xld = [kb.sb("a_xldc%d" % b, [128, 1024], F32) for b in range(2)]
    xld_r = RL(2)
    zt = [kb.sb("a_z%d" % b, [128, 1024], F32) for b in range(2)]
    zt_r = RL(2)
    x1 = [kb.sb("a_x1%d" % b, [128, 1024], F32) for b in range(2)]
    x1_r = RL(2)
    LS = ln_scratch(kb, "a_ln", LN_EPS)
    g_bc, b_bc, lnp_r = load_ln_params(kb, "a_lnp", prm["ln1_g"], prm["ln1_b"], li)
    for i in range(NT):
        b = i % 2
        kb.dma("sp", lambda e, i=i, b=b: e.dma_start(out=xld[b][:], in_=xin[i * 128:(i + 1) * 128, :]), reads=[xin_r[i]], writes=[xld_r[b]])
        for hf in range(2):
            pb = 2 * b + hf
            for hh in range(8):
                kb.op("pe", lambda e, pb=pb, hh=hh, hf=hf, i=i: e.matmul(ps[pb][:], OT[:, hh, i * 128:(i + 1) * 128], wo[:, hh, hf * 512:(hf + 1) * 512], start=(hh == 0), stop=(hh == 7)),
                      reads=[OT_r[hh][i // 4], wo_r], writes=[ps_r[pb]])
            kb.op("dve", lambda e, pb=pb, hf=hf, b=b: e.scalar_tensor_tensor(zt[b][:, hf * 512:(hf + 1) * 512], xld[b][:, hf * 512:(hf + 1) * 512], ALPHA, ps[pb][:], op0=ALU.mult, op1=ALU.add),
                  reads=[xld_r[b], ps_r[pb]], writes=[zt_r[b]])
        ln_tile(kb, LS, zt[b], zt_r[b], x1[b][:], x1_r[b], g_bc, b_bc, lnp_r)
        kb.dma("sp", lambda e, i=i, b=b: e.dma_start(out=xa[i * 128:(i + 1) * 128, :], in_=x1[b][:]), reads=[x1_r[b]], writes=[xa_r[i]])
    kb_pop(kb)


PSHAPES = {
    "ln1_g": (4, 1024), "ln1_b": (4, 1024), "ln2_g": (4, 1024), "ln2_b": (4, 1024),
    "attn_w_qkv": (2, 1024, 3072), "attn_w_o": (2, 1024, 1024), "attn_lambda": (2, 4, 64), "attn_subln_g": (2, 128),
    "rw_mu": (2, 6, 1024), "rw_w_rkv": (2, 3, 1024, 1024), "rw_w_o": (2, 1024, 1024), "rw_w0": (2, 1024),
    "rw_w1": (2, 1024, 64), "rw_w2": (2, 64, 1024), "rw_a0": (2, 1024), "rw_a1": (2, 1024, 64), "rw_a2": (2, 64, 1024),
    "rw_g1": (2, 1024, 160), "rw_g2": (2, 160, 1024), "rw_k_k": (2, 1024), "rw_k_a": (2, 1024), "rw_r_k": (2, 16, 64),
    "rw_lnx_g": (2, 1024), "rw_lnx_b": (2, 1024), "rw_v0": (1, 1024), "rw_v1": (1, 1024, 32), "rw_v2": (1, 32, 1024),
    "moe_rg_w": (4, 1024, 4), "moe_rg_b": (4, 4), "moe_re_w": (4, 1024, 32), "moe_re_b": (4, 32),
    "moe_w_gu": (4, 32, 1024, 1024), "moe_w_down": (4, 32, 512, 1024),
}


class Params(dict):
    def __init__(self, nc):
        super().__init__()
        self.nc = nc

    def __missing__(self, k):
        ap = self.nc.dram_tensor(k, list(PSHAPES[k]), F32, kind="ExternalInput").ap()
        self[k] = ap
        return ap


def build(T, plan, cap=512):
    nc = bass.Bass("TRN2", target_bir_lowering=False)
    prm = Params(nc)
    x = nc.dram_tensor("x", [T, D], F32, kind="ExternalInput").ap()
    out = nc.dram_tensor("out", [T, D], F32, kind="ExternalOutput").ap()
    xa = nc.dram_tensor("xa_s", [T, D], F32, kind="Internal").ap()
    xb = nc.dram_tensor("xb_s", [T, D], F32, kind="Internal").ap()
    NT = T // 128
    es = ExitStack()
    with es:
        kb = KB(nc, es)
        kb.es_t = es
        C = make_consts(kb)
        cur, cur_r = x, RL(NT)
        xa_r, xb_r, out_r = RL(NT), RL(NT), RL(NT)
        ST = dict(DEBUG_ST)
        for n, step in enumerate(plan):
            last = n == len(plan) - 1
            kind = step[0]
            if kind == "attn":
                attn_phase(kb, C, T, prm, step[1], step[2], cur, cur_r, xa, xa_r)
                cur, cur_r = xa, xa_r
            elif kind == "rwkv":
                rwkv_phase(kb, C, T, prm, step[1], step[2], cur, cur_r, xa, xa_r, ST, nc)
                cur, cur_r = xa, xa_r
            elif kind == "moe":
                dst, dst_r = (out, out_r) if last else (xb, xb_r)
                moe_phase(kb, C, T, prm, step[1], cur, cur_r, dst, dst_r, cap, nc, ST)
                cur, cur_r = dst, dst_r
        if cur is not out:
            for i in range(NT):
                kb.dma("sp", lambda e, i=i: e.dma_start(out=out[i * 128:(i + 1) * 128, :], in_=cur[i * 128:(i + 1) * 128, :]), reads=[cur_r[i]], writes=[out_r[i]])
        kb.finish()
        ninst = kb.nins
    return nc, list(prm.keys()), ninst


DEBUG_ST = {}
FULL_PLAN = [("attn", 0, 0), ("moe", 0), ("rwkv", 0, 1), ("moe", 1), ("attn", 1, 2), ("moe", 2), ("rwkv", 1, 3), ("moe", 3)]


def moe_phase(kb, C, T, prm, li, xin, xin_r, dst, dst_r, cap, nc, ST):
    NT = T // 128
    NSLOT = NE * cap
    NB = cap // 128
    if "xbuf" not in ST:
        ST["xbuf"] = nc.dram_tensor("xbuf_s", [NSLOT, D], BF16, kind="Internal").ap()
        ST["ybuf"] = nc.dram_tensor("ybuf_s", [NSLOT, D], F32, kind="Internal").ap()
        ST["bc_reg"] = nc.gpsimd.to_reg(NSLOT - 1)
    xbuf, ybuf = ST["xbuf"], ST["ybuf"]
    bc_reg = ST["bc_reg"]
    kb_push(kb)
    slots = kb.sb("m_slots", [128, NT, 2], I32)
    gates = kb.sb("m_gates", [128, NT, 2], F32)
    sg_r = Res()

    kb_push(kb)
    ps = [kb.ps("m1_ps%d" % b, [128, 512], F32) for b in range(6)]
    ps_r = RL(6)
    xt = [kb.sb("m1_xt%d" % b, [128, 1024], F32) for b in range(2)]
    xt_r = RL(2)
    xb16 = [kb.sb("m1_xb%d" % b, [128, 1024], BF16) for b in range(2)]
    xb_r = RL(2)
    xT = [kb.sb("m1_xT%d" % b, [128, 8, 128], F32) for b in range(2)]
    xT_r = RL(2)
    wr = kb.sb("m1_wr", [128, 8, 36], F32)
    rb = kb.sb("m1_rb", [128, 36], F32)
    offs_i = kb.sb("m1_offi", [128, 32], I32)
    offs = kb.sb("m1_off", [128, 32], F32)
    base = kb.sb("m1_base", [128, 32], F32)
    cr = Res()
    base_r = Res()
    kb.dma("sp", lambda e: e.dma_start(out=wr[:, :, 0:4], in_=prm["moe_rg_w"][li].rearrange("(kc p) n -> p kc n", p=128)), writes=[cr])
    kb.dma("sp", lambda e: e.dma_start(out=wr[:, :, 4:36], in_=prm["moe_re_w"][li].rearrange("(kc p) n -> p kc n", p=128)), writes=[cr])
    kb.dma("sp", lambda e: e.dma_start(out=rb[:, 0:4], in_=bcast_rows(prm["moe_rg_b"][li:li + 1, :])), writes=[cr])
    kb.dma("sp", lambda e: e.dma_start(out=rb[:, 4:36], in_=bcast_rows(prm["moe_re_b"][li:li + 1, :])), writes=[cr])
    kb.op("pool", lambda e: e.iota(offs_i[:], [[cap, 32]], base=-1, channel_multiplier=0), writes=[cr])
    kb.op("dve", lambda e: e.tensor_copy(offs[:], offs_i[:]), reads=[cr], writes=[cr])
    kb.op("dve", lambda e: e.memset(base[:], 0.0), writes=[base_r])
    Wk = {}
    for nm, w in (("L", 36), ("ohg", 4), ("eg", 4), ("lsel", 8), ("oh1", 8), ("lsel2", 8), ("oh2", 8), ("E1", 32), ("E2", 32),
                  ("val", 32), ("valid", 32), ("val2", 32), ("tmp", 32), ("sc", 16)):
        Wk[nm] = kb.sb("m1_w" + nm, [128, w], F32)
    G01 = kb.sb("m1_G01", [128, 32], BF16)
    wr_ = Res()
    BIG = float(NSLOT)
    for i in range(NT):
        b = i % 2
        kb.dma("sp", lambda e, i=i, b=b: e.dma_start(out=xt[b][:], in_=xin[i * 128:(i + 1) * 128, :]), reads=[xin_r[i]], writes=[xt_r[b]])
        kb.op("act", lambda e, b=b: e.activation(xb16[b][:], xt[b][:], AF.Copy), reads=[xt_r[b]], writes=[xb_r[b]])
        for hf in range(2):
            pb = hf
            for c4 in range(4):
                kc = hf * 4 + c4
                kb.op("pe", lambda e, pb=pb, c4=c4, kc=kc, b=b: e.transpose(ps[pb][:, c4 * 128:(c4 + 1) * 128], xt[b][:, kc * 128:(kc + 1) * 128], C.ident_f[:]),
                      reads=[xt_r[b], C.r], writes=[ps_r[pb]])
            if hf == 0:
                kb.op("act", lambda e, pb=pb, b=b: e.activation(xT[b][:, 0:4, :], ps[pb][:].rearrange("p (c t) -> p c t", c=4), AF.Copy), reads=[ps_r[pb]], writes=[xT_r[b]])
            else:
                kb.op("dve", lambda e, pb=pb, b=b: e.tensor_copy(xT[b][:, 4:8, :], ps[pb][:].rearrange("p (c t) -> p c t", c=4)), reads=[ps_r[pb]], writes=[xT_r[b]])
        for kc in range(8):
            kb.op("pe", lambda e, kc=kc, b=b: e.matmul(ps[2][:, 0:36], xT[b][:, kc, :], wr[:, kc, :], start=(kc == 0), stop=(kc == 7)), reads=[xT_r[b], cr], writes=[ps_r[2]])
        L, ohg, eg, lsel, oh1, lsel2, oh2, E1, E2 = (Wk[k] for k in ("L", "ohg", "eg", "lsel", "oh1", "lsel2", "oh2", "E1", "E2"))
        val, valid, val2, tmp, sc = (Wk[k] for k in ("val", "valid", "val2", "tmp", "sc"))

        def dv(fn, extra_r=(), extra_w=()):
            kb.op("dve", fn, reads=[wr_] + list(extra_r), writes=[wr_] + list(extra_w))

        dv(lambda e: e.tensor_tensor(L[:], ps[2][:, 0:36], rb[:], op=ALU.add), extra_r=[ps_r[2], cr])
        dv(lambda e: e.reduce_max(sc[:, 0:1], L[:, 0:4], axis=AX.X))
        dv(lambda e: e.tensor_scalar(ohg[:], L[:, 0:4], sc[:, 0:1], None, op0=ALU.is_equal))
        dv(lambda e: e.tensor_scalar(sc[:, 1:2], sc[:, 0:1], -1.0, None, op0=ALU.mult))
        kb.op("act", lambda e: e.activation(eg[:], L[:, 0:4], AF.Exp, bias=sc[:, 1:2]), reads=[wr_], writes=[wr_])
        dv(lambda e: e.reduce_sum(sc[:, 2:3], eg[:], axis=AX.X))
        dv(lambda e: e.reciprocal(sc[:, 3:4], sc[:, 2:3]))
        dv(lambda e: e.tensor_scalar(lsel[:], L[:, 4:12], ohg[:, 0:1], None, op0=ALU.mult))
        for g in range(1, 4):
            dv(lambda e, g=g: e.scalar_tensor_tensor(lsel[:], L[:, 4 + 8 * g:12 + 8 * g], ohg[:, g:g + 1], lsel[:], op0=ALU.mult, op1=ALU.add))
        dv(lambda e: e.reduce_max(sc[:, 4:5], lsel[:], axis=AX.X))
        dv(lambda e: e.tensor_scalar(oh1[:], lsel[:], sc[:, 4:5], None, op0=ALU.is_equal))
        dv(lambda e: e.scalar_tensor_tensor(lsel2[:], oh1[:], -1e30, lsel[:], op0=ALU.mult, op1=ALU.add))
        dv(lambda e: e.reduce_max(sc[:, 5:6], lsel2[:], axis=AX.X))
        dv(lambda e: e.tensor_scalar(oh2[:], lsel2[:], sc[:, 5:6], None, op0=ALU.is_equal))
        dv(lambda e: e.tensor_tensor(sc[:, 6:7], sc[:, 5:6], sc[:, 4:5], op=ALU.subtract))
        kb.op("act", lambda e: e.activation(sc[:, 7:8], sc[:, 6:7], AF.Exp), reads=[wr_], writes=[wr_])
        dv(lambda e: e.tensor_scalar(sc[:, 8:9], sc[:, 7:8], 1.0, None, op0=ALU.add))
        dv(lambda e: e.reciprocal(sc[:, 9:10], sc[:, 8:9]))
        dv(lambda e: e.tensor_tensor(sc[:, 10:11], sc[:, 7:8], sc[:, 9:10], op=ALU.mult))
        for g in range(4):
            dv(lambda e, g=g: e.tensor_scalar(E1[:, 8 * g:8 * g + 8], oh1[:], ohg[:, g:g + 1], None, op0=ALU.mult))
            dv(lambda e, g=g: e.tensor_scalar(E2[:, 8 * g:8 * g + 8], oh2[:], ohg[:, g:g + 1], None, op0=ALU.mult))
        dv(lambda e: e.tensor_tensor(G01[:], E1[:], E2[:], op=ALU.add))
        kb.op("pe", lambda e: e.matmul(ps[3][:, 0:32], C.triu_b[:], G01[:], start=True, stop=True), reads=[wr_, C.r], writes=[ps_r[3]])
        kb.op("pe", lambda e: e.matmul(ps[3][:, 32:64], C.ones_b[:], G01[:], start=True, stop=True), reads=[wr_, C.r], writes=[ps_r[3]])
        dv(lambda e: e.tensor_tensor(val[:], ps[3][:, 0:32], base[:], op=ALU.add), extra_r=[ps_r[3], base_r])
        dv(lambda e: e.tensor_scalar(valid[:], val[:], float(cap), None, op0=ALU.is_le))
        dv(lambda e: e.tensor_tensor(val2[:], val[:], offs[:], op=ALU.add), extra_r=[cr])
        dv(lambda e: e.scalar_tensor_tensor(val2[:], val2[:], -BIG, valid[:], op0=ALU.add, op1=ALU.mult))
        dv(lambda e: e.tensor_scalar(val2[:], val2[:], BIG, None, op0=ALU.add))
        dv(lambda e: e.tensor_tensor(tmp[:], E1[:], val2[:], op=ALU.mult))
        dv(lambda e: e.reduce_sum(sc[:, 11:12], tmp[:], axis=AX.X))
        dv(lambda e: e.tensor_tensor(tmp[:], E2[:], val2[:], op=ALU.mult))
        dv(lambda e: e.reduce_sum(sc[:, 12:13], tmp[:], axis=AX.X))
        dv(lambda e: e.tensor_tensor(tmp[:], E1[:], valid[:], op=ALU.mult))
        dv(lambda e: e.reduce_sum(sc[:, 13:14], tmp[:], axis=AX.X))
        dv(lambda e: e.tensor_tensor(tmp[:], E2[:], valid[:], op=ALU.mult))
        dv(lambda e: e.reduce_sum(sc[:, 14:15], tmp[:], axis=AX.X))
        dv(lambda e: e.tensor_tensor(base[:], base[:], ps[3][:, 32:64], op=ALU.add), extra_r=[ps_r[3]], extra_w=[base_r])
        dv(lambda e, i=i: e.tensor_copy(slots[:, i, :], sc[:, 11:13]), extra_w=[sg_r])
        dv(lambda e: e.tensor_scalar(sc[:, 9:11], sc[:, 9:11], sc[:, 3:4], None, op0=ALU.mult))
        dv(lambda e, i=i: e.tensor_tensor(gates[:, i, :], sc[:, 9:11], sc[:, 13:15], op=ALU.mult), extra_w=[sg_r])
        for k in range(2):
            kb.dma("pool", lambda e, i=i, k=k, b=b: e.indirect_dma_start(out=xbuf[:, :], out_offset=bass.IndirectOffsetOnAxis(ap=slots[:, i, k:k + 1], axis=0),
                                                                       in_=xb16[b][:], in_offset=None, bounds_check=bc_reg, oob_is_err=False),
                   reads=[sg_r, xb_r[b]])
    kb_pop(kb)

    kb_push(kb)
    psT = [kb.ps("m2_pT%d" % b, [128, 1024], BF16) for b in range(2)]
    psT_r = RL(2)
    psH = [kb.ps("m2_pH%d" % b, [128, 512], F32) for b in range(4)]
    psH_r = RL(4)
    psY = [kb.ps("m2_pY%d" % b, [128, 512], F32) for b in range(2)]
    psY_r = RL(2)
    wgu = [kb.sb("m2_wgu%d" % b, [128, 8, 1024], BF16) for b in range(2)]
    wgu_r = RL(2)
    wd = [kb.sb("m2_wd%d" % b, [128, 4, 1024], BF16) for b in range(2)]
    wd_r = RL(2)
    xblk = [kb.sb("m2_xb%d" % b, [128, 1024], BF16) for b in range(2)]
    xblk_r = RL(2)
    XT = [kb.sb("m2_XT%d" % b, [128, 8, cap], BF16) for b in range(2)]
    XT_r = RL(2)
    sil = [kb.sb("m2_sil%d" % b, [128, cap], F32) for b in range(2)]
    sil_r = RL(2)
    AT = [kb.sb("m2_AT%d" % b, [128, 4, cap], BF16) for b in range(2)]
    AT_r = RL(2)
    ysb = [kb.sb("m2_y%d" % b, [128, 1024], F32) for b in range(2)]
    ysb_r = RL(2)
    nblk = 0
    for ex in range(NE):
        wb = ex % 2
        gsrc = prm["moe_w_gu"][li, ex].rearrange("(kc p) n -> p kc n", p=128)
        dsrc = prm["moe_w_down"][li, ex].rearrange("(m p) n -> p m n", p=128)
        for kc in range(8):
            kb.dma("pool", lambda e, wb=wb, kc=kc, gsrc=gsrc: e.dma_start(out=wgu[wb][:, kc, :], in_=gsrc[:, kc, :]), writes=[wgu_r[wb]])
        for m in range(4):
            kb.dma("pool", lambda e, wb=wb, m=m, dsrc=dsrc: e.dma_start(out=wd[wb][:, m, :], in_=dsrc[:, m, :]), writes=[wd_r[wb]])
        for blk in range(NB):
            bb = nblk % 2
            nblk += 1
            r0 = ex * cap + blk * 128
            kb.dma("sp", lambda e, bb=bb, r0=r0: e.dma_start(out=xblk[bb][:], in_=xbuf[r0:r0 + 128, :]), writes=[xblk_r[bb]])
            for kc in range(8):
                kb.op("pe", lambda e, bb=bb, kc=kc: e.transpose(psT[bb][:, kc * 128:(kc + 1) * 128], xblk[bb][:, kc * 128:(kc + 1) * 128], C.ident_b[:]),
                      reads=[xblk_r[bb], C.r], writes=[psT_r[bb]])
            eng = "act" if blk % 2 == 0 else "dve"
            if eng == "act":
                kb.op("act", lambda e, bb=bb, wb=wb, blk=blk: e.activation(XT[wb][:, :, blk * 128:(blk + 1) * 128], psT[bb][:].rearrange("p (c t) -> p c t", c=8), AF.Copy),
                      reads=[psT_r[bb]], writes=[XT_r[wb]])
            else:
                kb.op("dve", lambda e, bb=bb, wb=wb, blk=blk: e.tensor_copy(XT[wb][:, :, blk * 128:(blk + 1) * 128], psT[bb][:].rearrange("p (c t) -> p c t", c=8)),
                      reads=[psT_r[bb]], writes=[XT_r[wb]])
        for m in range(4):
            pg, pu = (m % 2) * 2, (m % 2) * 2 + 1
            for (pb, col) in ((pg, m * 128), (pu, 512 + m * 128)):
                for kc in range(8):
                    kb.op("pe", lambda e, pb=pb, col=col, kc=kc, wb=wb: e.matmul(psH[pb][:, 0:cap], wgu[wb][:, kc, col:col + 128], XT[wb][:, kc, :], start=(kc == 0), stop=(kc == 7)),
                          reads=[wgu_r[wb], XT_r[wb]], writes=[psH_r[pb]])
            sb_ = m % 2
            kb.op("act", lambda e, pg=pg, sb_=sb_: e.activation(sil[sb_][:], psH[pg][:, 0:cap], AF.Silu), reads=[psH_r[pg]], writes=[sil_r[sb_]])
            kb.op("dve", lambda e, pu=pu, sb_=sb_, m=m, wb=wb: e.tensor_tensor(AT[wb][:, m, :], sil[sb_][:], psH[pu][:, 0:cap], op=ALU.mult),
                  reads=[psH_r[pu], sil_r[sb_]], writes=[AT_r[wb]])
        for blk in range(NB):
            yb = blk % 2
            for hf in range(2):
                for m in range(4):
                    kb.op("pe", lambda e, hf=hf, m=m, wb=wb, blk=blk: e.matmul(psY[hf][:], AT[wb][:, m, blk * 128:(blk + 1) * 128], wd[wb][:, m, hf * 512:(hf + 1) * 512], start=(m == 0), stop=(m == 3)),
                          reads=[AT_r[wb], wd_r[wb]], writes=[psY_r[hf]])
                if hf == 0:
                    kb.op("act", lambda e, yb=yb: e.activation(ysb[yb][:, 0:512], psY[0][:], AF.Copy), reads=[psY_r[0]], writes=[ysb_r[yb]])
                else:
                    kb.op("dve", lambda e, yb=yb: e.tensor_copy(ysb[yb][:, 512:1024], psY[1][:]), reads=[psY_r[1]], writes=[ysb_r[yb]])
            r0 = ex * cap + blk * 128
            kb.dma("sp", lambda e, yb=yb, r0=r0: e.dma_start(out=ybuf[r0:r0 + 128, :], in_=ysb[yb][:]), reads=[ysb_r[yb]])
    kb_pop(kb)

    kb_push(kb)
    y1 = [kb.sb("m3_y1%d" % b, [128, 1024], F32) for b in range(2)]
    y2 = [kb.sb("m3_y2%d" % b, [128, 1024], F32) for b in range(2)]
    y_r = RL(2)
    xt3 = [kb.sb("m3_xt%d" % b, [128, 1024], F32) for b in range(2)]
    xt3_r = RL(2)
    z3 = [kb.sb("m3_z%d" % b, [128, 1024], F32) for b in range(2)]
    z3_r = RL(2)
    x2 = [kb.sb("m3_x2%d" % b, [128, 1024], F32) for b in range(2)]
    x2_r = RL(2)
    LS = ln_scratch(kb, "m3_ln", LN_EPS)
    g_bc, b_bc, lnp_r = load_ln_params(kb, "m3_lnp", prm["ln2_g"], prm["ln2_b"], li)
    for b in range(2):
        kb.op("pool", lambda e, b=b: e.memset(y1[b][:], 0.0), writes=[y_r[b]])
        kb.op("pool", lambda e, b=b: e.memset(y2[b][:], 0.0), writes=[y_r[b]])
    for i in range(NT):
        b = i % 2
        kb.dma("sp", lambda e, i=i, b=b: e.dma_start(out=xt3[b][:], in_=xin[i * 128:(i + 1) * 128, :]), reads=[xin_r[i]], writes=[xt3_r[b]])
        for k, yy in ((0, y1), (1, y2)):
            kb.dma("pool", lambda e, i=i, k=k, b=b, yy=yy: e.indirect_dma_start(out=yy[b][:], out_offset=None, in_=ybuf[:, :],
                                                                              in_offset=bass.IndirectOffsetOnAxis(ap=slots[:, i, k:k + 1], axis=0),
                                                                              bounds_check=bc_reg, oob_is_err=False),
                   reads=[sg_r], writes=[y_r[b]])
        kb.op("act", lambda e, b=b: e.activation(z3[b][:], xt3[b][:], AF.Copy, scale=ALPHA), reads=[xt3_r[b]], writes=[z3_r[b]])
        kb.op("dve", lambda e, b=b, i=i: e.scalar_tensor_tensor(z3[b][:], y1[b][:], gates[:, i, 0:1], z3[b][:], op0=ALU.mult, op1=ALU.add), reads=[y_r[b], sg_r], writes=[z3_r[b]])
        kb.op("dve", lambda e, b=b, i=i: e.scalar_tensor_tensor(z3[b][:], y2[b][:], gates[:, i, 1:2], z3[b][:], op0=ALU.mult, op1=ALU.add), reads=[y_r[b], sg_r], writes=[z3_r[b]])
        ln_tile(kb, LS, z3[b], z3_r[b], x2[b][:], x2_r[b], g_bc, b_bc, lnp_r)
        kb.dma("sp", lambda e, i=i, b=b: e.dma_start(out=dst[i * 128:(i + 1) * 128, :], in_=x2[b][:]), reads=[x2_r[b]], writes=[dst_r[i]])
    kb_pop(kb)
    kb_pop(kb)


C0 = math.exp(-0.5)


def rwkv_phase(kb, C, T, prm, j, li, xin, xin_r, xa, xa_r, ST, nc):
    NT = T // 128
    NSUP = T // 256
    if "ARd" not in ST:
        ST["ARd"] = nc.dram_tensor("ARd_s", [NT, 128, 8 * 2 * 128], BF16, kind="Internal").ap()
        ST["BKd"] = nc.dram_tensor("BKd_s", [NT, 128, 8 * 2 * 128], BF16, kind="Internal").ap()
        ST["rkd"] = nc.dram_tensor("rkd_s", [NT, 128, 8 * 128], BF16, kind="Internal").ap()
        ST["Pcd"] = nc.dram_tensor("Pcd_s", [NT, 128, 8], F32, kind="Internal").ap()
        ST["Vd"] = nc.dram_tensor("Vd_s", [T, D], BF16, kind="Internal").ap()
        ST["Gd"] = nc.dram_tensor("Gd_s", [T, D], BF16, kind="Internal").ap()
        ST["vfirst"] = nc.dram_tensor("vfirst_s", [T, D], F32, kind="Internal").ap()
    ARd, BKd, rkd, Pcd, Vd, Gd, vfd = (ST[k] for k in ("ARd", "BKd", "rkd", "Pcd", "Vd", "Gd", "vfirst"))

    kb_push(kb)
    ps = [kb.ps("r1_ps%d" % b, [128, 512], F32) for b in range(8)]
    ps_r = RL(8)
    wrkv = kb.sb("r1_wrkv", [128, 3, 8, 1024], BF16)
    w1 = kb.sb("r1_w1", [128, 8, 64], BF16)
    a1 = kb.sb("r1_a1", [128, 8, 64], BF16)
    g1 = kb.sb("r1_g1", [128, 8, 160], BF16)
    w2 = kb.sb("r1_w2", [64, 1024], BF16)
    a2 = kb.sb("r1_a2", [64, 1024], BF16)
    g2a = kb.sb("r1_g2a", [128, 1024], BF16)
    g2b = kb.sb("r1_g2b", [32, 1024], BF16)
    wr_ = Res()
    for n in range(3):
        src = prm["rw_w_rkv"][j, n].rearrange("(kc p) n -> p kc n", p=128)
        for kc in range(8):
            kb.dma("pool", lambda e, n=n, kc=kc, src=src: e.dma_start(out=wrkv[:, n, kc, :], in_=src[:, kc, :]), writes=[wr_])
    kb.dma("pool", lambda e: e.dma_start(out=w1[:], in_=prm["rw_w1"][j].rearrange("(kc p) n -> p kc n", p=128)), writes=[wr_])
    kb.dma("pool", lambda e: e.dma_start(out=a1[:], in_=prm["rw_a1"][j].rearrange("(kc p) n -> p kc n", p=128)), writes=[wr_])
    kb.dma("pool", lambda e: e.dma_start(out=g1[:], in_=prm["rw_g1"][j].rearrange("(kc p) n -> p kc n", p=128)), writes=[wr_])
    kb.dma("pool", lambda e: e.dma_start(out=w2[:], in_=prm["rw_w2"][j]), writes=[wr_])
    kb.dma("pool", lambda e: e.dma_start(out=a2[:], in_=prm["rw_a2"][j]), writes=[wr_])
    kb.dma("pool", lambda e: e.dma_start(out=g2a[:], in_=prm["rw_g2"][j, 0:128, :]), writes=[wr_])
    kb.dma("pool", lambda e: e.dma_start(out=g2b[:], in_=prm["rw_g2"][j, 128:160, :]), writes=[wr_])
    if j > 0:
        v1 = kb.sb("r1_v1", [128, 8, 32], BF16)
        v2 = kb.sb("r1_v2", [32, 1024], BF16)
        v0b = kb.sb("r1_v0b", [128, 1024], F32)
        kb.dma("pool", lambda e: e.dma_start(out=v1[:], in_=prm["rw_v1"][j - 1].rearrange("(kc p) n -> p kc n", p=128)), writes=[wr_])
        kb.dma("pool", lambda e: e.dma_start(out=v2[:], in_=prm["rw_v2"][j - 1]), writes=[wr_])
        kb.dma("sp", lambda e: e.dma_start(out=v0b[:], in_=bcast_rows(prm["rw_v0"][j - 1:j, :])), writes=[wr_])
    pvin = kb.sb("r1_pvin", [88, 128], F32)
    pvall = kb.sb("r1_pvall", [128, 88], F32)
    oma = kb.sb("r1_oma", [128, 8], F32)
    pv_r = Res()
    kb.dma("sp", lambda e: e.dma_start(out=pvin[0:48, :], in_=prm["rw_mu"][j].rearrange("n (kc p) -> (n kc) p", p=128)), writes=[pv_r])
    for idx, nm in enumerate(("rw_w0", "rw_a0", "rw_k_k", "rw_k_a")):
        kb.dma("sp", lambda e, idx=idx, nm=nm: e.dma_start(out=pvin[48 + 8 * idx:56 + 8 * idx, :], in_=prm[nm][j].rearrange("(oc p) -> oc p", p=128)), writes=[pv_r])
    kb.dma("sp", lambda e: e.dma_start(out=pvin[80:88, :], in_=prm["rw_r_k"][j].rearrange("(oc hh) n -> oc (hh n)", hh=2)), writes=[pv_r])
    kb.op("pe", lambda e: e.transpose(ps[0][:, 0:88], pvin[:], C.ident_f[0:88, 0:88]), reads=[pv_r, C.r], writes=[ps_r[0]])
    kb.op("dve", lambda e: e.tensor_copy(pvall[:], ps[0][:, 0:88]), reads=[ps_r[0]], writes=[pv_r])
    kb.op("dve", lambda e: e.tensor_scalar(oma[:], pvall[:, 72:80], -1.0, 1.0, op0=ALU.mult, op1=ALU.add), reads=[pv_r], writes=[pv_r])

    class _PV:
        def __getitem__(self, key):
            p, idx, oc = key
            if idx == 5:
                return oma[p, oc]
            return pvall[p, (oc.start + 48 + 8 * idx):(oc.stop + 48 + 8 * idx)]

    class _MU:
        def __getitem__(self, key):
            p, n, kc = key
            return pvall[p, (n * 8 + kc.start):(n * 8 + kc.stop)]

    pv = _PV()
    mu = _MU()
    rst = kb.sb("r1_rst", [128, 256], F32)
    kb.op("dve", lambda e: e.memset(rst[:], 1.0), writes=[pv_r])
    kb.op("dve", lambda e: e.memset(rst[:, 0:1], 0.0), writes=[pv_r])
    kb.op("dve", lambda e: e.memset(rst[:, 128:129], 0.0), writes=[pv_r])
    bd64 = kb.sb("r1_bd64", [128, 128], BF16)
    kb.op("dve", lambda e: e.memset(bd64[:], 0.0), writes=[pv_r])
    kb.op("dve", lambda e: e.memset(bd64[0:64, 0:64], 1.0), writes=[pv_r])
    kb.op("dve", lambda e: e.memset(bd64[64:128, 64:128], 1.0), writes=[pv_r])

    xld = [kb.sb("r1_xld%d" % b, [128, 1024], F32) for b in range(2)]
    xld_r = RL(2)
    xTs = [kb.sb("r1_xTs%d" % b, [128, 8, 257], BF16) for b in range(2)]
    xTs_r = RL(2)
    xx = kb.sb("r1_xx", [128, 8, 256], F32)
    xx_r = Res()
    xm = [kb.sb("r1_xm%d" % b, [128, 8, 256], BF16) for b in range(3)]
    xm_r = RL(3)
    AR = kb.sb("r1_AR", [128, 8, 2, 2, 128], BF16)
    BK = kb.sb("r1_BK", [128, 8, 2, 2, 128], BF16)
    rk = kb.sb("r1_rk", [128, 8, 256], BF16)
    Pc = kb.sb("r1_Pc", [128, 2, 8], F32)
    out_r = Res()
    hw = kb.sb("r1_hw", [64, 256], BF16)
    ha = kb.sb("r1_ha", [64, 256], BF16)
    hg1 = kb.sb("r1_hg1", [128, 256], BF16)
    hg2 = kb.sb("r1_hg2", [32, 256], BF16)
    hid_r = Res()
    if j > 0:
        hv = kb.sb("r1_hv", [32, 256], BF16)
    tnames = ("sgw", "cum", "cumx", "pin", "pinv", "pprev", "asig", "kk", "lns", "rn", "kkn", "t1", "k2", "tb")
    tmS = [{k: kb.sb("r1_t%d%s" % (q, k), [128, 256], F32) for k in tnames} for q in range(2)]
    kk2S = [kb.sb("r1_kk2_%d" % q, [128, 256], BF16) for q in range(2)]
    tm_rS = RL(2)
    vsb = [kb.sb("r1_v%d" % b, [128, 1024], F32) for b in range(2)]
    vsb_r = RL(2)
    vb16 = [kb.sb("r1_vb%d" % b, [128, 1024], BF16) for b in range(2)]
    vb_r = RL(2)
    gsb = [kb.sb("r1_g%d" % b, [128, 1024], BF16) for b in range(2)]
    gsb_r = RL(2)
    if j > 0:
        vfs = [kb.sb("r1_vf%d" % b, [128, 1024], F32) for b in range(2)]
        vfs_r = RL(2)
        vmx = [kb.sb("r1_vm%d" % b, [128, 1024], F32) for b in range(2)]
        vmx_r = RL(2)
    kb.op("dve", lambda e: e.memset(xTs[0][:, :, 0:1], 0.0), writes=[xTs_r[0]])

    def mix(n, buf, xb):
        for kc in range(8):
            kb.op("dve", lambda e, kc=kc: e.scalar_tensor_tensor(xm[buf][:, kc, :], xx[:, kc, :], mu[:, n, kc:kc + 1], xTs[xb][:, kc, 1:257], op0=ALU.mult, op1=ALU.add),
                  reads=[xx_r, pv_r, xTs_r[xb]], writes=[xm_r[buf]])

    for s in range(NSUP):
        xb = s % 2
        if s > 0:
            kb.op("pool", lambda e, xb=xb: e.tensor_copy(xTs[xb][:, :, 0:1], xTs[1 - xb][:, :, 256:257]), reads=[xTs_r[1 - xb]], writes=[xTs_r[xb]])
        for tl in range(2):
            i = s * 2 + tl
            b = i % 2
            kb.dma("sp", lambda e, i=i, b=b: e.dma_start(out=xld[b][:], in_=xin[i * 128:(i + 1) * 128, :]), reads=[xin_r[i]], writes=[xld_r[b]])
            for hf in range(2):
                pb = 6 + hf
                for c4 in range(4):
                    kc = hf * 4 + c4
                    kb.op("pe", lambda e, pb=pb, c4=c4, kc=kc, b=b: e.transpose(ps[pb][:, c4 * 128:(c4 + 1) * 128], xld[b][:, kc * 128:(kc + 1) * 128], C.ident_f[:]),
                          reads=[xld_r[b], C.r], writes=[ps_r[pb]])
                if hf == 0:
                    kb.op("act", lambda e, pb=pb, xb=xb, tl=tl: e.activation(xTs[xb][:, 0:4, 1 + tl * 128:1 + (tl + 1) * 128], ps[pb][:].rearrange("p (c t) -> p c t", c=4), AF.Copy),
                          reads=[ps_r[pb]], writes=[xTs_r[xb]])
                else:
                    kb.op("dve", lambda e, pb=pb, xb=xb, tl=tl: e.tensor_copy(xTs[xb][:, 4:8, 1 + tl * 128:1 + (tl + 1) * 128], ps[pb][:].rearrange("p (c t) -> p c t", c=4)),
                          reads=[ps_r[pb]], writes=[xTs_r[xb]])
        kb.op("dve", lambda e, xb=xb: e.tensor_tensor(xx[:], xTs[xb][:, :, 0:256], xTs[xb][:, :, 1:257], op=ALU.subtract), reads=[xTs_r[xb]], writes=[xx_r])
        mix(3, 0, xb)
        for kc in range(8):
            kb.op("pe", lambda e, kc=kc: e.matmul(ps[5][0:64, 0:256], w1[:, kc, :], xm[0][:, kc, :], start=(kc == 0), stop=(kc == 7)), reads=[wr_, xm_r[0]], writes=[ps_r[5]])
        kb.op("act", lambda e: e.activation(hw[:], ps[5][0:64, 0:256], AF.Tanh), reads=[ps_r[5]], writes=[hid_r])
        mix(4, 1, xb)
        for kc in range(8):
            kb.op("pe", lambda e, kc=kc: e.matmul(ps[5][0:64, 256:512], a1[:, kc, :], xm[1][:, kc, :], start=(kc == 0), stop=(kc == 7)), reads=[wr_, xm_r[1]], writes=[ps_r[5]])
        kb.op("act", lambda e: e.activation(ha[:], ps[5][0:64, 256:512], AF.Copy), reads=[ps_r[5]], writes=[hid_r])
        mix(5, 2, xb)
        for kc in range(8):
            kb.op("pe", lambda e, kc=kc: e.matmul(ps[4][:, 0:256], g1[:, kc, 0:128], xm[2][:, kc, :], start=(kc == 0), stop=(kc == 7)), reads=[wr_, xm_r[2]], writes=[ps_r[4]])
        for kc in range(8):
            kb.op("pe", lambda e, kc=kc: e.matmul(ps[4][0:32, 256:512], g1[:, kc, 128:160], xm[2][:, kc, :], start=(kc == 0), stop=(kc == 7)), reads=[wr_, xm_r[2]], writes=[ps_r[4]])
        kb.op("act", lambda e: e.activation(hg1[:], ps[4][:, 0:256], AF.Sigmoid), reads=[ps_r[4]], writes=[hid_r])
        kb.op("act", lambda e: e.activation(hg2[:], ps[4][0:32, 256:512], AF.Sigmoid), reads=[ps_r[4]], writes=[hid_r])
        for tl in range(2):
            i = s * 2 + tl
            b = i % 2
            for hf in range(2):
                pb = 6 + hf
                kb.op("pe", lambda e, pb=pb, tl=tl, hf=hf: e.matmul(ps[pb][:], hg1[:, tl * 128:(tl + 1) * 128], g2a[:, hf * 512:(hf + 1) * 512], start=True, stop=False), reads=[hid_r, wr_], writes=[ps_r[pb]])
                kb.op("pe", lambda e, pb=pb, tl=tl, hf=hf: e.matmul(ps[pb][:], hg2[:, tl * 128:(tl + 1) * 128], g2b[:, hf * 512:(hf + 1) * 512], start=False, stop=True), reads=[hid_r, wr_], writes=[ps_r[pb]])
                if hf == 0:
                    kb.op("act", lambda e, pb=pb, b=b: e.activation(gsb[b][:, 0:512], ps[pb][:], AF.Copy), reads=[ps_r[pb]], writes=[gsb_r[b]])
                else:
                    kb.op("dve", lambda e, pb=pb, b=b: e.tensor_copy(gsb[b][:, 512:1024], ps[pb][:]), reads=[ps_r[pb]], writes=[gsb_r[b]])
            kb.dma("sp", lambda e, i=i, b=b: e.dma_start(out=Gd[i * 128:(i + 1) * 128, :], in_=gsb[b][:]), reads=[gsb_r[b]])
        mix(2, 0, xb)
        if j > 0:
            for kc in range(8):
                kb.op("pe", lambda e, kc=kc: e.matmul(ps[5][0:32, 0:256], v1[:, kc, :], xm[0][:, kc, :], start=(kc == 0), stop=(kc == 7)), reads=[wr_, xm_r[0]], writes=[ps_r[5]])
            kb.op("act", lambda e: e.activation(hv[:], ps[5][0:32, 0:256], AF.Copy), reads=[ps_r[5]], writes=[hid_r])
        for tl in range(2):
            i = s * 2 + tl
            b = i % 2
            for hf in range(2):
                pb = 6 + hf
                for kc in range(8):
                    kb.op("pe", lambda e, pb=pb, tl=tl, hf=hf, kc=kc: e.matmul(ps[pb][:], xm[0][:, kc, tl * 128:(tl + 1) * 128], wrkv[:, 2, kc, hf * 512:(hf + 1) * 512], start=(kc == 0), stop=(kc == 7)),
                          reads=[xm_r[0], wr_], writes=[ps_r[pb]])
                if hf == 0:
                    kb.op("act", lambda e, pb=pb, b=b: e.activation(vsb[b][:, 0:512], ps[pb][:], AF.Copy), reads=[ps_r[pb]], writes=[vsb_r[b]])
                else:
                    kb.op("dve", lambda e, pb=pb, b=b: e.tensor_copy(vsb[b][:, 512:1024], ps[pb][:]), reads=[ps_r[pb]], writes=[vsb_r[b]])
            if j == 0:
                kb.dma("sp", lambda e, i=i, b=b: e.dma_start(out=vfd[i * 128:(i + 1) * 128, :], in_=vsb[b][:]), reads=[vsb_r[b]])
                kb.op("pool", lambda e, b=b: e.tensor_copy(vb16[b][:], vsb[b][:]), reads=[vsb_r[b]], writes=[vb_r[b]])
            else:
                kb.dma("sp", lambda e, i=i, b=b: e.dma_start(out=vfs[b][:], in_=vfd[i * 128:(i + 1) * 128, :]), writes=[vfs_r[b]])
                for hf in range(2):
                    pb = 6 + hf
                    kb.op("pe", lambda e, pb=pb, tl=tl, hf=hf: e.matmul(ps[pb][:], hv[:, tl * 128:(tl + 1) * 128], v2[:, hf * 512:(hf + 1) * 512], start=True, stop=True), reads=[hid_r, wr_], writes=[ps_r[pb]])
                    kb.op("dve", lambda e, pb=pb, b=b, hf=hf: e.tensor_tensor(vmx[b][:, hf * 512:(hf + 1) * 512], ps[pb][:], v0b[:, hf * 512:(hf + 1) * 512], op=ALU.add), reads=[ps_r[pb], wr_], writes=[vmx_r[b]])
                kb.op("act", lambda e, b=b: e.activation(vmx[b][:], vmx[b][:], AF.Sigmoid), reads=[vmx_r[b]], writes=[vmx_r[b]])
                kb.op("pool", lambda e, b=b: e.tensor_tensor(vfs[b][:], vfs[b][:], vsb[b][:], op=ALU.subtract), reads=[vfs_r[b], vsb_r[b]], writes=[vfs_r[b]])
                kb.op("pool", lambda e, b=b: e.tensor_tensor(vfs[b][:], vfs[b][:], vmx[b][:], op=ALU.mult), reads=[vfs_r[b], vmx_r[b]], writes=[vfs_r[b]])
                kb.op("pool", lambda e, b=b: e.tensor_tensor(vb16[b][:], vfs[b][:], vsb[b][:], op=ALU.add), reads=[vfs_r[b], vsb_r[b]], writes=[vb_r[b]])
            kb.dma("sp", lambda e, i=i, b=b: e.dma_start(out=Vd[i * 128:(i + 1) * 128, :], in_=vb16[b][:]), reads=[vb_r[b]])
        mix(0, 1, xb)
        mix(1, 2, xb)
        def oc_chain(oc):
            par = oc % 2
            T_ = tmS[par]
            kk2_ = kk2S[par]
            tmr = tm_rS[par]
            psum_kk = ps[5] if par == 0 else ps[4]
            psum_kk_r = ps_r[5] if par == 0 else ps_r[4]
            osl = slice(oc * 128, (oc + 1) * 128)
            pr, pk = par * 2, par * 2 + 1
            for kc in range(8):
                kb.op("pe", lambda e, pr=pr, kc=kc, osl=osl: e.matmul(ps[pr][:, 0:256], wrkv[:, 0, kc, osl], xm[1][:, kc, :], start=(kc == 0), stop=(kc == 7)), reads=[wr_, xm_r[1]], writes=[ps_r[pr]])
            for kc in range(8):
                kb.op("pe", lambda e, pk=pk, kc=kc, osl=osl: e.matmul(ps[pk][:, 0:256], wrkv[:, 1, kc, osl], xm[2][:, kc, :], start=(kc == 0), stop=(kc == 7)), reads=[wr_, xm_r[2]], writes=[ps_r[pk]])
            kb.op("pe", lambda e, pr=pr, osl=osl: e.matmul(ps[pr][:, 256:512], w2[:, osl], hw[:], start=True, stop=True), reads=[wr_, hid_r], writes=[ps_r[pr]])
            kb.op("pe", lambda e, pk=pk, osl=osl: e.matmul(ps[pk][:, 256:512], a2[:, osl], ha[:], start=True, stop=True), reads=[wr_, hid_r], writes=[ps_r[pk]])
            r_ps, k_ps, w_ps, a_ps = ps[pr][:, 0:256], ps[pk][:, 0:256], ps[pr][:, 256:512], ps[pk][:, 256:512]
            R2 = [ps_r[pr], ps_r[pk], tmr, pv_r]
            L = []

            def o(eng, fn, extra_w=()):
                L.append((eng, fn, R2, [tmr] + list(extra_w)))

            o("act", lambda e: e.activation(T_["sgw"][:], w_ps, AF.Sigmoid, bias=pv[:, 0, oc:oc + 1]))
            o("act", lambda e: e.activation(T_["asig"][:], a_ps, AF.Sigmoid, bias=pv[:, 1, oc:oc + 1]))
            o("act", lambda e: e.activation(T_["kk"][:], k_ps, AF.Identity, scale=pv[:, 2, oc:oc + 1]))
            o("dve", lambda e: e.tensor_tensor_scan(T_["cum"][:], rst[:], T_["sgw"][:], 0.0, op0=ALU.mult, op1=ALU.add))
            o("pool", lambda e: e.tensor_tensor(kk2_[:], T_["kk"][:], T_["kk"][:], op=ALU.mult))
            L.append(("pe", lambda e: e.matmul(psum_kk[:, 0:256], bd64[:], kk2_[:], start=True, stop=True), [tmr, pv_r], [psum_kk_r]))
            o("pool", lambda e: e.tensor_tensor(T_["cumx"][:], T_["cum"][:], T_["sgw"][:], op=ALU.subtract))
            o("act", lambda e: e.activation(T_["pin"][:], T_["cum"][:], AF.Exp, scale=-C0))
            o("act", lambda e: e.activation(T_["pinv"][:], T_["cum"][:], AF.Exp, scale=C0))
            o("act", lambda e: e.activation(T_["pprev"][:], T_["cumx"][:], AF.Exp, scale=-C0))
            L.append(("dve", lambda e: e.tensor_scalar(T_["lns"][:], psum_kk[:, 0:256], 1e-30, None, op0=ALU.add), [psum_kk_r, tmr], [tmr]))
            o("act", lambda e: e.activation(T_["lns"][:], T_["lns"][:], AF.Ln))
            o("act", lambda e: e.activation(T_["rn"][:], T_["lns"][:], AF.Exp, scale=-0.5))
            o("pool", lambda e: e.tensor_tensor(T_["kkn"][:], T_["kk"][:], T_["rn"][:], op=ALU.mult))
            o("dve", lambda e: e.tensor_scalar(T_["t1"][:], T_["asig"][:], pv[:, 3, oc:oc + 1], pv[:, 5, oc:oc + 1], op0=ALU.mult, op1=ALU.add))
            o("dve", lambda e: e.tensor_tensor(T_["k2"][:], k_ps, T_["t1"][:], op=ALU.mult))
            for tl_ in range(2):
                o("dve", lambda e, tl_=tl_: e.tensor_copy(Pc[:, tl_, oc:oc + 1], T_["pin"][:, 127 + 128 * tl_:128 + 128 * tl_]), extra_w=[out_r])
            o("dve", lambda e: e.scalar_tensor_tensor(AR[:, oc, :, 0, :], T_["kkn"][:].rearrange("p (a t) -> p a t", a=2), -1.0, T_["pprev"][:].rearrange("p (a t) -> p a t", a=2), op0=ALU.mult, op1=ALU.mult), extra_w=[out_r])
            o("dve", lambda e: e.tensor_tensor(AR[:, oc, :, 1, :], r_ps.rearrange("p (a t) -> p a t", a=2), T_["pin"][:].rearrange("p (a t) -> p a t", a=2), op=ALU.mult), extra_w=[out_r])
            o("pool", lambda e: e.tensor_tensor(T_["tb"][:], T_["kkn"][:], T_["asig"][:], op=ALU.mult))
            o("pool", lambda e: e.tensor_tensor(BK[:, oc, :, 0, :], T_["tb"][:].rearrange("p (a t) -> p a t", a=2), T_["pinv"][:].rearrange("p (a t) -> p a t", a=2), op=ALU.mult), extra_w=[out_r])
            o("pool", lambda e: e.tensor_tensor(BK[:, oc, :, 1, :], T_["k2"][:].rearrange("p (a t) -> p a t", a=2), T_["pinv"][:].rearrange("p (a t) -> p a t", a=2), op=ALU.mult), extra_w=[out_r])
            o("dve", lambda e: e.scalar_tensor_tensor(rk[:, oc, :], r_ps, pv[:, 4, oc:oc + 1], T_["k2"][:], op0=ALU.mult, op1=ALU.mult), extra_w=[out_r])
            return L

        for ocp in range(4):
            La = oc_chain(2 * ocp)
            Lb = oc_chain(2 * ocp + 1)
            for ia in range(len(La)):
                kb.op(La[ia][0], La[ia][1], reads=La[ia][2], writes=La[ia][3])
                kb.op(Lb[ia][0], Lb[ia][1], reads=Lb[ia][2], writes=Lb[ia][3])
        for tl in range(2):
            i = s * 2 + tl
            kb.dma("sp", lambda e, i=i, tl=tl: e.dma_start(out=ARd[i].rearrange("p (o c t) -> p o c t", o=8, c=2), in_=AR[:, :, tl, :, :]), reads=[out_r])
            kb.dma("sp", lambda e, i=i, tl=tl: e.dma_start(out=BKd[i].rearrange("p (o c t) -> p o c t", o=8, c=2), in_=BK[:, :, tl, :, :]), reads=[out_r])
            kb.dma("sp", lambda e, i=i, tl=tl: e.dma_start(out=rkd[i].rearrange("p (o t) -> p o t", o=8), in_=rk[:, :, tl * 128:(tl + 1) * 128]), reads=[out_r])
            kb.dma("sp", lambda e, i=i, tl=tl: e.dma_start(out=Pcd[i], in_=Pc[:, tl, :]), reads=[out_r])
    kb_pop(kb)
    if not ST.get("skip_p2"):
        rwkv_pass2(kb, C, T, prm, j, li, xin, xin_r, xa, xa_r, ST, nc)


def rwkv_pass2(kb, C, T, prm, j, li, xin, xin_r, xa, xa_r, ST, nc):
    NT = T // 128
    ARd, BKd, rkd, Pcd, Vd, Gd = (ST[k] for k in ("ARd", "BKd", "rkd", "Pcd", "Vd", "Gd"))
    kb_push(kb)
    pF = [kb.ps("r2_pf%d" % b, [128, 512], F32) for b in range(4)]
    RG = [pF[b][:, 0:256] for b in range(4)]
    RG_r = RL(4)
    rgc = [0]

    def nrg():
        rgc[0] += 1
        return rgc[0] % 4
    pT = kb.ps("r2_pT", [128, 1024], BF16)
    pT_r = Res()
    pY = [kb.ps("r2_pY%d" % b, [128, 512], F32) for b in range(2)]
    pY_r = RL(2)
    pH = kb.ps("r2_pH", [128, 512], F32)
    pH_r = Res()
    wo = kb.sb("r2_wo", [128, 8, 1024], BF16)
    wo_r = Res()
    wsrc = prm["rw_w_o"][j].rearrange("(kc p) n -> p kc n", p=128)
    for kc in range(8):
        kb.dma("pool", lambda e, kc=kc: e.dma_start(out=wo[:, kc, :], in_=wsrc[:, kc, :]), writes=[wo_r])
    cst_r = Res()
    m2 = kb.sb("r2_m2", [128, 2, 128], F32)
    kb.op("dve", lambda e: e.tensor_copy(m2[:, 0, :], C.m_st[:]), reads=[C.r], writes=[cst_r])
    kb.op("dve", lambda e: e.tensor_copy(m2[:, 1, :], C.m_in[:]), reads=[C.r], writes=[cst_r])
    bdm = kb.sb("r2_bdm", [128, 128], F32)
    kb.op("dve", lambda e: e.memset(bdm[:], 0.0), writes=[cst_r])
    kb.op("dve", lambda e: e.memset(bdm[0:64, 0:64], 1.0), writes=[cst_r])
    kb.op("dve", lambda e: e.memset(bdm[64:128, 64:128], 1.0), writes=[cst_r])
    HS = kb.sb("r2_HS", [128, 8, 16], BF16)
    kb.op("dve", lambda e: e.memset(HS[:], 0.0), writes=[cst_r])
    for oc in range(8):
        for hh in range(2):
            kb.op("dve", lambda e, oc=oc, hh=hh: e.memset(HS[hh * 64:(hh + 1) * 64, oc, 2 * oc + hh:2 * oc + hh + 1], 1.0), writes=[cst_r])
    lng = kb.sb("r2_lng", [128, 1024], F32)
    lnb = kb.sb("r2_lnb", [128, 1024], F32)
    kb.dma("sp", lambda e: e.dma_start(out=lng[:], in_=bcast_rows(prm["rw_lnx_g"][j:j + 1, :])), writes=[cst_r])
    kb.dma("sp", lambda e: e.dma_start(out=lnb[:], in_=bcast_rows(prm["rw_lnx_b"][j:j + 1, :])), writes=[cst_r])
    gneps = kb.sb("r2_gneps", [128, 1], F32)
    kb.op("dve", lambda e: e.memset(gneps[:], GN_EPS), writes=[cst_r])
    LS = ln_scratch(kb, "r2_ln", LN_EPS)
    g_bc, b_bc, lnp_r = load_ln_params(kb, "r2_lnp", prm["ln1_g"], prm["ln1_b"], li)

    ARt = [kb.sb("r2_AR%d" % b, [128, 8, 2, 128], BF16) for b in range(2)]
    BKt = [kb.sb("r2_BK%d" % b, [128, 8, 2, 128], BF16) for b in range(2)]
    rkt = [kb.sb("r2_rk%d" % b, [128, 8, 128], BF16) for b in range(2)]
    Pct = [kb.sb("r2_Pc%d" % b, [128, 8], F32) for b in range(2)]
    Vt = [kb.sb("r2_V%d" % b, [128, 1024], BF16) for b in range(2)]
    Gt = [kb.sb("r2_G%d" % b, [128, 1024], BF16) for b in range(2)]
    xt = [kb.sb("r2_xt%d" % b, [128, 1024], F32) for b in range(2)]
    in_r = RL(2)
    Hb = kb.sb("r2_H", [128, 8, 64], BF16)
    Hb_r = RL(8)
    kb.op("dve", lambda e: e.memset(Hb[:], 0.0), writes=Hb_r)
    TK = [kb.sb("r2_TK%d" % u, [128, 3, 128], BF16) for u in range(8)]
    TK_r = RL(8)
    XA = [kb.sb("r2_XA%d" % u, [128, 2, 128], BF16) for u in range(16)]
    KA = [kb.sb("r2_KA%d" % u, [128, 2, 128], BF16) for u in range(16)]
    XAr, KAr = RL(16), RL(16)
    XN = [[kb.sb("r2_XN%d_%d" % (u, q), [128, 2, 128], BF16) for q in range(2)] for u in range(16)]
    XNr = [RL(2) for _ in range(16)]
    PQ = [[kb.sb("r2_PQ%d_%d" % (u, q), [128, 2, 128], BF16) for q in range(2)] for u in range(16)]
    PQr = [RL(2) for _ in range(16)]
    AV = [kb.sb("r2_AV%d" % u, [128, 64], BF16) for u in range(16)]
    AVr = RL(16)
    W12 = [kb.sb("r2_W12%d" % u, [128, 2, 2, 64], BF16) for u in range(8)]
    W12_r = RL(8)
    MT = [kb.sb("r2_MT%d" % u, [128, 128], BF16) for u in range(8)]
    MTf = [kb.sb("r2_MTf%d" % u, [128, 128], F32) for u in range(2)]
    mtf_r = RL(2)
    GS = [kb.sb("r2_GS%d" % u, [128, 64], F32) for u in range(8)]
    QT = [kb.sb("r2_QT%d" % u, [128, 128], BF16) for u in range(8)]
    MT_r, GS_r, QT_r = RL(8), RL(8), RL(8)
    pT_rr = RL(2)
    ysb = kb.sb("r2_y", [128, 1024], F32)
    ysq = kb.sb("r2_ysq", [128, 1024], F32)
    yn = kb.sb("r2_yn", [128, 1024], F32)
    yg = kb.sb("r2_yg", [128, 1024], BF16)
    ygT = kb.sb("r2_ygT", [128, 8, 128], BF16)
    st = kb.sb("r2_st", [128, 8, 16], F32)
    y_r = Res()
    zt = [kb.sb("r2_z%d" % b, [128, 1024], F32) for b in range(2)]
    zt_r = RL(2)
    x1 = [kb.sb("r2_x1%d" % b, [128, 1024], F32) for b in range(2)]
    x1_r = RL(2)

    for i in range(NT):
        b = i % 2
        kb.dma("sp", lambda e, i=i, b=b: e.dma_start(out=ARt[b][:], in_=ARd[i].rearrange("p (o c t) -> p o c t", o=8, c=2)), writes=[in_r[b]])
        kb.dma("sp", lambda e, i=i, b=b: e.dma_start(out=BKt[b][:], in_=BKd[i].rearrange("p (o c t) -> p o c t", o=8, c=2)), writes=[in_r[b]])
        kb.dma("sp", lambda e, i=i, b=b: e.dma_start(out=rkt[b][:], in_=rkd[i].rearrange("p (o t) -> p o t", o=8)), writes=[in_r[b]])
        kb.dma("sp", lambda e, i=i, b=b: e.dma_start(out=Pct[b][:], in_=Pcd[i]), writes=[in_r[b]])
        kb.dma("sp", lambda e, i=i, b=b: e.dma_start(out=Vt[b][:], in_=Vd[i * 128:(i + 1) * 128, :]), writes=[in_r[b]])
        kb.dma("sp", lambda e, i=i, b=b: e.dma_start(out=Gt[b][:], in_=Gd[i * 128:(i + 1) * 128, :]), writes=[in_r[b]])
        kb.dma("sp", lambda e, i=i, b=b: e.dma_start(out=xt[b][:], in_=xin[i * 128:(i + 1) * 128, :]), reads=[xin_r[i]], writes=[in_r[b]])
        ARb, BKb, Vb, Pcb = ARt[b], BKt[b], Vt[b], Pct[b]
        inr = in_r[b]
        for hp in range(8):
            tr = 0
            for n, src in enumerate((ARb[:, hp, 0, :], BKb[:, hp, 0, :], BKb[:, hp, 1, :])):
                kb.op("pe", lambda e, n=n, src=src, tr=tr: e.transpose(pT[:, tr * 384 + n * 128:tr * 384 + (n + 1) * 128], src, C.ident_b[:]), reads=[inr, C.r], writes=[pT_rr[tr]])
            kb.op("act", lambda e, hp=hp, tr=tr: e.activation(TK[hp][:], pT[:, tr * 384:tr * 384 + 384].rearrange("p (c t) -> p c t", c=3), AF.Copy), reads=[pT_rr[tr]], writes=[TK_r[hp]])
        for u in range(16):
            hp, hh = u // 2, u % 2
            psl = slice(64 * hh, 64 * hh + 64)
            rhs_ar = ARb[psl, hp, :, :].rearrange("p c t -> p (c t)")
            for which, dstT, dst_r in ((0, XA, XAr), (1, KA, KAr)):
                g = nrg()
                kb.op("pe", lambda e, g=g, psl=psl, hp=hp, which=which, rhs_ar=rhs_ar: e.matmul(RG[g], BKb[psl, hp, which, :], rhs_ar, start=True, stop=True), reads=[inr], writes=[RG_r[g]])
                kb.op("dve", lambda e, g=g, u=u, dstT=dstT: e.tensor_tensor(dstT[u][:], RG[g].rearrange("p (c t) -> p c t", c=2), m2[:], op=ALU.mult), reads=[RG_r[g], cst_r], writes=[dst_r[u]])
            g = nrg()
            kb.op("pe", lambda e, g=g, psl=psl, hp=hp: e.matmul(RG[g][:, 0:128], ARb[psl, hp, 0, :], BKb[psl, hp, 0, :], start=True, stop=True), reads=[inr], writes=[RG_r[g]])
            kb.op("dve", lambda e, g=g, u=u: e.tensor_tensor(XN[u][0][:, 1, :], RG[g][:, 0:128], C.m_lo[:], op=ALU.mult), reads=[RG_r[g], C.r], writes=[XNr[u][0]])
            kb.op("pool", lambda e, u=u: e.tensor_copy(XN[u][0][:, 0, :], XA[u][:, 0, :]), reads=[XAr[u]], writes=[XNr[u][0]])
        for u in range(16):
            for c in range(2):
                kb.op("pool", lambda e, u=u, c=c: e.tensor_tensor(PQ[u][0][:, c, :], XN[u][0][:, c, :], C.ident_b[:], op=ALU.add), reads=[XNr[u][0], C.r], writes=[PQr[u][0]])
        for k in range(1, 7):
            cur, prv = k % 2, (k - 1) % 2
            for u in range(16):
                g = nrg()
                Xp, Np = XN[u][prv][:, 0, :], XN[u][prv][:, 1, :]
                kb.op("pe", lambda e, g=g, Xp=Xp, Np=Np: e.matmul(RG[g][:, 0:128], Np, Xp, start=True, stop=True), reads=[XNr[u][prv]], writes=[RG_r[g]])
                if k < 6:
                    kb.op("pe", lambda e, g=g, Xp=Xp, Np=Np: e.matmul(RG[g][:, 128:256], Xp, Np, start=True, stop=True), reads=[XNr[u][prv]], writes=[RG_r[g]])
                    kb.op("act", lambda e, g=g, u=u, cur=cur: e.activation(XN[u][cur][:], RG[g].rearrange("p (c t) -> p c t", c=2), AF.Copy), reads=[RG_r[g]], writes=[XNr[u][cur]])
                else:
                    kb.op("act", lambda e, g=g, u=u, cur=cur: e.activation(XN[u][cur][:, 0, :], RG[g][:, 0:128], AF.Copy), reads=[RG_r[g]], writes=[XNr[u][cur]])
            for u in range(16):
                g = nrg()
                Xc = XN[u][cur][:, 0, :]
                Qp = PQ[u][prv][:, 1, :]
                kb.op("pe", lambda e, g=g, Xc=Xc, Qp=Qp: e.matmul(RG[g][:, 0:128], Qp, Xc, start=True, stop=True), reads=[XNr[u][cur], PQr[u][prv]], writes=[RG_r[g]])
                if k < 6:
                    kb.op("pe", lambda e, g=g, Xc=Xc, Qp=Qp: e.matmul(RG[g][:, 128:256], Xc, Qp, start=True, stop=True), reads=[XNr[u][cur], PQr[u][prv]], writes=[RG_r[g]])
                    kb.op("dve", lambda e, g=g, u=u, cur=cur, prv=prv: e.tensor_tensor(PQ[u][cur][:], RG[g].rearrange("p (c t) -> p c t", c=2), PQ[u][prv][:], op=ALU.add), reads=[RG_r[g], PQr[u][prv]], writes=[PQr[u][cur]])
                else:
                    kb.op("dve", lambda e, g=g, u=u, cur=cur, prv=prv: e.tensor_tensor(PQ[u][cur][:, 0, :], RG[g][:, 0:128], PQ[u][prv][:, 0, :], op=ALU.add), reads=[RG_r[g], PQr[u][prv]], writes=[PQr[u][cur]])
        Pfin = 0
        for u in range(16):
            hp, hh = u // 2, u % 2
            hc = slice(hp * 128 + hh * 64, hp * 128 + hh * 64 + 64)
            g = nrg()
            kb.op("pe", lambda e, g=g, u=u, hc=hc: e.matmul(RG[g][:, 0:64], KA[u][:, 0, :], Vb[:, hc], start=True, stop=True), reads=[KAr[u], inr], writes=[RG_r[g]])
            kb.op("act", lambda e, g=g, u=u: e.activation(AV[u][:], RG[g][:, 0:64], AF.Copy), reads=[RG_r[g]], writes=[AVr[u]])
        for u in range(16):
            hp, hh = u // 2, u % 2
            g = nrg()
            kb.op("pe", lambda e, g=g, u=u: e.matmul(RG[g][:, 0:64], PQ[u][Pfin][:, 0, :], AV[u][:], start=True, stop=True), reads=[PQr[u][Pfin], AVr[u]], writes=[RG_r[g]])
            kb.op("pe", lambda e, g=g, u=u, hp=hp, hh=hh: e.matmul(RG[g][:, 64:128], PQ[u][Pfin][:, 0, :], TK[hp][:, 0, hh * 64:(hh + 1) * 64], start=True, stop=True), reads=[PQr[u][Pfin], TK_r[hp]], writes=[RG_r[g]])
            kb.op("act", lambda e, g=g, hp=hp, hh=hh: e.activation(W12[hp][:, :, hh, :], RG[g][:, 0:128].rearrange("p (c t) -> p c t", c=2), AF.Copy), reads=[RG_r[g]], writes=[W12_r[hp]])
        for hp in range(8):
            W1p = W12[hp][:, 0, :, :].rearrange("p h t -> p (h t)")
            W2p = W12[hp][:, 1, :, :].rearrange("p h t -> p (h t)")
            g = nrg()
            mq = hp % 2
            kb.op("pe", lambda e, g=g, W2p=W2p, hp=hp: e.matmul(RG[g][:, 0:128], W2p, TK[hp][:, 1, :], start=True, stop=True), reads=[W12_r[hp], TK_r[hp]], writes=[RG_r[g]])
            kb.op("pe", lambda e, g=g, W1p=W1p, hp=hp: e.matmul(RG[g][:, 128:256], TK[hp][:, 1, :], W1p, start=True, stop=False), reads=[W12_r[hp], TK_r[hp]], writes=[RG_r[g]])
            kb.op("pe", lambda e, g=g, hp=hp: e.matmul(RG[g][:, 128:256], TK[hp][:, 2, :], Vb[:, hp * 128:(hp + 1) * 128], start=False, stop=True), reads=[TK_r[hp], inr], writes=[RG_r[g]])
            kb.op("dve", lambda e, g=g, mq=mq: e.tensor_tensor(MTf[mq][:], RG[g][:, 0:128], bdm[:], op=ALU.mult), reads=[RG_r[g], cst_r], writes=[mtf_r[mq]])
            kb.op("pool", lambda e, hp=hp, mq=mq: e.tensor_tensor(MT[hp][:], MTf[mq][:], C.ident_f[:], op=ALU.add), reads=[mtf_r[mq], C.r], writes=[MT_r[hp]])
            for hh in range(2):
                psl = slice(64 * hh, 64 * hh + 64)
                kb.op("dve", lambda e, g=g, psl=psl, hh=hh, hp=hp: e.tensor_scalar(GS[hp][psl, :], RG[g][psl, 128 + 64 * hh:192 + 64 * hh], Pcb[psl, hp:hp + 1], None, op0=ALU.mult), reads=[RG_r[g], inr], writes=[GS_r[hp]])
            g2 = nrg()
            for hh in range(2):
                u = 2 * hp + hh
                psl = slice(64 * hh, 64 * hh + 64)
                kb.op("pe", lambda e, g2=g2, hh=hh, W2p=W2p, u=u: e.matmul(RG[g2][:, 128 * hh:128 * hh + 128], W2p, XA[u][:, 1, :], start=True, stop=True), reads=[W12_r[hp], XAr[u]], writes=[RG_r[g2]])
            for hh in range(2):
                psl = slice(64 * hh, 64 * hh + 64)
                kb.op("dve", lambda e, g2=g2, psl=psl, hh=hh, hp=hp: e.tensor_tensor(QT[hp][psl, :], RG[g2][psl, 128 * hh:128 * hh + 128], ARb[psl, hp, 1, :], op=ALU.add), reads=[RG_r[g2], inr], writes=[QT_r[hp]])
        for hp in range(8):
            yb_, yc = pY[hp // 4], (hp % 4) * 128
            for hh in range(2):
                u = 2 * hp + hh
                psl = slice(64 * hh, 64 * hh + 64)
                hc = slice(hp * 128 + hh * 64, hp * 128 + hh * 64 + 64)
                yo = yb_[:, yc + 64 * hh:yc + 64 * hh + 64]
                kb.op("pe", lambda e, yo=yo, hh=hh, u=u, hp=hp: e.matmul(yo, XA[u][:, 1, :], W12[hp][:, 0, hh, :], start=True, stop=False), reads=[XAr[u], W12_r[hp]], writes=[pY_r[hp // 4]])
                kb.op("pe", lambda e, yo=yo, u=u, hc=hc: e.matmul(yo, KA[u][:, 1, :], Vb[:, hc], start=False, stop=False), reads=[KAr[u], inr], writes=[pY_r[hp // 4]])
                kb.op("pe", lambda e, yo=yo, psl=psl, hp=hp: e.matmul(yo, QT[hp][psl, :], Hb[psl, hp, :], start=False, stop=True), reads=[QT_r[hp], Hb_r[hp]], writes=[pY_r[hp // 4]])
            g = nrg()
            kb.op("pe", lambda e, g=g, hp=hp: e.matmul(RG[g][:, 0:64], MT[hp][:], Hb[:, hp, :], start=True, stop=True), reads=[MT_r[hp], Hb_r[hp]], writes=[RG_r[g]])
            kb.op("dve", lambda e, g=g, hp=hp: e.scalar_tensor_tensor(Hb[:, hp, :], RG[g][:, 0:64], Pcb[:, hp:hp + 1], GS[hp][:], op0=ALU.mult, op1=ALU.add), reads=[RG_r[g], inr, GS_r[hp]], writes=[Hb_r[hp]])
        kb.op("act", lambda e: e.activation(ysb[:, 0:512], pY[0][:], AF.Copy), reads=[pY_r[0]], writes=[y_r])
        kb.op("dve", lambda e: e.tensor_copy(ysb[:, 512:1024], pY[1][:]), reads=[pY_r[1]], writes=[y_r])
        kb.op("pool", lambda e: e.tensor_tensor(ysq[:], ysb[:], ysb[:], op=ALU.mult), reads=[y_r], writes=[y_r])
        kb.op("dve", lambda e: e.reduce_sum(st[:, 0, :], ysb[:].rearrange("p (h n) -> p h n", h=16), axis=AX.X), reads=[y_r], writes=[y_r])
        kb.op("dve", lambda e: e.reduce_sum(st[:, 1, :], ysq[:].rearrange("p (h n) -> p h n", h=16), axis=AX.X), reads=[y_r], writes=[y_r])
        kb.op("dve", lambda e: e.tensor_scalar(st[:, 2, :], st[:, 0, :], 1.0 / 64, None, op0=ALU.mult), reads=[y_r], writes=[y_r])
        kb.op("dve", lambda e: e.tensor_tensor(st[:, 3, :], st[:, 2, :], st[:, 2, :], op=ALU.mult), reads=[y_r], writes=[y_r])
        kb.op("dve", lambda e: e.scalar_tensor_tensor(st[:, 4, :], st[:, 1, :], 1.0 / 64, st[:, 3, :], op0=ALU.mult, op1=ALU.subtract), reads=[y_r], writes=[y_r])
        kb.op("act", lambda e: e.activation(st[:, 5, :], st[:, 4, :], AF.Sqrt, bias=gneps[:, 0:1]), reads=[y_r, cst_r], writes=[y_r])
        kb.op("dve", lambda e: e.reciprocal(st[:, 6, :], st[:, 5, :]), reads=[y_r], writes=[y_r])
        for hd in range(16):
            eng = "dve" if hd % 2 == 0 else "pool"
            kb.op(eng, lambda e, hd=hd: e.tensor_scalar(yn[:, hd * 64:(hd + 1) * 64], ysb[:, hd * 64:(hd + 1) * 64], st[:, 2, hd:hd + 1], st[:, 6, hd:hd + 1], op0=ALU.subtract, op1=ALU.mult), reads=[y_r], writes=[y_r])
        kb.op("pool", lambda e: e.tensor_tensor(yn[:], yn[:], lng[:], op=ALU.mult), reads=[y_r, cst_r], writes=[y_r])
        kb.op("pool", lambda e: e.tensor_tensor(yn[:], yn[:], lnb[:], op=ALU.add), reads=[y_r, cst_r], writes=[y_r])
        for oc in range(8):
            kb.op("pe", lambda e, oc=oc: e.matmul(pH[:, 0:16], rkt[b][:, oc, :], HS[:, oc, :], start=(oc == 0), stop=(oc == 7)), reads=[in_r[b], cst_r], writes=[pH_r])
        kb.op("act", lambda e: e.activation(st[:, 7, :], pH[:, 0:16], AF.Copy), reads=[pH_r], writes=[y_r])
        for hd in range(16):
            kb.op("dve", lambda e, hd=hd: e.scalar_tensor_tensor(yn[:, hd * 64:(hd + 1) * 64], Vt[b][:, hd * 64:(hd + 1) * 64], st[:, 7, hd:hd + 1], yn[:, hd * 64:(hd + 1) * 64], op0=ALU.mult, op1=ALU.add), reads=[y_r, in_r[b]], writes=[y_r])
        kb.op("pool", lambda e: e.tensor_tensor(yg[:], yn[:], Gt[b][:], op=ALU.mult), reads=[y_r, in_r[b]], writes=[y_r])
        for hf in range(2):
            for c4 in range(4):
                kc = hf * 4 + c4
                kb.op("pe", lambda e, c4=c4, kc=kc: e.transpose(pT[:, 512 + c4 * 128:512 + (c4 + 1) * 128], yg[:, kc * 128:(kc + 1) * 128], C.ident_b[:]), reads=[y_r, C.r], writes=[pT_rr[0], pT_rr[1]])
            kb.op("act", lambda e, hf=hf: e.activation(ygT[:, hf * 4:hf * 4 + 4, :], pT[:, 512:1024].rearrange("p (c t) -> p c t", c=4), AF.Copy), reads=[pT_rr[0], pT_rr[1]], writes=[y_r])
        for hf in range(2):
            for kc in range(8):
                kb.op("pe", lambda e, kc=kc, hf=hf: e.matmul(pH[:], ygT[:, kc, :], wo[:, kc, hf * 512:(hf + 1) * 512], start=(kc == 0), stop=(kc == 7)), reads=[y_r, wo_r], writes=[pH_r])
            kb.op("dve", lambda e, hf=hf, b=b: e.scalar_tensor_tensor(zt[b][:, hf * 512:(hf + 1) * 512], xt[b][:, hf * 512:(hf + 1) * 512], ALPHA, pH[:], op0=ALU.mult, op1=ALU.add),
                  reads=[in_r[b], pH_r], writes=[zt_r[b]])
        ln_tile(kb, LS, zt[b], zt_r[b], x1[b][:], x1_r[b], g_bc, b_bc, lnp_r)
        kb.dma("sp", lambda e, i=i, b=b: e.dma_start(out=xa[i * 128:(i + 1) * 128, :], in_=x1[b][:]), reads=[x1_r[b]], writes=[xa_r[i]])
    kb_pop(kb)


T_FULL = 4096
N_CORES = 8
_CACHE = {}


def kernel(**inputs):
    if "prog" not in _CACHE:
        _CACHE["prog"] = build(T_FULL, FULL_PLAN, cap=512)
    nc, names, _ = _CACHE["prog"]
    x = np.asarray(inputs["x"], dtype=np.float32)
    in_maps = []
    for c in range(N_CORES):
        m = {"x": np.ascontiguousarray(x[c])}
        for n in names:
            m[n] = np.ascontiguousarray(np.asarray(inputs[n], dtype=np.float32))
        in_maps.append(m)
    res = run_bass_kernel_spmd(nc, in_maps, core_ids=list(range(N_CORES)))
    return np.stack([np.asarray(res.results[c]["out"]) for c in range(N_CORES)], axis=0).astype(np.float32)
```

```python
import math
from contextlib import ExitStack
import numpy as np
import concourse.bass as bass
import concourse.mybir as mybir
from concourse.bass_utils import run_bass_kernel_spmd

F32 = mybir.dt.float32
BF16 = mybir.dt.bfloat16
I32 = mybir.dt.int32
AF = mybir.ActivationFunctionType
ALU = mybir.AluOpType
AX = mybir.AxisListType

D = 1024
DEPTH = 4
NH_A = 8
NE = 32
NG = 4
EPG = 8
HID = 512
ALPHA = (2 * DEPTH) ** 0.25
LN_EPS = 1e-5
RMS_EPS = 1e-5
GN_EPS = 64e-5


class Res:
    __slots__ = ("w", "r")

    def __init__(self):
        self.w = None
        self.r = {}


def RL(n):
    return [Res() for _ in range(n)]


class KB:
    EPOCH = 30000

    def __init__(self, nc, es):
        self.nc = nc
        self.es = es
        self.engs = {"pe": nc.tensor, "dve": nc.vector, "act": nc.scalar, "pool": nc.gpsimd, "sp": nc.sync}
        self.sems = {}
        self.cur = {}
        self.seen = {e: {} for e in self.engs}
        self.rings = {}
        self.ridx = {}
        self.nins = 0
        self.rec = None
        self.defer = None
        for q, n in (("sp", 16), ("pool", 12), ("act", 6)):
            self.rings[q] = [[self._new_sem("d%s%d" % (q, i)), 0] for i in range(n)]
            self.ridx[q] = 0

    def _new_sem(self, name):
        s = self.es.enter_context(self.nc.semaphore(name))
        self.sems[name] = s
        return name

    def sb(self, name, shape, dt):
        return self.es.enter_context(self.nc.sbuf_tensor(name, list(shape), dt))

    def ps(self, name, shape, dt=F32):
        return self.es.enter_context(self.nc.psum_tensor(name, list(shape), dt))

    def _deps(self, reads, writes):
        deps = {}
        for r in reads:
            if r.w:
                for k, v in r.w.items():
                    if deps.get(k, 0) < v:
                        deps[k] = v
        for w in writes:
            if w.w:
                for k, v in w.w.items():
                    if deps.get(k, 0) < v:
                        deps[k] = v
            for k, v in w.r.items():
                if deps.get(k, 0) < v:
                    deps[k] = v
        return deps

    def _waits(self, eng, deps):
        E = self.engs[eng]
        seen = self.seen[eng]
        for k, v in deps.items():
            if eng == "pe" and k.startswith("epe"):
                continue
            if seen.get(k, 0) >= v:
                continue
            E.wait_ge(self.sems[k], v)
            seen[k] = v
            if self.rec is not None:
                self.rec.append((eng, "w", k, v))

    def _mark(self, tok, reads, writes):
        (k, v), = tok.items()
        for w in writes:
            if w.w is None:
                w.w = dict(tok)
            else:
                w.w = dict(w.w)
                w.w[k] = v
            w.r = {}
        for r in reads:
            if r.r.get(k, 0) < v:
                r.r[k] = v

    def op(self, eng, fn, reads=(), writes=()):
        if self.defer is not None:
            self.defer.append((0, eng, fn, list(reads), list(writes)))
            return
        self._waits(eng, self._deps(reads, writes))
        c = self.cur.get(eng)
        if c is None or c[1] >= self.EPOCH:
            n = len([k for k in self.sems if k.startswith("e" + eng)])
            c = [self._new_sem("e%s%d" % (eng, n)), 0]
            self.cur[eng] = c
        ins = fn(self.engs[eng])
        c[1] += 1
        ins.then_inc(self.sems[c[0]], 1)
        self.nins += 1
        if self.rec is not None:
            self.rec.append((eng, "i", c[0], 1))
        self._mark({c[0]: c[1]}, reads, writes)

    def dma(self, q, fn, reads=(), writes=()):
        if self.defer is not None:
            self.defer.append((1, q, fn, list(reads), list(writes)))
            return
        ring = self.rings[q]
        slot = ring[self.ridx[q] % len(ring)]
        self.ridx[q] += 1
        deps = self._deps(reads, writes)
        if slot[1] > 0:
            deps[slot[0]] = max(deps.get(slot[0], 0), slot[1] * 16)
        self._waits(q, deps)
        ins = fn(self.engs[q])
        slot[1] += 1
        ins.then_inc(self.sems[slot[0]], 16)
        self.nins += 1
        if self.rec is not None:
            self.rec.append((q, "i", slot[0], 16))
        self._mark({slot[0]: slot[1] * 16}, reads, writes)

    def finish(self):
        deps = {}
        for q, ring in self.rings.items():
            for name, cnt in ring:
                if cnt:
                    deps[name] = cnt * 16
        for name in self.sems:
            if name.startswith("e"):
                pass
        for e, c in self.cur.items():
            deps[c[0]] = c[1]
        self._waits("sp", deps)


class Consts:
    pass


def make_consts(kb):
    nc = kb.nc
    C = Consts()
    C.r = Res()
    it = kb.sb("c_iota", [128, 512], I32)
    itf = kb.sb("c_iotaf", [128, 512], F32)
    C.ident_f = kb.sb("c_identf", [128, 128], F32)
    C.ident_b = kb.sb("c_identb", [128, 128], BF16)
    C.ones_b = kb.sb("c_onesb", [128, 128], BF16)
    C.triu_b = kb.sb("c_triub", [128, 128], BF16)
    C.cmask = kb.sb("c_cmask", [128, 4, 512], BF16)
    C.m_st = kb.sb("c_mst", [128, 128], F32)
    C.m_in = kb.sb("c_min", [128, 128], F32)
    C.m_lo = kb.sb("c_mlo", [128, 128], F32)
    kb.op("pool", lambda e: e.iota(it[:], [[1, 512]], base=0, channel_multiplier=-1), writes=[C.r])
    kb.op("dve", lambda e: e.tensor_copy(itf[:], it[:]), reads=[C.r], writes=[C.r])
    kb.op("dve", lambda e: e.tensor_scalar(C.ident_f[:], itf[:, 0:128], 0.0, None, op0=ALU.is_equal), reads=[C.r], writes=[C.r])
    kb.op("dve", lambda e: e.tensor_copy(C.ident_b[:], C.ident_f[:]), reads=[C.r], writes=[C.r])
    kb.op("dve", lambda e: e.memset(C.ones_b[:], 1.0), writes=[C.r])
    kb.op("dve", lambda e: e.tensor_scalar(C.triu_b[:], itf[:, 0:128], 0.0, None, op0=ALU.is_ge), reads=[C.r], writes=[C.r])
    for o in range(4):
        kb.op("dve", lambda e, o=o: e.tensor_scalar(C.cmask[:, o, :], itf[:], float(128 * o), None, op0=ALU.is_ge), reads=[C.r], writes=[C.r])
    kb.op("dve", lambda e: e.tensor_scalar(C.m_st[:], itf[:, 0:128], 1.0, None, op0=ALU.is_ge), reads=[C.r], writes=[C.r])
    kb.op("dve", lambda e: e.tensor_scalar(C.m_in[:], itf[:, 0:128], 0.0, None, op0=ALU.is_ge), reads=[C.r], writes=[C.r])
    kb.op("dve", lambda e: e.tensor_scalar(C.m_lo[:], itf[:, 0:128], -1.0, None, op0=ALU.is_le), reads=[C.r], writes=[C.r])
    C.itf = itf
    return C


def kb_push(kb):
    kb._stack = getattr(kb, "_stack", [])
    kb._stack.append(kb.es_t)
    kb.es_t = kb.es_root.enter_context(ExitStack()) if False else ExitStack()
    kb.es_t.__enter__()


def kb_pop(kb):
    kb.barrier()
    kb.es_t.__exit__(None, None, None)
    kb.es_t = kb._stack.pop()


def _kb_sb(self, name, shape, dt):
    self.nalloc = getattr(self, "nalloc", 0) + 1
    return self.es_t.enter_context(self.nc.sbuf_tensor("%s_%d" % (name, self.nalloc), list(shape), dt))


def _kb_ps(self, name, shape, dt=F32):
    self.nalloc = getattr(self, "nalloc", 0) + 1
    return self.es_t.enter_context(self.nc.psum_tensor("%s_%d" % (name, self.nalloc), list(shape), dt))


def kb_flush(kb, pending, n):
    sv = kb.defer
    kb.defer = None
    for _ in range(min(n, len(pending))):
        kind, e, fn, rd, wr = pending.pop(0)
        if kind == 0:
            kb.op(e, fn, reads=rd, writes=wr)
        else:
            kb.dma(e, fn, reads=rd, writes=wr)
    kb.defer = sv


def _kb_barrier(self):
    deps = {}
    for q, ring in self.rings.items():
        for name, cnt in ring:
            if cnt:
                deps[name] = cnt * 16
    for name in self.sems:
        if name.startswith("e"):
            eng = [e for e in self.engs if name.startswith("e" + e)][0]
            c = self.cur[eng]
            if c[0] == name:
                deps[name] = c[1]
    for eng in self.engs:
        self._waits(eng, dict(deps))


KB.sb = _kb_sb
KB.ps = _kb_ps
KB.barrier = _kb_barrier


def bcast_rows(ap_row, n=128):
    return ap_row.partition_broadcast(n)


def ln_tile(kb, S, z, z_r, out, out_r, g_bc, b_bc, par_r):
    st, mv, sd, rs, xn = S["st"], S["mv"], S["sd"], S["rs"], S["xn"]
    r = S["r"]
    kb.op("dve", lambda e: e.bn_stats(st[:, 0, :], z[:, 0:512]), reads=[z_r], writes=[r])
    kb.op("dve", lambda e: e.bn_stats(st[:, 1, :], z[:, 512:1024]), reads=[z_r], writes=[r])
    kb.op("dve", lambda e: e.bn_aggr(mv[:, 0:2], st[:].rearrange("p a b -> p (a b)")), reads=[r], writes=[r])
    kb.op("act", lambda e: e.activation(sd[:, 0:1], mv[:, 1:2], AF.Sqrt, bias=S["eps"][:, 0:1]), reads=[r], writes=[r])
    kb.op("dve", lambda e: e.reciprocal(rs[:, 0:1], sd[:, 0:1]), reads=[r], writes=[r])
    kb.op("dve", lambda e: e.tensor_scalar(xn[:], z[:], mv[:, 0:1], rs[:, 0:1], op0=ALU.subtract, op1=ALU.mult), reads=[r, z_r], writes=[S["xn_r"]])
    kb.op("pool", lambda e: e.tensor_tensor(xn[:], xn[:], g_bc[:], op=ALU.mult), reads=[S["xn_r"], par_r], writes=[S["xn_r"]])
    kb.op("pool", lambda e: e.tensor_tensor(out, xn[:], b_bc[:], op=ALU.add), reads=[S["xn_r"], par_r], writes=[out_r])


def ln_scratch(kb, pfx, eps):
    S = {}
    S["st"] = kb.sb(pfx + "st", [128, 2, 6], F32)
    S["mv"] = kb.sb(pfx + "mv", [128, 2], F32)
    S["sd"] = kb.sb(pfx + "sd", [128, 1], F32)
    S["rs"] = kb.sb(pfx + "rs", [128, 1], F32)
    S["xn"] = kb.sb(pfx + "xn", [128, 1024], F32)
    S["eps"] = kb.sb(pfx + "eps", [128, 1], F32)
    S["r"] = Res()
    S["xn_r"] = Res()
    kb.op("dve", lambda e: e.memset(S["eps"][:], eps), writes=[S["r"]])
    return S


def load_ln_params(kb, pfx, g_dram, b_dram, li):
    g = kb.sb(pfx + "g", [128, 1024], F32)
    b = kb.sb(pfx + "b", [128, 1024], F32)
    r = Res()
    kb.dma("sp", lambda e: e.dma_start(out=g[:], in_=bcast_rows(g_dram[li:li + 1, :])), writes=[r])
    kb.dma("sp", lambda e: e.dma_start(out=b[:], in_=bcast_rows(b_dram[li:li + 1, :])), writes=[r])
    return g, b, r


def attn_phase(kb, C, T, prm, j, li, xin, xin_r, xa, xa_r):
    nc = kb.nc
    NT = T // 128
    NS = T // 512
    lambda_init = 0.8 - 0.6 * math.exp(-0.3 * li)
    kb_push(kb)
    OT = kb.sb("a_OT", [128, 8, T], BF16)
    OT_r = [RL(NS) for _ in range(8)]
    ps = [kb.ps("a_ps%d" % b, [128, 512], F32) for b in range(8)]
    ps_r = RL(8)
    kb_push(kb)
    xld = [kb.sb("a_xld%d" % b, [128, 1024], F32) for b in range(2)]
    xld_r = RL(2)
    xT = kb.sb("a_xT", [128, 8, T], BF16)
    xT_r = RL(NT)
    QT = kb.sb("a_QT", [128, T], BF16)
    QT_r = RL(NS)
    KT = kb.sb("a_KT", [128, T], BF16)
    KT_r = RL(NS)
    V = kb.sb("a_V", [128, NT, 128], BF16)
    V_r = RL(NT // 4)
    wh = [kb.sb("a_wh%d" % b, [128, 8, 3, 128], BF16) for b in range(2)]
    wh_r = RL(2)
    pt = [kb.sb("a_pt%d" % b, [128, 512], BF16) for b in range(4)]
    pt_r = RL(4)
    lam = kb.sb("a_lam", [128, 256], F32)
    lsc = kb.sb("a_lsc", [128, 8], F32)
    lam_r = Res()
    s1 = kb.sb("a_s1", [128, 512], F32)
    s2 = kb.sb("a_s2", [128, 512], F32)
    t1 = kb.sb("a_t1", [128, 512], F32)
    t2 = kb.sb("a_t2", [128, 512], F32)
    sq = kb.sb("a_sq", [128, 512], BF16)
    op_ = t1
    s12 = s1
    e2 = s2
    tot = s2
    rstd = s1
    fin_r = Res()
    fin2_r = fin_r

    kb.dma("sp", lambda e: e.dma_start(out=lam[:], in_=bcast_rows(prm["attn_lambda"][j:j + 1].rearrange("o a b -> o (a b)"))), writes=[lam_r])
    kb.dma("sp", lambda e: e.dma_start(out=lsc[:, 4:5], in_=prm["attn_subln_g"][j].rearrange("(p o) -> p o", o=1)), writes=[lam_r])
    kb.op("dve", lambda e: e.tensor_tensor(lam[:, 0:64], lam[:, 0:64], lam[:, 64:128], op=ALU.mult), reads=[lam_r], writes=[lam_r])
    kb.op("dve", lambda e: e.tensor_tensor(lam[:, 128:192], lam[:, 128:192], lam[:, 192:256], op=ALU.mult), reads=[lam_r], writes=[lam_r])
    kb.op("dve", lambda e: e.reduce_sum(lsc[:, 0:1], lam[:, 0:64], axis=AX.X), reads=[lam_r], writes=[lam_r])
    kb.op("dve", lambda e: e.reduce_sum(lsc[:, 1:2], lam[:, 128:192], axis=AX.X), reads=[lam_r], writes=[lam_r])
    kb.op("act", lambda e: e.activation(lsc[:, 2:4], lsc[:, 0:2], AF.Exp), reads=[lam_r], writes=[lam_r])
    kb.op("dve", lambda e: e.tensor_tensor(lsc[:, 5:6], lsc[:, 3:4], lsc[:, 2:3], op=ALU.subtract), reads=[lam_r], writes=[lam_r])
    kb.op("dve", lambda e: e.tensor_scalar(lsc[:, 5:6], lsc[:, 5:6], -lambda_init, None, op0=ALU.add), reads=[lam_r], writes=[lam_r])
    kb.op("dve", lambda e: e.tensor_scalar(lsc[:, 6:7], lsc[:, 4:5], 1.0 - lambda_init, None, op0=ALU.mult), reads=[lam_r], writes=[lam_r])
    nlam = lsc[:, 5:6]
    gsc = lsc[:, 6:7]

    for i in range(NT):
        b = i % 2
        kb.dma("sp", lambda e, i=i, b=b: e.dma_start(out=xld[b][:], in_=xin[i * 128:(i + 1) * 128, :]), reads=[xin_r[i]], writes=[xld_r[b]])
        for hf in range(2):
            pb = (2 * i + hf) % 8
            for c4 in range(4):
                kc = hf * 4 + c4
                kb.op("pe", lambda e, pb=pb, c4=c4, kc=kc, b=b: e.transpose(ps[pb][:, c4 * 128:(c4 + 1) * 128], xld[b][:, kc * 128:(kc + 1) * 128], C.ident_f[:]),
                      reads=[xld_r[b], C.r], writes=[ps_r[pb]])
            eng = "act" if hf == 0 else "dve"
            if eng == "act":
                kb.op("act", lambda e, pb=pb, hf=hf, i=i: e.activation(xT[:, hf * 4:hf * 4 + 4, i * 128:(i + 1) * 128], ps[pb][:].rearrange("p (c t) -> p c t", c=4), AF.Copy),
                      reads=[ps_r[pb]], writes=[xT_r[i]])
            else:
                kb.op("dve", lambda e, pb=pb, hf=hf, i=i: e.tensor_copy(xT[:, hf * 4:hf * 4 + 4, i * 128:(i + 1) * 128], ps[pb][:].rearrange("p (c t) -> p c t", c=4)),
                      reads=[ps_r[pb]], writes=[xT_r[i]])


    wq = prm["attn_w_qkv"][j].rearrange("(kc p) n -> p kc n", p=128)
    pcnt = [0]

    def nps():
        pcnt[0] += 1
        return pcnt[0] % 2

    for h in range(NH_A):
        wb = h % 2
        for part in range(3):
            kb.dma("pool", lambda e, wb=wb, part=part, h=h: e.dma_start(out=wh[wb][:, :, part, :], in_=wq[:, :, part * 1024 + h * 128: part * 1024 + (h + 1) * 128]),
                   writes=[wh_r[wb]])
        for s in range(NS):
            for part, dst, dst_r, scl in ((0, QT, QT_r, 0.125), (1, KT, KT_r, 1.0)):
                pb = nps()
                for kc in range(8):
                    kb.op("pe", lambda e, pb=pb, wb=wb, kc=kc, part=part, s=s: e.matmul(ps[pb][:], wh[wb][:, kc, part, :], xT[:, kc, s * 512:(s + 1) * 512], start=(kc == 0), stop=(kc == 7)),
                          reads=[wh_r[wb]] + xT_r[s * 4:s * 4 + 4], writes=[ps_r[pb]])
                kb.op("act", lambda e, pb=pb, dst=dst, s=s, scl=scl: e.activation(dst[:, s * 512:(s + 1) * 512], ps[pb][:], AF.Copy, scale=scl),
                      reads=[ps_r[pb]], writes=[dst_r[s]])
        for g4 in range(NT // 4):
            pb = nps()
            for t4 in range(4):
                i = g4 * 4 + t4
                for kc in range(8):
                    kb.op("pe", lambda e, pb=pb, wb=wb, kc=kc, i=i, t4=t4: e.matmul(ps[pb][:, t4 * 128:(t4 + 1) * 128], xT[:, kc, i * 128:(i + 1) * 128], wh[wb][:, kc, 2, :], start=(kc == 0), stop=(kc == 7)),
                          reads=[wh_r[wb], xT_r[i]], writes=[ps_r[pb]])
            kb.op("dve", lambda e, pb=pb, g4=g4: e.tensor_copy(V[:, g4 * 4:g4 * 4 + 4, :], ps[pb][:].rearrange("p (c t) -> p c t", c=4)),
                  reads=[ps_r[pb]], writes=[V_r[g4]])
        pti = 0
        scnt = [0]
        for qs in range(NS):
            nkt = qs * 4 + 4
            items = [(kt, c) for kt in range(nkt) for c in range(2)]
            sbank = {}
            LOOK = 2

            def emit_s(n):
                kt, c = items[n]
                pb = (0, 1, 7)[scnt[0] % 3]
                scnt[0] += 1
                sbank[n] = pb
                kb.op("pe", lambda e, pb=pb, c=c, kt=kt, qs=qs: e.matmul(ps[pb][:], KT[c * 64:(c + 1) * 64, kt * 128:(kt + 1) * 128], QT[c * 64:(c + 1) * 64, qs * 512:(qs + 1) * 512], start=True, stop=True),
                      reads=[KT_r[kt // 4], QT_r[qs]], writes=[ps_r[pb]])

            for n in range(min(LOOK, len(items))):
                emit_s(n)
            for n, (kt, c) in enumerate(items):
                if n + LOOK < len(items):
                    emit_s(n + LOOK)
                pb = sbank[n]
                pi = pti % 4
                pti += 1
                kb.op("act", lambda e, pb=pb, pi=pi: e.activation(pt[pi][:], ps[pb][:], AF.Exp), reads=[ps_r[pb]], writes=[pt_r[pi]])
                if kt >= qs * 4:
                    o = kt - qs * 4
                    kb.op("pool", lambda e, pi=pi, o=o: e.tensor_tensor(pt[pi][:], pt[pi][:], C.cmask[:, o, :], op=ALU.mult), reads=[pt_r[pi], C.r], writes=[pt_r[pi]])
                kb.op("pe", lambda e, c=c, kt=kt, pi=pi, nkt=nkt: e.matmul(ps[2 + c][:], V[:, kt, :], pt[pi][:], start=(kt == 0), stop=(kt == nkt - 1)),
                      reads=[V_r[kt // 4], pt_r[pi]], writes=[ps_r[2 + c]])
                kb.op("pe", lambda e, c=c, kt=kt, pi=pi, nkt=nkt: e.matmul(ps[4 + c][:], C.ones_b[:], pt[pi][:], start=(kt == 0), stop=(kt == nkt - 1)),
                      reads=[C.r, pt_r[pi]], writes=[ps_r[4 + c]])
            kb.op("act", lambda e: e.activation(s1[:], ps[4][:], AF.Copy), reads=[ps_r[4]], writes=[fin_r])
            kb.op("act", lambda e: e.activation(s2[:], ps[5][:], AF.Copy), reads=[ps_r[5]], writes=[fin_r])
            kb.op("dve", lambda e: e.tensor_tensor(t1[:], ps[2][:], s2[:], op=ALU.mult), reads=[ps_r[2], fin_r], writes=[fin2_r])
            kb.op("dve", lambda e: e.tensor_tensor(t2[:], ps[3][:], s1[:], op=ALU.mult), reads=[ps_r[3], fin_r], writes=[fin2_r])
            kb.op("dve", lambda e: e.scalar_tensor_tensor(op_[:], t2[:], nlam, t1[:], op0=ALU.mult, op1=ALU.add), reads=[fin2_r, lam_r], writes=[fin2_r])
            kb.op("pool", lambda e: e.tensor_tensor(sq[:], op_[:], op_[:], op=ALU.mult), reads=[fin2_r], writes=[fin2_r])
            kb.op("pool", lambda e: e.tensor_tensor(s12[:], s1[:], s2[:], op=ALU.mult), reads=[fin_r], writes=[fin2_r])
            kb.op("dve", lambda e: e.scalar_tensor_tensor(e2[:], s12[:], RMS_EPS, s12[:], op0=ALU.mult, op1=ALU.mult), reads=[fin2_r], writes=[fin2_r])
            kb.op("pe", lambda e: e.matmul(ps[6][:], C.ones_b[:], sq[:], start=True, stop=True), reads=[C.r, fin2_r], writes=[ps_r[6]])
            kb.op("dve", lambda e: e.scalar_tensor_tensor(tot[:], ps[6][:], 1.0 / 128.0, e2[:], op0=ALU.mult, op1=ALU.add), reads=[ps_r[6], fin2_r], writes=[fin2_r])
            kb.op("act", lambda e: e.activation(tot[:], tot[:], AF.Ln), reads=[fin2_r], writes=[fin2_r])
            kb.op("act", lambda e: e.activation(rstd[:], tot[:], AF.Exp, scale=-0.5), reads=[fin2_r], writes=[fin2_r])
            kb.op("dve", lambda e, h=h, qs=qs: e.scalar_tensor_tensor(OT[:, h, qs * 512:(qs + 1) * 512], op_[:], gsc, rstd[:], op0=ALU.mult, op1=ALU.mult),
                  reads=[fin2_r, lam_r], writes=[OT_r[h][qs]])

    kb_pop(kb)
    wo = kb.sb("a_wo", [128, 8, 1024], BF16)
    wo_r = Res()
    wo_src = prm["attn_w_o"][j].rearrange("(h p) n -> p h n", p=128)
    for hh in range(8):
        kb.dma("pool", lambda e, hh=hh: e.dma_start(out=wo[:, hh, :], in_=wo_src[:, hh, :]), writes=[wo_r])
    xld = [kb.sb("a_xldc%d" % b, [128, 1024], F32) for b in range(2)]
    xld_r = RL(2)
    zt = [kb.sb("a_z%d" % b, [128, 1024], F32) for b in range(2)]
    zt_r = RL(2)
    x1 = [kb.sb("a_x1%d" % b, [128, 1024], F32) for b in range(2)]
    x1_r = RL(2)
    LS = ln_scratch(kb, "a_ln", LN_EPS)
    g_bc, b_bc, lnp_r = load_ln_params(kb, "a_lnp", prm["ln1_g"], prm["ln1_b"], li)
    for i in range(NT):
        b = i % 2
        kb.dma("sp", lambda e, i=i, b=b: e.dma_start(out=xld[b][:], in_=xin[i * 128:(i + 1) * 128, :]), reads=[xin_r[i]], writes=[xld_r[b]])
        for hf in range(2):
            pb = 2 * b + hf
            for hh in range(8):
                kb.op("pe", lambda e, pb=pb, hh=hh, hf=hf, i=i: e.matmul(ps[pb][:], OT[:, hh, i * 128:(i + 1) * 128], wo[:, hh, hf * 512:(hf + 1) * 512], start=(hh == 0), stop=(hh == 7)),
                      reads=[OT_r[hh][i // 4], wo_r], writes=[ps_r[pb]])
            kb.op("dve", lambda e, pb=pb, hf=hf, b=b: e.scalar_tensor_tensor(zt[b][:, hf * 512:(hf + 1) * 512], xld[b][:, hf * 512:(hf + 1) * 512], ALPHA, ps[pb][:], op0=ALU.mult, op1=ALU.add),
                  reads=[xld_r[b], ps_r[pb]], writes=[zt_r[b]])
        ln_tile(kb, LS, zt[b], zt_r[b], x1[b][:], x1_r[b], g_bc, b_bc, lnp_r)
        kb.dma("sp", lambda e, i=i, b=b: e.dma_start(out=xa[i * 128:(i + 1) * 128, :], in_=x1[b][:]), reads=[x1_r[b]], writes=[xa_r[i]])
    kb_pop(kb)


PSHAPES = {
    "ln1_g": (4, 1024), "ln1_b": (4, 1024), "ln2_g": (4, 1024), "ln2_b": (4, 1024),
    "attn_w_qkv": (2, 1024, 3072), "attn_w_o": (2, 1024, 1024), "attn_lambda": (2, 4, 64), "attn_subln_g": (2, 128),
    "rw_mu": (2, 6, 1024), "rw_w_rkv": (2, 3, 1024, 1024), "rw_w_o": (2, 1024, 1024), "rw_w0": (2, 1024),
    "rw_w1": (2, 1024, 64), "rw_w2": (2, 64, 1024), "rw_a0": (2, 1024), "rw_a1": (2, 1024, 64), "rw_a2": (2, 64, 1024),
    "rw_g1": (2, 1024, 160), "rw_g2": (2, 160, 1024), "rw_k_k": (2, 1024), "rw_k_a": (2, 1024), "rw_r_k": (2, 16, 64),
    "rw_lnx_g": (2, 1024), "rw_lnx_b": (2, 1024), "rw_v0": (1, 1024), "rw_v1": (1, 1024, 32), "rw_v2": (1, 32, 1024),
    "moe_rg_w": (4, 1024, 4), "moe_rg_b": (4, 4), "moe_re_w": (4, 1024, 32), "moe_re_b": (4, 32),
    "moe_w_gu": (4, 32, 1024, 1024), "moe_w_down": (4, 32, 512, 1024),
}


class Params(dict):
    def __init__(self, nc):
        super().__init__()
        self.nc = nc

    def __missing__(self, k):
        ap = self.nc.dram_tensor(k, list(PSHAPES[k]), F32, kind="ExternalInput").ap()
        self[k] = ap
        return ap


def build(T, plan, cap=512):
    nc = bass.Bass("TRN2", target_bir_lowering=False)
    prm = Params(nc)
    x = nc.dram_tensor("x", [T, D], F32, kind="ExternalInput").ap()
    out = nc.dram_tensor("out", [T, D], F32, kind="ExternalOutput").ap()
    xa = nc.dram_tensor("xa_s", [T, D], F32, kind="Internal").ap()
    xb = nc.dram_tensor("xb_s", [T, D], F32, kind="Internal").ap()
    NT = T // 128
    es = ExitStack()
    with es:
        kb = KB(nc, es)
        kb.es_t = es
        C = make_consts(kb)
        cur, cur_r = x, RL(NT)
        xa_r, xb_r, out_r = RL(NT), RL(NT), RL(NT)
        ST = dict(DEBUG_ST)
        for n, step in enumerate(plan):
            last = n == len(plan) - 1
            kind = step[0]
            if kind == "attn":
                attn_phase(kb, C, T, prm, step[1], step[2], cur, cur_r, xa, xa_r)
                cur, cur_r = xa, xa_r
            elif kind == "rwkv":
                rwkv_phase(kb, C, T, prm, step[1], step[2], cur, cur_r, xa, xa_r, ST, nc)
                cur, cur_r = xa, xa_r
            elif kind == "moe":
                dst, dst_r = (out, out_r) if last else (xb, xb_r)
                moe_phase(kb, C, T, prm, step[1], cur, cur_r, dst, dst_r, cap, nc, ST)
                cur, cur_r = dst, dst_r
        if cur is not out:
            for i in range(NT):
                kb.dma("sp", lambda e, i=i: e.dma_start(out=out[i * 128:(i + 1) * 128, :], in_=cur[i * 128:(i + 1) * 128, :]), reads=[cur_r[i]], writes=[out_r[i]])
        kb.finish()
        ninst = kb.nins
    return nc, list(prm.keys()), ninst


DEBUG_ST = {}
FULL_PLAN = [("attn", 0, 0), ("moe", 0), ("rwkv", 0, 1), ("moe", 1), ("attn", 1, 2), ("moe", 2), ("rwkv", 1, 3), ("moe", 3)]


def moe_phase(kb, C, T, prm, li, xin, xin_r, dst, dst_r, cap, nc, ST):
    NT = T // 128
    NSLOT = NE * cap
    NB = cap // 128
    if "xbuf" not in ST:
        ST["xbuf"] = nc.dram_tensor("xbuf_s", [NSLOT, D], BF16, kind="Internal").ap()
        ST["ybuf"] = nc.dram_tensor("ybuf_s", [NSLOT, D], F32, kind="Internal").ap()
        ST["bc_reg"] = nc.gpsimd.to_reg(NSLOT - 1)
    xbuf, ybuf = ST["xbuf"], ST["ybuf"]
    bc_reg = ST["bc_reg"]
    kb_push(kb)
    slots = kb.sb("m_slots", [128, NT, 2], I32)
    gates = kb.sb("m_gates", [128, NT, 2], F32)
    sg_r = Res()

    kb_push(kb)
    ps = [kb.ps("m1_ps%d" % b, [128, 512], F32) for b in range(6)]
    ps_r = RL(6)
    xt = [kb.sb("m1_xt%d" % b, [128, 1024], F32) for b in range(2)]
    xt_r = RL(2)
    xb16 = [kb.sb("m1_xb%d" % b, [128, 1024], BF16) for b in range(2)]
    xb_r = RL(2)
    xT = [kb.sb("m1_xT%d" % b, [128, 8, 128], F32) for b in range(2)]
    xT_r = RL(2)
    wr = kb.sb("m1_wr", [128, 8, 36], F32)
    rb = kb.sb("m1_rb", [128, 36], F32)
    offs_i = kb.sb("m1_offi", [128, 32], I32)
    offs = kb.sb("m1_off", [128, 32], F32)
    base = kb.sb("m1_base", [128, 32], F32)
    cr = Res()
    base_r = Res()
    kb.dma("sp", lambda e: e.dma_start(out=wr[:, :, 0:4], in_=prm["moe_rg_w"][li].rearrange("(kc p) n -> p kc n", p=128)), writes=[cr])
    kb.dma("sp", lambda e: e.dma_start(out=wr[:, :, 4:36], in_=prm["moe_re_w"][li].rearrange("(kc p) n -> p kc n", p=128)), writes=[cr])
    kb.dma("sp", lambda e: e.dma_start(out=rb[:, 0:4], in_=bcast_rows(prm["moe_rg_b"][li:li + 1, :])), writes=[cr])
    kb.dma("sp", lambda e: e.dma_start(out=rb[:, 4:36], in_=bcast_rows(prm["moe_re_b"][li:li + 1, :])), writes=[cr])
    kb.op("pool", lambda e: e.iota(offs_i[:], [[cap, 32]], base=-1, channel_multiplier=0), writes=[cr])
    kb.op("dve", lambda e: e.tensor_copy(offs[:], offs_i[:]), reads=[cr], writes=[cr])
    kb.op("dve", lambda e: e.memset(base[:], 0.0), writes=[base_r])
    Wk = {}
    for nm, w in (("L", 36), ("ohg", 4), ("eg", 4), ("lsel", 8), ("oh1", 8), ("lsel2", 8), ("oh2", 8), ("E1", 32), ("E2", 32),
                  ("val", 32), ("valid", 32), ("val2", 32), ("tmp", 32), ("sc", 16)):
        Wk[nm] = kb.sb("m1_w" + nm, [128, w], F32)
    G01 = kb.sb("m1_G01", [128, 32], BF16)
    wr_ = Res()
    BIG = float(NSLOT)
    for i in range(NT):
        b = i % 2
        kb.dma("sp", lambda e, i=i, b=b: e.dma_start(out=xt[b][:], in_=xin[i * 128:(i + 1) * 128, :]), reads=[xin_r[i]], writes=[xt_r[b]])
        kb.op("act", lambda e, b=b: e.activation(xb16[b][:], xt[b][:], AF.Copy), reads=[xt_r[b]], writes=[xb_r[b]])
        for hf in range(2):
            pb = hf
            for c4 in range(4):
                kc = hf * 4 + c4
                kb.op("pe", lambda e, pb=pb, c4=c4, kc=kc, b=b: e.transpose(ps[pb][:, c4 * 128:(c4 + 1) * 128], xt[b][:, kc * 128:(kc + 1) * 128], C.ident_f[:]),
                      reads=[xt_r[b], C.r], writes=[ps_r[pb]])
            if hf == 0:
                kb.op("act", lambda e, pb=pb, b=b: e.activation(xT[b][:, 0:4, :], ps[pb][:].rearrange("p (c t) -> p c t", c=4), AF.Copy), reads=[ps_r[pb]], writes=[xT_r[b]])
            else:
                kb.op("dve", lambda e, pb=pb, b=b: e.tensor_copy(xT[b][:, 4:8, :], ps[pb][:].rearrange("p (c t) -> p c t", c=4)), reads=[ps_r[pb]], writes=[xT_r[b]])
        for kc in range(8):
            kb.op("pe", lambda e, kc=kc, b=b: e.matmul(ps[2][:, 0:36], xT[b][:, kc, :], wr[:, kc, :], start=(kc == 0), stop=(kc == 7)), reads=[xT_r[b], cr], writes=[ps_r[2]])
        L, ohg, eg, lsel, oh1, lsel2, oh2, E1, E2 = (Wk[k] for k in ("L", "ohg", "eg", "lsel", "oh1", "lsel2", "oh2", "E1", "E2"))
        val, valid, val2, tmp, sc = (Wk[k] for k in ("val", "valid", "val2", "tmp", "sc"))

        def dv(fn, extra_r=(), extra_w=()):
            kb.op("dve", fn, reads=[wr_] + list(extra_r), writes=[wr_] + list(extra_w))

        dv(lambda e: e.tensor_tensor(L[:], ps[2][:, 0:36], rb[:], op=ALU.add), extra_r=[ps_r[2], cr])
        dv(lambda e: e.reduce_max(sc[:, 0:1], L[:, 0:4], axis=AX.X))
        dv(lambda e: e.tensor_scalar(ohg[:], L[:, 0:4], sc[:, 0:1], None, op0=ALU.is_equal))
        dv(lambda e: e.tensor_scalar(sc[:, 1:2], sc[:, 0:1], -1.0, None, op0=ALU.mult))
        kb.op("act", lambda e: e.activation(eg[:], L[:, 0:4], AF.Exp, bias=sc[:, 1:2]), reads=[wr_], writes=[wr_])
        dv(lambda e: e.reduce_sum(sc[:, 2:3], eg[:], axis=AX.X))
        dv(lambda e: e.reciprocal(sc[:, 3:4], sc[:, 2:3]))
        dv(lambda e: e.tensor_scalar(lsel[:], L[:, 4:12], ohg[:, 0:1], None, op0=ALU.mult))
        for g in range(1, 4):
            dv(lambda e, g=g: e.scalar_tensor_tensor(lsel[:], L[:, 4 + 8 * g:12 + 8 * g], ohg[:, g:g + 1], lsel[:], op0=ALU.mult, op1=ALU.add))
        dv(lambda e: e.reduce_max(sc[:, 4:5], lsel[:], axis=AX.X))
        dv(lambda e: e.tensor_scalar(oh1[:], lsel[:], sc[:, 4:5], None, op0=ALU.is_equal))
        dv(lambda e: e.scalar_tensor_tensor(lsel2[:], oh1[:], -1e30, lsel[:], op0=ALU.mult, op1=ALU.add))
        dv(lambda e: e.reduce_max(sc[:, 5:6], lsel2[:], axis=AX.X))
        dv(lambda e: e.tensor_scalar(oh2[:], lsel2[:], sc[:, 5:6], None, op0=ALU.is_equal))
        dv(lambda e: e.tensor_tensor(sc[:, 6:7], sc[:, 5:6], sc[:, 4:5], op=ALU.subtract))
        kb.op("act", lambda e: e.activation(sc[:, 7:8], sc[:, 6:7], AF.Exp), reads=[wr_], writes=[wr_])
        dv(lambda e: e.tensor_scalar(sc[:, 8:9], sc[:, 7:8], 1.0, None, op0=ALU.add))
        dv(lambda e: e.reciprocal(sc[:, 9:10], sc[:, 8:9]))
        dv(lambda e: e.tensor_tensor(sc[:, 10:11], sc[:, 7:8], sc[:, 9:10], op=ALU.mult))
        for g in range(4):
            dv(lambda e, g=g: e.tensor_scalar(E1[:, 8 * g:8 * g + 8], oh1[:], ohg[:, g:g + 1], None, op0=ALU.mult))
            dv(lambda e, g=g: e.tensor_scalar(E2[:, 8 * g:8 * g + 8], oh2[:], ohg[:, g:g + 1], None, op0=ALU.mult))
        dv(lambda e: e.tensor_tensor(G01[:], E1[:], E2[:], op=ALU.add))
        kb.op("pe", lambda e: e.matmul(ps[3][:, 0:32], C.triu_b[:], G01[:], start=True, stop=True), reads=[wr_, C.r], writes=[ps_r[3]])
        kb.op("pe", lambda e: e.matmul(ps[3][:, 32:64], C.ones_b[:], G01[:], start=True, stop=True), reads=[wr_, C.r], writes=[ps_r[3]])
        dv(lambda e: e.tensor_tensor(val[:], ps[3][:, 0:32], base[:], op=ALU.add), extra_r=[ps_r[3], base_r])
        dv(lambda e: e.tensor_scalar(valid[:], val[:], float(cap), None, op0=ALU.is_le))
        dv(lambda e: e.tensor_tensor(val2[:], val[:], offs[:], op=ALU.add), extra_r=[cr])
        dv(lambda e: e.scalar_tensor_tensor(val2[:], val2[:], -BIG, valid[:], op0=ALU.add, op1=ALU.mult))
        dv(lambda e: e.tensor_scalar(val2[:], val2[:], BIG, None, op0=ALU.add))
        dv(lambda e: e.tensor_tensor(tmp[:], E1[:], val2[:], op=ALU.mult))
        dv(lambda e: e.reduce_sum(sc[:, 11:12], tmp[:], axis=AX.X))
        dv(lambda e: e.tensor_tensor(tmp[:], E2[:], val2[:], op=ALU.mult))
        dv(lambda e: e.reduce_sum(sc[:, 12:13], tmp[:], axis=AX.X))
        dv(lambda e: e.tensor_tensor(tmp[:], E1[:], valid[:], op=ALU.mult))
        dv(lambda e: e.reduce_sum(sc[:, 13:14], tmp[:], axis=AX.X))
        dv(lambda e: e.tensor_tensor(tmp[:], E2[:], valid[:], op=ALU.mult))
        dv(lambda e: e.reduce_sum(sc[:, 14:15], tmp[:], axis=AX.X))
        dv(lambda e: e.tensor_tensor(base[:], base[:], ps[3][:, 32:64], op=ALU.add), extra_r=[ps_r[3]], extra_w=[base_r])
        dv(lambda e, i=i: e.tensor_copy(slots[:, i, :], sc[:, 11:13]), extra_w=[sg_r])
        dv(lambda e: e.tensor_scalar(sc[:, 9:11], sc[:, 9:11], sc[:, 3:4], None, op0=ALU.mult))
        dv(lambda e, i=i: e.tensor_tensor(gates[:, i, :], sc[:, 9:11], sc[:, 13:15], op=ALU.mult), extra_w=[sg_r])
        for k in range(2):
            kb.dma("pool", lambda e, i=i, k=k, b=b: e.indirect_dma_start(out=xbuf[:, :], out_offset=bass.IndirectOffsetOnAxis(ap=slots[:, i, k:k + 1], axis=0),
                                                                       in_=xb16[b][:], in_offset=None, bounds_check=bc_reg, oob_is_err=False),
                   reads=[sg_r, xb_r[b]])
    kb_pop(kb)

    kb_push(kb)
    psT = [kb.ps("m2_pT%d" % b, [128, 1024], BF16) for b in range(2)]
    psT_r = RL(2)
    psH = [kb.ps("m2_pH%d" % b, [128, 512], F32) for b in range(4)]
    psH_r = RL(4)
    psY = [kb.ps("m2_pY%d" % b, [128, 512], F32) for b in range(2)]
    psY_r = RL(2)
    wgu = [kb.sb("m2_wgu%d" % b, [128, 8, 1024], BF16) for b in range(2)]
    wgu_r = RL(2)
    wd = [kb.sb("m2_wd%d" % b, [128, 4, 1024], BF16) for b in range(2)]
    wd_r = RL(2)
    xblk = [kb.sb("m2_xb%d" % b, [128, 1024], BF16) for b in range(2)]
    xblk_r = RL(2)
    XT = [kb.sb("m2_XT%d" % b, [128, 8, cap], BF16) for b in range(2)]
    XT_r = RL(2)
    sil = [kb.sb("m2_sil%d" % b, [128, cap], F32) for b in range(2)]
    sil_r = RL(2)
    AT = [kb.sb("m2_AT%d" % b, [128, 4, cap], BF16) for b in range(2)]
    AT_r = RL(2)
    ysb = [kb.sb("m2_y%d" % b, [128, 1024], F32) for b in range(2)]
    ysb_r = RL(2)
    nblk = 0
    for ex in range(NE):
        wb = ex % 2
        gsrc = prm["moe_w_gu"][li, ex].rearrange("(kc p) n -> p kc n", p=128)
        dsrc = prm["moe_w_down"][li, ex].rearrange("(m p) n -> p m n", p=128)
        for kc in range(8):
            kb.dma("pool", lambda e, wb=wb, kc=kc, gsrc=gsrc: e.dma_start(out=wgu[wb][:, kc, :], in_=gsrc[:, kc, :]), writes=[wgu_r[wb]])
        for m in range(4):
            kb.dma("pool", lambda e, wb=wb, m=m, dsrc=dsrc: e.dma_start(out=wd[wb][:, m, :], in_=dsrc[:, m, :]), writes=[wd_r[wb]])
        for blk in range(NB):
            bb = nblk % 2
            nblk += 1
            r0 = ex * cap + blk * 128
            kb.dma("sp", lambda e, bb=bb, r0=r0: e.dma_start(out=xblk[bb][:], in_=xbuf[r0:r0 + 128, :]), writes=[xblk_r[bb]])
            for kc in range(8):
                kb.op("pe", lambda e, bb=bb, kc=kc: e.transpose(psT[bb][:, kc * 128:(kc + 1) * 128], xblk[bb][:, kc * 128:(kc + 1) * 128], C.ident_b[:]),
                      reads=[xblk_r[bb], C.r], writes=[psT_r[bb]])
            eng = "act" if blk % 2 == 0 else "dve"
            if eng == "act":
                kb.op("act", lambda e, bb=bb, wb=wb, blk=blk: e.activation(XT[wb][:, :, blk * 128:(blk + 1) * 128], psT[bb][:].rearrange("p (c t) -> p c t", c=8), AF.Copy),
                      reads=[psT_r[bb]], writes=[XT_r[wb]])
            else:
                kb.op("dve", lambda e, bb=bb, wb=wb, blk=blk: e.tensor_copy(XT[wb][:, :, blk * 128:(blk + 1) * 128], psT[bb][:].rearrange("p (c t) -> p c t", c=8)),
                      reads=[psT_r[bb]], writes=[XT_r[wb]])
        for m in range(4):
            pg, pu = (m % 2) * 2, (m % 2) * 2 + 1
            for (pb, col) in ((pg, m * 128), (pu, 512 + m * 128)):
                for kc in range(8):
                    kb.op("pe", lambda e, pb=pb, col=col, kc=kc, wb=wb: e.matmul(psH[pb][:, 0:cap], wgu[wb][:, kc, col:col + 128], XT[wb][:, kc, :], start=(kc == 0), stop=(kc == 7)),
                          reads=[wgu_r[wb], XT_r[wb]], writes=[psH_r[pb]])
            sb_ = m % 2
            kb.op("act", lambda e, pg=pg, sb_=sb_: e.activation(sil[sb_][:], psH[pg][:, 0:cap], AF.Silu), reads=[psH_r[pg]], writes=[sil_r[sb_]])
            kb.op("dve", lambda e, pu=pu, sb_=sb_, m=m, wb=wb: e.tensor_tensor(AT[wb][:, m, :], sil[sb_][:], psH[pu][:, 0:cap], op=ALU.mult),
                  reads=[psH_r[pu], sil_r[sb_]], writes=[AT_r[wb]])
        for blk in range(NB):
            yb = blk % 2
            for hf in range(2):
                for m in range(4):
                    kb.op("pe", lambda e, hf=hf, m=m, wb=wb, blk=blk: e.matmul(psY[hf][:], AT[wb][:, m, blk * 128:(blk + 1) * 128], wd[wb][:, m, hf * 512:(hf + 1) * 512], start=(m == 0), stop=(m == 3)),
                          reads=[AT_r[wb], wd_r[wb]], writes=[psY_r[hf]])
                if hf == 0:
                    kb.op("act", lambda e, yb=yb: e.activation(ysb[yb][:, 0:512], psY[0][:], AF.Copy), reads=[psY_r[0]], writes=[ysb_r[yb]])
                else:
                    kb.op("dve", lambda e, yb=yb: e.tensor_copy(ysb[yb][:, 512:1024], psY[1][:]), reads=[psY_r[1]], writes=[ysb_r[yb]])
            r0 = ex * cap + blk * 128
            kb.dma("sp", lambda e, yb=yb, r0=r0: e.dma_start(out=ybuf[r0:r0 + 128, :], in_=ysb[yb][:]), reads=[ysb_r[yb]])
    kb_pop(kb)

    kb_push(kb)
    y1 = [kb.sb("m3_y1%d" % b, [128, 1024], F32) for b in range(2)]
    y2 = [kb.sb("m3_y2%d" % b, [128, 1024], F32) for b in range(2)]
    y_r = RL(2)
    xt3 = [kb.sb("m3_xt%d" % b, [128, 1024], F32) for b in range(2)]
    xt3_r = RL(2)
    z3 = [kb.sb("m3_z%d" % b, [128, 1024], F32) for b in range(2)]
    z3_r = RL(2)
    x2 = [kb.sb("m3_x2%d" % b, [128, 1024], F32) for b in range(2)]
    x2_r = RL(2)
    LS = ln_scratch(kb, "m3_ln", LN_EPS)
    g_bc, b_bc, lnp_r = load_ln_params(kb, "m3_lnp", prm["ln2_g"], prm["ln2_b"], li)
    for b in range(2):
        kb.op("pool", lambda e, b=b: e.memset(y1[b][:], 0.0), writes=[y_r[b]])
        kb.op("pool", lambda e, b=b: e.memset(y2[b][:], 0.0), writes=[y_r[b]])
    for i in range(NT):
        b = i % 2
        kb.dma("sp", lambda e, i=i, b=b: e.dma_start(out=xt3[b][:], in_=xin[i * 128:(i + 1) * 128, :]), reads=[xin_r[i]], writes=[xt3_r[b]])
        for k, yy in ((0, y1), (1, y2)):
            kb.dma("pool", lambda e, i=i, k=k, b=b, yy=yy: e.indirect_dma_start(out=yy[b][:], out_offset=None, in_=ybuf[:, :],
                                                                              in_offset=bass.IndirectOffsetOnAxis(ap=slots[:, i, k:k + 1], axis=0),
                                                                              bounds_check=bc_reg, oob_is_err=False),
                   reads=[sg_r], writes=[y_r[b]])
        kb.op("act", lambda e, b=b: e.activation(z3[b][:], xt3[b][:], AF.Copy, scale=ALPHA), reads=[xt3_r[b]], writes=[z3_r[b]])
        kb.op("dve", lambda e, b=b, i=i: e.scalar_tensor_tensor(z3[b][:], y1[b][:], gates[:, i, 0:1], z3[b][:], op0=ALU.mult, op1=ALU.add), reads=[y_r[b], sg_r], writes=[z3_r[b]])
        kb.op("dve", lambda e, b=b, i=i: e.scalar_tensor_tensor(z3[b][:], y2[b][:], gates[:, i, 1:2], z3[b][:], op0=ALU.mult, op1=ALU.add), reads=[y_r[b], sg_r], writes=[z3_r[b]])
        ln_tile(kb, LS, z3[b], z3_r[b], x2[b][:], x2_r[b], g_bc, b_bc, lnp_r)
        kb.dma("sp", lambda e, i=i, b=b: e.dma_start(out=dst[i * 128:(i + 1) * 128, :], in_=x2[b][:]), reads=[x2_r[b]], writes=[dst_r[i]])
    kb_pop(kb)
    kb_pop(kb)


C0 = math.exp(-0.5)


def rwkv_phase(kb, C, T, prm, j, li, xin, xin_r, xa, xa_r, ST, nc):
    NT = T // 128
    NSUP = T // 256
    if "ARd" not in ST:
        ST["ARd"] = nc.dram_tensor("ARd_s", [NT, 128, 8 * 2 * 128], BF16, kind="Internal").ap()
        ST["BKd"] = nc.dram_tensor("BKd_s", [NT, 128, 8 * 2 * 128], BF16, kind="Internal").ap()
        ST["rkd"] = nc.dram_tensor("rkd_s", [NT, 128, 8 * 128], BF16, kind="Internal").ap()
        ST["Pcd"] = nc.dram_tensor("Pcd_s", [NT, 128, 8], F32, kind="Internal").ap()
        ST["Vd"] = nc.dram_tensor("Vd_s", [T, D], BF16, kind="Internal").ap()
        ST["Gd"] = nc.dram_tensor("Gd_s", [T, D], BF16, kind="Internal").ap()
        ST["vfirst"] = nc.dram_tensor("vfirst_s", [T, D], F32, kind="Internal").ap()
    ARd, BKd, rkd, Pcd, Vd, Gd, vfd = (ST[k] for k in ("ARd", "BKd", "rkd", "Pcd", "Vd", "Gd", "vfirst"))

    kb_push(kb)
    ps = [kb.ps("r1_ps%d" % b, [128, 512], F32) for b in range(8)]
    ps_r = RL(8)
    wrkv = kb.sb("r1_wrkv", [128, 3, 8, 1024], BF16)
    w1 = kb.sb("r1_w1", [128, 8, 64], BF16)
    a1 = kb.sb("r1_a1", [128, 8, 64], BF16)
    g1 = kb.sb("r1_g1", [128, 8, 160], BF16)
    w2 = kb.sb("r1_w2", [64, 1024], BF16)
    a2 = kb.sb("r1_a2", [64, 1024], BF16)
    g2a = kb.sb("r1_g2a", [128, 1024], BF16)
    g2b = kb.sb("r1_g2b", [32, 1024], BF16)
    wr_ = Res()
    for n in range(3):
        src = prm["rw_w_rkv"][j, n].rearrange("(kc p) n -> p kc n", p=128)
        for kc in range(8):
            kb.dma("pool", lambda e, n=n, kc=kc, src=src: e.dma_start(out=wrkv[:, n, kc, :], in_=src[:, kc, :]), writes=[wr_])
    kb.dma("pool", lambda e: e.dma_start(out=w1[:], in_=prm["rw_w1"][j].rearrange("(kc p) n -> p kc n", p=128)), writes=[wr_])
    kb.dma("pool", lambda e: e.dma_start(out=a1[:], in_=prm["rw_a1"][j].rearrange("(kc p) n -> p kc n", p=128)), writes=[wr_])
    kb.dma("pool", lambda e: e.dma_start(out=g1[:], in_=prm["rw_g1"][j].rearrange("(kc p) n -> p kc n", p=128)), writes=[wr_])
    kb.dma("pool", lambda e: e.dma_start(out=w2[:], in_=prm["rw_w2"][j]), writes=[wr_])
    kb.dma("pool", lambda e: e.dma_start(out=a2[:], in_=prm["rw_a2"][j]), writes=[wr_])
    kb.dma("pool", lambda e: e.dma_start(out=g2a[:], in_=prm["rw_g2"][j, 0:128, :]), writes=[wr_])
    kb.dma("pool", lambda e: e.dma_start(out=g2b[:], in_=prm["rw_g2"][j, 128:160, :]), writes=[wr_])
    if j > 0:
        v1 = kb.sb("r1_v1", [128, 8, 32], BF16)
        v2 = kb.sb("r1_v2", [32, 1024], BF16)
        v0b = kb.sb("r1_v0b", [128, 1024], F32)
        kb.dma("pool", lambda e: e.dma_start(out=v1[:], in_=prm["rw_v1"][j - 1].rearrange("(kc p) n -> p kc n", p=128)), writes=[wr_])
        kb.dma("pool", lambda e: e.dma_start(out=v2[:], in_=prm["rw_v2"][j - 1]), writes=[wr_])
        kb.dma("sp", lambda e: e.dma_start(out=v0b[:], in_=bcast_rows(prm["rw_v0"][j - 1:j, :])), writes=[wr_])
    pvin = kb.sb("r1_pvin", [88, 128], F32)
    pvall = kb.sb("r1_pvall", [128, 88], F32)
    oma = kb.sb("r1_oma", [128, 8], F32)
    pv_r = Res()
    kb.dma("sp", lambda e: e.dma_start(out=pvin[0:48, :], in_=prm["rw_mu"][j].rearrange("n (kc p) -> (n kc) p", p=128)), writes=[pv_r])
    for idx, nm in enumerate(("rw_w0", "rw_a0", "rw_k_k", "rw_k_a")):
        kb.dma("sp", lambda e, idx=idx, nm=nm: e.dma_start(out=pvin[48 + 8 * idx:56 + 8 * idx, :], in_=prm[nm][j].rearrange("(oc p) -> oc p", p=128)), writes=[pv_r])
    kb.dma("sp", lambda e: e.dma_start(out=pvin[80:88, :], in_=prm["rw_r_k"][j].rearrange("(oc hh) n -> oc (hh n)", hh=2)), writes=[pv_r])
    kb.op("pe", lambda e: e.transpose(ps[0][:, 0:88], pvin[:], C.ident_f[0:88, 0:88]), reads=[pv_r, C.r], writes=[ps_r[0]])
    kb.op("dve", lambda e: e.tensor_copy(pvall[:], ps[0][:, 0:88]), reads=[ps_r[0]], writes=[pv_r])
    kb.op("dve", lambda e: e.tensor_scalar(oma[:], pvall[:, 72:80], -1.0, 1.0, op0=ALU.mult, op1=ALU.add), reads=[pv_r], writes=[pv_r])

    class _PV:
        def __getitem__(self, key):
            p, idx, oc = key
            if idx == 5:
                return oma[p, oc]
            return pvall[p, (oc.start + 48 + 8 * idx):(oc.stop + 48 + 8 * idx)]

    class _MU:
        def __getitem__(self, key):
            p, n, kc = key
            return pvall[p, (n * 8 + kc.start):(n * 8 + kc.stop)]

    pv = _PV()
    mu = _MU()
    rst = kb.sb("r1_rst", [128, 256], F32)
    kb.op("dve", lambda e: e.memset(rst[:], 1.0), writes=[pv_r])
    kb.op("dve", lambda e: e.memset(rst[:, 0:1], 0.0), writes=[pv_r])
    kb.op("dve", lambda e: e.memset(rst[:, 128:129], 0.0), writes=[pv_r])
    bd64 = kb.sb("r1_bd64", [128, 128], BF16)
    kb.op("dve", lambda e: e.memset(bd64[:], 0.0), writes=[pv_r])
    kb.op("dve", lambda e: e.memset(bd64[0:64, 0:64], 1.0), writes=[pv_r])
    kb.op("dve", lambda e: e.memset(bd64[64:128, 64:128], 1.0), writes=[pv_r])

    xld = [kb.sb("r1_xld%d" % b, [128, 1024], F32) for b in range(2)]
    xld_r = RL(2)
    xTs = [kb.sb("r1_xTs%d" % b, [128, 8, 257], BF16) for b in range(2)]
    xTs_r = RL(2)
    xx = kb.sb("r1_xx", [128, 8, 256], F32)
    xx_r = Res()
    xm = [kb.sb("r1_xm%d" % b, [128, 8, 256], BF16) for b in range(3)]
    xm_r = RL(3)
    AR = kb.sb("r1_AR", [128, 8, 2, 2, 128], BF16)
    BK = kb.sb("r1_BK", [128, 8, 2, 2, 128], BF16)
    rk = kb.sb("r1_rk", [128, 8, 256], BF16)
    Pc = kb.sb("r1_Pc", [128, 2, 8], F32)
    out_r = Res()
    hw = kb.sb("r1_hw", [64, 256], BF16)
    ha = kb.sb("r1_ha", [64, 256], BF16)
    hg1 = kb.sb("r1_hg1", [128, 256], BF16)
    hg2 = kb.sb("r1_hg2", [32, 256], BF16)
    hid_r = Res()
    if j > 0:
        hv = kb.sb("r1_hv", [32, 256], BF16)
    tnames = ("sgw", "cum", "cumx", "pin", "pinv", "pprev", "asig", "kk", "lns", "rn", "kkn", "t1", "k2", "tb")
    tmS = [{k: kb.sb("r1_t%d%s" % (q, k), [128, 256], F32) for k in tnames} for q in range(2)]
    kk2S = [kb.sb("r1_kk2_%d" % q, [128, 256], BF16) for q in range(2)]
    tm_rS = RL(2)
    vsb = [kb.sb("r1_v%d" % b, [128, 1024], F32) for b in range(2)]
    vsb_r = RL(2)
    vb16 = [kb.sb("r1_vb%d" % b, [128, 1024], BF16) for b in range(2)]
    vb_r = RL(2)
    gsb = [kb.sb("r1_g%d" % b, [128, 1024], BF16) for b in range(2)]
    gsb_r = RL(2)
    if j > 0:
        vfs = [kb.sb("r1_vf%d" % b, [128, 1024], F32) for b in range(2)]
        vfs_r = RL(2)
        vmx = [kb.sb("r1_vm%d" % b, [128, 1024], F32) for b in range(2)]
        vmx_r = RL(2)
    kb.op("dve", lambda e: e.memset(xTs[0][:, :, 0:1], 0.0), writes=[xTs_r[0]])

    def mix(n, buf, xb):
        for kc in range(8):
            kb.op("dve", lambda e, kc=kc: e.scalar_tensor_tensor(xm[buf][:, kc, :], xx[:, kc, :], mu[:, n, kc:kc + 1], xTs[xb][:, kc, 1:257], op0=ALU.mult, op1=ALU.add),
                  reads=[xx_r, pv_r, xTs_r[xb]], writes=[xm_r[buf]])

    for s in range(NSUP):
        xb = s % 2
        if s > 0:
            kb.op("pool", lambda e, xb=xb: e.tensor_copy(xTs[xb][:, :, 0:1], xTs[1 - xb][:, :, 256:257]), reads=[xTs_r[1 - xb]], writes=[xTs_r[xb]])
        for tl in range(2):
            i = s * 2 + tl
            b = i % 2
            kb.dma("sp", lambda e, i=i, b=b: e.dma_start(out=xld[b][:], in_=xin[i * 128:(i + 1) * 128, :]), reads=[xin_r[i]], writes=[xld_r[b]])
            for hf in range(2):
                pb = 6 + hf
                for c4 in range(4):
                    kc = hf * 4 + c4
                    kb.op("pe", lambda e, pb=pb, c4=c4, kc=kc, b=b: e.transpose(ps[pb][:, c4 * 128:(c4 + 1) * 128], xld[b][:, kc * 128:(kc + 1) * 128], C.ident_f[:]),
                          reads=[xld_r[b], C.r], writes=[ps_r[pb]])
                if hf == 0:
                    kb.op("act", lambda e, pb=pb, xb=xb, tl=tl: e.activation(xTs[xb][:, 0:4, 1 + tl * 128:1 + (tl + 1) * 128], ps[pb][:].rearrange("p (c t) -> p c t", c=4), AF.Copy),
                          reads=[ps_r[pb]], writes=[xTs_r[xb]])
                else:
                    kb.op("dve", lambda e, pb=pb, xb=xb, tl=tl: e.tensor_copy(xTs[xb][:, 4:8, 1 + tl * 128:1 + (tl + 1) * 128], ps[pb][:].rearrange("p (c t) -> p c t", c=4)),
                          reads=[ps_r[pb]], writes=[xTs_r[xb]])
        kb.op("dve", lambda e, xb=xb: e.tensor_tensor(xx[:], xTs[xb][:, :, 0:256], xTs[xb][:, :, 1:257], op=ALU.subtract), reads=[xTs_r[xb]], writes=[xx_r])
        mix(3, 0, xb)
        for kc in range(8):
            kb.op("pe", lambda e, kc=kc: e.matmul(ps[5][0:64, 0:256], w1[:, kc, :], xm[0][:, kc, :], start=(kc == 0), stop=(kc == 7)), reads=[wr_, xm_r[0]], writes=[ps_r[5]])
        kb.op("act", lambda e: e.activation(hw[:], ps[5][0:64, 0:256], AF.Tanh), reads=[ps_r[5]], writes=[hid_r])
        mix(4, 1, xb)
        for kc in range(8):
            kb.op("pe", lambda e, kc=kc: e.matmul(ps[5][0:64, 256:512], a1[:, kc, :], xm[1][:, kc, :], start=(kc == 0), stop=(kc == 7)), reads=[wr_, xm_r[1]], writes=[ps_r[5]])
        kb.op("act", lambda e: e.activation(ha[:], ps[5][0:64, 256:512], AF.Copy), reads=[ps_r[5]], writes=[hid_r])
        mix(5, 2, xb)
        for kc in range(8):
            kb.op("pe", lambda e, kc=kc: e.matmul(ps[4][:, 0:256], g1[:, kc, 0:128], xm[2][:, kc, :], start=(kc == 0), stop=(kc == 7)), reads=[wr_, xm_r[2]], writes=[ps_r[4]])
        for kc in range(8):
            kb.op("pe", lambda e, kc=kc: e.matmul(ps[4][0:32, 256:512], g1[:, kc, 128:160], xm[2][:, kc, :], start=(kc == 0), stop=(kc == 7)), reads=[wr_, xm_r[2]], writes=[ps_r[4]])
        kb.op("act", lambda e: e.activation(hg1[:], ps[4][:, 0:256], AF.Sigmoid), reads=[ps_r[4]], writes=[hid_r])
        kb.op("act", lambda e: e.activation(hg2[:], ps[4][0:32, 256:512], AF.Sigmoid), reads=[ps_r[4]], writes=[hid_r])
        for tl in range(2):
            i = s * 2 + tl
            b = i % 2
            for hf in range(2):
                pb = 6 + hf
                kb.op("pe", lambda e, pb=pb, tl=tl, hf=hf: e.matmul(ps[pb][:], hg1[:, tl * 128:(tl + 1) * 128], g2a[:, hf * 512:(hf + 1) * 512], start=True, stop=False), reads=[hid_r, wr_], writes=[ps_r[pb]])
                kb.op("pe", lambda e, pb=pb, tl=tl, hf=hf: e.matmul(ps[pb][:], hg2[:, tl * 128:(tl + 1) * 128], g2b[:, hf * 512:(hf + 1) * 512], start=False, stop=True), reads=[hid_r, wr_], writes=[ps_r[pb]])
                if hf == 0:
                    kb.op("act", lambda e, pb=pb, b=b: e.activation(gsb[b][:, 0:512], ps[pb][:], AF.Copy), reads=[ps_r[pb]], writes=[gsb_r[b]])
                else:
                    kb.op("dve", lambda e, pb=pb, b=b: e.tensor_copy(gsb[b][:, 512:1024], ps[pb][:]), reads=[ps_r[pb]], writes=[gsb_r[b]])
            kb.dma("sp", lambda e, i=i, b=b: e.dma_start(out=Gd[i * 128:(i + 1) * 128, :], in_=gsb[b][:]), reads=[gsb_r[b]])
        mix(2, 0, xb)
        if j > 0:
            for kc in range(8):
                kb.op("pe", lambda e, kc=kc: e.matmul(ps[5][0:32, 0:256], v1[:, kc, :], xm[0][:, kc, :], start=(kc == 0), stop=(kc == 7)), reads=[wr_, xm_r[0]], writes=[ps_r[5]])
            kb.op("act", lambda e: e.activation(hv[:], ps[5][0:32, 0:256], AF.Copy), reads=[ps_r[5]], writes=[hid_r])
        for tl in range(2):
            i = s * 2 + tl
            b = i % 2
            for hf in range(2):
                pb = 6 + hf
                for kc in range(8):
                    kb.op("pe", lambda e, pb=pb, tl=tl, hf=hf, kc=kc: e.matmul(ps[pb][:], xm[0][:, kc, tl * 128:(tl + 1) * 128], wrkv[:, 2, kc, hf * 512:(hf + 1) * 512], start=(kc == 0), stop=(kc == 7)),
                          reads=[xm_r[0], wr_], writes=[ps_r[pb]])
                if hf == 0:
                    kb.op("act", lambda e, pb=pb, b=b: e.activation(vsb[b][:, 0:512], ps[pb][:], AF.Copy), reads=[ps_r[pb]], writes=[vsb_r[b]])
                else:
                    kb.op("dve", lambda e, pb=pb, b=b: e.tensor_copy(vsb[b][:, 512:1024], ps[pb][:]), reads=[ps_r[pb]], writes=[vsb_r[b]])
            if j == 0:
                kb.dma("sp", lambda e, i=i, b=b: e.dma_start(out=vfd[i * 128:(i + 1) * 128, :], in_=vsb[b][:]), reads=[vsb_r[b]])
                kb.op("pool", lambda e, b=b: e.tensor_copy(vb16[b][:], vsb[b][:]), reads=[vsb_r[b]], writes=[vb_r[b]])
            else:
                kb.dma("sp", lambda e, i=i, b=b: e.dma_start(out=vfs[b][:], in_=vfd[i * 128:(i + 1) * 128, :]), writes=[vfs_r[b]])
                for hf in range(2):
                    pb = 6 + hf
                    kb.op("pe", lambda e, pb=pb, tl=tl, hf=hf: e.matmul(ps[pb][:], hv[:, tl * 128:(tl + 1) * 128], v2[:, hf * 512:(hf + 1) * 512], start=True, stop=True), reads=[hid_r, wr_], writes=[ps_r[pb]])
                    kb.op("dve", lambda e, pb=pb, b=b, hf=hf: e.tensor_tensor(vmx[b][:, hf * 512:(hf + 1) * 512], ps[pb][:], v0b[:, hf * 512:(hf + 1) * 512], op=ALU.add), reads=[ps_r[pb], wr_], writes=[vmx_r[b]])
                kb.op("act", lambda e, b=b: e.activation(vmx[b][:], vmx[b][:], AF.Sigmoid), reads=[vmx_r[b]], writes=[vmx_r[b]])
                kb.op("pool", lambda e, b=b: e.tensor_tensor(vfs[b][:], vfs[b][:], vsb[b][:], op=ALU.subtract), reads=[vfs_r[b], vsb_r[b]], writes=[vfs_r[b]])
                kb.op("pool", lambda e, b=b: e.tensor_tensor(vfs[b][:], vfs[b][:], vmx[b][:], op=ALU.mult), reads=[vfs_r[b], vmx_r[b]], writes=[vfs_r[b]])
                kb.op("pool", lambda e, b=b: e.tensor_tensor(vb16[b][:], vfs[b][:], vsb[b][:], op=ALU.add), reads=[vfs_r[b], vsb_r[b]], writes=[vb_r[b]])
            kb.dma("sp", lambda e, i=i, b=b: e.dma_start(out=Vd[i * 128:(i + 1) * 128, :], in_=vb16[b][:]), reads=[vb_r[b]])
        mix(0, 1, xb)
        mix(1, 2, xb)
        def oc_chain(oc):
            par = oc % 2
            T_ = tmS[par]
            kk2_ = kk2S[par]
            tmr = tm_rS[par]
            psum_kk = ps[5] if par == 0 else ps[4]
            psum_kk_r = ps_r[5] if par == 0 else ps_r[4]
            osl = slice(oc * 128, (oc + 1) * 128)
            pr, pk = par * 2, par * 2 + 1
            for kc in range(8):
                kb.op("pe", lambda e, pr=pr, kc=kc, osl=osl: e.matmul(ps[pr][:, 0:256], wrkv[:, 0, kc, osl], xm[1][:, kc, :], start=(kc == 0), stop=(kc == 7)), reads=[wr_, xm_r[1]], writes=[ps_r[pr]])
            for kc in range(8):
                kb.op("pe", lambda e, pk=pk, kc=kc, osl=osl: e.matmul(ps[pk][:, 0:256], wrkv[:, 1, kc, osl], xm[2][:, kc, :], start=(kc == 0), stop=(kc == 7)), reads=[wr_, xm_r[2]], writes=[ps_r[pk]])
            kb.op("pe", lambda e, pr=pr, osl=osl: e.matmul(ps[pr][:, 256:512], w2[:, osl], hw[:], start=True, stop=True), reads=[wr_, hid_r], writes=[ps_r[pr]])
            kb.op("pe", lambda e, pk=pk, osl=osl: e.matmul(ps[pk][:, 256:512], a2[:, osl], ha[:], start=True, stop=True), reads=[wr_, hid_r], writes=[ps_r[pk]])
            r_ps, k_ps, w_ps, a_ps = ps[pr][:, 0:256], ps[pk][:, 0:256], ps[pr][:, 256:512], ps[pk][:, 256:512]
            R2 = [ps_r[pr], ps_r[pk], tmr, pv_r]
            L = []

            def o(eng, fn, extra_w=()):
                L.append((eng, fn, R2, [tmr] + list(extra_w)))

            o("act", lambda e: e.activation(T_["sgw"][:], w_ps, AF.Sigmoid, bias=pv[:, 0, oc:oc + 1]))
            o("act", lambda e: e.activation(T_["asig"][:], a_ps, AF.Sigmoid, bias=pv[:, 1, oc:oc + 1]))
            o("act", lambda e: e.activation(T_["kk"][:], k_ps, AF.Identity, scale=pv[:, 2, oc:oc + 1]))
            o("dve", lambda e: e.tensor_tensor_scan(T_["cum"][:], rst[:], T_["sgw"][:], 0.0, op0=ALU.mult, op1=ALU.add))
            o("pool", lambda e: e.tensor_tensor(kk2_[:], T_["kk"][:], T_["kk"][:], op=ALU.mult))
            L.append(("pe", lambda e: e.matmul(psum_kk[:, 0:256], bd64[:], kk2_[:], start=True, stop=True), [tmr, pv_r], [psum_kk_r]))
            o("pool", lambda e: e.tensor_tensor(T_["cumx"][:], T_["cum"][:], T_["sgw"][:], op=ALU.subtract))
            o("act", lambda e: e.activation(T_["pin"][:], T_["cum"][:], AF.Exp, scale=-C0))
            o("act", lambda e: e.activation(T_["pinv"][:], T_["cum"][:], AF.Exp, scale=C0))
            o("act", lambda e: e.activation(T_["pprev"][:], T_["cumx"][:], AF.Exp, scale=-C0))
            L.append(("dve", lambda e: e.tensor_scalar(T_["lns"][:], psum_kk[:, 0:256], 1e-30, None, op0=ALU.add), [psum_kk_r, tmr], [tmr]))
            o("act", lambda e: e.activation(T_["lns"][:], T_["lns"][:], AF.Ln))
            o("act", lambda e: e.activation(T_["rn"][:], T_["lns"][:], AF.Exp, scale=-0.5))
            o("pool", lambda e: e.tensor_tensor(T_["kkn"][:], T_["kk"][:], T_["rn"][:], op=ALU.mult))
            o("dve", lambda e: e.tensor_scalar(T_["t1"][:], T_["asig"][:], pv[:, 3, oc:oc + 1], pv[:, 5, oc:oc + 1], op0=ALU.mult, op1=ALU.add))
            o("dve", lambda e: e.tensor_tensor(T_["k2"][:], k_ps, T_["t1"][:], op=ALU.mult))
            for tl_ in range(2):
                o("dve", lambda e, tl_=tl_: e.tensor_copy(Pc[:, tl_, oc:oc + 1], T_["pin"][:, 127 + 128 * tl_:128 + 128 * tl_]), extra_w=[out_r])
            o("dve", lambda e: e.scalar_tensor_tensor(AR[:, oc, :, 0, :], T_["kkn"][:].rearrange("p (a t) -> p a t", a=2), -1.0, T_["pprev"][:].rearrange("p (a t) -> p a t", a=2), op0=ALU.mult, op1=ALU.mult), extra_w=[out_r])
            o("dve", lambda e: e.tensor_tensor(AR[:, oc, :, 1, :], r_ps.rearrange("p (a t) -> p a t", a=2), T_["pin"][:].rearrange("p (a t) -> p a t", a=2), op=ALU.mult), extra_w=[out_r])
            o("pool", lambda e: e.tensor_tensor(T_["tb"][:], T_["kkn"][:], T_["asig"][:], op=ALU.mult))
            o("pool", lambda e: e.tensor_tensor(BK[:, oc, :, 0, :], T_["tb"][:].rearrange("p (a t) -> p a t", a=2), T_["pinv"][:].rearrange("p (a t) -> p a t", a=2), op=ALU.mult), extra_w=[out_r])
            o("pool", lambda e: e.tensor_tensor(BK[:, oc, :, 1, :], T_["k2"][:].rearrange("p (a t) -> p a t", a=2), T_["pinv"][:].rearrange("p (a t) -> p a t", a=2), op=ALU.mult), extra_w=[out_r])
            o("dve", lambda e: e.scalar_tensor_tensor(rk[:, oc, :], r_ps, pv[:, 4, oc:oc + 1], T_["k2"][:], op0=ALU.mult, op1=ALU.mult), extra_w=[out_r])
            return L

        for ocp in range(4):
            La = oc_chain(2 * ocp)
            Lb = oc_chain(2 * ocp + 1)
            for ia in range(len(La)):
                kb.op(La[ia][0], La[ia][1], reads=La[ia][2], writes=La[ia][3])
                kb.op(Lb[ia][0], Lb[ia][1], reads=Lb[ia][2], writes=Lb[ia][3])
        for tl in range(2):
            i = s * 2 + tl
            kb.dma("sp", lambda e, i=i, tl=tl: e.dma_start(out=ARd[i].rearrange("p (o c t) -> p o c t", o=8, c=2), in_=AR[:, :, tl, :, :]), reads=[out_r])
            kb.dma("sp", lambda e, i=i, tl=tl: e.dma_start(out=BKd[i].rearrange("p (o c t) -> p o c t", o=8, c=2), in_=BK[:, :, tl, :, :]), reads=[out_r])
            kb.dma("sp", lambda e, i=i, tl=tl: e.dma_start(out=rkd[i].rearrange("p (o t) -> p o t", o=8), in_=rk[:, :, tl * 128:(tl + 1) * 128]), reads=[out_r])
            kb.dma("sp", lambda e, i=i, tl=tl: e.dma_start(out=Pcd[i], in_=Pc[:, tl, :]), reads=[out_r])
    kb_pop(kb)
    if not ST.get("skip_p2"):
        rwkv_pass2(kb, C, T, prm, j, li, xin, xin_r, xa, xa_r, ST, nc)


def rwkv_pass2(kb, C, T, prm, j, li, xin, xin_r, xa, xa_r, ST, nc):
    NT = T // 128
    ARd, BKd, rkd, Pcd, Vd, Gd = (ST[k] for k in ("ARd", "BKd", "rkd", "Pcd", "Vd", "Gd"))
    kb_push(kb)
    pF = [kb.ps("r2_pf%d" % b, [128, 512], F32) for b in range(4)]
    RG = [pF[b][:, 0:256] for b in range(4)]
    RG_r = RL(4)
    rgc = [0]

    def nrg():
        rgc[0] += 1
        return rgc[0] % 4
    pT = kb.ps("r2_pT", [128, 1024], BF16)
    pT_r = Res()
    pY = [kb.ps("r2_pY%d" % b, [128, 512], F32) for b in range(2)]
    pY_r = RL(2)
    pH = kb.ps("r2_pH", [128, 512], F32)
    pH_r = Res()
    wo = kb.sb("r2_wo", [128, 8, 1024], BF16)
    wo_r = Res()
    wsrc = prm["rw_w_o"][j].rearrange("(kc p) n -> p kc n", p=128)
    for kc in range(8):
        kb.dma("pool", lambda e, kc=kc: e.dma_start(out=wo[:, kc, :], in_=wsrc[:, kc, :]), writes=[wo_r])
    cst_r = Res()
    m2 = kb.sb("r2_m2", [128, 2, 128], F32)
    kb.op("dve", lambda e: e.tensor_copy(m2[:, 0, :], C.m_st[:]), reads=[C.r], writes=[cst_r])
    kb.op("dve", lambda e: e.tensor_copy(m2[:, 1, :], C.m_in[:]), reads=[C.r], writes=[cst_r])
    bdm = kb.sb("r2_bdm", [128, 128], F32)
    kb.op("dve", lambda e: e.memset(bdm[:], 0.0), writes=[cst_r])
    kb.op("dve", lambda e: e.memset(bdm[0:64, 0:64], 1.0), writes=[cst_r])
    kb.op("dve", lambda e: e.memset(bdm[64:128, 64:128], 1.0), writes=[cst_r])
    HS = kb.sb("r2_HS", [128, 8, 16], BF16)
    kb.op("dve", lambda e: e.memset(HS[:], 0.0), writes=[cst_r])
    for oc in range(8):
        for hh in range(2):
            kb.op("dve", lambda e, oc=oc, hh=hh: e.memset(HS[hh * 64:(hh + 1) * 64, oc, 2 * oc + hh:2 * oc + hh + 1], 1.0), writes=[cst_r])
    lng = kb.sb("r2_lng", [128, 1024], F32)
    lnb = kb.sb("r2_lnb", [128, 1024], F32)
    kb.dma("sp", lambda e: e.dma_start(out=lng[:], in_=bcast_rows(prm["rw_lnx_g"][j:j + 1, :])), writes=[cst_r])
    kb.dma("sp", lambda e: e.dma_start(out=lnb[:], in_=bcast_rows(prm["rw_lnx_b"][j:j + 1, :])), writes=[cst_r])
    gneps = kb.sb("r2_gneps", [128, 1], F32)
    kb.op("dve", lambda e: e.memset(gneps[:], GN_EPS), writes=[cst_r])
    LS = ln_scratch(kb, "r2_ln", LN_EPS)
    g_bc, b_bc, lnp_r = load_ln_params(kb, "r2_lnp", prm["ln1_g"], prm["ln1_b"], li)

    ARt = [kb.sb("r2_AR%d" % b, [128, 8, 2, 128], BF16) for b in range(2)]
    BKt = [kb.sb("r2_BK%d" % b, [128, 8, 2, 128], BF16) for b in range(2)]
    rkt = [kb.sb("r2_rk%d" % b, [128, 8, 128], BF16) for b in range(2)]
    Pct = [kb.sb("r2_Pc%d" % b, [128, 8], F32) for b in range(2)]
    Vt = [kb.sb("r2_V%d" % b, [128, 1024], BF16) for b in range(2)]
    Gt = [kb.sb("r2_G%d" % b, [128, 1024], BF16) for b in range(2)]
    xt = [kb.sb("r2_xt%d" % b, [128, 1024], F32) for b in range(2)]
    in_r = RL(2)
    Hb = kb.sb("r2_H", [128, 8, 64], BF16)
    Hb_r = RL(8)
    kb.op("dve", lambda e: e.memset(Hb[:], 0.0), writes=Hb_r)
    TK = [kb.sb("r2_TK%d" % u, [128, 3, 128], BF16) for u in range(8)]
    TK_r = RL(8)
    XA = [kb.sb("r2_XA%d" % u, [128, 2, 128], BF16) for u in range(16)]
    KA = [kb.sb("r2_KA%d" % u, [128, 2, 128], BF16) for u in range(16)]
    XAr, KAr = RL(16), RL(16)
    XN = [[kb.sb("r2_XN%d_%d" % (u, q), [128, 2, 128], BF16) for q in range(2)] for u in range(16)]
    XNr = [RL(2) for _ in range(16)]
    PQ = [[kb.sb("r2_PQ%d_%d" % (u, q), [128, 2, 128], BF16) for q in range(2)] for u in range(16)]
    PQr = [RL(2) for _ in range(16)]
    AV = [kb.sb("r2_AV%d" % u, [128, 64], BF16) for u in range(16)]
    AVr = RL(16)
    W12 = [kb.sb("r2_W12%d" % u, [128, 2, 2, 64], BF16) for u in range(8)]
    W12_r = RL(8)
    MT = [kb.sb("r2_MT%d" % u, [128, 128], BF16) for u in range(8)]
    MTf = [kb.sb("r2_MTf%d" % u, [128, 128], F32) for u in range(2)]
    mtf_r = RL(2)
    GS = [kb.sb("r2_GS%d" % u, [128, 64], F32) for u in range(8)]
    QT = [kb.sb("r2_QT%d" % u, [128, 128], BF16) for u in range(8)]
    MT_r, GS_r, QT_r = RL(8), RL(8), RL(8)
    pT_rr = RL(2)
    ysb = kb.sb("r2_y", [128, 1024], F32)
    ysq = kb.sb("r2_ysq", [128, 1024], F32)
    yn = kb.sb("r2_yn", [128, 1024], F32)
    yg = kb.sb("r2_yg", [128, 1024], BF16)
    ygT = kb.sb("r2_ygT", [128, 8, 128], BF16)
    st = kb.sb("r2_st", [128, 8, 16], F32)
    y_r = Res()
    zt = [kb.sb("r2_z%d" % b, [128, 1024], F32) for b in range(2)]
    zt_r = RL(2)
    x1 = [kb.sb("r2_x1%d" % b, [128, 1024], F32) for b in range(2)]
    x1_r = RL(2)

    pending = []
    for i in range(NT):
        b = i % 2
        kb.dma("sp", lambda e, i=i, b=b: e.dma_start(out=ARt[b][:], in_=ARd[i].rearrange("p (o c t) -> p o c t", o=8, c=2)), writes=[in_r[b]])
        kb.dma("sp", lambda e, i=i, b=b: e.dma_start(out=BKt[b][:], in_=BKd[i].rearrange("p (o c t) -> p o c t", o=8, c=2)), writes=[in_r[b]])
        kb.dma("sp", lambda e, i=i, b=b: e.dma_start(out=rkt[b][:], in_=rkd[i].rearrange("p (o t) -> p o t", o=8)), writes=[in_r[b]])
        kb.dma("sp", lambda e, i=i, b=b: e.dma_start(out=Pct[b][:], in_=Pcd[i]), writes=[in_r[b]])
        kb.dma("sp", lambda e, i=i, b=b: e.dma_start(out=Vt[b][:], in_=Vd[i * 128:(i + 1) * 128, :]), writes=[in_r[b]])
        kb.dma("sp", lambda e, i=i, b=b: e.dma_start(out=Gt[b][:], in_=Gd[i * 128:(i + 1) * 128, :]), writes=[in_r[b]])
        kb.dma("sp", lambda e, i=i, b=b: e.dma_start(out=xt[b][:], in_=xin[i * 128:(i + 1) * 128, :]), reads=[xin_r[i]], writes=[in_r[b]])
        ARb, BKb, Vb, Pcb = ARt[b], BKt[b], Vt[b], Pct[b]
        inr = in_r[b]
        for hp in range(8):
            tr = 0
            for n, src in enumerate((ARb[:, hp, 0, :], BKb[:, hp, 0, :], BKb[:, hp, 1, :])):
                kb.op("pe", lambda e, n=n, src=src, tr=tr: e.transpose(pT[:, tr * 384 + n * 128:tr * 384 + (n + 1) * 128], src, C.ident_b[:]), reads=[inr, C.r], writes=[pT_rr[tr]])
            kb.op("act", lambda e, hp=hp, tr=tr: e.activation(TK[hp][:], pT[:, tr * 384:tr * 384 + 384].rearrange("p (c t) -> p c t", c=3), AF.Copy), reads=[pT_rr[tr]], writes=[TK_r[hp]])
        for u in range(16):
            hp, hh = u // 2, u % 2
            psl = slice(64 * hh, 64 * hh + 64)
            rhs_ar = ARb[psl, hp, :, :].rearrange("p c t -> p (c t)")
            for which, dstT, dst_r in ((0, XA, XAr), (1, KA, KAr)):
                g = nrg()
                kb.op("pe", lambda e, g=g, psl=psl, hp=hp, which=which, rhs_ar=rhs_ar: e.matmul(RG[g], BKb[psl, hp, which, :], rhs_ar, start=True, stop=True), reads=[inr], writes=[RG_r[g]])
                kb.op("dve", lambda e, g=g, u=u, dstT=dstT: e.tensor_tensor(dstT[u][:], RG[g].rearrange("p (c t) -> p c t", c=2), m2[:], op=ALU.mult), reads=[RG_r[g], cst_r], writes=[dst_r[u]])
            g = nrg()
            kb.op("pe", lambda e, g=g, psl=psl, hp=hp: e.matmul(RG[g][:, 0:128], ARb[psl, hp, 0, :], BKb[psl, hp, 0, :], start=True, stop=True), reads=[inr], writes=[RG_r[g]])
            kb.op("dve", lambda e, g=g, u=u: e.tensor_tensor(XN[u][0][:, 1, :], RG[g][:, 0:128], C.m_lo[:], op=ALU.mult), reads=[RG_r[g], C.r], writes=[XNr[u][0]])
            kb.op("pool", lambda e, u=u: e.tensor_copy(XN[u][0][:, 0, :], XA[u][:, 0, :]), reads=[XAr[u]], writes=[XNr[u][0]])
        for u in range(16):
            for c in range(2):
                kb.op("pool", lambda e, u=u, c=c: e.tensor_tensor(PQ[u][0][:, c, :], XN[u][0][:, c, :], C.ident_b[:], op=ALU.add), reads=[XNr[u][0], C.r], writes=[PQr[u][0]])
        for k in range(1, 7):
            cur, prv = k % 2, (k - 1) % 2
            for u in range(16):
                g = nrg()
                Xp, Np = XN[u][prv][:, 0, :], XN[u][prv][:, 1, :]
                kb.op("pe", lambda e, g=g, Xp=Xp, Np=Np: e.matmul(RG[g][:, 0:128], Np, Xp, start=True, stop=True), reads=[XNr[u][prv]], writes=[RG_r[g]])
                if k < 6:
                    kb.op("pe", lambda e, g=g, Xp=Xp, Np=Np: e.matmul(RG[g][:, 128:256], Xp, Np, start=True, stop=True), reads=[XNr[u][prv]], writes=[RG_r[g]])
                    kb.op("act", lambda e, g=g, u=u, cur=cur: e.activation(XN[u][cur][:], RG[g].rearrange("p (c t) -> p c t", c=2), AF.Copy), reads=[RG_r[g]], writes=[XNr[u][cur]])
                else:
                    kb.op("act", lambda e, g=g, u=u, cur=cur: e.activation(XN[u][cur][:, 0, :], RG[g][:, 0:128], AF.Copy), reads=[RG_r[g]], writes=[XNr[u][cur]])
            kb_flush(kb, pending, (len(pending) + (6 - k)) // (7 - k))
            for u in range(16):
                g = nrg()
                Xc = XN[u][cur][:, 0, :]
                Qp = PQ[u][prv][:, 1, :]
                kb.op("pe", lambda e, g=g, Xc=Xc, Qp=Qp: e.matmul(RG[g][:, 0:128], Qp, Xc, start=True, stop=True), reads=[XNr[u][cur], PQr[u][prv]], writes=[RG_r[g]])
                if k < 6:
                    kb.op("pe", lambda e, g=g, Xc=Xc, Qp=Qp: e.matmul(RG[g][:, 128:256], Xc, Qp, start=True, stop=True), reads=[XNr[u][cur], PQr[u][prv]], writes=[RG_r[g]])
                    kb.op("dve", lambda e, g=g, u=u, cur=cur, prv=prv: e.tensor_tensor(PQ[u][cur][:], RG[g].rearrange("p (c t) -> p c t", c=2), PQ[u][prv][:], op=ALU.add), reads=[RG_r[g], PQr[u][prv]], writes=[PQr[u][cur]])
                else:
                    kb.op("dve", lambda e, g=g, u=u, cur=cur, prv=prv: e.tensor_tensor(PQ[u][cur][:, 0, :], RG[g][:, 0:128], PQ[u][prv][:, 0, :], op=ALU.add), reads=[RG_r[g], PQr[u][prv]], writes=[PQr[u][cur]])
        kb_flush(kb, pending, len(pending))
        Pfin = 0
        for u in range(16):
            hp, hh = u // 2, u % 2
            hc = slice(hp * 128 + hh * 64, hp * 128 + hh * 64 + 64)
            g = nrg()
            kb.op("pe", lambda e, g=g, u=u, hc=hc: e.matmul(RG[g][:, 0:64], KA[u][:, 0, :], Vb[:, hc], start=True, stop=True), reads=[KAr[u], inr], writes=[RG_r[g]])
            kb.op("act", lambda e, g=g, u=u: e.activation(AV[u][:], RG[g][:, 0:64], AF.Copy), reads=[RG_r[g]], writes=[AVr[u]])
        for u in range(16):
            hp, hh = u // 2, u % 2
            g = nrg()
            kb.op("pe", lambda e, g=g, u=u: e.matmul(RG[g][:, 0:64], PQ[u][Pfin][:, 0, :], AV[u][:], start=True, stop=True), reads=[PQr[u][Pfin], AVr[u]], writes=[RG_r[g]])
            kb.op("pe", lambda e, g=g, u=u, hp=hp, hh=hh: e.matmul(RG[g][:, 64:128], PQ[u][Pfin][:, 0, :], TK[hp][:, 0, hh * 64:(hh + 1) * 64], start=True, stop=True), reads=[PQr[u][Pfin], TK_r[hp]], writes=[RG_r[g]])
            kb.op("act", lambda e, g=g, hp=hp, hh=hh: e.activation(W12[hp][:, :, hh, :], RG[g][:, 0:128].rearrange("p (c t) -> p c t", c=2), AF.Copy), reads=[RG_r[g]], writes=[W12_r[hp]])
        for hp in range(8):
            W1p = W12[hp][:, 0, :, :].rearrange("p h t -> p (h t)")
            W2p = W12[hp][:, 1, :, :].rearrange("p h t -> p (h t)")
            g = nrg()
            mq = hp % 2
            kb.op("pe", lambda e, g=g, W2p=W2p, hp=hp: e.matmul(RG[g][:, 0:128], W2p, TK[hp][:, 1, :], start=True, stop=True), reads=[W12_r[hp], TK_r[hp]], writes=[RG_r[g]])
            kb.op("pe", lambda e, g=g, W1p=W1p, hp=hp: e.matmul(RG[g][:, 128:256], TK[hp][:, 1, :], W1p, start=True, stop=False), reads=[W12_r[hp], TK_r[hp]], writes=[RG_r[g]])
            kb.op("pe", lambda e, g=g, hp=hp: e.matmul(RG[g][:, 128:256], TK[hp][:, 2, :], Vb[:, hp * 128:(hp + 1) * 128], start=False, stop=True), reads=[TK_r[hp], inr], writes=[RG_r[g]])
            kb.op("dve", lambda e, g=g, mq=mq: e.tensor_tensor(MTf[mq][:], RG[g][:, 0:128], bdm[:], op=ALU.mult), reads=[RG_r[g], cst_r], writes=[mtf_r[mq]])
            kb.op("pool", lambda e, hp=hp, mq=mq: e.tensor_tensor(MT[hp][:], MTf[mq][:], C.ident_f[:], op=ALU.add), reads=[mtf_r[mq], C.r], writes=[MT_r[hp]])
            for hh in range(2):
                psl = slice(64 * hh, 64 * hh + 64)
                kb.op("dve", lambda e, g=g, psl=psl, hh=hh, hp=hp: e.tensor_scalar(GS[hp][psl, :], RG[g][psl, 128 + 64 * hh:192 + 64 * hh], Pcb[psl, hp:hp + 1], None, op0=ALU.mult), reads=[RG_r[g], inr], writes=[GS_r[hp]])
            g2 = nrg()
            for hh in range(2):
                u = 2 * hp + hh
                psl = slice(64 * hh, 64 * hh + 64)
                kb.op("pe", lambda e, g2=g2, hh=hh, W2p=W2p, u=u: e.matmul(RG[g2][:, 128 * hh:128 * hh + 128], W2p, XA[u][:, 1, :], start=True, stop=True), reads=[W12_r[hp], XAr[u]], writes=[RG_r[g2]])
            for hh in range(2):
                psl = slice(64 * hh, 64 * hh + 64)
                kb.op("dve", lambda e, g2=g2, psl=psl, hh=hh, hp=hp: e.tensor_tensor(QT[hp][psl, :], RG[g2][psl, 128 * hh:128 * hh + 128], ARb[psl, hp, 1, :], op=ALU.add), reads=[RG_r[g2], inr], writes=[QT_r[hp]])
        for hp in range(8):
            yb_, yc = pY[hp // 4], (hp % 4) * 128
            for hh in range(2):
                u = 2 * hp + hh
                psl = slice(64 * hh, 64 * hh + 64)
                hc = slice(hp * 128 + hh * 64, hp * 128 + hh * 64 + 64)
                yo = yb_[:, yc + 64 * hh:yc + 64 * hh + 64]
                kb.op("pe", lambda e, yo=yo, hh=hh, u=u, hp=hp: e.matmul(yo, XA[u][:, 1, :], W12[hp][:, 0, hh, :], start=True, stop=False), reads=[XAr[u], W12_r[hp]], writes=[pY_r[hp // 4]])
                kb.op("pe", lambda e, yo=yo, u=u, hc=hc: e.matmul(yo, KA[u][:, 1, :], Vb[:, hc], start=False, stop=False), reads=[KAr[u], inr], writes=[pY_r[hp // 4]])
                kb.op("pe", lambda e, yo=yo, psl=psl, hp=hp: e.matmul(yo, QT[hp][psl, :], Hb[psl, hp, :], start=False, stop=True), reads=[QT_r[hp], Hb_r[hp]], writes=[pY_r[hp // 4]])
            g = nrg()
            kb.op("pe", lambda e, g=g, hp=hp: e.matmul(RG[g][:, 0:64], MT[hp][:], Hb[:, hp, :], start=True, stop=True), reads=[MT_r[hp], Hb_r[hp]], writes=[RG_r[g]])
            kb.op("dve", lambda e, g=g, hp=hp: e.scalar_tensor_tensor(Hb[:, hp, :], RG[g][:, 0:64], Pcb[:, hp:hp + 1], GS[hp][:], op0=ALU.mult, op1=ALU.add), reads=[RG_r[g], inr, GS_r[hp]], writes=[Hb_r[hp]])
        kb.op("act", lambda e: e.activation(ysb[:, 0:512], pY[0][:], AF.Copy), reads=[pY_r[0]], writes=[y_r])
        kb.op("dve", lambda e: e.tensor_copy(ysb[:, 512:1024], pY[1][:]), reads=[pY_r[1]], writes=[y_r])
        def post(i=i, b=b):
            kb.op("pool", lambda e: e.tensor_tensor(ysq[:], ysb[:], ysb[:], op=ALU.mult), reads=[y_r], writes=[y_r])
            kb.op("dve", lambda e: e.reduce_sum(st[:, 0, :], ysb[:].rearrange("p (h n) -> p h n", h=16), axis=AX.X), reads=[y_r], writes=[y_r])
            kb.op("dve", lambda e: e.reduce_sum(st[:, 1, :], ysq[:].rearrange("p (h n) -> p h n", h=16), axis=AX.X), reads=[y_r], writes=[y_r])
            kb.op("dve", lambda e: e.tensor_scalar(st[:, 2, :], st[:, 0, :], 1.0 / 64, None, op0=ALU.mult), reads=[y_r], writes=[y_r])
            kb.op("dve", lambda e: e.tensor_tensor(st[:, 3, :], st[:, 2, :], st[:, 2, :], op=ALU.mult), reads=[y_r], writes=[y_r])
            kb.op("dve", lambda e: e.scalar_tensor_tensor(st[:, 4, :], st[:, 1, :], 1.0 / 64, st[:, 3, :], op0=ALU.mult, op1=ALU.subtract), reads=[y_r], writes=[y_r])
            kb.op("act", lambda e: e.activation(st[:, 5, :], st[:, 4, :], AF.Sqrt, bias=gneps[:, 0:1]), reads=[y_r, cst_r], writes=[y_r])
            kb.op("dve", lambda e: e.reciprocal(st[:, 6, :], st[:, 5, :]), reads=[y_r], writes=[y_r])
            for hd in range(16):
                eng = "dve" if hd % 2 == 0 else "pool"
                kb.op(eng, lambda e, hd=hd: e.tensor_scalar(yn[:, hd * 64:(hd + 1) * 64], ysb[:, hd * 64:(hd + 1) * 64], st[:, 2, hd:hd + 1], st[:, 6, hd:hd + 1], op0=ALU.subtract, op1=ALU.mult), reads=[y_r], writes=[y_r])
            kb.op("pool", lambda e: e.tensor_tensor(yn[:], yn[:], lng[:], op=ALU.mult), reads=[y_r, cst_r], writes=[y_r])
            kb.op("pool", lambda e: e.tensor_tensor(yn[:], yn[:], lnb[:], op=ALU.add), reads=[y_r, cst_r], writes=[y_r])
            for oc in range(8):
                kb.op("pe", lambda e, oc=oc: e.matmul(pH[:, 0:16], rkt[b][:, oc, :], HS[:, oc, :], start=(oc == 0), stop=(oc == 7)), reads=[in_r[b], cst_r], writes=[pH_r])
            kb.op("act", lambda e: e.activation(st[:, 7, :], pH[:, 0:16], AF.Copy), reads=[pH_r], writes=[y_r])
            for hd in range(16):
                kb.op("dve", lambda e, hd=hd: e.scalar_tensor_tensor(yn[:, hd * 64:(hd + 1) * 64], Vt[b][:, hd * 64:(hd + 1) * 64], st[:, 7, hd:hd + 1], yn[:, hd * 64:(hd + 1) * 64], op0=ALU.mult, op1=ALU.add), reads=[y_r, in_r[b]], writes=[y_r])
            kb.op("pool", lambda e: e.tensor_tensor(yg[:], yn[:], Gt[b][:], op=ALU.mult), reads=[y_r, in_r[b]], writes=[y_r])
            for hf in range(2):
                for c4 in range(4):
                    kc = hf * 4 + c4
                    kb.op("pe", lambda e, c4=c4, kc=kc: e.transpose(pT[:, 512 + c4 * 128:512 + (c4 + 1) * 128], yg[:, kc * 128:(kc + 1) * 128], C.ident_b[:]), reads=[y_r, C.r], writes=[pT_rr[0], pT_rr[1]])
                kb.op("act", lambda e, hf=hf: e.activation(ygT[:, hf * 4:hf * 4 + 4, :], pT[:, 512:1024].rearrange("p (c t) -> p c t", c=4), AF.Copy), reads=[pT_rr[0], pT_rr[1]], writes=[y_r])
            for hf in range(2):
                for kc in range(8):
                    kb.op("pe", lambda e, kc=kc, hf=hf: e.matmul(pH[:], ygT[:, kc, :], wo[:, kc, hf * 512:(hf + 1) * 512], start=(kc == 0), stop=(kc == 7)), reads=[y_r, wo_r], writes=[pH_r])
                kb.op("dve", lambda e, hf=hf, b=b: e.scalar_tensor_tensor(zt[b][:, hf * 512:(hf + 1) * 512], xt[b][:, hf * 512:(hf + 1) * 512], ALPHA, pH[:], op0=ALU.mult, op1=ALU.add),
                      reads=[in_r[b], pH_r], writes=[zt_r[b]])
            ln_tile(kb, LS, zt[b], zt_r[b], x1[b][:], x1_r[b], g_bc, b_bc, lnp_r)
            kb.dma("sp", lambda e, i=i, b=b: e.dma_start(out=xa[i * 128:(i + 1) * 128, :], in_=x1[b][:]), reads=[x1_r[b]], writes=[xa_r[i]])

        kb.defer = pending
        post()
        kb.defer = None
    kb_flush(kb, pending, len(pending))
    kb_pop(kb)


T_FULL = 4096
N_CORES = 8
_CACHE = {}


def kernel(**inputs):
    if "prog" not in _CACHE:
        _CACHE["prog"] = build(T_FULL, FULL_PLAN, cap=512)
    nc, names, _ = _CACHE["prog"]
    x = np.asarray(inputs["x"], dtype=np.float32)
    in_maps = []
    for c in range(N_CORES):
        m = {"x": np.ascontiguousarray(x[c])}
        for n in names:
            m[n] = np.ascontiguousarray(np.asarray(inputs[n], dtype=np.float32))
        in_maps.append(m)
    res = run_bass_kernel_spmd(nc, in_maps, core_ids=list(range(N_CORES)))
    return np.stack([np.asarray(res.results[c]["out"]) for c in range(N_CORES)], axis=0).astype(np.float32)
```

```python
import math
from contextlib import ExitStack
import numpy as np
import concourse.bass as bass
import concourse.mybir as mybir
from concourse.bass_utils import run_bass_kernel_spmd

F32 = mybir.dt.float32
BF16 = mybir.dt.bfloat16
I32 = mybir.dt.int32
AF = mybir.ActivationFunctionType
ALU = mybir.AluOpType
AX = mybir.AxisListType

D = 1024
DEPTH = 4
NH_A = 8
NE = 32
NG = 4
EPG = 8
HID = 512
ALPHA = (2 * DEPTH) ** 0.25
LN_EPS = 1e-5
RMS_EPS = 1e-5
GN_EPS = 64e-5


class Res:
    __slots__ = ("w", "r")

    def __init__(self):
        self.w = None
        self.r = {}


def RL(n):
    return [Res() for _ in range(n)]


class KB:
    EPOCH = 30000

    def __init__(self, nc, es):
        self.nc = nc
        self.es = es
        self.engs = {"pe": nc.tensor, "dve": nc.vector, "act": nc.scalar, "pool": nc.gpsimd, "sp": nc.sync}
        self.sems = {}
        self.cur = {}
        self.seen = {e: {} for e in self.engs}
        self.rings = {}
        self.ridx = {}
        self.nins = 0
        self.rec = None
        self.defer = None
        for q, n in (("sp", 16), ("pool", 12), ("act", 6)):
            self.rings[q] = [[self._new_sem("d%s%d" % (q, i)), 0] for i in range(n)]
            self.ridx[q] = 0

    def _new_sem(self, name):
        s = self.es.enter_context(self.nc.semaphore(name))
        self.sems[name] = s
        return name

    def sb(self, name, shape, dt):
        return self.es.enter_context(self.nc.sbuf_tensor(name, list(shape), dt))

    def ps(self, name, shape, dt=F32):
        return self.es.enter_context(self.nc.psum_tensor(name, list(shape), dt))

    def _deps(self, reads, writes):
        deps = {}
        for r in reads:
            if r.w:
                for k, v in r.w.items():
                    if deps.get(k, 0) < v:
                        deps[k] = v
        for w in writes:
            if w.w:
                for k, v in w.w.items():
                    if deps.get(k, 0) < v:
                        deps[k] = v
            for k, v in w.r.items():
                if deps.get(k, 0) < v:
                    deps[k] = v
        return deps

    def _waits(self, eng, deps):
        E = self.engs[eng]
        seen = self.seen[eng]
        for k, v in deps.items():
            if eng == "pe" and k.startswith("epe"):
                continue
            if seen.get(k, 0) >= v:
                continue
            E.wait_ge(self.sems[k], v)
            seen[k] = v
            if self.rec is not None:
                self.rec.append((eng, "w", k, v))

    def _mark(self, tok, reads, writes):
        (k, v), = tok.items()
        for w in writes:
            if w.w is None:
                w.w = dict(tok)
            else:
                w.w = dict(w.w)
                w.w[k] = v
            w.r = {}
        for r in reads:
            if r.r.get(k, 0) < v:
                r.r[k] = v

    def op(self, eng, fn, reads=(), writes=()):
        if self.defer is not None:
            self.defer.append((0, eng, fn, list(reads), list(writes)))
            return
        self._waits(eng, self._deps(reads, writes))
        c = self.cur.get(eng)
        if c is None or c[1] >= self.EPOCH:
            n = len([k for k in self.sems if k.startswith("e" + eng)])
            c = [self._new_sem("e%s%d" % (eng, n)), 0]
            self.cur[eng] = c
        ins = fn(self.engs[eng])
        c[1] += 1
        ins.then_inc(self.sems[c[0]], 1)
        self.nins += 1
        if self.rec is not None:
            self.rec.append((eng, "i", c[0], 1))
        self._mark({c[0]: c[1]}, reads, writes)

    def dma(self, q, fn, reads=(), writes=()):
        if self.defer is not None:
            self.defer.append((1, q, fn, list(reads), list(writes)))
            return
        ring = self.rings[q]
        slot = ring[self.ridx[q] % len(ring)]
        self.ridx[q] += 1
        deps = self._deps(reads, writes)
        if slot[1] > 0:
            deps[slot[0]] = max(deps.get(slot[0], 0), slot[1] * 16)
        self._waits(q, deps)
        ins = fn(self.engs[q])
        slot[1] += 1
        ins.then_inc(self.sems[slot[0]], 16)
        self.nins += 1
        if self.rec is not None:
            self.rec.append((q, "i", slot[0], 16))
        self._mark({slot[0]: slot[1] * 16}, reads, writes)

    def finish(self):
        deps = {}
        for q, ring in self.rings.items():
            for name, cnt in ring:
                if cnt:
                    deps[name] = cnt * 16
        for name in self.sems:
            if name.startswith("e"):
                pass
        for e, c in self.cur.items():
            deps[c[0]] = c[1]
        self._waits("sp", deps)


class Consts:
    pass


def make_consts(kb):
    nc = kb.nc
    C = Consts()
    C.r = Res()
    it = kb.sb("c_iota", [128, 512], I32)
    itf = kb.sb("c_iotaf", [128, 512], F32)
    C.ident_f = kb.sb("c_identf", [128, 128], F32)
    C.ident_b = kb.sb("c_identb", [128, 128], BF16)
    C.ones_b = kb.sb("c_onesb", [128, 128], BF16)
    C.triu_b = kb.sb("c_triub", [128, 128], BF16)
    C.cmask = kb.sb("c_cmask", [128, 4, 512], BF16)
    C.m_st = kb.sb("c_mst", [128, 128], F32)
    C.m_in = kb.sb("c_min", [128, 128], F32)
    C.m_lo = kb.sb("c_mlo", [128, 128], F32)
    kb.op("pool", lambda e: e.iota(it[:], [[1, 512]], base=0, channel_multiplier=-1), writes=[C.r])
    kb.op("dve", lambda e: e.tensor_copy(itf[:], it[:]), reads=[C.r], writes=[C.r])
    kb.op("dve", lambda e: e.tensor_scalar(C.ident_f[:], itf[:, 0:128], 0.0, None, op0=ALU.is_equal), reads=[C.r], writes=[C.r])
    kb.op("dve", lambda e: e.tensor_copy(C.ident_b[:], C.ident_f[:]), reads=[C.r], writes=[C.r])
    kb.op("dve", lambda e: e.memset(C.ones_b[:], 1.0), writes=[C.r])
    kb.op("dve", lambda e: e.tensor_scalar(C.triu_b[:], itf[:, 0:128], 0.0, None, op0=ALU.is_ge), reads=[C.r], writes=[C.r])
    for o in range(4):
        kb.op("dve", lambda e, o=o: e.tensor_scalar(C.cmask[:, o, :], itf[:], float(128 * o), None, op0=ALU.is_ge), reads=[C.r], writes=[C.r])
    kb.op("dve", lambda e: e.tensor_scalar(C.m_st[:], itf[:, 0:128], 1.0, None, op0=ALU.is_ge), reads=[C.r], writes=[C.r])
    kb.op("dve", lambda e: e.tensor_scalar(C.m_in[:], itf[:, 0:128], 0.0, None, op0=ALU.is_ge), reads=[C.r], writes=[C.r])
    kb.op("dve", lambda e: e.tensor_scalar(C.m_lo[:], itf[:, 0:128], -1.0, None, op0=ALU.is_le), reads=[C.r], writes=[C.r])
    C.itf = itf
    return C


def kb_push(kb):
    kb._stack = getattr(kb, "_stack", [])
    kb._stack.append(kb.es_t)
    kb.es_t = kb.es_root.enter_context(ExitStack()) if False else ExitStack()
    kb.es_t.__enter__()


def kb_pop(kb):
    kb.barrier()
    kb.es_t.__exit__(None, None, None)
    kb.es_t = kb._stack.pop()


def _kb_sb(self, name, shape, dt):
    self.nalloc = getattr(self, "nalloc", 0) + 1
    return self.es_t.enter_context(self.nc.sbuf_tensor("%s_%d" % (name, self.nalloc), list(shape), dt))


def _kb_ps(self, name, shape, dt=F32):
    self.nalloc = getattr(self, "nalloc", 0) + 1
    return self.es_t.enter_context(self.nc.psum_tensor("%s_%d" % (name, self.nalloc), list(shape), dt))


def kb_flush(kb, pending, n):
    sv = kb.defer
    kb.defer = None
    for _ in range(min(n, len(pending))):
        kind, e, fn, rd, wr = pending.pop(0)
        if kind == 0:
            kb.op(e, fn, reads=rd, writes=wr)
        else:
            kb.dma(e, fn, reads=rd, writes=wr)
    kb.defer = sv


def _kb_barrier(self):
    deps = {}
    for q, ring in self.rings.items():
        for name, cnt in ring:
            if cnt:
                deps[name] = cnt * 16
    for name in self.sems:
        if name.startswith("e"):
            eng = [e for e in self.engs if name.startswith("e" + e)][0]
            c = self.cur[eng]
            if c[0] == name:
                deps[name] = c[1]
    for eng in self.engs:
        self._waits(eng, dict(deps))


KB.sb = _kb_sb
KB.ps = _kb_ps
KB.barrier = _kb_barrier


def bcast_rows(ap_row, n=128):
    return ap_row.partition_broadcast(n)


def ln_tile(kb, S, z, z_r, out, out_r, g_bc, b_bc, par_r):
    st, mv, sd, rs, xn = S["st"], S["mv"], S["sd"], S["rs"], S["xn"]
    r = S["r"]
    kb.op("dve", lambda e: e.bn_stats(st[:, 0, :], z[:, 0:512]), reads=[z_r], writes=[r])
    kb.op("dve", lambda e: e.bn_stats(st[:, 1, :], z[:, 512:1024]), reads=[z_r], writes=[r])
    kb.op("dve", lambda e: e.bn_aggr(mv[:, 0:2], st[:].rearrange("p a b -> p (a b)")), reads=[r], writes=[r])
    kb.op("act", lambda e: e.activation(sd[:, 0:1], mv[:, 1:2], AF.Sqrt, bias=S["eps"][:, 0:1]), reads=[r], writes=[r])
    kb.op("dve", lambda e: e.reciprocal(rs[:, 0:1], sd[:, 0:1]), reads=[r], writes=[r])
    kb.op("dve", lambda e: e.tensor_scalar(xn[:], z[:], mv[:, 0:1], rs[:, 0:1], op0=ALU.subtract, op1=ALU.mult), reads=[r, z_r], writes=[S["xn_r"]])
    kb.op("pool", lambda e: e.tensor_tensor(xn[:], xn[:], g_bc[:], op=ALU.mult), reads=[S["xn_r"], par_r], writes=[S["xn_r"]])
    kb.op("pool", lambda e: e.tensor_tensor(out, xn[:], b_bc[:], op=ALU.add), reads=[S["xn_r"], par_r], writes=[out_r])


def ln_scratch(kb, pfx, eps):
    S = {}
    S["st"] = kb.sb(pfx + "st", [128, 2, 6], F32)
    S["mv"] = kb.sb(pfx + "mv", [128, 2], F32)
    S["sd"] = kb.sb(pfx + "sd", [128, 1], F32)
    S["rs"] = kb.sb(pfx + "rs", [128, 1], F32)
    S["xn"] = kb.sb(pfx + "xn", [128, 1024], F32)
    S["eps"] = kb.sb(pfx + "eps", [128, 1], F32)
    S["r"] = Res()
    S["xn_r"] = Res()
    kb.op("dve", lambda e: e.memset(S["eps"][:], eps), writes=[S["r"]])
    return S


def load_ln_params(kb, pfx, g_dram, b_dram, li):
    g = kb.sb(pfx + "g", [128, 1024], F32)
    b = kb.sb(pfx + "b", [128, 1024], F32)
    r = Res()
    kb.dma("sp", lambda e: e.dma_start(out=g[:], in_=bcast_rows(g_dram[li:li + 1, :])), writes=[r])
    kb.dma("sp", lambda e: e.dma_start(out=b[:], in_=bcast_rows(b_dram[li:li + 1, :])), writes=[r])
    return g, b, r


def attn_phase(kb, C, T, prm, j, li, xin, xin_r, xa, xa_r):
    nc = kb.nc
    NT = T // 128
    NS = T // 512
    lambda_init = 0.8 - 0.6 * math.exp(-0.3 * li)
    kb_push(kb)
    OT = kb.sb("a_OT", [128, 8, T], BF16)
    OT_r = [RL(NS) for _ in range(8)]
    ps = [kb.ps("a_ps%d" % b, [128, 512], F32) for b in range(8)]
    ps_r = RL(8)
    kb_push(kb)
    xld = [kb.sb("a_xld%d" % b, [128, 1024], F32) for b in range(2)]
    xld_r = RL(2)
    xT = kb.sb("a_xT", [128, 8, T], BF16)
    xT_r = RL(NT)
    QT = kb.sb("a_QT", [128, T], BF16)
    QT_r = RL(NS)
    KT = kb.sb("a_KT", [128, T], BF16)
    KT_r = RL(NS)
    V = kb.sb("a_V", [128, NT, 128], BF16)
    V_r = RL(NT // 4)
    wh = [kb.sb("a_wh%d" % b, [128, 8, 3, 128], BF16) for b in range(2)]
    wh_r = RL(2)
    pt = [kb.sb("a_pt%d" % b, [128, 512], BF16) for b in range(4)]
    pt_r = RL(4)
    lam = kb.sb("a_lam", [128, 256], F32)
    lsc = kb.sb("a_lsc", [128, 8], F32)
    lam_r = Res()
    s1 = kb.sb("a_s1", [128, 512], F32)
    s2 = kb.sb("a_s2", [128, 512], F32)
    t1 = kb.sb("a_t1", [128, 512], F32)
    t2 = kb.sb("a_t2", [128, 512], F32)
    sq = kb.sb("a_sq", [128, 512], BF16)
    op_ = t1
    s12 = s1
    e2 = s2
    tot = s2
    rstd = s1
    fin_r = Res()
    fin2_r = fin_r

    kb.dma("sp", lambda e: e.dma_start(out=lam[:], in_=bcast_rows(prm["attn_lambda"][j:j + 1].rearrange("o a b -> o (a b)"))), writes=[lam_r])
    kb.dma("sp", lambda e: e.dma_start(out=lsc[:, 4:5], in_=prm["attn_subln_g"][j].rearrange("(p o) -> p o", o=1)), writes=[lam_r])
    kb.op("dve", lambda e: e.tensor_tensor(lam[:, 0:64], lam[:, 0:64], lam[:, 64:128], op=ALU.mult), reads=[lam_r], writes=[lam_r])
    kb.op("dve", lambda e: e.tensor_tensor(lam[:, 128:192], lam[:, 128:192], lam[:, 192:256], op=ALU.mult), reads=[lam_r], writes=[lam_r])
    kb.op("dve", lambda e: e.reduce_sum(lsc[:, 0:1], lam[:, 0:64], axis=AX.X), reads=[lam_r], writes=[lam_r])
    kb.op("dve", lambda e: e.reduce_sum(lsc[:, 1:2], lam[:, 128:192], axis=AX.X), reads=[lam_r], writes=[lam_r])
    kb.op("act", lambda e: e.activation(lsc[:, 2:4], lsc[:, 0:2], AF.Exp), reads=[lam_r], writes=[lam_r])
    kb.op("dve", lambda e: e.tensor_tensor(lsc[:, 5:6], lsc[:, 3:4], lsc[:, 2:3], op=ALU.subtract), reads=[lam_r], writes=[lam_r])
    kb.op("dve", lambda e: e.tensor_scalar(lsc[:, 5:6], lsc[:, 5:6], -lambda_init, None, op0=ALU.add), reads=[lam_r], writes=[lam_r])
    kb.op("dve", lambda e: e.tensor_scalar(lsc[:, 6:7], lsc[:, 4:5], 1.0 - lambda_init, None, op0=ALU.mult), reads=[lam_r], writes=[lam_r])
    nlam = lsc[:, 5:6]
    gsc = lsc[:, 6:7]

    for i in range(NT):
        b = i % 2
        kb.dma("sp", lambda e, i=i, b=b: e.dma_start(out=xld[b][:], in_=xin[i * 128:(i + 1) * 128, :]), reads=[xin_r[i]], writes=[xld_r[b]])
        for hf in range(2):
            pb = (2 * i + hf) % 8
            for c4 in range(4):
                kc = hf * 4 + c4
                kb.op("pe", lambda e, pb=pb, c4=c4, kc=kc, b=b: e.transpose(ps[pb][:, c4 * 128:(c4 + 1) * 128], xld[b][:, kc * 128:(kc + 1) * 128], C.ident_f[:]),
                      reads=[xld_r[b], C.r], writes=[ps_r[pb]])
            eng = "act" if hf == 0 else "dve"
            if eng == "act":
                kb.op("act", lambda e, pb=pb, hf=hf, i=i: e.activation(xT[:, hf * 4:hf * 4 + 4, i * 128:(i + 1) * 128], ps[pb][:].rearrange("p (c t) -> p c t", c=4), AF.Copy),
                      reads=[ps_r[pb]], writes=[xT_r[i]])
            else:
                kb.op("dve", lambda e, pb=pb, hf=hf, i=i: e.tensor_copy(xT[:, hf * 4:hf * 4 + 4, i * 128:(i + 1) * 128], ps[pb][:].rearrange("p (c t) -> p c t", c=4)),
                      reads=[ps_r[pb]], writes=[xT_r[i]])


    wq = prm["attn_w_qkv"][j].rearrange("(kc p) n -> p kc n", p=128)
    pcnt = [0]

    def nps():
        pcnt[0] += 1
        return pcnt[0] % 2

    for h in range(NH_A):
        wb = h % 2
        for part in range(3):
            kb.dma("pool", lambda e, wb=wb, part=part, h=h: e.dma_start(out=wh[wb][:, :, part, :], in_=wq[:, :, part * 1024 + h * 128: part * 1024 + (h + 1) * 128]),
                   writes=[wh_r[wb]])
        for s in range(NS):
            for part, dst, dst_r, scl in ((0, QT, QT_r, 0.125), (1, KT, KT_r, 1.0)):
                pb = nps()
                for kc in range(8):
                    kb.op("pe", lambda e, pb=pb, wb=wb, kc=kc, part=part, s=s: e.matmul(ps[pb][:], wh[wb][:, kc, part, :], xT[:, kc, s * 512:(s + 1) * 512], start=(kc == 0), stop=(kc == 7)),
                          reads=[wh_r[wb]] + xT_r[s * 4:s * 4 + 4], writes=[ps_r[pb]])
                kb.op("act", lambda e, pb=pb, dst=dst, s=s, scl=scl: e.activation(dst[:, s * 512:(s + 1) * 512], ps[pb][:], AF.Copy, scale=scl),
                      reads=[ps_r[pb]], writes=[dst_r[s]])
        for g4 in range(NT // 4):
            pb = nps()
            for t4 in range(4):
                i = g4 * 4 + t4
                for kc in range(8):
                    kb.op("pe", lambda e, pb=pb, wb=wb, kc=kc, i=i, t4=t4: e.matmul(ps[pb][:, t4 * 128:(t4 + 1) * 128], xT[:, kc, i * 128:(i + 1) * 128], wh[wb][:, kc, 2, :], start=(kc == 0), stop=(kc == 7)),
                          reads=[wh_r[wb], xT_r[i]], writes=[ps_r[pb]])
            kb.op("dve", lambda e, pb=pb, g4=g4: e.tensor_copy(V[:, g4 * 4:g4 * 4 + 4, :], ps[pb][:].rearrange("p (c t) -> p c t", c=4)),
                  reads=[ps_r[pb]], writes=[V_r[g4]])
        pti = 0
        scnt = [0]
        for qs in range(NS):
            nkt = qs * 4 + 4
            items = [(kt, c) for kt in range(nkt) for c in range(2)]
            sbank = {}
            LOOK = 2

            def emit_s(n):
                kt, c = items[n]
                pb = (0, 1, 7)[scnt[0] % 3]
                scnt[0] += 1
                sbank[n] = pb
                q0 = max(0, kt - qs * 4) * 128
                kb.op("pe", lambda e, pb=pb, c=c, kt=kt, qs=qs, q0=q0: e.matmul(ps[pb][:, q0:512], KT[c * 64:(c + 1) * 64, kt * 128:(kt + 1) * 128], QT[c * 64:(c + 1) * 64, qs * 512 + q0:(qs + 1) * 512], start=True, stop=True),
                      reads=[KT_r[kt // 4], QT_r[qs]], writes=[ps_r[pb]])

            for n in range(min(LOOK, len(items))):
                emit_s(n)
            for n, (kt, c) in enumerate(items):
                if n + LOOK < len(items):
                    emit_s(n + LOOK)
                pb = sbank[n]
                pi = pti % 4
                pti += 1
                q0 = max(0, kt - qs * 4) * 128
                kb.op("act", lambda e, pb=pb, pi=pi, q0=q0: e.activation(pt[pi][:, q0:512], ps[pb][:, q0:512], AF.Exp), reads=[ps_r[pb]], writes=[pt_r[pi]])
                if kt >= qs * 4:
                    o = kt - qs * 4
                    kb.op("pool", lambda e, pi=pi, o=o, q0=q0: e.tensor_tensor(pt[pi][:, q0:512], pt[pi][:, q0:512], C.cmask[:, o, q0:512], op=ALU.mult), reads=[pt_r[pi], C.r], writes=[pt_r[pi]])
                kb.op("pe", lambda e, c=c, kt=kt, pi=pi, nkt=nkt, q0=q0: e.matmul(ps[2 + c][:, q0:512], V[:, kt, :], pt[pi][:, q0:512], start=(kt == 0), stop=(kt == nkt - 1)),
                      reads=[V_r[kt // 4], pt_r[pi]], writes=[ps_r[2 + c]])
                kb.op("pe", lambda e, c=c, kt=kt, pi=pi, nkt=nkt, q0=q0: e.matmul(ps[4 + c][:, q0:512], C.ones_b[:], pt[pi][:, q0:512], start=(kt == 0), stop=(kt == nkt - 1)),
                      reads=[C.r, pt_r[pi]], writes=[ps_r[4 + c]])
            kb.op("act", lambda e: e.activation(s1[:], ps[4][:], AF.Copy), reads=[ps_r[4]], writes=[fin_r])
            kb.op("act", lambda e: e.activation(s2[:], ps[5][:], AF.Copy), reads=[ps_r[5]], writes=[fin_r])
            kb.op("dve", lambda e: e.tensor_tensor(t1[:], ps[2][:], s2[:], op=ALU.mult), reads=[ps_r[2], fin_r], writes=[fin2_r])
            kb.op("dve", lambda e: e.tensor_tensor(t2[:], ps[3][:], s1[:], op=ALU.mult), reads=[ps_r[3], fin_r], writes=[fin2_r])
            kb.op("dve", lambda e: e.scalar_tensor_tensor(op_[:], t2[:], nlam, t1[:], op0=ALU.mult, op1=ALU.add), reads=[fin2_r, lam_r], writes=[fin2_r])
            kb.op("pool", lambda e: e.tensor_tensor(sq[:], op_[:], op_[:], op=ALU.mult), reads=[fin2_r], writes=[fin2_r])
            kb.op("pool", lambda e: e.tensor_tensor(s12[:], s1[:], s2[:], op=ALU.mult), reads=[fin_r], writes=[fin2_r])
            kb.op("dve", lambda e: e.scalar_tensor_tensor(e2[:], s12[:], RMS_EPS, s12[:], op0=ALU.mult, op1=ALU.mult), reads=[fin2_r], writes=[fin2_r])
            kb.op("pe", lambda e: e.matmul(ps[6][:], C.ones_b[:], sq[:], start=True, stop=True), reads=[C.r, fin2_r], writes=[ps_r[6]])
            kb.op("dve", lambda e: e.scalar_tensor_tensor(tot[:], ps[6][:], 1.0 / 128.0, e2[:], op0=ALU.mult, op1=ALU.add), reads=[ps_r[6], fin2_r], writes=[fin2_r])
            kb.op("act", lambda e: e.activation(tot[:], tot[:], AF.Ln), reads=[fin2_r], writes=[fin2_r])
            kb.op("act", lambda e: e.activation(rstd[:], tot[:], AF.Exp, scale=-0.5), reads=[fin2_r], writes=[fin2_r])
            kb.op("dve", lambda e, h=h, qs=qs: e.scalar_tensor_tensor(OT[:, h, qs * 512:(qs + 1) * 512], op_[:], gsc, rstd[:], op0=ALU.mult, op1=ALU.mult),
                  reads=[fin2_r, lam_r], writes=[OT_r[h][qs]])

    kb_pop(kb)
    wo = kb.sb("a_wo", [128, 8, 1024], BF16)
    wo_r = Res()
    wo_src = prm["attn_w_o"][j].rearrange("(h p) n -> p h n", p=128)
    for hh in range(8):
        kb.dma("pool", lambda e, hh=hh: e.dma_start(out=wo[:, hh, :], in_=wo_src[:, hh, :]), writes=[wo_r])
    xld = [kb.sb("a_xldc%d" % b, [128, 1024], F32) for b in range(2)]
    xld_r = RL(2)
    zt = [kb.sb("a_z%d" % b, [128, 1024], F32) for b in range(2)]
    zt_r = RL(2)
    x1 = [kb.sb("a_x1%d" % b, [128, 1024], F32) for b in range(2)]
    x1_r = RL(2)
    LS = ln_scratch(kb, "a_ln", LN_EPS)
    g_bc, b_bc, lnp_r = load_ln_params(kb, "a_lnp", prm["ln1_g"], prm["ln1_b"], li)
    for i in range(NT):
        b = i % 2
        kb.dma("sp", lambda e, i=i, b=b: e.dma_start(out=xld[b][:], in_=xin[i * 128:(i + 1) * 128, :]), reads=[xin_r[i]], writes=[xld_r[b]])
        for hf in range(2):
            pb = 2 * b + hf
            for hh in range(8):
                kb.op("pe", lambda e, pb=pb, hh=hh, hf=hf, i=i: e.matmul(ps[pb][:], OT[:, hh, i * 128:(i + 1) * 128], wo[:, hh, hf * 512:(hf + 1) * 512], start=(hh == 0), stop=(hh == 7)),
                      reads=[OT_r[hh][i // 4], wo_r], writes=[ps_r[pb]])
            kb.op("dve", lambda e, pb=pb, hf=hf, b=b: e.scalar_tensor_tensor(zt[b][:, hf * 512:(hf + 1) * 512], xld[b][:, hf * 512:(hf + 1) * 512], ALPHA, ps[pb][:], op0=ALU.mult, op1=ALU.add),
                  reads=[xld_r[b], ps_r[pb]], writes=[zt_r[b]])
        ln_tile(kb, LS, zt[b], zt_r[b], x1[b][:], x1_r[b], g_bc, b_bc, lnp_r)
        kb.dma("sp", lambda e, i=i, b=b: e.dma_start(out=xa[i * 128:(i + 1) * 128, :], in_=x1[b][:]), reads=[x1_r[b]], writes=[xa_r[i]])
    kb_pop(kb)


PSHAPES = {
    "ln1_g": (4, 1024), "ln1_b": (4, 1024), "ln2_g": (4, 1024), "ln2_b": (4, 1024),
    "attn_w_qkv": (2, 1024, 3072), "attn_w_o": (2, 1024, 1024), "attn_lambda": (2, 4, 64), "attn_subln_g": (2, 128),
    "rw_mu": (2, 6, 1024), "rw_w_rkv": (2, 3, 1024, 1024), "rw_w_o": (2, 1024, 1024), "rw_w0": (2, 1024),
    "rw_w1": (2, 1024, 64), "rw_w2": (2, 64, 1024), "rw_a0": (2, 1024), "rw_a1": (2, 1024, 64), "rw_a2": (2, 64, 1024),
    "rw_g1": (2, 1024, 160), "rw_g2": (2, 160, 1024), "rw_k_k": (2, 1024), "rw_k_a": (2, 1024), "rw_r_k": (2, 16, 64),
    "rw_lnx_g": (2, 1024), "rw_lnx_b": (2, 1024), "rw_v0": (1, 1024), "rw_v1": (1, 1024, 32), "rw_v2": (1, 32, 1024),
    "moe_rg_w": (4, 1024, 4), "moe_rg_b": (4, 4), "moe_re_w": (4, 1024, 32), "moe_re_b": (4, 32),
    "moe_w_gu": (4, 32, 1024, 1024), "moe_w_down": (4, 32, 512, 1024),
}


class Params(dict):
    def __init__(self, nc):
        super().__init__()
        self.nc = nc

    def __missing__(self, k):
        ap = self.nc.dram_tensor(k, list(PSHAPES[k]), F32, kind="ExternalInput").ap()
        self[k] = ap
        return ap


def build(T, plan, cap=512):
    nc = bass.Bass("TRN2", target_bir_lowering=False)
    prm = Params(nc)
    x = nc.dram_tensor("x", [T, D], F32, kind="ExternalInput").ap()
    out = nc.dram_tensor("out", [T, D], F32, kind="ExternalOutput").ap()
    xa = nc.dram_tensor("xa_s", [T, D], F32, kind="Internal").ap()
    xb = nc.dram_tensor("xb_s", [T, D], F32, kind="Internal").ap()
    NT = T // 128
    es = ExitStack()
    with es:
        kb = KB(nc, es)
        kb.es_t = es
        C = make_consts(kb)
        cur, cur_r = x, RL(NT)
        xa_r, xb_r, out_r = RL(NT), RL(NT), RL(NT)
        ST = dict(DEBUG_ST)
        for n, step in enumerate(plan):
            last = n == len(plan) - 1
            kind = step[0]
            if kind == "attn":
                attn_phase(kb, C, T, prm, step[1], step[2], cur, cur_r, xa, xa_r)
                cur, cur_r = xa, xa_r
            elif kind == "rwkv":
                rwkv_phase(kb, C, T, prm, step[1], step[2], cur, cur_r, xa, xa_r, ST, nc)
                cur, cur_r = xa, xa_r
            elif kind == "moe":
                dst, dst_r = (out, out_r) if last else (xb, xb_r)
                moe_phase(kb, C, T, prm, step[1], cur, cur_r, dst, dst_r, cap, nc, ST)
                cur, cur_r = dst, dst_r
        if cur is not out:
            for i in range(NT):
                kb.dma("sp", lambda e, i=i: e.dma_start(out=out[i * 128:(i + 1) * 128, :], in_=cur[i * 128:(i + 1) * 128, :]), reads=[cur_r[i]], writes=[out_r[i]])
        kb.finish()
        ninst = kb.nins
    return nc, list(prm.keys()), ninst


DEBUG_ST = {}
FULL_PLAN = [("attn", 0, 0), ("moe", 0), ("rwkv", 0, 1), ("moe", 1), ("attn", 1, 2), ("moe", 2), ("rwkv", 1, 3), ("moe", 3)]


def moe_phase(kb, C, T, prm, li, xin, xin_r, dst, dst_r, cap, nc, ST):
    NT = T // 128
    NSLOT = NE * cap
    NB = cap // 128
    if "xbuf" not in ST:
        ST["xbuf"] = nc.dram_tensor("xbuf_s", [NSLOT, D], BF16, kind="Internal").ap()
        ST["ybuf"] = nc.dram_tensor("ybuf_s", [NSLOT, D], F32, kind="Internal").ap()
        ST["bc_reg"] = nc.gpsimd.to_reg(NSLOT - 1)
    xbuf, ybuf = ST["xbuf"], ST["ybuf"]
    bc_reg = ST["bc_reg"]
    kb_push(kb)
    slots = kb.sb("m_slots", [128, NT, 2], I32)
    gates = kb.sb("m_gates", [128, NT, 2], F32)
    sg_r = Res()

    kb_push(kb)
    ps = [kb.ps("m1_ps%d" % b, [128, 512], F32) for b in range(6)]
    ps_r = RL(6)
    xt = [kb.sb("m1_xt%d" % b, [128, 1024], F32) for b in range(2)]
    xt_r = RL(2)
    xb16 = [kb.sb("m1_xb%d" % b, [128, 1024], BF16) for b in range(2)]
    xb_r = RL(2)
    xT = [kb.sb("m1_xT%d" % b, [128, 8, 128], F32) for b in range(2)]
    xT_r = RL(2)
    wr = kb.sb("m1_wr", [128, 8, 36], F32)
    rb = kb.sb("m1_rb", [128, 36], F32)
    offs_i = kb.sb("m1_offi", [128, 32], I32)
    offs = kb.sb("m1_off", [128, 32], F32)
    base = kb.sb("m1_base", [128, 32], F32)
    cr = Res()
    base_r = Res()
    kb.dma("sp", lambda e: e.dma_start(out=wr[:, :, 0:4], in_=prm["moe_rg_w"][li].rearrange("(kc p) n -> p kc n", p=128)), writes=[cr])
    kb.dma("sp", lambda e: e.dma_start(out=wr[:, :, 4:36], in_=prm["moe_re_w"][li].rearrange("(kc p) n -> p kc n", p=128)), writes=[cr])
    kb.dma("sp", lambda e: e.dma_start(out=rb[:, 0:4], in_=bcast_rows(prm["moe_rg_b"][li:li + 1, :])), writes=[cr])
    kb.dma("sp", lambda e: e.dma_start(out=rb[:, 4:36], in_=bcast_rows(prm["moe_re_b"][li:li + 1, :])), writes=[cr])
    kb.op("pool", lambda e: e.iota(offs_i[:], [[cap, 32]], base=-1, channel_multiplier=0), writes=[cr])
    kb.op("dve", lambda e: e.tensor_copy(offs[:], offs_i[:]), reads=[cr], writes=[cr])
    kb.op("dve", lambda e: e.memset(base[:], 0.0), writes=[base_r])
    Wk = {}
    for nm, w in (("L", 36), ("ohg", 4), ("eg", 4), ("lsel", 8), ("oh1", 8), ("lsel2", 8), ("oh2", 8), ("E1", 32), ("E2", 32),
                  ("val", 32), ("valid", 32), ("val2", 32), ("tmp", 32), ("sc", 16)):
        Wk[nm] = kb.sb("m1_w" + nm, [128, w], F32)
    G01 = kb.sb("m1_G01", [128, 32], BF16)
    wr_ = Res()
    BIG = float(NSLOT)
    for i in range(NT):
        b = i % 2
        kb.dma("sp", lambda e, i=i, b=b: e.dma_start(out=xt[b][:], in_=xin[i * 128:(i + 1) * 128, :]), reads=[xin_r[i]], writes=[xt_r[b]])
        kb.op("act", lambda e, b=b: e.activation(xb16[b][:], xt[b][:], AF.Copy), reads=[xt_r[b]], writes=[xb_r[b]])
        for hf in range(2):
            pb = hf
            for c4 in range(4):
                kc = hf * 4 + c4
                kb.op("pe", lambda e, pb=pb, c4=c4, kc=kc, b=b: e.transpose(ps[pb][:, c4 * 128:(c4 + 1) * 128], xt[b][:, kc * 128:(kc + 1) * 128], C.ident_f[:]),
                      reads=[xt_r[b], C.r], writes=[ps_r[pb]])
            if hf == 0:
                kb.op("act", lambda e, pb=pb, b=b: e.activation(xT[b][:, 0:4, :], ps[pb][:].rearrange("p (c t) -> p c t", c=4), AF.Copy), reads=[ps_r[pb]], writes=[xT_r[b]])
            else:
                kb.op("dve", lambda e, pb=pb, b=b: e.tensor_copy(xT[b][:, 4:8, :], ps[pb][:].rearrange("p (c t) -> p c t", c=4)), reads=[ps_r[pb]], writes=[xT_r[b]])
        for kc in range(8):
            kb.op("pe", lambda e, kc=kc, b=b: e.matmul(ps[2][:, 0:36], xT[b][:, kc, :], wr[:, kc, :], start=(kc == 0), stop=(kc == 7)), reads=[xT_r[b], cr], writes=[ps_r[2]])
        L, ohg, eg, lsel, oh1, lsel2, oh2, E1, E2 = (Wk[k] for k in ("L", "ohg", "eg", "lsel", "oh1", "lsel2", "oh2", "E1", "E2"))
        val, valid, val2, tmp, sc = (Wk[k] for k in ("val", "valid", "val2", "tmp", "sc"))

        def dv(fn, extra_r=(), extra_w=()):
            kb.op("dve", fn, reads=[wr_] + list(extra_r), writes=[wr_] + list(extra_w))

        dv(lambda e: e.tensor_tensor(L[:], ps[2][:, 0:36], rb[:], op=ALU.add), extra_r=[ps_r[2], cr])
        dv(lambda e: e.reduce_max(sc[:, 0:1], L[:, 0:4], axis=AX.X))
        dv(lambda e: e.tensor_scalar(ohg[:], L[:, 0:4], sc[:, 0:1], None, op0=ALU.is_equal))
        dv(lambda e: e.tensor_scalar(sc[:, 1:2], sc[:, 0:1], -1.0, None, op0=ALU.mult))
        kb.op("act", lambda e: e.activation(eg[:], L[:, 0:4], AF.Exp, bias=sc[:, 1:2]), reads=[wr_], writes=[wr_])
        dv(lambda e: e.reduce_sum(sc[:, 2:3], eg[:], axis=AX.X))
        dv(lambda e: e.reciprocal(sc[:, 3:4], sc[:, 2:3]))
        dv(lambda e: e.tensor_scalar(lsel[:], L[:, 4:12], ohg[:, 0:1], None, op0=ALU.mult))
        for g in range(1, 4):
            dv(lambda e, g=g: e.scalar_tensor_tensor(lsel[:], L[:, 4 + 8 * g:12 + 8 * g], ohg[:, g:g + 1], lsel[:], op0=ALU.mult, op1=ALU.add))
        dv(lambda e: e.reduce_max(sc[:, 4:5], lsel[:], axis=AX.X))
        dv(lambda e: e.tensor_scalar(oh1[:], lsel[:], sc[:, 4:5], None, op0=ALU.is_equal))
        dv(lambda e: e.scalar_tensor_tensor(lsel2[:], oh1[:], -1e30, lsel[:], op0=ALU.mult, op1=ALU.add))
        dv(lambda e: e.reduce_max(sc[:, 5:6], lsel2[:], axis=AX.X))
        dv(lambda e: e.tensor_scalar(oh2[:], lsel2[:], sc[:, 5:6], None, op0=ALU.is_equal))
        dv(lambda e: e.tensor_tensor(sc[:, 6:7], sc[:, 5:6], sc[:, 4:5], op=ALU.subtract))
        kb.op("act", lambda e: e.activation(sc[:, 7:8], sc[:, 6:7], AF.Exp), reads=[wr_], writes=[wr_])
        dv(lambda e: e.tensor_scalar(sc[:, 8:9], sc[:, 7:8], 1.0, None, op0=ALU.add))
        dv(lambda e: e.reciprocal(sc[:, 9:10], sc[:, 8:9]))
        dv(lambda e: e.tensor_tensor(sc[:, 10:11], sc[:, 7:8], sc[:, 9:10], op=ALU.mult))
        for g in range(4):
            dv(lambda e, g=g: e.tensor_scalar(E1[:, 8 * g:8 * g + 8], oh1[:], ohg[:, g:g + 1], None, op0=ALU.mult))
            dv(lambda e, g=g: e.tensor_scalar(E2[:, 8 * g:8 * g + 8], oh2[:], ohg[:, g:g + 1], None, op0=ALU.mult))
        dv(lambda e: e.tensor_tensor(G01[:], E1[:], E2[:], op=ALU.add))
        kb.op("pe", lambda e: e.matmul(ps[3][:, 0:32], C.triu_b[:], G01[:], start=True, stop=True), reads=[wr_, C.r], writes=[ps_r[3]])
        kb.op("pe", lambda e: e.matmul(ps[3][:, 32:64], C.ones_b[:], G01[:], start=True, stop=True), reads=[wr_, C.r], writes=[ps_r[3]])
        dv(lambda e: e.tensor_tensor(val[:], ps[3][:, 0:32], base[:], op=ALU.add), extra_r=[ps_r[3], base_r])
        dv(lambda e: e.tensor_scalar(valid[:], val[:], float(cap), None, op0=ALU.is_le))
        dv(lambda e: e.tensor_tensor(val2[:], val[:], offs[:], op=ALU.add), extra_r=[cr])
        dv(lambda e: e.scalar_tensor_tensor(val2[:], val2[:], -BIG, valid[:], op0=ALU.add, op1=ALU.mult))
        dv(lambda e: e.tensor_scalar(val2[:], val2[:], BIG, None, op0=ALU.add))
        dv(lambda e: e.tensor_tensor(tmp[:], E1[:], val2[:], op=ALU.mult))
        dv(lambda e: e.reduce_sum(sc[:, 11:12], tmp[:], axis=AX.X))
        dv(lambda e: e.tensor_tensor(tmp[:], E2[:], val2[:], op=ALU.mult))
        dv(lambda e: e.reduce_sum(sc[:, 12:13], tmp[:], axis=AX.X))
        dv(lambda e: e.tensor_tensor(tmp[:], E1[:], valid[:], op=ALU.mult))
        dv(lambda e: e.reduce_sum(sc[:, 13:14], tmp[:], axis=AX.X))
        dv(lambda e: e.tensor_tensor(tmp[:], E2[:], valid[:], op=ALU.mult))
        dv(lambda e: e.reduce_sum(sc[:, 14:15], tmp[:], axis=AX.X))
        dv(lambda e: e.tensor_tensor(base[:], base[:], ps[3][:, 32:64], op=ALU.add), extra_r=[ps_r[3]], extra_w=[base_r])
        dv(lambda e, i=i: e.tensor_copy(slots[:, i, :], sc[:, 11:13]), extra_w=[sg_r])
        dv(lambda e: e.tensor_scalar(sc[:, 9:11], sc[:, 9:11], sc[:, 3:4], None, op0=ALU.mult))
        dv(lambda e, i=i: e.tensor_tensor(gates[:, i, :], sc[:, 9:11], sc[:, 13:15], op=ALU.mult), extra_w=[sg_r])
        for k in range(2):
            kb.dma("pool", lambda e, i=i, k=k, b=b: e.indirect_dma_start(out=xbuf[:, :], out_offset=bass.IndirectOffsetOnAxis(ap=slots[:, i, k:k + 1], axis=0),
                                                                       in_=xb16[b][:], in_offset=None, bounds_check=bc_reg, oob_is_err=False),
                   reads=[sg_r, xb_r[b]])
    kb_pop(kb)

    kb_push(kb)
    psT = [kb.ps("m2_pT%d" % b, [128, 1024], BF16) for b in range(2)]
    psT_r = RL(2)
    psH = [kb.ps("m2_pH%d" % b, [128, 512], F32) for b in range(4)]
    psH_r = RL(4)
    psY = [kb.ps("m2_pY%d" % b, [128, 512], F32) for b in range(2)]
    psY_r = RL(2)
    wgu = [kb.sb("m2_wgu%d" % b, [128, 8, 1024], BF16) for b in range(2)]
    wgu_r = RL(2)
    wd = [kb.sb("m2_wd%d" % b, [128, 4, 1024], BF16) for b in range(2)]
    wd_r = RL(2)
    xblk = [kb.sb("m2_xb%d" % b, [128, 1024], BF16) for b in range(2)]
    xblk_r = RL(2)
    XT = [kb.sb("m2_XT%d" % b, [128, 8, cap], BF16) for b in range(2)]
    XT_r = RL(2)
    sil = [kb.sb("m2_sil%d" % b, [128, cap], F32) for b in range(2)]
    sil_r = RL(2)
    AT = [kb.sb("m2_AT%d" % b, [128, 4, cap], BF16) for b in range(2)]
    AT_r = RL(2)
    ysb = [kb.sb("m2_y%d" % b, [128, 1024], F32) for b in range(2)]
    ysb_r = RL(2)
    nblk = 0
    for ex in range(NE):
        wb = ex % 2
        gsrc = prm["moe_w_gu"][li, ex].rearrange("(kc p) n -> p kc n", p=128)
        dsrc = prm["moe_w_down"][li, ex].rearrange("(m p) n -> p m n", p=128)
        for kc in range(8):
            kb.dma("pool", lambda e, wb=wb, kc=kc, gsrc=gsrc: e.dma_start(out=wgu[wb][:, kc, :], in_=gsrc[:, kc, :]), writes=[wgu_r[wb]])
        for m in range(4):
            kb.dma("pool", lambda e, wb=wb, m=m, dsrc=dsrc: e.dma_start(out=wd[wb][:, m, :], in_=dsrc[:, m, :]), writes=[wd_r[wb]])
        for blk in range(NB):
            bb = nblk % 2
            nblk += 1
            r0 = ex * cap + blk * 128
            kb.dma("sp", lambda e, bb=bb, r0=r0: e.dma_start(out=xblk[bb][:], in_=xbuf[r0:r0 + 128, :]), writes=[xblk_r[bb]])
            for kc in range(8):
                kb.op("pe", lambda e, bb=bb, kc=kc: e.transpose(psT[bb][:, kc * 128:(kc + 1) * 128], xblk[bb][:, kc * 128:(kc + 1) * 128], C.ident_b[:]),
                      reads=[xblk_r[bb], C.r], writes=[psT_r[bb]])
            eng = "act" if blk % 2 == 0 else "dve"
            if eng == "act":
                kb.op("act", lambda e, bb=bb, wb=wb, blk=blk: e.activation(XT[wb][:, :, blk * 128:(blk + 1) * 128], psT[bb][:].rearrange("p (c t) -> p c t", c=8), AF.Copy),
                      reads=[psT_r[bb]], writes=[XT_r[wb]])
            else:
                kb.op("dve", lambda e, bb=bb, wb=wb, blk=blk: e.tensor_copy(XT[wb][:, :, blk * 128:(blk + 1) * 128], psT[bb][:].rearrange("p (c t) -> p c t", c=8)),
                      reads=[psT_r[bb]], writes=[XT_r[wb]])
        for m in range(4):
            pg, pu = (m % 2) * 2, (m % 2) * 2 + 1
            for (pb, col) in ((pg, m * 128), (pu, 512 + m * 128)):
                for kc in range(8):
                    kb.op("pe", lambda e, pb=pb, col=col, kc=kc, wb=wb: e.matmul(psH[pb][:, 0:cap], wgu[wb][:, kc, col:col + 128], XT[wb][:, kc, :], start=(kc == 0), stop=(kc == 7)),
                          reads=[wgu_r[wb], XT_r[wb]], writes=[psH_r[pb]])
            sb_ = m % 2
            kb.op("act", lambda e, pg=pg, sb_=sb_: e.activation(sil[sb_][:], psH[pg][:, 0:cap], AF.Silu), reads=[psH_r[pg]], writes=[sil_r[sb_]])
            kb.op("dve", lambda e, pu=pu, sb_=sb_, m=m, wb=wb: e.tensor_tensor(AT[wb][:, m, :], sil[sb_][:], psH[pu][:, 0:cap], op=ALU.mult),
                  reads=[psH_r[pu], sil_r[sb_]], writes=[AT_r[wb]])
        for blk in range(NB):
            yb = blk % 2
            for hf in range(2):
                for m in range(4):
                    kb.op("pe", lambda e, hf=hf, m=m, wb=wb, blk=blk: e.matmul(psY[hf][:], AT[wb][:, m, blk * 128:(blk + 1) * 128], wd[wb][:, m, hf * 512:(hf + 1) * 512], start=(m == 0), stop=(m == 3)),
                          reads=[AT_r[wb], wd_r[wb]], writes=[psY_r[hf]])
                if hf == 0:
                    kb.op("act", lambda e, yb=yb: e.activation(ysb[yb][:, 0:512], psY[0][:], AF.Copy), reads=[psY_r[0]], writes=[ysb_r[yb]])
                else:
                    kb.op("dve", lambda e, yb=yb: e.tensor_copy(ysb[yb][:, 512:1024], psY[1][:]), reads=[psY_r[1]], writes=[ysb_r[yb]])
            r0 = ex * cap + blk * 128
            kb.dma("sp", lambda e, yb=yb, r0=r0: e.dma_start(out=ybuf[r0:r0 + 128, :], in_=ysb[yb][:]), reads=[ysb_r[yb]])
    kb_pop(kb)

    kb_push(kb)
    y1 = [kb.sb("m3_y1%d" % b, [128, 1024], F32) for b in range(2)]
    y2 = [kb.sb("m3_y2%d" % b, [128, 1024], F32) for b in range(2)]
    y_r = RL(2)
    xt3 = [kb.sb("m3_xt%d" % b, [128, 1024], F32) for b in range(2)]
    xt3_r = RL(2)
    z3 = [kb.sb("m3_z%d" % b, [128, 1024], F32) for b in range(2)]
    z3_r = RL(2)
    x2 = [kb.sb("m3_x2%d" % b, [128, 1024], F32) for b in range(2)]
    x2_r = RL(2)
    LS = ln_scratch(kb, "m3_ln", LN_EPS)
    g_bc, b_bc, lnp_r = load_ln_params(kb, "m3_lnp", prm["ln2_g"], prm["ln2_b"], li)
    for b in range(2):
        kb.op("pool", lambda e, b=b: e.memset(y1[b][:], 0.0), writes=[y_r[b]])
        kb.op("pool", lambda e, b=b: e.memset(y2[b][:], 0.0), writes=[y_r[b]])
    for i in range(NT):
        b = i % 2
        kb.dma("sp", lambda e, i=i, b=b: e.dma_start(out=xt3[b][:], in_=xin[i * 128:(i + 1) * 128, :]), reads=[xin_r[i]], writes=[xt3_r[b]])
        for k, yy in ((0, y1), (1, y2)):
            kb.dma("pool", lambda e, i=i, k=k, b=b, yy=yy: e.indirect_dma_start(out=yy[b][:], out_offset=None, in_=ybuf[:, :],
                                                                              in_offset=bass.IndirectOffsetOnAxis(ap=slots[:, i, k:k + 1], axis=0),
                                                                              bounds_check=bc_reg, oob_is_err=False),
                   reads=[sg_r], writes=[y_r[b]])
        kb.op("act", lambda e, b=b: e.activation(z3[b][:], xt3[b][:], AF.Copy, scale=ALPHA), reads=[xt3_r[b]], writes=[z3_r[b]])
        kb.op("dve", lambda e, b=b, i=i: e.scalar_tensor_tensor(z3[b][:], y1[b][:], gates[:, i, 0:1], z3[b][:], op0=ALU.mult, op1=ALU.add), reads=[y_r[b], sg_r], writes=[z3_r[b]])
        kb.op("dve", lambda e, b=b, i=i: e.scalar_tensor_tensor(z3[b][:], y2[b][:], gates[:, i, 1:2], z3[b][:], op0=ALU.mult, op1=ALU.add), reads=[y_r[b], sg_r], writes=[z3_r[b]])
        ln_tile(kb, LS, z3[b], z3_r[b], x2[b][:], x2_r[b], g_bc, b_bc, lnp_r)
        kb.dma("sp", lambda e, i=i, b=b: e.dma_start(out=dst[i * 128:(i + 1) * 128, :], in_=x2[b][:]), reads=[x2_r[b]], writes=[dst_r[i]])
    kb_pop(kb)
    kb_pop(kb)


C0 = math.exp(-0.5)


def rwkv_phase(kb, C, T, prm, j, li, xin, xin_r, xa, xa_r, ST, nc):
    NT = T // 128
    NSUP = T // 256
    if "ARd" not in ST:
        ST["ARd"] = nc.dram_tensor("ARd_s", [NT, 128, 8 * 2 * 128], BF16, kind="Internal").ap()
        ST["BKd"] = nc.dram_tensor("BKd_s", [NT, 128, 8 * 2 * 128], BF16, kind="Internal").ap()
        ST["rkd"] = nc.dram_tensor("rkd_s", [NT, 128, 8 * 128], BF16, kind="Internal").ap()
        ST["Pcd"] = nc.dram_tensor("Pcd_s", [NT, 128, 8], F32, kind="Internal").ap()
        ST["Vd"] = nc.dram_tensor("Vd_s", [T, D], BF16, kind="Internal").ap()
        ST["Gd"] = nc.dram_tensor("Gd_s", [T, D], BF16, kind="Internal").ap()
        ST["vfirst"] = nc.dram_tensor("vfirst_s", [T, D], F32, kind="Internal").ap()
    ARd, BKd, rkd, Pcd, Vd, Gd, vfd = (ST[k] for k in ("ARd", "BKd", "rkd", "Pcd", "Vd", "Gd", "vfirst"))

    kb_push(kb)
    ps = [kb.ps("r1_ps%d" % b, [128, 512], F32) for b in range(8)]
    ps_r = RL(8)
    wrkv = kb.sb("r1_wrkv", [128, 3, 8, 1024], BF16)
    w1 = kb.sb("r1_w1", [128, 8, 64], BF16)
    a1 = kb.sb("r1_a1", [128, 8, 64], BF16)
    g1 = kb.sb("r1_g1", [128, 8, 160], BF16)
    w2 = kb.sb("r1_w2", [64, 1024], BF16)
    a2 = kb.sb("r1_a2", [64, 1024], BF16)
    g2a = kb.sb("r1_g2a", [128, 1024], BF16)
    g2b = kb.sb("r1_g2b", [32, 1024], BF16)
    wr_ = Res()
    for n in range(3):
        src = prm["rw_w_rkv"][j, n].rearrange("(kc p) n -> p kc n", p=128)
        for kc in range(8):
            kb.dma("pool", lambda e, n=n, kc=kc, src=src: e.dma_start(out=wrkv[:, n, kc, :], in_=src[:, kc, :]), writes=[wr_])
    kb.dma("pool", lambda e: e.dma_start(out=w1[:], in_=prm["rw_w1"][j].rearrange("(kc p) n -> p kc n", p=128)), writes=[wr_])
    kb.dma("pool", lambda e: e.dma_start(out=a1[:], in_=prm["rw_a1"][j].rearrange("(kc p) n -> p kc n", p=128)), writes=[wr_])
    kb.dma("pool", lambda e: e.dma_start(out=g1[:], in_=prm["rw_g1"][j].rearrange("(kc p) n -> p kc n", p=128)), writes=[wr_])
    kb.dma("pool", lambda e: e.dma_start(out=w2[:], in_=prm["rw_w2"][j]), writes=[wr_])
    kb.dma("pool", lambda e: e.dma_start(out=a2[:], in_=prm["rw_a2"][j]), writes=[wr_])
    kb.dma("pool", lambda e: e.dma_start(out=g2a[:], in_=prm["rw_g2"][j, 0:128, :]), writes=[wr_])
    kb.dma("pool", lambda e: e.dma_start(out=g2b[:], in_=prm["rw_g2"][j, 128:160, :]), writes=[wr_])
    if j > 0:
        v1 = kb.sb("r1_v1", [128, 8, 32], BF16)
        v2 = kb.sb("r1_v2", [32, 1024], BF16)
        v0b = kb.sb("r1_v0b", [128, 1024], F32)
        kb.dma("pool", lambda e: e.dma_start(out=v1[:], in_=prm["rw_v1"][j - 1].rearrange("(kc p) n -> p kc n", p=128)), writes=[wr_])
        kb.dma("pool", lambda e: e.dma_start(out=v2[:], in_=prm["rw_v2"][j - 1]), writes=[wr_])
        kb.dma("sp", lambda e: e.dma_start(out=v0b[:], in_=bcast_rows(prm["rw_v0"][j - 1:j, :])), writes=[wr_])
    pvin = kb.sb("r1_pvin", [88, 128], F32)
    pvall = kb.sb("r1_pvall", [128, 88], F32)
    oma = kb.sb("r1_oma", [128, 8], F32)
    pv_r = Res()
    kb.dma("sp", lambda e: e.dma_start(out=pvin[0:48, :], in_=prm["rw_mu"][j].rearrange("n (kc p) -> (n kc) p", p=128)), writes=[pv_r])
    for idx, nm in enumerate(("rw_w0", "rw_a0", "rw_k_k", "rw_k_a")):
        kb.dma("sp", lambda e, idx=idx, nm=nm: e.dma_start(out=pvin[48 + 8 * idx:56 + 8 * idx, :], in_=prm[nm][j].rearrange("(oc p) -> oc p", p=128)), writes=[pv_r])
    kb.dma("sp", lambda e: e.dma_start(out=pvin[80:88, :], in_=prm["rw_r_k"][j].rearrange("(oc hh) n -> oc (hh n)", hh=2)), writes=[pv_r])
    kb.op("pe", lambda e: e.transpose(ps[0][:, 0:88], pvin[:], C.ident_f[0:88, 0:88]), reads=[pv_r, C.r], writes=[ps_r[0]])
    kb.op("dve", lambda e: e.tensor_copy(pvall[:], ps[0][:, 0:88]), reads=[ps_r[0]], writes=[pv_r])
    kb.op("dve", lambda e: e.tensor_scalar(oma[:], pvall[:, 72:80], -1.0, 1.0, op0=ALU.mult, op1=ALU.add), reads=[pv_r], writes=[pv_r])

    class _PV:
        def __getitem__(self, key):
            p, idx, oc = key
            if idx == 5:
                return oma[p, oc]
            return pvall[p, (oc.start + 48 + 8 * idx):(oc.stop + 48 + 8 * idx)]

    class _MU:
        def __getitem__(self, key):
            p, n, kc = key
            return pvall[p, (n * 8 + kc.start):(n * 8 + kc.stop)]

    pv = _PV()
    mu = _MU()
    rst = kb.sb("r1_rst", [128, 256], F32)
    kb.op("dve", lambda e: e.memset(rst[:], 1.0), writes=[pv_r])
    kb.op("dve", lambda e: e.memset(rst[:, 0:1], 0.0), writes=[pv_r])
    kb.op("dve", lambda e: e.memset(rst[:, 128:129], 0.0), writes=[pv_r])
    bd64 = kb.sb("r1_bd64", [128, 128], BF16)
    kb.op("dve", lambda e: e.memset(bd64[:], 0.0), writes=[pv_r])
    kb.op("dve", lambda e: e.memset(bd64[0:64, 0:64], 1.0), writes=[pv_r])
    kb.op("dve", lambda e: e.memset(bd64[64:128, 64:128], 1.0), writes=[pv_r])

    xld = [kb.sb("r1_xld%d" % b, [128, 1024], F32) for b in range(2)]
    xld_r = RL(2)
    xTs = [kb.sb("r1_xTs%d" % b, [128, 8, 257], BF16) for b in range(2)]
    xTs_r = RL(2)
    xx = kb.sb("r1_xx", [128, 8, 256], F32)
    xx_r = Res()
    xm = [kb.sb("r1_xm%d" % b, [128, 8, 256], BF16) for b in range(3)]
    xm_r = RL(3)
    AR = kb.sb("r1_AR", [128, 8, 2, 2, 128], BF16)
    BK = kb.sb("r1_BK", [128, 8, 2, 2, 128], BF16)
    rk = kb.sb("r1_rk", [128, 8, 256], BF16)
    Pc = kb.sb("r1_Pc", [128, 2, 8], F32)
    out_r = Res()
    hw = kb.sb("r1_hw", [64, 256], BF16)
    ha = kb.sb("r1_ha", [64, 256], BF16)
    hg1 = kb.sb("r1_hg1", [128, 256], BF16)
    hg2 = kb.sb("r1_hg2", [32, 256], BF16)
    hid_r = Res()
    if j > 0:
        hv = kb.sb("r1_hv", [32, 256], BF16)
    tnames = ("sgw", "cum", "cumx", "pin", "pinv", "pprev", "asig", "kk", "lns", "rn", "kkn", "t1", "k2", "tb")
    tmS = [{k: kb.sb("r1_t%d%s" % (q, k), [128, 256], F32) for k in tnames} for q in range(2)]
    kk2S = [kb.sb("r1_kk2_%d" % q, [128, 256], BF16) for q in range(2)]
    tm_rS = RL(2)
    vsb = [kb.sb("r1_v%d" % b, [128, 1024], F32) for b in range(2)]
    vsb_r = RL(2)
    vb16 = [kb.sb("r1_vb%d" % b, [128, 1024], BF16) for b in range(2)]
    vb_r = RL(2)
    gsb = [kb.sb("r1_g%d" % b, [128, 1024], BF16) for b in range(2)]
    gsb_r = RL(2)
    if j > 0:
        vfs = [kb.sb("r1_vf%d" % b, [128, 1024], F32) for b in range(2)]
        vfs_r = RL(2)
        vmx = [kb.sb("r1_vm%d" % b, [128, 1024], F32) for b in range(2)]
        vmx_r = RL(2)
    kb.op("dve", lambda e: e.memset(xTs[0][:, :, 0:1], 0.0), writes=[xTs_r[0]])

    def mix(n, buf, xb):
        for kc in range(8):
            kb.op("dve", lambda e, kc=kc: e.scalar_tensor_tensor(xm[buf][:, kc, :], xx[:, kc, :], mu[:, n, kc:kc + 1], xTs[xb][:, kc, 1:257], op0=ALU.mult, op1=ALU.add),
                  reads=[xx_r, pv_r, xTs_r[xb]], writes=[xm_r[buf]])

    for s in range(NSUP):
        xb = s % 2
        if s > 0:
            kb.op("pool", lambda e, xb=xb: e.tensor_copy(xTs[xb][:, :, 0:1], xTs[1 - xb][:, :, 256:257]), reads=[xTs_r[1 - xb]], writes=[xTs_r[xb]])
        for tl in range(2):
            i = s * 2 + tl
            b = i % 2
            kb.dma("sp", lambda e, i=i, b=b: e.dma_start(out=xld[b][:], in_=xin[i * 128:(i + 1) * 128, :]), reads=[xin_r[i]], writes=[xld_r[b]])
            for hf in range(2):
                pb = 6 + hf
                for c4 in range(4):
                    kc = hf * 4 + c4
                    kb.op("pe", lambda e, pb=pb, c4=c4, kc=kc, b=b: e.transpose(ps[pb][:, c4 * 128:(c4 + 1) * 128], xld[b][:, kc * 128:(kc + 1) * 128], C.ident_f[:]),
                          reads=[xld_r[b], C.r], writes=[ps_r[pb]])
                if hf == 0:
                    kb.op("act", lambda e, pb=pb, xb=xb, tl=tl: e.activation(xTs[xb][:, 0:4, 1 + tl * 128:1 + (tl + 1) * 128], ps[pb][:].rearrange("p (c t) -> p c t", c=4), AF.Copy),
                          reads=[ps_r[pb]], writes=[xTs_r[xb]])
                else:
                    kb.op("dve", lambda e, pb=pb, xb=xb, tl=tl: e.tensor_copy(xTs[xb][:, 4:8, 1 + tl * 128:1 + (tl + 1) * 128], ps[pb][:].rearrange("p (c t) -> p c t", c=4)),
                          reads=[ps_r[pb]], writes=[xTs_r[xb]])
        kb.op("dve", lambda e, xb=xb: e.tensor_tensor(xx[:], xTs[xb][:, :, 0:256], xTs[xb][:, :, 1:257], op=ALU.subtract), reads=[xTs_r[xb]], writes=[xx_r])
        mix(3, 0, xb)
        for kc in range(8):
            kb.op("pe", lambda e, kc=kc: e.matmul(ps[5][0:64, 0:256], w1[:, kc, :], xm[0][:, kc, :], start=(kc == 0), stop=(kc == 7)), reads=[wr_, xm_r[0]], writes=[ps_r[5]])
        kb.op("act", lambda e: e.activation(hw[:], ps[5][0:64, 0:256], AF.Tanh), reads=[ps_r[5]], writes=[hid_r])
        mix(4, 1, xb)
        for kc in range(8):
            kb.op("pe", lambda e, kc=kc: e.matmul(ps[5][0:64, 256:512], a1[:, kc, :], xm[1][:, kc, :], start=(kc == 0), stop=(kc == 7)), reads=[wr_, xm_r[1]], writes=[ps_r[5]])
        kb.op("act", lambda e: e.activation(ha[:], ps[5][0:64, 256:512], AF.Copy), reads=[ps_r[5]], writes=[hid_r])
        mix(5, 2, xb)
        for kc in range(8):
            kb.op("pe", lambda e, kc=kc: e.matmul(ps[4][:, 0:256], g1[:, kc, 0:128], xm[2][:, kc, :], start=(kc == 0), stop=(kc == 7)), reads=[wr_, xm_r[2]], writes=[ps_r[4]])
        for kc in range(8):
            kb.op("pe", lambda e, kc=kc: e.matmul(ps[4][0:32, 256:512], g1[:, kc, 128:160], xm[2][:, kc, :], start=(kc == 0), stop=(kc == 7)), reads=[wr_, xm_r[2]], writes=[ps_r[4]])
        kb.op("act", lambda e: e.activation(hg1[:], ps[4][:, 0:256], AF.Sigmoid), reads=[ps_r[4]], writes=[hid_r])
        kb.op("act", lambda e: e.activation(hg2[:], ps[4][0:32, 256:512], AF.Sigmoid), reads=[ps_r[4]], writes=[hid_r])
        for tl in range(2):
            i = s * 2 + tl
            b = i % 2
            for hf in range(2):
                pb = 6 + hf
                kb.op("pe", lambda e, pb=pb, tl=tl, hf=hf: e.matmul(ps[pb][:], hg1[:, tl * 128:(tl + 1) * 128], g2a[:, hf * 512:(hf + 1) * 512], start=True, stop=False), reads=[hid_r, wr_], writes=[ps_r[pb]])
                kb.op("pe", lambda e, pb=pb, tl=tl, hf=hf: e.matmul(ps[pb][:], hg2[:, tl * 128:(tl + 1) * 128], g2b[:, hf * 512:(hf + 1) * 512], start=False, stop=True), reads=[hid_r, wr_], writes=[ps_r[pb]])
                if hf == 0:
                    kb.op("act", lambda e, pb=pb, b=b: e.activation(gsb[b][:, 0:512], ps[pb][:], AF.Copy), reads=[ps_r[pb]], writes=[gsb_r[b]])
                else:
                    kb.op("dve", lambda e, pb=pb, b=b: e.tensor_copy(gsb[b][:, 512:1024], ps[pb][:]), reads=[ps_r[pb]], writes=[gsb_r[b]])
            kb.dma("sp", lambda e, i=i, b=b: e.dma_start(out=Gd[i * 128:(i + 1) * 128, :], in_=gsb[b][:]), reads=[gsb_r[b]])
        mix(2, 0, xb)
        if j > 0:
            for kc in range(8):
                kb.op("pe", lambda e, kc=kc: e.matmul(ps[5][0:32, 0:256], v1[:, kc, :], xm[0][:, kc, :], start=(kc == 0), stop=(kc == 7)), reads=[wr_, xm_r[0]], writes=[ps_r[5]])
            kb.op("act", lambda e: e.activation(hv[:], ps[5][0:32, 0:256], AF.Copy), reads=[ps_r[5]], writes=[hid_r])
        for tl in range(2):
            i = s * 2 + tl
            b = i % 2
            for hf in range(2):
                pb = 6 + hf
                for kc in range(8):
                    kb.op("pe", lambda e, pb=pb, tl=tl, hf=hf, kc=kc: e.matmul(ps[pb][:], xm[0][:, kc, tl * 128:(tl + 1) * 128], wrkv[:, 2, kc, hf * 512:(hf + 1) * 512], start=(kc == 0), stop=(kc == 7)),
                          reads=[xm_r[0], wr_], writes=[ps_r[pb]])
                if hf == 0:
                    kb.op("act", lambda e, pb=pb, b=b: e.activation(vsb[b][:, 0:512], ps[pb][:], AF.Copy), reads=[ps_r[pb]], writes=[vsb_r[b]])
                else:
                    kb.op("dve", lambda e, pb=pb, b=b: e.tensor_copy(vsb[b][:, 512:1024], ps[pb][:]), reads=[ps_r[pb]], writes=[vsb_r[b]])
            if j == 0:
                kb.dma("sp", lambda e, i=i, b=b: e.dma_start(out=vfd[i * 128:(i + 1) * 128, :], in_=vsb[b][:]), reads=[vsb_r[b]])
                kb.op("pool", lambda e, b=b: e.tensor_copy(vb16[b][:], vsb[b][:]), reads=[vsb_r[b]], writes=[vb_r[b]])
            else:
                kb.dma("sp", lambda e, i=i, b=b: e.dma_start(out=vfs[b][:], in_=vfd[i * 128:(i + 1) * 128, :]), writes=[vfs_r[b]])
                for hf in range(2):
                    pb = 6 + hf
                    kb.op("pe", lambda e, pb=pb, tl=tl, hf=hf: e.matmul(ps[pb][:], hv[:, tl * 128:(tl + 1) * 128], v2[:, hf * 512:(hf + 1) * 512], start=True, stop=True), reads=[hid_r, wr_], writes=[ps_r[pb]])
                    kb.op("dve", lambda e, pb=pb, b=b, hf=hf: e.tensor_tensor(vmx[b][:, hf * 512:(hf + 1) * 512], ps[pb][:], v0b[:, hf * 512:(hf + 1) * 512], op=ALU.add), reads=[ps_r[pb], wr_], writes=[vmx_r[b]])
                kb.op("act", lambda e, b=b: e.activation(vmx[b][:], vmx[b][:], AF.Sigmoid), reads=[vmx_r[b]], writes=[vmx_r[b]])
                kb.op("pool", lambda e, b=b: e.tensor_tensor(vfs[b][:], vfs[b][:], vsb[b][:], op=ALU.subtract), reads=[vfs_r[b], vsb_r[b]], writes=[vfs_r[b]])
                kb.op("pool", lambda e, b=b: e.tensor_tensor(vfs[b][:], vfs[b][:], vmx[b][:], op=ALU.mult), reads=[vfs_r[b], vmx_r[b]], writes=[vfs_r[b]])
                kb.op("pool", lambda e, b=b: e.tensor_tensor(vb16[b][:], vfs[b][:], vsb[b][:], op=ALU.add), reads=[vfs_r[b], vsb_r[b]], writes=[vb_r[b]])
            kb.dma("sp", lambda e, i=i, b=b: e.dma_start(out=Vd[i * 128:(i + 1) * 128, :], in_=vb16[b][:]), reads=[vb_r[b]])
        mix(0, 1, xb)
        mix(1, 2, xb)
        def oc_chain(oc):
            par = oc % 2
            T_ = tmS[par]
            kk2_ = kk2S[par]
            tmr = tm_rS[par]
            psum_kk = ps[5] if par == 0 else ps[4]
            psum_kk_r = ps_r[5] if par == 0 else ps_r[4]
            osl = slice(oc * 128, (oc + 1) * 128)
            pr, pk = par * 2, par * 2 + 1
            for kc in range(8):
                kb.op("pe", lambda e, pr=pr, kc=kc, osl=osl: e.matmul(ps[pr][:, 0:256], wrkv[:, 0, kc, osl], xm[1][:, kc, :], start=(kc == 0), stop=(kc == 7)), reads=[wr_, xm_r[1]], writes=[ps_r[pr]])
            for kc in range(8):
                kb.op("pe", lambda e, pk=pk, kc=kc, osl=osl: e.matmul(ps[pk][:, 0:256], wrkv[:, 1, kc, osl], xm[2][:, kc, :], start=(kc == 0), stop=(kc == 7)), reads=[wr_, xm_r[2]], writes=[ps_r[pk]])
            kb.op("pe", lambda e, pr=pr, osl=osl: e.matmul(ps[pr][:, 256:512], w2[:, osl], hw[:], start=True, stop=True), reads=[wr_, hid_r], writes=[ps_r[pr]])
            kb.op("pe", lambda e, pk=pk, osl=osl: e.matmul(ps[pk][:, 256:512], a2[:, osl], ha[:], start=True, stop=True), reads=[wr_, hid_r], writes=[ps_r[pk]])
            r_ps, k_ps, w_ps, a_ps = ps[pr][:, 0:256], ps[pk][:, 0:256], ps[pr][:, 256:512], ps[pk][:, 256:512]
            R2 = [ps_r[pr], ps_r[pk], tmr, pv_r]
            L = []

            def o(eng, fn, extra_w=()):
                L.append((eng, fn, R2, [tmr] + list(extra_w)))

            o("act", lambda e: e.activation(T_["sgw"][:], w_ps, AF.Sigmoid, bias=pv[:, 0, oc:oc + 1]))
            o("act", lambda e: e.activation(T_["asig"][:], a_ps, AF.Sigmoid, bias=pv[:, 1, oc:oc + 1]))
            o("act", lambda e: e.activation(T_["kk"][:], k_ps, AF.Identity, scale=pv[:, 2, oc:oc + 1]))
            o("dve", lambda e: e.tensor_tensor_scan(T_["cum"][:], rst[:], T_["sgw"][:], 0.0, op0=ALU.mult, op1=ALU.add))
            o("pool", lambda e: e.tensor_tensor(kk2_[:], T_["kk"][:], T_["kk"][:], op=ALU.mult))
            L.append(("pe", lambda e: e.matmul(psum_kk[:, 0:256], bd64[:], kk2_[:], start=True, stop=True), [tmr, pv_r], [psum_kk_r]))
            o("pool", lambda e: e.tensor_tensor(T_["cumx"][:], T_["cum"][:], T_["sgw"][:], op=ALU.subtract))
            o("act", lambda e: e.activation(T_["pin"][:], T_["cum"][:], AF.Exp, scale=-C0))
            o("act", lambda e: e.activation(T_["pinv"][:], T_["cum"][:], AF.Exp, scale=C0))
            o("act", lambda e: e.activation(T_["pprev"][:], T_["cumx"][:], AF.Exp, scale=-C0))
            L.append(("dve", lambda e: e.tensor_scalar(T_["lns"][:], psum_kk[:, 0:256], 1e-30, None, op0=ALU.add), [psum_kk_r, tmr], [tmr]))
            o("act", lambda e: e.activation(T_["lns"][:], T_["lns"][:], AF.Ln))
            o("act", lambda e: e.activation(T_["rn"][:], T_["lns"][:], AF.Exp, scale=-0.5))
            o("pool", lambda e: e.tensor_tensor(T_["kkn"][:], T_["kk"][:], T_["rn"][:], op=ALU.mult))
            o("dve", lambda e: e.tensor_scalar(T_["t1"][:], T_["asig"][:], pv[:, 3, oc:oc + 1], pv[:, 5, oc:oc + 1], op0=ALU.mult, op1=ALU.add))
            o("dve", lambda e: e.tensor_tensor(T_["k2"][:], k_ps, T_["t1"][:], op=ALU.mult))
            for tl_ in range(2):
                o("dve", lambda e, tl_=tl_: e.tensor_copy(Pc[:, tl_, oc:oc + 1], T_["pin"][:, 127 + 128 * tl_:128 + 128 * tl_]), extra_w=[out_r])
            o("dve", lambda e: e.scalar_tensor_tensor(AR[:, oc, :, 0, :], T_["kkn"][:].rearrange("p (a t) -> p a t", a=2), -1.0, T_["pprev"][:].rearrange("p (a t) -> p a t", a=2), op0=ALU.mult, op1=ALU.mult), extra_w=[out_r])
            o("dve", lambda e: e.tensor_tensor(AR[:, oc, :, 1, :], r_ps.rearrange("p (a t) -> p a t", a=2), T_["pin"][:].rearrange("p (a t) -> p a t", a=2), op=ALU.mult), extra_w=[out_r])
            o("pool", lambda e: e.tensor_tensor(T_["tb"][:], T_["kkn"][:], T_["asig"][:], op=ALU.mult))
            o("pool", lambda e: e.tensor_tensor(BK[:, oc, :, 0, :], T_["tb"][:].rearrange("p (a t) -> p a t", a=2), T_["pinv"][:].rearrange("p (a t) -> p a t", a=2), op=ALU.mult), extra_w=[out_r])
            o("pool", lambda e: e.tensor_tensor(BK[:, oc, :, 1, :], T_["k2"][:].rearrange("p (a t) -> p a t", a=2), T_["pinv"][:].rearrange("p (a t) -> p a t", a=2), op=ALU.mult), extra_w=[out_r])
            o("dve", lambda e: e.scalar_tensor_tensor(rk[:, oc, :], r_ps, pv[:, 4, oc:oc + 1], T_["k2"][:], op0=ALU.mult, op1=ALU.mult), extra_w=[out_r])
            return L

        for ocp in range(4):
            La = oc_chain(2 * ocp)
            Lb = oc_chain(2 * ocp + 1)
            for ia in range(len(La)):
                kb.op(La[ia][0], La[ia][1], reads=La[ia][2], writes=La[ia][3])
                kb.op(Lb[ia][0], Lb[ia][1], reads=Lb[ia][2], writes=Lb[ia][3])
        for tl in range(2):
            i = s * 2 + tl
            kb.dma("sp", lambda e, i=i, tl=tl: e.dma_start(out=ARd[i].rearrange("p (o c t) -> p o c t", o=8, c=2), in_=AR[:, :, tl, :, :]), reads=[out_r])
            kb.dma("sp", lambda e, i=i, tl=tl: e.dma_start(out=BKd[i].rearrange("p (o c t) -> p o c t", o=8, c=2), in_=BK[:, :, tl, :, :]), reads=[out_r])
            kb.dma("sp", lambda e, i=i, tl=tl: e.dma_start(out=rkd[i].rearrange("p (o t) -> p o t", o=8), in_=rk[:, :, tl * 128:(tl + 1) * 128]), reads=[out_r])
            kb.dma("sp", lambda e, i=i, tl=tl: e.dma_start(out=Pcd[i], in_=Pc[:, tl, :]), reads=[out_r])
    kb_pop(kb)
    if not ST.get("skip_p2"):
        rwkv_pass2(kb, C, T, prm, j, li, xin, xin_r, xa, xa_r, ST, nc)


def rwkv_pass2(kb, C, T, prm, j, li, xin, xin_r, xa, xa_r, ST, nc):
    NT = T // 128
    ARd, BKd, rkd, Pcd, Vd, Gd = (ST[k] for k in ("ARd", "BKd", "rkd", "Pcd", "Vd", "Gd"))
    kb_push(kb)
    pF = [kb.ps("r2_pf%d" % b, [128, 512], F32) for b in range(4)]
    RG = [pF[b][:, 0:256] for b in range(4)]
    RG_r = RL(4)
    rgc = [0]

    def nrg():
        rgc[0] += 1
        return rgc[0] % 4
    pT = kb.ps("r2_pT", [128, 1024], BF16)
    pT_r = Res()
    pY = [kb.ps("r2_pY%d" % b, [128, 512], F32) for b in range(2)]
    pY_r = RL(2)
    pH = kb.ps("r2_pH", [128, 512], F32)
    pH_r = Res()
    wo = kb.sb("r2_wo", [128, 8, 1024], BF16)
    wo_r = Res()
    wsrc = prm["rw_w_o"][j].rearrange("(kc p) n -> p kc n", p=128)
    for kc in range(8):
        kb.dma("pool", lambda e, kc=kc: e.dma_start(out=wo[:, kc, :], in_=wsrc[:, kc, :]), writes=[wo_r])
    cst_r = Res()
    m2 = kb.sb("r2_m2", [128, 2, 128], F32)
    kb.op("dve", lambda e: e.tensor_copy(m2[:, 0, :], C.m_st[:]), reads=[C.r], writes=[cst_r])
    kb.op("dve", lambda e: e.tensor_copy(m2[:, 1, :], C.m_in[:]), reads=[C.r], writes=[cst_r])
    bdm = kb.sb("r2_bdm", [128, 128], F32)
    kb.op("dve", lambda e: e.memset(bdm[:], 0.0), writes=[cst_r])
    kb.op("dve", lambda e: e.memset(bdm[0:64, 0:64], 1.0), writes=[cst_r])
    kb.op("dve", lambda e: e.memset(bdm[64:128, 64:128], 1.0), writes=[cst_r])
    HS = kb.sb("r2_HS", [128, 8, 16], BF16)
    kb.op("dve", lambda e: e.memset(HS[:], 0.0), writes=[cst_r])
    for oc in range(8):
        for hh in range(2):
            kb.op("dve", lambda e, oc=oc, hh=hh: e.memset(HS[hh * 64:(hh + 1) * 64, oc, 2 * oc + hh:2 * oc + hh + 1], 1.0), writes=[cst_r])
    lng = kb.sb("r2_lng", [128, 1024], F32)
    lnb = kb.sb("r2_lnb", [128, 1024], F32)
    kb.dma("sp", lambda e: e.dma_start(out=lng[:], in_=bcast_rows(prm["rw_lnx_g"][j:j + 1, :])), writes=[cst_r])
    kb.dma("sp", lambda e: e.dma_start(out=lnb[:], in_=bcast_rows(prm["rw_lnx_b"][j:j + 1, :])), writes=[cst_r])
    gneps = kb.sb("r2_gneps", [128, 1], F32)
    kb.op("dve", lambda e: e.memset(gneps[:], GN_EPS), writes=[cst_r])
    LS = ln_scratch(kb, "r2_ln", LN_EPS)
    g_bc, b_bc, lnp_r = load_ln_params(kb, "r2_lnp", prm["ln1_g"], prm["ln1_b"], li)

    ARt = [kb.sb("r2_AR%d" % b, [128, 8, 2, 128], BF16) for b in range(2)]
    BKt = [kb.sb("r2_BK%d" % b, [128, 8, 2, 128], BF16) for b in range(2)]
    rkt = [kb.sb("r2_rk%d" % b, [128, 8, 128], BF16) for b in range(2)]
    Pct = [kb.sb("r2_Pc%d" % b, [128, 8], F32) for b in range(2)]
    Vt = [kb.sb("r2_V%d" % b, [128, 1024], BF16) for b in range(2)]
    Gt = [kb.sb("r2_G%d" % b, [128, 1024], BF16) for b in range(2)]
    xt = [kb.sb("r2_xt%d" % b, [128, 1024], F32) for b in range(2)]
    in_r = RL(2)
    Hb = kb.sb("r2_H", [128, 8, 64], BF16)
    Hb_r = RL(8)
    kb.op("dve", lambda e: e.memset(Hb[:], 0.0), writes=Hb_r)
    TK = [kb.sb("r2_TK%d" % u, [128, 3, 128], BF16) for u in range(8)]
    TK_r = RL(8)
    XA = [kb.sb("r2_XA%d" % u, [128, 2, 128], BF16) for u in range(16)]
    KA = [kb.sb("r2_KA%d" % u, [128, 2, 128], BF16) for u in range(16)]
    XAr, KAr = RL(16), RL(16)
    XN = [[kb.sb("r2_XN%d_%d" % (u, q), [128, 2, 128], BF16) for q in range(2)] for u in range(16)]
    XNr = [RL(2) for _ in range(16)]
    PQ = [[kb.sb("r2_PQ%d_%d" % (u, q), [128, 2, 128], BF16) for q in range(2)] for u in range(16)]
    PQr = [RL(2) for _ in range(16)]
    AV = [kb.sb("r2_AV%d" % u, [128, 64], BF16) for u in range(16)]
    AVr = RL(16)
    W12 = [kb.sb("r2_W12%d" % u, [128, 2, 2, 64], BF16) for u in range(8)]
    W12_r = RL(8)
    MT = [kb.sb("r2_MT%d" % u, [128, 128], BF16) for u in range(8)]
    MTf = [kb.sb("r2_MTf%d" % u, [128, 128], F32) for u in range(2)]
    mtf_r = RL(2)
    GS = [kb.sb("r2_GS%d" % u, [128, 64], F32) for u in range(8)]
    QT = [kb.sb("r2_QT%d" % u, [128, 128], BF16) for u in range(8)]
    MT_r, GS_r, QT_r = RL(8), RL(8), RL(8)
    pT_rr = RL(2)
    ysb = kb.sb("r2_y", [128, 1024], F32)
    ysq = kb.sb("r2_ysq", [128, 1024], F32)
    yn = kb.sb("r2_yn", [128, 1024], F32)
    yg = kb.sb("r2_yg", [128, 1024], BF16)
    ygT = kb.sb("r2_ygT", [128, 8, 128], BF16)
    st = kb.sb("r2_st", [128, 8, 16], F32)
    y_r = Res()
    zt = [kb.sb("r2_z%d" % b, [128, 1024], F32) for b in range(2)]
    zt_r = RL(2)
    x1 = [kb.sb("r2_x1%d" % b, [128, 1024], F32) for b in range(2)]
    x1_r = RL(2)

    pending = []
    for i in range(NT):
        b = i % 2
        kb.dma("sp", lambda e, i=i, b=b: e.dma_start(out=ARt[b][:], in_=ARd[i].rearrange("p (o c t) -> p o c t", o=8, c=2)), writes=[in_r[b]])
        kb.dma("sp", lambda e, i=i, b=b: e.dma_start(out=BKt[b][:], in_=BKd[i].rearrange("p (o c t) -> p o c t", o=8, c=2)), writes=[in_r[b]])
        kb.dma("sp", lambda e, i=i, b=b: e.dma_start(out=rkt[b][:], in_=rkd[i].rearrange("p (o t) -> p o t", o=8)), writes=[in_r[b]])
        kb.dma("sp", lambda e, i=i, b=b: e.dma_start(out=Pct[b][:], in_=Pcd[i]), writes=[in_r[b]])
        kb.dma("sp", lambda e, i=i, b=b: e.dma_start(out=Vt[b][:], in_=Vd[i * 128:(i + 1) * 128, :]), writes=[in_r[b]])
        kb.dma("sp", lambda e, i=i, b=b: e.dma_start(out=Gt[b][:], in_=Gd[i * 128:(i + 1) * 128, :]), writes=[in_r[b]])
        kb.dma("sp", lambda e, i=i, b=b: e.dma_start(out=xt[b][:], in_=xin[i * 128:(i + 1) * 128, :]), reads=[xin_r[i]], writes=[in_r[b]])
        ARb, BKb, Vb, Pcb = ARt[b], BKt[b], Vt[b], Pct[b]
        inr = in_r[b]
        for hp in range(8):
            tr = 0
            for n, src in enumerate((ARb[:, hp, 0, :], BKb[:, hp, 0, :], BKb[:, hp, 1, :])):
                kb.op("pe", lambda e, n=n, src=src, tr=tr: e.transpose(pT[:, tr * 384 + n * 128:tr * 384 + (n + 1) * 128], src, C.ident_b[:]), reads=[inr, C.r], writes=[pT_rr[tr]])
            kb.op("act", lambda e, hp=hp, tr=tr: e.activation(TK[hp][:], pT[:, tr * 384:tr * 384 + 384].rearrange("p (c t) -> p c t", c=3), AF.Copy), reads=[pT_rr[tr]], writes=[TK_r[hp]])
        for u in range(16):
            hp, hh = u // 2, u % 2
            psl = slice(64 * hh, 64 * hh + 64)
            rhs_ar = ARb[psl, hp, :, :].rearrange("p c t -> p (c t)")
            for which, dstT, dst_r in ((0, XA, XAr), (1, KA, KAr)):
                g = nrg()
                kb.op("pe", lambda e, g=g, psl=psl, hp=hp, which=which, rhs_ar=rhs_ar: e.matmul(RG[g], BKb[psl, hp, which, :], rhs_ar, start=True, stop=True), reads=[inr], writes=[RG_r[g]])
                kb.op("dve", lambda e, g=g, u=u, dstT=dstT: e.tensor_tensor(dstT[u][:], RG[g].rearrange("p (c t) -> p c t", c=2), m2[:], op=ALU.mult), reads=[RG_r[g], cst_r], writes=[dst_r[u]])
            g = nrg()
            kb.op("pe", lambda e, g=g, psl=psl, hp=hp: e.matmul(RG[g][:, 0:128], ARb[psl, hp, 0, :], BKb[psl, hp, 0, :], start=True, stop=True), reads=[inr], writes=[RG_r[g]])
            kb.op("dve", lambda e, g=g, u=u: e.tensor_tensor(XN[u][0][:, 1, :], RG[g][:, 0:128], C.m_lo[:], op=ALU.mult), reads=[RG_r[g], C.r], writes=[XNr[u][0]])
            kb.op("pool", lambda e, u=u: e.tensor_copy(XN[u][0][:, 0, :], XA[u][:, 0, :]), reads=[XAr[u]], writes=[XNr[u][0]])
        for u in range(16):
            for c in range(2):
                kb.op("pool", lambda e, u=u, c=c: e.tensor_tensor(PQ[u][0][:, c, :], XN[u][0][:, c, :], C.ident_b[:], op=ALU.add), reads=[XNr[u][0], C.r], writes=[PQr[u][0]])
        for k in range(1, 7):
            cur, prv = k % 2, (k - 1) % 2
            for u in range(16):
                g = nrg()
                Xp, Np = XN[u][prv][:, 0, :], XN[u][prv][:, 1, :]
                kb.op("pe", lambda e, g=g, Xp=Xp, Np=Np: e.matmul(RG[g][:, 0:128], Np, Xp, start=True, stop=True), reads=[XNr[u][prv]], writes=[RG_r[g]])
                if k < 6:
                    kb.op("pe", lambda e, g=g, Xp=Xp, Np=Np: e.matmul(RG[g][:, 128:256], Xp, Np, start=True, stop=True), reads=[XNr[u][prv]], writes=[RG_r[g]])
                    kb.op("act", lambda e, g=g, u=u, cur=cur: e.activation(XN[u][cur][:], RG[g].rearrange("p (c t) -> p c t", c=2), AF.Copy), reads=[RG_r[g]], writes=[XNr[u][cur]])
                else:
                    kb.op("act", lambda e, g=g, u=u, cur=cur: e.activation(XN[u][cur][:, 0, :], RG[g][:, 0:128], AF.Copy), reads=[RG_r[g]], writes=[XNr[u][cur]])
            kb_flush(kb, pending, (len(pending) + (6 - k)) // (7 - k))
            for u in range(16):
                g = nrg()
                Xc = XN[u][cur][:, 0, :]
                Qp = PQ[u][prv][:, 1, :]
                kb.op("pe", lambda e, g=g, Xc=Xc, Qp=Qp: e.matmul(RG[g][:, 0:128], Qp, Xc, start=True, stop=True), reads=[XNr[u][cur], PQr[u][prv]], writes=[RG_r[g]])
                if k < 6:
                    kb.op("pe", lambda e, g=g, Xc=Xc, Qp=Qp: e.matmul(RG[g][:, 128:256], Xc, Qp, start=True, stop=True), reads=[XNr[u][cur], PQr[u][prv]], writes=[RG_r[g]])
                    kb.op("dve", lambda e, g=g, u=u, cur=cur, prv=prv: e.tensor_tensor(PQ[u][cur][:], RG[g].rearrange("p (c t) -> p c t", c=2), PQ[u][prv][:], op=ALU.add), reads=[RG_r[g], PQr[u][prv]], writes=[PQr[u][cur]])
                else:
                    kb.op("dve", lambda e, g=g, u=u, cur=cur, prv=prv: e.tensor_tensor(PQ[u][cur][:, 0, :], RG[g][:, 0:128], PQ[u][prv][:, 0, :], op=ALU.add), reads=[RG_r[g], PQr[u][prv]], writes=[PQr[u][cur]])
        kb_flush(kb, pending, len(pending))
        Pfin = 0
        for u in range(16):
            hp, hh = u // 2, u % 2
            hc = slice(hp * 128 + hh * 64, hp * 128 + hh * 64 + 64)
            g = nrg()
            kb.op("pe", lambda e, g=g, u=u, hc=hc: e.matmul(RG[g][:, 0:64], KA[u][:, 0, :], Vb[:, hc], start=True, stop=True), reads=[KAr[u], inr], writes=[RG_r[g]])
            kb.op("act", lambda e, g=g, u=u: e.activation(AV[u][:], RG[g][:, 0:64], AF.Copy), reads=[RG_r[g]], writes=[AVr[u]])
        for u in range(16):
            hp, hh = u // 2, u % 2
            g = nrg()
            kb.op("pe", lambda e, g=g, u=u: e.matmul(RG[g][:, 0:64], PQ[u][Pfin][:, 0, :], AV[u][:], start=True, stop=True), reads=[PQr[u][Pfin], AVr[u]], writes=[RG_r[g]])
            kb.op("pe", lambda e, g=g, u=u, hp=hp, hh=hh: e.matmul(RG[g][:, 64:128], PQ[u][Pfin][:, 0, :], TK[hp][:, 0, hh * 64:(hh + 1) * 64], start=True, stop=True), reads=[PQr[u][Pfin], TK_r[hp]], writes=[RG_r[g]])
            kb.op("act", lambda e, g=g, hp=hp, hh=hh: e.activation(W12[hp][:, :, hh, :], RG[g][:, 0:128].rearrange("p (c t) -> p c t", c=2), AF.Copy), reads=[RG_r[g]], writes=[W12_r[hp]])
        for hp in range(8):
            W1p = W12[hp][:, 0, :, :].rearrange("p h t -> p (h t)")
            W2p = W12[hp][:, 1, :, :].rearrange("p h t -> p (h t)")
            g = nrg()
            mq = hp % 2
            kb.op("pe", lambda e, g=g, W2p=W2p, hp=hp: e.matmul(RG[g][:, 0:128], W2p, TK[hp][:, 1, :], start=True, stop=True), reads=[W12_r[hp], TK_r[hp]], writes=[RG_r[g]])
            kb.op("pe", lambda e, g=g, W1p=W1p, hp=hp: e.matmul(RG[g][:, 128:256], TK[hp][:, 1, :], W1p, start=True, stop=False), reads=[W12_r[hp], TK_r[hp]], writes=[RG_r[g]])
            kb.op("pe", lambda e, g=g, hp=hp: e.matmul(RG[g][:, 128:256], TK[hp][:, 2, :], Vb[:, hp * 128:(hp + 1) * 128], start=False, stop=True), reads=[TK_r[hp], inr], writes=[RG_r[g]])
            kb.op("dve", lambda e, g=g, mq=mq: e.tensor_tensor(MTf[mq][:], RG[g][:, 0:128], bdm[:], op=ALU.mult), reads=[RG_r[g], cst_r], writes=[mtf_r[mq]])
            kb.op("pool", lambda e, hp=hp, mq=mq: e.tensor_tensor(MT[hp][:], MTf[mq][:], C.ident_f[:], op=ALU.add), reads=[mtf_r[mq], C.r], writes=[MT_r[hp]])
            for hh in range(2):
                psl = slice(64 * hh, 64 * hh + 64)
                kb.op("dve", lambda e, g=g, psl=psl, hh=hh, hp=hp: e.tensor_scalar(GS[hp][psl, :], RG[g][psl, 128 + 64 * hh:192 + 64 * hh], Pcb[psl, hp:hp + 1], None, op0=ALU.mult), reads=[RG_r[g], inr], writes=[GS_r[hp]])
            g2 = nrg()
            for hh in range(2):
                u = 2 * hp + hh
                psl = slice(64 * hh, 64 * hh + 64)
                kb.op("pe", lambda e, g2=g2, hh=hh, W2p=W2p, u=u: e.matmul(RG[g2][:, 128 * hh:128 * hh + 128], W2p, XA[u][:, 1, :], start=True, stop=True), reads=[W12_r[hp], XAr[u]], writes=[RG_r[g2]])
            for hh in range(2):
                psl = slice(64 * hh, 64 * hh + 64)
                kb.op("dve", lambda e, g2=g2, psl=psl, hh=hh, hp=hp: e.tensor_tensor(QT[hp][psl, :], RG[g2][psl, 128 * hh:128 * hh + 128], ARb[psl, hp, 1, :], op=ALU.add), reads=[RG_r[g2], inr], writes=[QT_r[hp]])
        for hp in range(8):
            yb_, yc = pY[hp // 4], (hp % 4) * 128
            for hh in range(2):
                u = 2 * hp + hh
                psl = slice(64 * hh, 64 * hh + 64)
                hc = slice(hp * 128 + hh * 64, hp * 128 + hh * 64 + 64)
                yo = yb_[:, yc + 64 * hh:yc + 64 * hh + 64]
                kb.op("pe", lambda e, yo=yo, hh=hh, u=u, hp=hp: e.matmul(yo, XA[u][:, 1, :], W12[hp][:, 0, hh, :], start=True, stop=False), reads=[XAr[u], W12_r[hp]], writes=[pY_r[hp // 4]])
                kb.op("pe", lambda e, yo=yo, u=u, hc=hc: e.matmul(yo, KA[u][:, 1, :], Vb[:, hc], start=False, stop=False), reads=[KAr[u], inr], writes=[pY_r[hp // 4]])
                kb.op("pe", lambda e, yo=yo, psl=psl, hp=hp: e.matmul(yo, QT[hp][psl, :], Hb[psl, hp, :], start=False, stop=True), reads=[QT_r[hp], Hb_r[hp]], writes=[pY_r[hp // 4]])
            g = nrg()
            kb.op("pe", lambda e, g=g, hp=hp: e.matmul(RG[g][:, 0:64], MT[hp][:], Hb[:, hp, :], start=True, stop=True), reads=[MT_r[hp], Hb_r[hp]], writes=[RG_r[g]])
            kb.op("dve", lambda e, g=g, hp=hp: e.scalar_tensor_tensor(Hb[:, hp, :], RG[g][:, 0:64], Pcb[:, hp:hp + 1], GS[hp][:], op0=ALU.mult, op1=ALU.add), reads=[RG_r[g], inr, GS_r[hp]], writes=[Hb_r[hp]])
        kb.op("act", lambda e: e.activation(ysb[:, 0:512], pY[0][:], AF.Copy), reads=[pY_r[0]], writes=[y_r])
        kb.op("dve", lambda e: e.tensor_copy(ysb[:, 512:1024], pY[1][:]), reads=[pY_r[1]], writes=[y_r])
        def post(i=i, b=b):
            kb.op("pool", lambda e: e.tensor_tensor(ysq[:], ysb[:], ysb[:], op=ALU.mult), reads=[y_r], writes=[y_r])
            kb.op("dve", lambda e: e.reduce_sum(st[:, 0, :], ysb[:].rearrange("p (h n) -> p h n", h=16), axis=AX.X), reads=[y_r], writes=[y_r])
            kb.op("dve", lambda e: e.reduce_sum(st[:, 1, :], ysq[:].rearrange("p (h n) -> p h n", h=16), axis=AX.X), reads=[y_r], writes=[y_r])
            kb.op("dve", lambda e: e.tensor_scalar(st[:, 2, :], st[:, 0, :], 1.0 / 64, None, op0=ALU.mult), reads=[y_r], writes=[y_r])
            kb.op("dve", lambda e: e.tensor_tensor(st[:, 3, :], st[:, 2, :], st[:, 2, :], op=ALU.mult), reads=[y_r], writes=[y_r])
            kb.op("dve", lambda e: e.scalar_tensor_tensor(st[:, 4, :], st[:, 1, :], 1.0 / 64, st[:, 3, :], op0=ALU.mult, op1=ALU.subtract), reads=[y_r], writes=[y_r])
            kb.op("act", lambda e: e.activation(st[:, 5, :], st[:, 4, :], AF.Sqrt, bias=gneps[:, 0:1]), reads=[y_r, cst_r], writes=[y_r])
            kb.op("dve", lambda e: e.reciprocal(st[:, 6, :], st[:, 5, :]), reads=[y_r], writes=[y_r])
            for hd in range(16):
                eng = "dve" if hd % 2 == 0 else "pool"
                kb.op(eng, lambda e, hd=hd: e.tensor_scalar(yn[:, hd * 64:(hd + 1) * 64], ysb[:, hd * 64:(hd + 1) * 64], st[:, 2, hd:hd + 1], st[:, 6, hd:hd + 1], op0=ALU.subtract, op1=ALU.mult), reads=[y_r], writes=[y_r])
            kb.op("pool", lambda e: e.tensor_tensor(yn[:], yn[:], lng[:], op=ALU.mult), reads=[y_r, cst_r], writes=[y_r])
            kb.op("pool", lambda e: e.tensor_tensor(yn[:], yn[:], lnb[:], op=ALU.add), reads=[y_r, cst_r], writes=[y_r])
            for oc in range(8):
                kb.op("pe", lambda e, oc=oc: e.matmul(pH[:, 0:16], rkt[b][:, oc, :], HS[:, oc, :], start=(oc == 0), stop=(oc == 7)), reads=[in_r[b], cst_r], writes=[pH_r])
            kb.op("act", lambda e: e.activation(st[:, 7, :], pH[:, 0:16], AF.Copy), reads=[pH_r], writes=[y_r])
            for hd in range(16):
                kb.op("dve", lambda e, hd=hd: e.scalar_tensor_tensor(yn[:, hd * 64:(hd + 1) * 64], Vt[b][:, hd * 64:(hd + 1) * 64], st[:, 7, hd:hd + 1], yn[:, hd * 64:(hd + 1) * 64], op0=ALU.mult, op1=ALU.add), reads=[y_r, in_r[b]], writes=[y_r])
            kb.op("pool", lambda e: e.tensor_tensor(yg[:], yn[:], Gt[b][:], op=ALU.mult), reads=[y_r, in_r[b]], writes=[y_r])
            for hf in range(2):
                for c4 in range(4):
                    kc = hf * 4 + c4
                    kb.op("pe", lambda e, c4=c4, kc=kc: e.transpose(pT[:, 512 + c4 * 128:512 + (c4 + 1) * 128], yg[:, kc * 128:(kc + 1) * 128], C.ident_b[:]), reads=[y_r, C.r], writes=[pT_rr[0], pT_rr[1]])
                kb.op("act", lambda e, hf=hf: e.activation(ygT[:, hf * 4:hf * 4 + 4, :], pT[:, 512:1024].rearrange("p (c t) -> p c t", c=4), AF.Copy), reads=[pT_rr[0], pT_rr[1]], writes=[y_r])
            for hf in range(2):
                for kc in range(8):
                    kb.op("pe", lambda e, kc=kc, hf=hf: e.matmul(pH[:], ygT[:, kc, :], wo[:, kc, hf * 512:(hf + 1) * 512], start=(kc == 0), stop=(kc == 7)), reads=[y_r, wo_r], writes=[pH_r])
                kb.op("dve", lambda e, hf=hf, b=b: e.scalar_tensor_tensor(zt[b][:, hf * 512:(hf + 1) * 512], xt[b][:, hf * 512:(hf + 1) * 512], ALPHA, pH[:], op0=ALU.mult, op1=ALU.add),
                      reads=[in_r[b], pH_r], writes=[zt_r[b]])
            ln_tile(kb, LS, zt[b], zt_r[b], x1[b][:], x1_r[b], g_bc, b_bc, lnp_r)
            kb.dma("sp", lambda e, i=i, b=b: e.dma_start(out=xa[i * 128:(i + 1) * 128, :], in_=x1[b][:]), reads=[x1_r[b]], writes=[xa_r[i]])

        kb.defer = pending
        post()
        kb.defer = None
    kb_flush(kb, pending, len(pending))
    kb_pop(kb)


T_FULL = 4096
N_CORES = 8
_CACHE = {}


def kernel(**inputs):
    if "prog" not in _CACHE:
        _CACHE["prog"] = build(T_FULL, FULL_PLAN, cap=512)
    nc, names, _ = _CACHE["prog"]
    x = np.asarray(inputs["x"], dtype=np.float32)
    in_maps = []
    for c in range(N_CORES):
        m = {"x": np.ascontiguousarray(x[c])}
        for n in names:
            m[n] = np.ascontiguousarray(np.asarray(inputs[n], dtype=np.float32))
        in_maps.append(m)
    res = run_bass_kernel_spmd(nc, in_maps, core_ids=list(range(N_CORES)))
    return np.stack([np.asarray(res.results[c]["out"]) for c in range(N_CORES)], axis=0).astype(np.float32)
```
